# Optimizing a Trainium2 kernel written in Bass

```python
import math
import jax, jax.numpy as jnp
from jax import lax
import numpy as np

D_MODEL = 2048
BATCH = 4
SEQ = 2048
DEPTH = 1
DEC_BATCH = 128
DEC_SEQ = 8
PAST_LEN = 16384
PAGE_SIZE = 128

M_W = D_MODEL // 2
M_H = 4
M_DK = M_W // M_H
M_DV = M_W // M_H
CONV_K = 4
M_CHUNK = 64
HG_W = D_MODEL // 2
HG_DK = 128
HG_H = HG_W // HG_DK
HG_DV = HG_W // HG_H
HG_CHUNK = 64
MEM_LEN = 256
XA_H = 4
XA_D = D_MODEL // XA_H
N_GROUPS = 4
EXP_PER_GROUP = 8
N_EXPERTS = N_GROUPS * EXP_PER_GROUP
TOP_K = 2
EXP_FF = D_MODEL // 4
N_IN = 4 * M_W + 2 * M_H + 4 * HG_W + 2 * D_MODEL
ALPHA = (2 * DEPTH) ** 0.25
BETA = (8 * DEPTH) ** -0.25
LN_EPS = 1e-5
F32 = jnp.float32

kernel_name = "mlstm_hgrn2_gated_merge_memxattn_hiermoe_step"


def layer_norm(x, g, b):
    xf = x.astype(F32)
    mu = xf.mean(-1, keepdims=True)
    var = jnp.mean(jnp.square(xf - mu), -1, keepdims=True)
    return ((xf - mu) * lax.rsqrt(var + LN_EPS) * g.astype(F32) + b.astype(F32)).astype(x.dtype)


def head_norm(h, gain, center):
    hf = h.astype(F32)
    if center:
        hf = hf - hf.mean(-1, keepdims=True)
    return hf * lax.rsqrt(jnp.mean(jnp.square(hf), -1, keepdims=True) + LN_EPS) * gain.astype(F32)


def to_chunks(a, L):
    B, T = a.shape[:2]
    return jnp.moveaxis(a.reshape(B, T // L, L, *a.shape[2:]), 1, 0)


def from_chunks(a):
    nc, B, L = a.shape[:3]
    return jnp.moveaxis(a, 0, 1).reshape(B, nc * L, *a.shape[3:])


def causal_conv(u, buf, w):
    T = u.shape[1]
    full = jnp.concatenate([buf.astype(u.dtype), u], axis=1)
    out = sum(full[:, j:j + T] * w[j] for j in range(CONV_K))
    return out, full[:, T:]


def mlstm_recurrence(q, k, v, ig, lf, C0, n0, m0):
    T = q.shape[1]
    L = math.gcd(T, M_CHUNK)
    causal = jnp.tril(jnp.ones((L, L), dtype=bool))[None, :, :, None]

    def step(carry, inp):
        C, n, m = carry
        qc, kc, vc, ic, fc = inp
        F = jnp.cumsum(fc, axis=1)
        Dm = F[:, :, None, :] - F[:, None, :, :] + ic[:, None, :, :]
        Dm = jnp.where(causal, Dm, -jnp.inf)
        init_w = F + m[:, None, :]
        m_t = jnp.maximum(init_w, Dm.max(axis=2))
        P = jnp.exp(Dm - m_t[:, :, None, :])
        a0 = jnp.exp(init_w - m_t)
        Sc = jnp.einsum('bthd,bshd->btsh', qc, kc) * P
        num = jnp.einsum('btsh,bshv->bthv', Sc, vc) + a0[..., None] * jnp.einsum('bthd,bhdv->bthv', qc, C)
        den = Sc.sum(axis=2) + a0 * jnp.einsum('bthd,bhd->bth', qc, n)
        h = num / jnp.maximum(jnp.abs(den), jnp.exp(-m_t))[..., None]
        mL = m_t[:, -1]
        wL = jnp.exp(F[:, -1:, :] - F + ic - mL[:, None, :])
        decay = jnp.exp(F[:, -1] + m - mL)
        C_new = decay[:, :, None, None] * C + jnp.einsum('bshd,bshv->bhdv', wL[..., None] * kc, vc)
        n_new = decay[..., None] * n + jnp.einsum('bsh,bshd->bhd', wL, kc)
        return (C_new, n_new, mL), h

    xs = tuple(to_chunks(a, L) for a in (q, k, v, ig, lf))
    (C, n, m), h = lax.scan(step, (C0, n0, m0), xs)
    return from_chunks(h), C, n, m


def hgrn2_recurrence(q, k, lf, v, S0):
    T = q.shape[1]
    L = math.gcd(T, HG_CHUNK)
    causal = jnp.tril(jnp.ones((L, L), dtype=bool))[None, :, :, None, None]

    def step(S, inp):
        qc, kc, fc, vc = inp
        b = jnp.cumsum(fc, axis=1)
        rel = jnp.exp(jnp.where(causal, b[:, :, None] - b[:, None], -jnp.inf))
        A = jnp.einsum('bthc,btshc->btsh', qc, rel * kc[:, None])
        o = jnp.einsum('btsh,bshv->bthv', A, vc) + jnp.einsum('bthc,bhcv->bthv', qc * jnp.exp(b), S)
        bL = b[:, -1]
        S_new = jnp.exp(bL)[..., None] * S + jnp.einsum('bshc,bshv->bhcv', kc * jnp.exp(bL[:, None] - b), vc)
        return S_new, o

    xs = tuple(to_chunks(a, L) for a in (q, k, lf, v))
    S, o = lax.scan(step, S0, xs)
    return from_chunks(o), S


def token_mixer(x, C0, n0, m0, conv0, S0, lb, w_in, b_in, conv_w, mlstm_gn, hgrn_gn, w_bm, w_bh, w_out):
    B, T, _ = x.shape
    u = x @ w_in + b_in
    sizes = (2 * M_W, M_W, M_W, M_H, M_H, HG_W, HG_W, HG_W, HG_W, D_MODEL, D_MODEL)
    offs = np.cumsum(sizes)[:-1].tolist()
    qk_m, v_m, o_m, i_m, f_m, q_h, f_h, i_h, g_h, gate_m, gate_h = jnp.split(u, offs, axis=-1)

    qk_c, conv_new = causal_conv(qk_m, conv0, conv_w)
    q_m, k_m = jnp.split(jax.nn.silu(qk_c).astype(F32), 2, axis=-1)
    q_m = q_m.reshape(B, T, M_H, M_DK) * (M_DK ** -0.5)
    k_m = k_m.reshape(B, T, M_H, M_DK)
    v_m = v_m.astype(F32).reshape(B, T, M_H, M_DV)
    h_m, C, n, m = mlstm_recurrence(q_m, k_m, v_m, i_m.astype(F32), jax.nn.log_sigmoid(f_m.astype(F32)),
                                    C0.astype(F32), n0.astype(F32), m0.astype(F32))
    h_m = jax.nn.sigmoid(o_m.astype(F32)).reshape(B, T, M_H, M_DV) * h_m
    h_m = head_norm(h_m, mlstm_gn.reshape(M_H, M_DV), True).reshape(B, T, M_W).astype(x.dtype)

    f = lb + (1.0 - lb) * jax.nn.sigmoid(f_h.astype(F32))
    q_g = jax.nn.silu(q_h.astype(F32)).reshape(B, T, HG_H, HG_DK)
    k_g = (1.0 - f).reshape(B, T, HG_H, HG_DK)
    lf_g = jnp.log(f).reshape(B, T, HG_H, HG_DK)
    i_g = i_h.astype(F32).reshape(B, T, HG_H, HG_DV)
    o_g, S = hgrn2_recurrence(q_g, k_g, lf_g, i_g, S0.astype(F32))
    o_g = head_norm(o_g, hgrn_gn.reshape(HG_H, HG_DV), False).reshape(B, T, HG_W) * jax.nn.silu(g_h.astype(F32))
    o_g = o_g.astype(x.dtype)

    merged = jax.nn.sigmoid(gate_m) * (h_m @ w_bm) + jax.nn.sigmoid(gate_h) * (o_g @ w_bh)
    return merged @ w_out, (C, n, m, conv_new, S)


def memory_kv(mem, wk, wv):
    B, M, _ = mem.shape
    return (mem @ wk).reshape(B, M, XA_H, XA_D), (mem @ wv).reshape(B, M, XA_H, XA_D)


def cross_attention(h, mk, mv, wq, wo):
    B, T, _ = h.shape
    q = (h @ wq).reshape(B, T, XA_H, XA_D)
    s = jnp.einsum('bthd,bmhd->bhtm', q, mk.astype(q.dtype)).astype(F32) * (XA_D ** -0.5)
    p = jax.nn.softmax(s, axis=-1).astype(h.dtype)
    o = jnp.einsum('bhtm,bmhd->bthd', p, mv.astype(h.dtype))
    return o.reshape(B, T, D_MODEL) @ wo


def hier_moe(h, r1_w, r1_b, r2_w, r2_b, e_wg, e_wu, e_wd):
    B, T, D = h.shape
    x = h.reshape(B * T, D)
    lg_group = (x @ r1_w).astype(F32) + r1_b.astype(F32)
    grp = jnp.argmax(lg_group, axis=-1)
    p_grp = jnp.take_along_axis(jax.nn.softmax(lg_group, axis=-1), grp[:, None], axis=-1)
    lg_exp = jnp.einsum('nd,gde->nge', x, r2_w).astype(F32) + r2_b.astype(F32)
    lg_exp = jnp.take_along_axis(lg_exp, grp[:, None, None], axis=1)[:, 0]
    top_val, top_idx = lax.top_k(lg_exp, TOP_K)
    w_top = jax.nn.softmax(top_val, axis=-1) * p_grp
    w_in_group = jnp.einsum('nk,nke->ne', w_top, jax.nn.one_hot(top_idx, EXP_PER_GROUP, dtype=F32))
    combine = (jax.nn.one_hot(grp, N_GROUPS, dtype=F32)[:, :, None] * w_in_group[:, None, :]).reshape(-1, N_EXPERTS)
    hid = jax.nn.silu(jnp.einsum('nd,edf->nef', x, e_wg)) * jnp.einsum('nd,edf->nef', x, e_wu)
    out = jnp.einsum('nef,efd->nd', hid * combine[:, :, None].astype(hid.dtype), e_wd)
    return out.reshape(B, T, D)


def decoder_layer(x, mem_k, mem_v, C0, n0, m0, conv0, S0, lb, w_in, b_in, conv_w, mlstm_gn, hgrn_gn,
                  w_bm, w_bh, w_out, ln1_g, ln1_b, xa_wq, xa_wo, ln2_g, ln2_b,
                  r1_w, r1_b, r2_w, r2_b, e_wg, e_wu, e_wd, ln3_g, ln3_b):
    mix, state = token_mixer(x, C0, n0, m0, conv0, S0, lb, w_in, b_in, conv_w, mlstm_gn, hgrn_gn, w_bm, w_bh, w_out)
    h = layer_norm(ALPHA * x + mix, ln1_g, ln1_b)
    h = layer_norm(ALPHA * h + cross_attention(h, mem_k, mem_v, xa_wq, xa_wo), ln2_g, ln2_b)
    y = layer_norm(ALPHA * h + hier_moe(h, r1_w, r1_b, r2_w, r2_b, e_wg, e_wu, e_wd), ln3_g, ln3_b)
    return y, state


def setup_inputs(seed: int = 0) -> dict:
    key = jax.random.key(seed)
    ks = iter(jax.random.split(key, 48))

    def nrm(shape, scale):
        return jax.random.normal(next(ks), shape, F32) * scale

    f_lo = 4 * M_W + M_H
    forget_offset = jnp.zeros((N_IN,), F32).at[f_lo:f_lo + M_H].set(jnp.linspace(3.0, 6.0, M_H))
    return {
        "x_prompt": nrm((BATCH, SEQ, D_MODEL), 1.0),
        "x_sample": nrm((DEC_BATCH, DEC_SEQ, D_MODEL), 1.0),
        "mem_prompt": nrm((BATCH, MEM_LEN, D_MODEL), 1.0),
        "cache_mem_k": nrm((DEPTH, DEC_BATCH, MEM_LEN, XA_H, XA_D), 1.0),
        "cache_mem_v": nrm((DEPTH, DEC_BATCH, MEM_LEN, XA_H, XA_D), 1.0),
        "state_mlstm_C": nrm((DEPTH, DEC_BATCH, M_H, M_DK, M_DV), 0.5),
        "state_mlstm_n": nrm((DEPTH, DEC_BATCH, M_H, M_DK), 0.5),
        "state_mlstm_m": nrm((DEPTH, DEC_BATCH, M_H), 1.0),
        "state_mlstm_conv": nrm((DEPTH, DEC_BATCH, CONV_K - 1, 2 * M_W), 1.0),
        "state_hgrn_S": nrm((DEPTH, DEC_BATCH, HG_H, HG_DK, HG_DV), 0.5),
        "w_in": nrm((DEPTH, D_MODEL, N_IN), D_MODEL ** -0.5),
        "b_in": nrm((DEPTH, N_IN), 0.02) + forget_offset[None],
        "conv_w": nrm((DEPTH, CONV_K, 2 * M_W), CONV_K ** -0.5),
        "mlstm_gn": 1.0 + nrm((DEPTH, M_W), 0.02),
        "lb_logits": nrm((DEPTH + 1, HG_W), 0.1),
        "hgrn_gn": 1.0 + nrm((DEPTH, HG_W), 0.02),
        "w_bm": nrm((DEPTH, M_W, D_MODEL), M_W ** -0.5),
        "w_bh": nrm((DEPTH, HG_W, D_MODEL), HG_W ** -0.5),
        "w_out": nrm((DEPTH, D_MODEL, D_MODEL), BETA * D_MODEL ** -0.5),
        "ln1_g": 1.0 + nrm((DEPTH, D_MODEL), 0.02),
        "ln1_b": nrm((DEPTH, D_MODEL), 0.02),
        "xa_wq": nrm((DEPTH, D_MODEL, D_MODEL), D_MODEL ** -0.5),
        "xa_wk": nrm((DEPTH, D_MODEL, D_MODEL), D_MODEL ** -0.5),
        "xa_wv": nrm((DEPTH, D_MODEL, D_MODEL), BETA * D_MODEL ** -0.5),
        "xa_wo": nrm((DEPTH, D_MODEL, D_MODEL), BETA * D_MODEL ** -0.5),
        "ln2_g": 1.0 + nrm((DEPTH, D_MODEL), 0.02),
        "ln2_b": nrm((DEPTH, D_MODEL), 0.02),
        "r1_w": nrm((DEPTH, D_MODEL, N_GROUPS), D_MODEL ** -0.5),
        "r1_b": nrm((DEPTH, N_GROUPS), 0.01),
        "r2_w": nrm((DEPTH, N_GROUPS, D_MODEL, EXP_PER_GROUP), D_MODEL ** -0.5),
        "r2_b": nrm((DEPTH, N_GROUPS, EXP_PER_GROUP), 0.01),
        "e_wg": nrm((DEPTH, N_EXPERTS, D_MODEL, EXP_FF), D_MODEL ** -0.5),
        "e_wu": nrm((DEPTH, N_EXPERTS, D_MODEL, EXP_FF), BETA * D_MODEL ** -0.5),
        "e_wd": nrm((DEPTH, N_EXPERTS, EXP_FF, D_MODEL), BETA * EXP_FF ** -0.5),
        "ln3_g": 1.0 + nrm((DEPTH, D_MODEL), 0.02),
        "ln3_b": nrm((DEPTH, D_MODEL), 0.02),
    }


def reference(x_prompt, x_sample, mem_prompt, cache_mem_k, cache_mem_v, state_mlstm_C, state_mlstm_n,
              state_mlstm_m, state_mlstm_conv, state_hgrn_S, w_in, b_in, conv_w, mlstm_gn, lb_logits, hgrn_gn,
              w_bm, w_bh, w_out, ln1_g, ln1_b, xa_wq, xa_wk, xa_wv, xa_wo, ln2_g, ln2_b,
              r1_w, r1_b, r2_w, r2_b, e_wg, e_wu, e_wd, ln3_g, ln3_b):
    lb_all = jnp.cumsum(jax.nn.softmax(lb_logits.astype(F32), axis=0), axis=0)
    Bp = x_prompt.shape[0]
    hp, hs = x_prompt, x_sample
    p_states, s_states, p_mk, p_mv = [], [], [], []
    for l in range(DEPTH):
        lw = (w_in[l], b_in[l], conv_w[l], mlstm_gn[l], hgrn_gn[l], w_bm[l], w_bh[l], w_out[l],
              ln1_g[l], ln1_b[l], xa_wq[l], xa_wo[l], ln2_g[l], ln2_b[l],
              r1_w[l], r1_b[l], r2_w[l], r2_b[l], e_wg[l], e_wu[l], e_wd[l], ln3_g[l], ln3_b[l])
        mk, mv = memory_kv(mem_prompt, xa_wk[l], xa_wv[l])
        zero_state = (jnp.zeros((Bp, M_H, M_DK, M_DV), F32), jnp.zeros((Bp, M_H, M_DK), F32),
                      jnp.zeros((Bp, M_H), F32), jnp.zeros((Bp, CONV_K - 1, 2 * M_W), hp.dtype),
                      jnp.zeros((Bp, HG_H, HG_DK, HG_DV), F32))
        hp, sp = decoder_layer(hp, mk, mv, *zero_state, lb_all[l], *lw)
        hs, ss = decoder_layer(hs, cache_mem_k[l], cache_mem_v[l], state_mlstm_C[l], state_mlstm_n[l],
                               state_mlstm_m[l], state_mlstm_conv[l], state_hgrn_S[l], lb_all[l], *lw)
        p_mk.append(mk)
        p_mv.append(mv)
        p_states.append(sp)
        s_states.append(ss)
    mem_k_p = jnp.stack(p_mk)
    mem_v_p = jnp.stack(p_mv)
    C_p, n_p, m_p, conv_p, S_p = [jnp.stack([s[i] for s in p_states]) for i in range(5)]
    C_s, n_s, m_s, conv_s, S_s = [jnp.stack([s[i] for s in s_states]) for i in range(5)]
    return (hp, hs, mem_k_p, mem_v_p, C_p, n_p, m_p, conv_p, S_p, C_s, n_s, m_s, conv_s, S_s)
```

```python
import math
import numpy as np
import concourse.bass as bass
import concourse.mybir as mybir
from concourse.bass_utils import run_bass_kernel_spmd

F32 = mybir.dt.float32
BF16 = mybir.dt.bfloat16
ALU = mybir.AluOpType
AF = mybir.ActivationFunctionType
AX = mybir.AxisListType

D = 2048
NIN = 12296
NCORES = 8
TP = 1024
NTOK = 1152
NT = 9
ALPHA = 2.0 ** 0.25
EPS = 1e-5
LN16 = math.log(16.0)
NEGBIG = -1.0e30


class Buf:
    def __init__(self, name, t=None, parent=None):
        self.name = name
        self.t = t
        self.parent = parent
        self.kids = {}
        self.last_w = None
        self.readers = []

    def sub(self, key):
        if key not in self.kids:
            self.kids[key] = Buf(f"{self.name}.{key}", self.t, self)
        return self.kids[key]

    def __getitem__(self, idx):
        return self.t[idx]

    def related(self):
        out = [self]
        p = self.parent
        while p is not None:
            out.append(p)
            p = p.parent
        st = list(self.kids.values())
        while st:
            k = st.pop()
            out.append(k)
            st.extend(k.kids.values())
        return out


class Op:
    __slots__ = ("eng", "fn", "deps", "needs_inc", "count", "is_dma", "sem", "val")

    def __init__(self, eng, fn):
        self.eng = eng
        self.fn = fn
        self.deps = []
        self.needs_inc = False
        self.count = None
        self.is_dma = False
        self.sem = None
        self.val = None


class Sched:
    ENGS = ("pe", "act", "dve", "pool", "sp")

    def __init__(self, nc, n_dma_sems=16, same_engine_sync=True):
        self.nc = nc
        self.ops = {e: [] for e in self.ENGS}
        self.esem = {e: nc.alloc_semaphore(f"s_{e}") for e in self.ENGS}
        self.dsem = {q: [nc.alloc_semaphore(f"d_{q}{i}") for i in range(n_dma_sems)] for q in ("sp", "pool")}
        self.dcount = {q: 0 for q in self.dsem}
        self.dlast = {q: [None] * n_dma_sems for q in self.dsem}
        self.same_engine_sync = same_engine_sync
        self.all_dmas = []

    def _add_dep(self, op, dep):
        if dep is None or dep is op:
            return
        if not dep.is_dma and dep.eng == op.eng and not op.is_dma:
            if op.eng == "pe" or not self.same_engine_sync:
                return
        if dep in op.deps:
            return
        op.deps.append(dep)
        if not dep.is_dma:
            dep.needs_inc = True

    def _track(self, op, reads, writes):
        for b in reads:
            for r in b.related():
                self._add_dep(op, r.last_w)
            if getattr(b, "excl", False):
                for rd in b.readers:
                    if rd.eng != op.eng:
                        self._add_dep(op, rd)
        for b in writes:
            for r in b.related():
                self._add_dep(op, r.last_w)
                for rd in r.readers:
                    self._add_dep(op, rd)
        for b in reads:
            b.readers.append(op)
        for b in writes:
            b.last_w = op
            b.readers = []

    def op(self, eng, fn, reads=(), writes=()):
        o = Op(eng, fn)
        self._track(o, list(reads), list(writes))
        self.ops[eng].append(o)
        return o

    def dma(self, q, out, in_, reads=(), writes=(), **kw):
        o = Op(q, lambda e: e.dma_start(out=out, in_=in_, **kw))
        o.is_dma = True
        k = self.dcount[q]
        ns = len(self.dsem[q])
        slot = k % ns
        o.sem = self.dsem[q][slot]
        o.val = 16 * (k // ns + 1)
        prev = self.dlast[q][slot]
        if prev is not None:
            o.deps.append(prev)
        self.dlast[q][slot] = o
        self.dcount[q] = k + 1
        self._track(o, list(reads), list(writes))
        self.ops[q].append(o)
        self.all_dmas.append(o)
        return o

    def barrier(self):
        lasts = []
        for e in self.ENGS:
            for o in reversed(self.ops[e]):
                if not o.is_dma and o.fn is not None:
                    lasts.append(o)
                    break
        dm = [d for q in self.dlast for d in self.dlast[q] if d is not None]
        for e in self.ENGS:
            o = Op(e, None)
            for l in lasts:
                if l.eng != e:
                    o.deps.append(l)
                    l.needs_inc = True
            o.deps.extend(dm)
            self.ops[e].append(o)

    def finalize(self):
        fin = Op("sp", None)
        for q in self.dlast:
            for d in self.dlast[q]:
                if d is not None:
                    fin.deps.append(d)
        self.ops["sp"].append(fin)
        for e in self.ENGS:
            c = 0
            for o in self.ops[e]:
                if (not o.is_dma) and o.needs_inc:
                    c += 1
                    o.count = c
        nc = self.nc
        esem = self.esem
        with nc.Block() as block:
            regs = {"pe": block.tensor, "act": block.scalar, "dve": block.vector, "pool": block.gpsimd, "sp": block.sync}
            for e in self.ENGS:
                ops = self.ops[e]

                def body(h, ops=ops, e=e):
                    waited = {}
                    pending = None
                    for o in ops:
                        for d in o.deps:
                            if d.is_dma:
                                s, v = d.sem, d.val
                            else:
                                s, v = esem[d.eng], d.count
                            key = id(s)
                            if waited.get(key, 0) >= v:
                                continue
                            waited[key] = v
                            h.wait_ge(s, v)
                        if o.fn is None:
                            continue
                        ins = o.fn(h)
                        if o.is_dma:
                            ins.then_inc(o.sem, 16)
                        elif o.needs_inc:
                            ins.then_inc(esem[e], 1)

                regs[e](body)


def V(t, off, *dims, npart=128, p0=0):
    n = t.shape[1]
    return bass.AP(tensor=t, offset=p0 * n + off, ap=[[n, npart]] + [[s, c] for s, c in dims])


class Arena:
    def __init__(self, nc, base=16384, limit=196608):
        self.nc = nc
        self.off = base
        self.limit = limit
        self.n = 0

    def alloc(self, name, nelem, dt):
        sz = nelem * (4 if dt == F32 else 2)
        self.off = (self.off + 63) // 64 * 64
        assert self.off + sz <= self.limit, f"SBUF overflow at {name}: {self.off + sz}"
        self.n += 1
        t = self.nc.alloc_sbuf_tensor_at(f"{name}_{self.n}", [128, nelem], dt, offset=self.off)
        self.off += sz
        return Buf(name, t)

    def mark(self):
        return self.off

    def release(self, m):
        self.off = m


def host_consts():
    c = np.zeros((128, 1056 + NTOK), np.float32)
    idx = np.arange(128)
    c[:, 0:128] = np.eye(128)
    s = idx[:, None]
    t = idx[None, :]
    c[:, 128:256] = (s <= t)
    c[:, 256:384] = np.where(t <= s, 0.0, NEGBIG)
    c[:, 384:512] = (s == 127)
    same = (s // 8) == (t // 8)
    c[:, 512:640] = same & (s <= t)
    c[:, 640:768] = np.where(same & (t <= s), 0.0, NEGBIG)
    c[:, 768:896] = (s == 8 * (t // 8) + 7)
    c[:, 896:912] = (idx[:, None] // 8) == np.arange(16)[None, :]
    c[:, 912:928] = idx[:, None] == 8 * np.arange(16)[None, :]
    c[:, 928:1056] = 1.0
    rm = np.ones(NTOK, np.float32)
    rm[0:1024:128] = 0.0
    rm[1024:NTOK:8] = 0.0
    c[:, 1056:] = rm[None, :]
    return c


C_ID, C_MP, C_NEGP, C_LASTP, C_MS, C_NEGS, C_LASTS, C_SEQM, C_SEQ1, C_ONES, C_RM = 0, 128, 256, 384, 512, 640, 768, 896, 912, 928, 1056


def build_program(stage=99, same_engine_sync=True, cut=99):
    nc = bass.Bass("TRN2", target_bir_lowering=False)

    def din(name, shape):
        return nc.dram_tensor(name, list(shape), F32, kind="ExternalInput").ap()

    def dout(name, shape):
        return nc.dram_tensor(name, list(shape), F32, kind="ExternalOutput").ap()

    I = {}
    for name, shape in [
        ("xp", (TP, D)), ("xpre", (TP, D)), ("xs", (128, D)), ("mem", (256, D)),
        ("ck", (16, 256, D)), ("cv", (16, 256, D)), ("C0", (16, 4, 256, 256)), ("n0", (16, 4, 256)), ("m0", (16, 4)),
        ("conv0", (48, D)), ("S0", (16, 8, 128, 128)), ("flag", (128, 1)), ("consts", (128, 1056 + NTOK)),
        ("w_in", (D, NIN)), ("b_in", (1, NIN)), ("conv_w", (4, D)), ("mlstm_gn", (1, 1024)), ("lb_logits", (2, 1024)),
        ("hgrn_gn", (1, 1024)), ("w_bm", (1024, D)), ("w_bh", (1024, D)), ("w_out", (D, D)),
        ("ln1_g", (1, D)), ("ln1_b", (1, D)), ("xa_wq", (D, D)), ("xa_wk", (D, D)), ("xa_wv", (D, D)), ("xa_wo", (D, D)),
        ("ln2_g", (1, D)), ("ln2_b", (1, D)), ("rw", (D, 36)), ("rb", (1, 36)),
        ("e_wg", (32, D, 512)), ("e_wu", (32, D, 512)), ("e_wd", (32, 512, D)), ("ln3_g", (1, D)), ("ln3_b", (1, D)),
    ]:
        I[name] = din(name, shape)
    O = {}
    for name, shape in [
        ("y_p", (TP, D)), ("y_s", (128, D)), ("mk", (256, D)), ("mv", (256, D)),
        ("Cp", (4, 256, 256)), ("np_", (4, 256)), ("mp", (1, 4)), ("convp", (3, D)), ("Sp", (8, 128, 128)),
        ("Cs", (16, 4, 256, 256)), ("ns", (16, 4, 256)), ("ms", (16, 4)), ("convs", (16, 3, D)), ("Ss", (16, 8, 128, 128)),
    ]:
        O[name] = dout(name, shape)

    K = Sched(nc, same_engine_sync=same_engine_sync)
    A = Arena(nc)
    psum = [Buf(f"ps{i}", nc.alloc_psum_tensor(f"ps{i}", [128, 512], F32)) for i in range(8)]
    for b_ in psum:
        b_.excl = True
    pstate = {"i": 0}

    def PS():
        b = psum[pstate["i"] % 6]
        pstate["i"] += 1
        return b


    cst = A.alloc("cst", 1056 + NTOK, F32)
    K.dma("sp", cst[:, :], I["consts"], writes=[cst])
    identB = A.alloc("identB", 128, BF16)
    K.op("dve", lambda e: e.tensor_copy(out=identB[:, :], in_=cst[:, C_ID:C_ID + 128]), reads=[cst], writes=[identB])
    flag = A.alloc("flag", 1, F32)
    K.dma("sp", flag[:, :], I["flag"], writes=[flag])

    def cs(c0, n=128):
        return cst[:, c0:c0 + n]

    ident = cs(C_ID)
    ones = cs(C_ONES)

    NSLOT = 4
    wslots = [A.alloc(f"wslot{i}", 4096, BF16) for i in range(NSLOT)]
    wstate = {"i": 0}

    def wload(parts):
        sl = wslots[wstate["i"] % NSLOT]
        wstate["i"] += 1
        kc = parts[0].shape[0] // 128
        tot = sum(p.shape[1] for p in parts)
        assert kc * tot <= 4096
        view = sl[:, 0:kc * tot].rearrange("p (k c) -> p k c", k=kc)
        c0 = 0
        for p in parts:
            n = p.shape[1]
            K.dma("pool", view[:, :, c0:c0 + n], p.rearrange("(k p) c -> p k c", p=128), writes=[sl])
            c0 += n
        return sl, view

    w_in = I["w_in"]
    b_in = I["b_in"]

    def fm_bias(name, c0, nblk):
        b = A.alloc(name, nblk, F32)
        K.dma("sp", b[:, :], b_in[0, c0:c0 + nblk * 128].rearrange("(b p) -> p b", p=128), writes=[b])
        return b

    bqk = fm_bias("bqk", 0, 16)
    bqh = fm_bias("bqh", 4104, 8)
    bfh = fm_bias("bfh", 5128, 8)
    bgm = fm_bias("bgm", 8200, 16)
    bgh = fm_bias("bgh", 10248, 16)
    cw = A.alloc("cw", 64, F32)
    for j in range(4):
        K.dma("sp", V(cw.t, j, (4, 16)), I["conv_w"][j, :].rearrange("(c p) -> p c", p=128), writes=[cw])
    lbt = A.alloc("lbt", 16, F32)
    for r in range(2):
        K.dma("sp", lbt[:, r * 8:(r + 1) * 8], I["lb_logits"][r, :].rearrange("(h p) -> p h", p=128), writes=[lbt])
    lb = A.alloc("lb", 8, F32)
    oml = A.alloc("oml", 8, F32)
    K.op("dve", lambda e: e.tensor_tensor(out=lb[:, :], in0=lbt[:, 0:8], in1=lbt[:, 8:16], op=ALU.subtract), reads=[lbt], writes=[lb])
    K.op("act", lambda e: e.activation(out=lb[:, :], in_=lb[:, :], func=AF.Sigmoid), reads=[lb], writes=[lb])
    K.op("dve", lambda e: e.tensor_scalar(out=oml[:, :], in0=lb[:, :], scalar1=-1.0, scalar2=1.0, op0=ALU.mult, op1=ALU.add), reads=[lb], writes=[oml])
    bgate = A.alloc("bgate", 8, F32)
    K.dma("sp", bgate[:, :], b_in[0:1, 4096:4104].partition_broadcast(128), writes=[bgate])
    gnT = A.alloc("gnT", 16, F32)
    K.dma("sp", gnT[:, 0:8], I["mlstm_gn"][0, :].rearrange("(k p) -> p k", p=128), writes=[gnT])
    K.dma("sp", gnT[:, 8:16], I["hgrn_gn"][0, :].rearrange("(k p) -> p k", p=128), writes=[gnT])
    hm_scr = nc.dram_tensor("hm_scr", [NTOK, 1024], BF16, kind="Internal").ap()
    og_scr = nc.dram_tensor("og_scr", [NTOK, 1024], BF16, kind="Internal").ap()
    hm_scr_b = Buf("hm_scr")
    og_scr_b = Buf("og_scr")

    xT = A.alloc("xT", 16 * NTOK, BF16)
    xtile = []
    xst = {"i": 0}

    def xTv(ntok):
        return xT[:, 0:16 * ntok].rearrange("p (k t) -> p k t", k=16)

    def transpose_tile_f32(src_buf, src_ap_fn, dstT_buf, dst_view, tok0):
        for g in range(4):
            ps = PS()
            for j in range(4):
                kc = g * 4 + j
                K.op("pe", lambda e, ps=ps, j=j, kc=kc: e.transpose(ps[:, j * 128:(j + 1) * 128], src_ap_fn(kc), ident),
                     reads=[src_buf, cst], writes=[ps])
            eng = "act" if g % 2 == 0 else "dve"
            outv = dst_view[:, g * 4:(g + 1) * 4, tok0:tok0 + 128]
            inv = ps[:, :].rearrange("p (j t) -> p j t", j=4)
            if eng == "act":
                K.op("act", lambda e, outv=outv, inv=inv: e.activation(out=outv, in_=inv, func=AF.Copy), reads=[ps], writes=[dstT_buf])
            else:
                K.op("dve", lambda e, outv=outv, inv=inv: e.tensor_copy(out=outv, in_=inv), reads=[ps], writes=[dstT_buf])

    def build_xT(srcs, ntok):
        view = xTv(ntok)
        t = 0
        for src, ntile in srcs:
            for i in range(ntile):
                xb = xtile[xst["i"] % 2]
                xst["i"] += 1
                K.dma("sp", xb[:, :], src[i * 128:(i + 1) * 128, :], writes=[xb])
                transpose_tile_f32(xb, lambda kc, xb=xb: xb[:, kc * 128:(kc + 1) * 128], xT, view, t * 128)
                t += 1
        return view

    def mm_group(ps_ap, ps_buf, pairs, reads):
        n = len(pairs)
        for i, (l, r) in enumerate(pairs):
            K.op("pe", lambda e, l=l, r=r, i=i: e.matmul(ps_ap, l, r, start=(i == 0), stop=(i == n - 1)), reads=reads, writes=[ps_buf])

    m_mix = A.mark()

    Cst = [A.alloc(f"Cst{h}", 2 * 257, F32) for h in range(4)]
    Sst = [A.alloc(f"Sst{h}", 128, F32) for h in range(8)]
    mstate = A.alloc("mstate", 4, F32)
    xhalo = A.alloc("xhalo", 48, BF16)
    for h in range(4):
        K.op("pool", lambda e, h=h: e.memset(Cst[h][:, :], 0.0), writes=[Cst[h]])
    for h in range(8):
        K.op("pool", lambda e, h=h: e.memset(Sst[h][:, :], 0.0), writes=[Sst[h]])
    K.op("pool", lambda e: e.memset(mstate[:, :], 0.0), writes=[mstate])


    sm = A.alloc("gate_small", 160, F32)
    Dg = A.alloc("Dg", 512, F32)
    Dm = A.alloc("Dm", 512, F32)

    def smv(i, n=4):
        return sm[:, i:i + n]

    def gate_tile(gt_ap, Mc, NEGc, LASTc, mprev_ap, mprev_buf, out, need_P):
        gi = gt_ap[:, 0:4]
        gf = gt_ap[:, 4:8]
        gb = out["gbuf"]
        e1, nlf, g, Fv, mx, iw, mt, negm, d1, L8, d2, t1 = (smv(0), smv(4), smv(8), smv(12), smv(16), smv(20), smv(24), smv(28), smv(32), smv(36, 8), smv(44), smv(48))
        mf = smv(52, 8)
        K.op("act", lambda e: e.activation(out=e1, in_=gf, func=AF.Exp, scale=-1.0), reads=[gb], writes=[sm])
        K.op("act", lambda e: e.activation(out=nlf, in_=e1, func=AF.Ln, bias=1.0), reads=[sm], writes=[sm])
        ps = PS()
        K.op("pe", lambda e, ps=ps: e.matmul(ps[:, 0:4], cs(Mc), nlf, start=True, stop=True), reads=[sm, cst], writes=[ps])
        K.op("dve", lambda e, ps=ps: e.tensor_tensor(out=g, in0=gi, in1=ps[:, 0:4], op=ALU.add), reads=[ps, gb], writes=[sm])
        K.op("dve", lambda e, ps=ps: e.tensor_scalar(out=Fv, in0=ps[:, 0:4], scalar1=-1.0, scalar2=None, op0=ALU.mult), reads=[ps], writes=[sm])
        K.op("dve", lambda e: e.tensor_tensor(out=iw, in0=Fv, in1=mprev_ap, op=ALU.add), reads=[sm, mprev_buf], writes=[sm])
        for h in range(4):
            K.op("dve", lambda e, h=h: e.tensor_scalar(out=Dg[:, h * 128:(h + 1) * 128], in0=ident, scalar1=g[:, h:h + 1], scalar2=None, op0=ALU.mult),
                 reads=[sm, cst], writes=[Dg])
        ps2 = PS()
        K.op("pe", lambda e, ps2=ps2: e.matmul(ps2[:, :], ones, Dg[:, :], start=True, stop=True), reads=[Dg, cst], writes=[ps2])
        for h in range(4):
            K.op("dve", lambda e, h=h, ps2=ps2: e.scalar_tensor_tensor(out=Dm[:, h * 128:(h + 1) * 128], in0=ps2[:, h * 128:(h + 1) * 128], scalar=Fv[:, h:h + 1],
                                                                 in1=cs(NEGc), op0=ALU.add, op1=ALU.add), reads=[ps2, sm, cst], writes=[Dm])
        K.op("dve", lambda e: e.tensor_reduce(out=mx, in_=Dm[:, :].rearrange("p (h s) -> p h s", h=4), axis=AX.X, op=ALU.max), reads=[Dm], writes=[sm])
        K.op("dve", lambda e: e.tensor_tensor(out=mf[:, 0:4], in0=iw, in1=mx, op=ALU.max), reads=[sm], writes=[sm])
        K.op("dve", lambda e: e.tensor_copy(out=mf[:, 4:8], in_=Fv), reads=[sm], writes=[sm])
        mtv = mf[:, 0:4]
        K.op("dve", lambda e: e.tensor_scalar(out=negm, in0=mtv, scalar1=-1.0, scalar2=None, op0=ALU.mult), reads=[sm], writes=[sm])
        if need_P:
            for h in range(4):
                K.op("act", lambda e, h=h: e.activation(out=out["P"][:, h, :], in_=Dm[:, h * 128:(h + 1) * 128], func=AF.Exp, bias=negm[:, h:h + 1]),
                     reads=[Dm, sm], writes=[out["Pbuf"]])
            K.op("dve", lambda e: e.tensor_tensor(out=d1, in0=iw, in1=mtv, op=ALU.subtract), reads=[sm], writes=[sm])
            K.op("act", lambda e: e.activation(out=out["a0"], in_=d1, func=AF.Exp), reads=[sm], writes=[out["gsb"]])
            K.op("act", lambda e: e.activation(out=out["floor"], in_=negm, func=AF.Exp, bias=LN16), reads=[sm], writes=[out["gsb"]])
        ps3 = PS()
        K.op("pe", lambda e, ps3=ps3: e.matmul(ps3[:, 0:8], cs(LASTc), mf, start=True, stop=True), reads=[sm, cst], writes=[ps3])
        K.op("act", lambda e, ps3=ps3: e.activation(out=L8, in_=ps3[:, 0:8], func=AF.Copy), reads=[ps3], writes=[sm])
        K.op("dve", lambda e: e.tensor_tensor(out=d2, in0=L8[:, 4:8], in1=L8[:, 0:4], op=ALU.subtract), reads=[sm], writes=[sm])
        K.op("dve", lambda e: e.tensor_tensor(out=t1, in0=g, in1=d2, op=ALU.add), reads=[sm], writes=[sm])
        K.op("act", lambda e: e.activation(out=out["wL"], in_=t1, func=AF.Exp), reads=[sm], writes=[out["gsb"]])
        K.op("dve", lambda e: e.tensor_tensor(out=d1, in0=d2, in1=mprev_ap, op=ALU.add), reads=[sm, mprev_buf], writes=[sm])
        K.op("act", lambda e: e.activation(out=out["decay"], in_=d1, func=AF.Exp), reads=[sm], writes=[out["gsb"]])
        K.op("dve", lambda e: e.tensor_copy(out=out["mlast"], in_=L8[:, 0:4]), reads=[sm], writes=[out["mlast_buf"]])

    def gates_proj(view, ntile, gt):
        sl, wv = wload([w_in[:, 4096:4104]])
        for t in range(ntile):
            ps = PS()
            mm_group(ps[:, 0:8], ps, [(view[:, kc, t * 128:(t + 1) * 128], wv[:, kc, :]) for kc in range(16)], [xT, sl])
            K.op("dve", lambda e, ps=ps, t=t: e.tensor_tensor(out=gt[:, t * 8:(t + 1) * 8], in0=ps[:, 0:8], in1=bgate[:, :], op=ALU.add),
                 reads=[ps, bgate], writes=[gt])

    brow = [A.alloc(f"brow{i}", 256, F32) for i in range(2)]
    browst = {"i": 0}
    st6 = A.alloc("st6", 16, F32)
    hmo = [A.alloc(f"hmo{i}", 256, BF16) for i in range(2)]
    hmost = {"i": 0}
    convo = A.alloc("convo", 48, F32)
    convso = A.alloc("convso", 16 * 48, F32)
    c0T = A.alloc("c0T", 16 * 48, F32)
    cacc = A.alloc("cacc", NTOK, F32)
    m_head = A.mark()
    xtile.append(A.alloc("xtile0", D, F32))
    xtile.append(xtile[0])
    cvb = A.alloc("cvb", 3 + TP, F32)
    cvs = A.alloc("cvs", 16 * 11, F32)
    qT = A.alloc("qT", 2 * NTOK, BF16)
    kT = A.alloc("kT", 2 * NTOK, BF16)
    vaug = A.alloc("vaug", NT * 257, BF16)
    sigo = A.alloc("sigo", NT * 256, BF16)
    gt = A.alloc("gt", NT * 8, F32)
    Pm = A.alloc("Pm", NT * 512, BF16)
    gsb = A.alloc("gsb", NT * 16, F32)
    mprev_s = A.alloc("mprev_s", 4, F32)
    mprev_tok = A.alloc("mprev_tok", 4, F32)
    mlast_smp = A.alloc("mlast_smp", 4, F32)
    dbc = A.alloc("dbc", 64, F32)
    rhsd = A.alloc("rhsd", 64, F32)
    Cb = A.alloc("Cb", 2 * 257, BF16)
    Sc = A.alloc("Sc", 128, BF16)
    ScT = A.alloc("ScT", 128, BF16)
    ktok = A.alloc("ktok", 256, BF16)
    wvt = A.alloc("wvt", 257, BF16)
    tmpi = A.alloc("tmpi", 257, F32)
    num = A.alloc("num", 257, F32)
    hm = A.alloc("hm", 256, F32)
    tmpo = hm
    C0f = [A.alloc(f"C0f{i}", 2 * 257, F32) for i in range(2)]
    C0b = [A.alloc(f"C0b{i}", 2 * 257, BF16) for i in range(2)]
    Cnew = [A.alloc(f"Cnew{i}", 2 * 257, F32) for i in range(2)]
    wvz = [A.alloc(f"wvz{i}", 257, BF16) for i in range(2)]
    Qz = A.alloc("Qz", 2 * 16 * 128, BF16)
    m_end_mlstm = A.mark()

    def load_brow(c0, n):
        b = brow[browst["i"] % 2]
        browst["i"] += 1
        K.dma("sp", b[:, 0:n], b_in[0:1, c0:c0 + n].partition_broadcast(128), writes=[b])
        return b

    def gates_out(t):
        o = t * 16
        return dict(a0=gsb[:, o:o + 4], floor=gsb[:, o + 4:o + 8], wL=gsb[:, o + 8:o + 12], decay=gsb[:, o + 12:o + 16], gsb=gsb.sub(t),
                    P=Pm[:, t * 512:(t + 1) * 512].rearrange("p (h s) -> p h s", h=4), Pbuf=Pm.sub(t), gbuf=gt)

    def conv_silu(cvbuf, T, cb, dst_ap, dst_buf, seg=None):
        if seg is None:
            src = lambda j: cvbuf[:, j:j + T]
            acc = cacc[:, 0:T]
        else:
            nb, L = seg
            src = lambda j: V(cvbuf.t, j, (3 + L, nb), (1, L))
            acc = cacc[:, 0:nb * L].rearrange("p (b l) -> p b l", b=nb)
        K.op("dve", lambda e: e.tensor_scalar(out=acc, in0=src(0), scalar1=cw[:, cb * 4:cb * 4 + 1], scalar2=None, op0=ALU.mult), reads=[cvbuf, cw], writes=[cacc])
        for j in range(1, 4):
            K.op("dve", lambda e, j=j: e.scalar_tensor_tensor(out=acc, in0=src(j), scalar=cw[:, cb * 4 + j:cb * 4 + j + 1], in1=acc, op0=ALU.mult, op1=ALU.add),
                 reads=[cvbuf, cw, cacc], writes=[cacc])
        K.op("act", lambda e: e.activation(out=dst_ap, in_=acc, func=AF.Silu), reads=[cacc], writes=[dst_buf])

    def make_ktok(kT_ap_fn):
        ps = PS()
        psb = ps[:, 0:128].bitcast(BF16)
        for dc in range(2):
            K.op("pe", lambda e, dc=dc, psb=psb: e.transpose(psb[:, dc * 128:(dc + 1) * 128], kT_ap_fn(dc), identB[:, :]), reads=[kT, identB], writes=[ps])
        K.op("act", lambda e, psb=psb: e.activation(out=ktok[:, :], in_=psb, func=AF.Copy), reads=[ps], writes=[ktok])

    def mlstm_state_update(h, t, wL_ap, decay_ap):
        make_ktok(lambda dc: kT[:, dc * NTOK + t * 128:dc * NTOK + (t + 1) * 128])
        K.op("pool", lambda e: e.tensor_scalar(out=wvt[:, :], in0=vaug[:, t * 257:(t + 1) * 257], scalar1=wL_ap, scalar2=None, op0=ALU.mult), reads=[vaug, gsb], writes=[wvt])
        for dc in range(2):
            pc = PS()
            K.op("pe", lambda e, pc=pc, dc=dc: e.matmul(pc[:, 0:257], ktok[:, dc * 128:(dc + 1) * 128], wvt[:, :], start=True, stop=True), reads=[ktok, wvt], writes=[pc])
            cv_ = Cst[h][:, dc * 257:(dc + 1) * 257]
            K.op("dve", lambda e, pc=pc, cv_=cv_: e.scalar_tensor_tensor(out=cv_, in0=cv_, scalar=decay_ap, in1=pc[:, 0:257], op0=ALU.mult, op1=ALU.add),
                 reads=[pc, gsb], writes=[Cst[h]])

    def head_v_proj(view, ntile, h, slv, wv_):
        bv = load_brow(2048 + h * 256, 256)
        K.op("pool", lambda e: e.memset(V(vaug.t, 256, (257, NT), (1, 1)), 1.0), writes=[vaug])
        for t in range(ntile):
            ps = PS()
            mm_group(ps[:, 0:256], ps, [(view[:, kc, t * 128:(t + 1) * 128], wv_[:, kc, :]) for kc in range(16)], [slv, xT])
            K.op("dve", lambda e, ps=ps, t=t, bv=bv: e.tensor_tensor(out=vaug[:, t * 257:t * 257 + 256], in0=ps[:, 0:256], in1=bv[:, 0:256], op=ALU.add),
                 reads=[ps, bv], writes=[vaug])

    def phase_A_mlstm():
        viewA = xTv(TP)
        gates_proj(viewA, 8, gt)
        for t in range(8):
            og = gates_out(t)
            og["mlast"] = mstate[:, :]
            og["mlast_buf"] = mstate
            K.op("dve", lambda e: e.tensor_copy(out=mprev_s[:, :], in_=mstate[:, :]), reads=[mstate], writes=[mprev_s])
            gate_tile(gt[:, t * 8:(t + 1) * 8], C_MP, C_NEGP, C_LASTP, mprev_s[:, :], mprev_s, og, need_P=False)
        K.op("pool", lambda e: e.memset(cvb[:, 0:3], 0.0), writes=[cvb])
        for h in range(4):
            slk, wk = wload([w_in[:, 1024 + h * 256:1024 + (h + 1) * 256]])
            slv, wv_ = wload([w_in[:, 2048 + h * 256:2048 + (h + 1) * 256]])
            for cbk in range(2):
                cb = 8 + h * 2 + cbk
                for tb in range(2):
                    ps = PS()
                    mm_group(ps[:, :], ps, [(wk[:, kc, cbk * 128:(cbk + 1) * 128], viewA[:, kc, tb * 512:(tb + 1) * 512]) for kc in range(16)], [slk, xT])
                    K.op("act", lambda e, ps=ps, tb=tb, cb=cb: e.activation(out=cvb[:, 3 + tb * 512:3 + (tb + 1) * 512], in_=ps[:, :], func=AF.Identity, bias=bqk[:, cb:cb + 1]),
                         reads=[ps, bqk], writes=[cvb])
                conv_silu(cvb, TP, cb, kT[:, cbk * NTOK:cbk * NTOK + TP], kT)
            head_v_proj(viewA, 8, h, slv, wv_)
            for t in range(8):
                og = gates_out(t)
                mlstm_state_update(h, t, og["wL"][:, h:h + 1], og["decay"][:, h:h + 1])
            K.op("dve", lambda e, h=h: e.tensor_scalar(out=Cst[h][:, :], in0=Cst[h][:, :], scalar1=flag[:, 0:1], scalar2=None, op0=ALU.mult), reads=[flag], writes=[Cst[h]])
        K.op("dve", lambda e: e.tensor_scalar(out=mstate[:, :], in0=mstate[:, :], scalar1=flag[:, 0:1], scalar2=None, op0=ALU.mult), reads=[flag], writes=[mstate])

    def mlstm_tile_full(h, t, sample, part="all"):
        og = gates_out(t)
        qs = lambda dc: qT[:, dc * NTOK + t * 128:dc * NTOK + (t + 1) * 128]
        ks = lambda dc: kT[:, dc * NTOK + t * 128:dc * NTOK + (t + 1) * 128]
        alt = (not sample) and (t % 2 == 1)
        Sc_ = wvz[0] if alt else Sc
        ScT_ = wvz[1] if alt else ScT
        tmpi_ = Cnew[1] if alt else tmpi
        num_ = Cnew[0] if alt else num
        ktok_ = C0b[0] if alt else ktok
        wvt_ = C0b[1] if alt else wvt
        if part in ("all", "front"):
            ps = PS()
            mm_group(ps[:, 0:128], ps, [(qs(dc), ks(dc)) for dc in range(2)], [qT, kT])
            K.op("dve", lambda e, ps=ps: e.tensor_tensor(out=Sc_[:, 0:128], in0=ps[:, 0:128], in1=og["P"][:, h, :], op=ALU.mult), reads=[ps, og["Pbuf"]], writes=[Sc_])
            pt = PS()
            ptb = pt[:, 0:64].bitcast(BF16)
            K.op("pe", lambda e, ptb=ptb: e.transpose(ptb, Sc_[:, 0:128], identB[:, :]), reads=[Sc_, identB], writes=[pt])
            K.op("act", lambda e, ptb=ptb: e.activation(out=ScT_[:, 0:128], in_=ptb, func=AF.Copy), reads=[pt], writes=[ScT_])
            pa = psum[7] if sample else PS()
            K.op("pe", lambda e, pa=pa: e.matmul(pa[:, 0:257], ScT_[:, 0:128], vaug[:, t * 257:(t + 1) * 257], start=True, stop=True), reads=[ScT_, vaug], writes=[pa])
            pb = psum[6] if sample else PS()
            if not sample:
                mm_group(pb[:, 0:257], pb, [(qs(dc), Cb[:, dc * 257:(dc + 1) * 257]) for dc in range(2)], [qT, Cb])
            else:
                for dc in range(2):
                    K.op("pool", lambda e, dc=dc: e.tensor_copy(out=V(Qz.t, dc * 2048, (136, 16), (1, 8)),
                                                               in_=qT[:, dc * NTOK + TP:dc * NTOK + NTOK].rearrange("p (b l) -> p b l", b=16)), reads=[qT], writes=[Qz])
                make_ktok(ks)

                def load_c0(b):
                    cf_ = C0f[b % 2]
                    K.dma("sp", V(cf_.t, 0, (257, 2), (1, 256)), I["C0"][b, h].rearrange("(dc p) v -> p dc v", p=128), writes=[cf_])
                    K.dma("sp", V(cf_.t, 256, (257, 2)), I["n0"][b, h].rearrange("(dc p) -> p dc", p=128), writes=[cf_])

                load_c0(0)
                for b in range(16):
                    cf = C0f[b % 2]
                    cbf = C0b[b % 2]
                    cn = Cnew[b % 2]
                    wz = wvz[b % 2]
                    if b + 1 < 16:
                        load_c0(b + 1)
                    K.op("act", lambda e, cf=cf, cbf=cbf: e.activation(out=cbf[:, :], in_=cf[:, :], func=AF.Copy), reads=[cf], writes=[cbf])
                    for dc in range(2):
                        first = (b == 0 and dc == 0)
                        last = (b == 15 and dc == 1)
                        K.op("pe", lambda e, pb=pb, dc=dc, b=b, cbf=cbf, first=first, last=last: e.matmul(
                            pb[:, 0:257], Qz[:, dc * 2048 + b * 128:dc * 2048 + (b + 1) * 128], cbf[:, dc * 257:(dc + 1) * 257], start=first, stop=last),
                            reads=[Qz, cbf], writes=[pb])
                    K.op("pool", lambda e, wz=wz, b=b: e.tensor_scalar(out=wz[:, :], in0=vaug[:, t * 257:(t + 1) * 257], scalar1=og["wL"][:, h:h + 1],
                                                                        scalar2=cst[:, C_SEQM + b:C_SEQM + b + 1], op0=ALU.mult, op1=ALU.mult), reads=[vaug, gsb, cst], writes=[wz])
                    for dc in range(2):
                        pc = PS()
                        K.op("pe", lambda e, pc=pc, dc=dc, wz=wz: e.matmul(pc[:, 0:257], ktok[:, dc * 128:(dc + 1) * 128], wz[:, :], start=True, stop=True), reads=[ktok, wz], writes=[pc])
                        K.op("dve", lambda e, pc=pc, dc=dc, cf=cf, cn=cn, b=b: e.scalar_tensor_tensor(
                            out=cn[:, dc * 257:(dc + 1) * 257], in0=cf[:, dc * 257:(dc + 1) * 257], scalar=dbc[:, h * 16 + b:h * 16 + b + 1], in1=pc[:, 0:257],
                            op0=ALU.mult, op1=ALU.add), reads=[pc, cf, dbc], writes=[cn])
                    K.dma("sp", O["Cs"][b, h].rearrange("(dc p) v -> p dc v", p=128), V(cn.t, 0, (257, 2), (1, 256)), reads=[cn])
                    K.dma("sp", O["ns"][b, h].rearrange("(dc p) -> p dc", p=128), V(cn.t, 256, (257, 2)), reads=[cn])
            K.op("act", lambda e, pb=pb: e.activation(out=tmpi_[:, 0:257], in_=pb[:, 0:257], func=AF.Copy, scale=og["a0"][:, h:h + 1]), reads=[pb, gsb], writes=[tmpi_])
            K.op("dve", lambda e, pa=pa: e.tensor_tensor(out=num_[:, 0:257], in0=tmpi_[:, 0:257], in1=pa[:, 0:257], op=ALU.add), reads=[pa, tmpi_], writes=[num_])
        if part in ("all", "state") and not sample:
            ps = PS()
            psb = ps[:, 0:128].bitcast(BF16)
            for dc in range(2):
                K.op("pe", lambda e, dc=dc, psb=psb: e.transpose(psb[:, dc * 128:(dc + 1) * 128], ks(dc), identB[:, :]), reads=[kT, identB], writes=[ps])
            K.op("act", lambda e, psb=psb: e.activation(out=ktok_[:, 0:256], in_=psb, func=AF.Copy), reads=[ps], writes=[ktok_])
            K.op("pool", lambda e: e.tensor_scalar(out=wvt_[:, 0:257], in0=vaug[:, t * 257:(t + 1) * 257], scalar1=og["wL"][:, h:h + 1], scalar2=None, op0=ALU.mult),
                 reads=[vaug, gsb], writes=[wvt_])
            for dc in range(2):
                pc = PS()
                K.op("pe", lambda e, pc=pc, dc=dc: e.matmul(pc[:, 0:257], ktok_[:, dc * 128:(dc + 1) * 128], wvt_[:, 0:257], start=True, stop=True), reads=[ktok_, wvt_], writes=[pc])
                cv_ = Cst[h][:, dc * 257:(dc + 1) * 257]
                K.op("dve", lambda e, pc=pc, cv_=cv_: e.scalar_tensor_tensor(out=cv_, in0=cv_, scalar=og["decay"][:, h:h + 1], in1=pc[:, 0:257], op0=ALU.mult, op1=ALU.add),
                     reads=[pc, gsb], writes=[Cst[h]])
            K.op("act", lambda e: e.activation(out=Cb[:, :], in_=Cst[h][:, :], func=AF.Copy), reads=[Cst[h]], writes=[Cb])
        if part in ("all", "tail"):
            K.op("dve", lambda e: e.tensor_scalar(out=st6[:, 12:13], in0=num_[:, 256:257], scalar1=-1.0, scalar2=None, op0=ALU.mult), reads=[num_], writes=[st6])
            K.op("dve", lambda e: e.scalar_tensor_tensor(out=st6[:, 0:1], in0=num_[:, 256:257], scalar=og["floor"][:, h:h + 1], in1=st6[:, 12:13], op0=ALU.max, op1=ALU.max),
                 reads=[num_, gsb, st6], writes=[st6])
            K.op("dve", lambda e: e.reciprocal(out=st6[:, 1:2], in_=st6[:, 0:1]), reads=[st6], writes=[st6])
            K.op("dve", lambda e: e.scalar_tensor_tensor(out=hm[:, :], in0=num_[:, 0:256], scalar=st6[:, 1:2], in1=sigo[:, t * 256:(t + 1) * 256], op0=ALU.mult, op1=ALU.mult),
                 reads=[num_, st6, sigo], writes=[hm])
            K.op("dve", lambda e: e.bn_stats(out=st6[:, 2:8], in_=hm[:, :]), reads=[hm], writes=[st6])
            K.op("dve", lambda e: e.bn_aggr(out=st6[:, 8:10], in_=st6[:, 2:8]), reads=[st6], writes=[st6])
            K.op("act", lambda e: e.activation(out=st6[:, 10:11], in_=st6[:, 9:10], func=AF.Sqrt, bias=EPS), reads=[st6], writes=[st6])
            K.op("dve", lambda e: e.reciprocal(out=st6[:, 11:12], in_=st6[:, 10:11]), reads=[st6], writes=[st6])
            ho = hmo[hmost["i"] % 2]
            hmost["i"] += 1
            K.op("dve", lambda e, ho=ho: e.tensor_scalar(out=ho[:, :], in0=hm[:, :], scalar1=st6[:, 8:9], scalar2=st6[:, 11:12], op0=ALU.subtract, op1=ALU.mult),
                 reads=[hm, st6], writes=[ho])
            K.dma("sp", hm_scr[t * 128:(t + 1) * 128, h * 256:(h + 1) * 256], ho[:, :], reads=[ho], writes=[hm_scr_b])

    def phase_B_mlstm():
        viewB = xTv(NTOK)
        cvin = xtile[0]
        K.dma("sp", cvin[0:48, :], I["conv0"], writes=[cvin])
        for g in range(4):
            ps = PS()
            for j in range(4):
                cb = g * 4 + j
                K.op("pe", lambda e, ps=ps, j=j, cb=cb: e.transpose(ps[:, j * 48:(j + 1) * 48], cvin[0:48, cb * 128:(cb + 1) * 128], cst[0:48, C_ID:C_ID + 48]),
                     reads=[cvin, cst], writes=[ps])
            K.op("act", lambda e, ps=ps, g=g: e.activation(out=c0T[:, g * 192:(g + 1) * 192], in_=ps[:, 0:192], func=AF.Copy), reads=[ps], writes=[c0T])
        if cut <= 1:
            return
        gates_proj(viewB, 9, gt)
        for t in range(8):
            og = gates_out(t)
            og["mlast"] = mstate[:, :]
            og["mlast_buf"] = mstate
            K.op("dve", lambda e: e.tensor_copy(out=mprev_s[:, :], in_=mstate[:, :]), reads=[mstate], writes=[mprev_s])
            gate_tile(gt[:, t * 8:(t + 1) * 8], C_MP, C_NEGP, C_LASTP, mprev_s[:, :], mprev_s, og, need_P=True)
        K.dma("sp", O["mp"], mstate[0:1, :], reads=[mstate])
        if cut <= 2:
            return
        K.dma("sp", mprev_tok[:, :], bass.AP(tensor=I["m0"].tensor, offset=0, ap=[[4, 16], [0, 8], [1, 4]]), writes=[mprev_tok])
        og = gates_out(8)
        og["mlast"] = mlast_smp[:, :]
        og["mlast_buf"] = mlast_smp
        gate_tile(gt[:, 64:72], C_MS, C_NEGS, C_LASTS, mprev_tok[:, :], mprev_tok, og, need_P=True)
        K.dma("sp", O["ms"], bass.AP(tensor=mlast_smp.t, offset=7 * 4, ap=[[32, 16], [1, 4]]), reads=[mlast_smp])
        for h in range(4):
            K.op("dve", lambda e, h=h: e.tensor_scalar(out=rhsd[:, h * 16:(h + 1) * 16], in0=cst[:, C_SEQ1:C_SEQ1 + 16], scalar1=og["decay"][:, h:h + 1], scalar2=None, op0=ALU.mult),
                 reads=[cst, gsb], writes=[rhsd])
        ps = PS()
        K.op("pe", lambda e, ps=ps: e.matmul(ps[:, 0:64], ones, rhsd[:, :], start=True, stop=True), reads=[rhsd, cst], writes=[ps])
        K.op("act", lambda e, ps=ps: e.activation(out=dbc[:, :], in_=ps[:, 0:64], func=AF.Copy), reads=[ps], writes=[dbc])
        K.op("pool", lambda e: e.memset(Qz[:, :], 0.0), writes=[Qz])
        xh = xhalo[:, :].rearrange("p (k j) -> p k j", k=16)
        if cut <= 3:
            return
        for h in range(4):
            slq, wq = wload([w_in[:, h * 256:(h + 1) * 256]])
            slk, wk = wload([w_in[:, 1024 + h * 256:1024 + (h + 1) * 256]])
            slv, wv_ = wload([w_in[:, 2048 + h * 256:2048 + (h + 1) * 256]])
            slo, wo_ = wload([w_in[:, 3072 + h * 256:3072 + (h + 1) * 256]])
            for (sl, wt, cb0, dstT) in ((slq, wq, h * 2, qT), (slk, wk, 8 + h * 2, kT)):
                for cbk in range(2):
                    cb = cb0 + cbk
                    lw = lambda kc, wt=wt, cbk=cbk: wt[:, kc, cbk * 128:(cbk + 1) * 128]
                    ps = PS()
                    mm_group(ps[:, 0:3], ps, [(lw(kc), xh[:, kc, :]) for kc in range(16)], [sl, xhalo])
                    K.op("act", lambda e, ps=ps, cb=cb: e.activation(out=cvb[:, 0:3], in_=ps[:, 0:3], func=AF.Identity, bias=bqk[:, cb:cb + 1]), reads=[ps, bqk], writes=[cvb])
                    K.op("dve", lambda e: e.tensor_scalar(out=cvb[:, 0:3], in0=cvb[:, 0:3], scalar1=flag[:, 0:1], scalar2=None, op0=ALU.mult), reads=[flag], writes=[cvb])
                    for tb in range(2):
                        ps = PS()
                        mm_group(ps[:, :], ps, [(lw(kc), viewB[:, kc, tb * 512:(tb + 1) * 512]) for kc in range(16)], [sl, xT])
                        K.op("act", lambda e, ps=ps, tb=tb, cb=cb: e.activation(out=cvb[:, 3 + tb * 512:3 + (tb + 1) * 512], in_=ps[:, :], func=AF.Identity, bias=bqk[:, cb:cb + 1]),
                             reads=[ps, bqk], writes=[cvb])
                    ps = PS()
                    mm_group(ps[:, 0:128], ps, [(lw(kc), viewB[:, kc, TP:NTOK]) for kc in range(16)], [sl, xT])
                    K.op("act", lambda e, ps=ps, cb=cb: e.activation(out=V(cvs.t, 3, (11, 16), (1, 8)), in_=ps[:, 0:128].rearrange("p (b l) -> p b l", b=16), func=AF.Identity,
                                                                     bias=bqk[:, cb:cb + 1]), reads=[ps, bqk], writes=[cvs])
                    K.op("dve", lambda e, cb=cb: e.tensor_copy(out=V(cvs.t, 0, (11, 16), (1, 3)), in_=c0T[:, cb * 48:(cb + 1) * 48].rearrange("p (b j) -> p b j", b=16)),
                         reads=[c0T], writes=[cvs])
                    conv_silu(cvb, TP, cb, dstT[:, cbk * NTOK:cbk * NTOK + TP], dstT)
                    conv_silu(cvs, 128, cb, dstT[:, cbk * NTOK + TP:cbk * NTOK + NTOK].rearrange("p (b l) -> p b l", b=16), dstT, seg=(16, 8))
                    K.op("pool", lambda e, cb=cb: e.tensor_copy(out=convo[:, cb * 3:(cb + 1) * 3], in_=cvb[:, TP:TP + 3]), reads=[cvb], writes=[convo])
                    K.op("pool", lambda e, cb=cb: e.tensor_copy(out=convso[:, cb * 48:(cb + 1) * 48].rearrange("p (b j) -> p b j", b=16), in_=V(cvs.t, 8, (11, 16), (1, 3))),
                         reads=[cvs], writes=[convso])
            head_v_proj(viewB, 9, h, slv, wv_)
            bo = load_brow(3072 + h * 256, 256)
            for t in range(9):
                ps = PS()
                mm_group(ps[:, 0:256], ps, [(viewB[:, kc, t * 128:(t + 1) * 128], wo_[:, kc, :]) for kc in range(16)], [slo, xT])
                K.op("dve", lambda e, ps=ps, bo=bo: e.tensor_tensor(out=tmpo[:, :], in0=ps[:, 0:256], in1=bo[:, 0:256], op=ALU.add), reads=[ps, bo], writes=[tmpo])
                K.op("act", lambda e, t=t: e.activation(out=sigo[:, t * 256:(t + 1) * 256], in_=tmpo[:, :], func=AF.Sigmoid), reads=[tmpo], writes=[sigo])
            K.op("act", lambda e, h=h: e.activation(out=Cb[:, :], in_=Cst[h][:, :], func=AF.Copy), reads=[Cst[h]], writes=[Cb])
            if cut <= 4:
                continue
            mlstm_tile_full(h, 0, False, "front")
            mlstm_tile_full(h, 0, False, "state")
            for t in range(1, 8):
                mlstm_tile_full(h, t, False, "front")
                mlstm_tile_full(h, t, False, "state")
                mlstm_tile_full(h, t - 1, False, "tail")
            mlstm_tile_full(h, 7, False, "tail")
            K.dma("sp", O["Cp"][h].rearrange("(dc p) v -> p dc v", p=128), V(Cst[h].t, 0, (257, 2), (1, 256)), reads=[Cst[h]])
            K.dma("sp", O["np_"][h].rearrange("(dc p) -> p dc", p=128), V(Cst[h].t, 256, (257, 2)), reads=[Cst[h]])
            if cut <= 5:
                continue
            mlstm_tile_full(h, 8, True)
        for cb in range(16):
            K.dma("sp", O["convp"][:, cb * 128:(cb + 1) * 128].rearrange("j p -> p j"), convo[:, cb * 3:(cb + 1) * 3], reads=[convo])
            K.dma("sp", O["convs"][:, :, cb * 128:(cb + 1) * 128].rearrange("b j p -> p b j"), convso[:, cb * 48:(cb + 1) * 48].rearrange("p (b j) -> p b j", b=16), reads=[convso])

    A.release(m_head)
    sg = A.alloc("sg", NTOK, F32)
    lf = A.alloc("lf", NTOK, F32)
    kk = A.alloc("kk", NTOK, F32)
    bb = A.alloc("bb", NTOK, F32)
    qh = A.alloc("qh", NTOK, F32)
    ex = lf
    qb = A.alloc("qb", NTOK, BF16)
    kb = A.alloc("kb", NTOK, BF16)
    qS = A.alloc("qS", NTOK, BF16)
    kLT = A.alloc("kLT", NTOK, BF16)
    vtok = A.alloc("vtok", NT * 128, BF16)
    gg = A.alloc("gg", NT * 128, BF16)
    eL = A.alloc("eL", 24, F32)
    Sb = A.alloc("Sb", 128, BF16)
    ATb = A.alloc("ATb", 128, BF16)
    kLtok = A.alloc("kLtok", 128, BF16)
    ogo = [A.alloc(f"ogo{i}", 128, BF16) for i in range(2)]
    ogst = {"i": 0}
    junk = A.alloc("junk", 128, F32)
    S0f = A.alloc("S0f", 2048, F32)
    S0b = A.alloc("S0b", 2048, BF16)
    Vz = [A.alloc(f"Vz{i}", 512, BF16) for i in range(2)]
    QzH = A.alloc("QzH", 2048, BF16)
    m_end_hgrn = A.mark()

    def hgrn_head(hh, view, ntok, full):
        ntile = ntok // 128
        tbs = [(0, 512), (512, 512)] + ([(1024, 128)] if ntok > 1024 else [])
        if full and cut > 7:
            K.dma("sp", S0f[:, :].rearrange("p (b v) -> p b v", b=16), I["S0"][:, hh].rearrange("b c v -> c b v"), writes=[S0f])
        cf0 = 5128 + hh * 128
        ci0 = 6152 + hh * 128
        if full:
            sl1, w1 = wload([w_in[:, 4104 + hh * 128:4104 + (hh + 1) * 128], w_in[:, cf0:cf0 + 128]])
            sl2, w2 = wload([w_in[:, ci0:ci0 + 128], w_in[:, 7176 + hh * 128:7176 + (hh + 1) * 128]])
            fcol, icol = 128, 0
        else:
            sl1, w1 = wload([w_in[:, cf0:cf0 + 128], w_in[:, ci0:ci0 + 128]])
            sl2, w2 = sl1, w1
            fcol, icol = 0, 128
        for (t0, n) in tbs:
            ps = PS()
            mm_group(ps[:, 0:n], ps, [(w1[:, kc, fcol:fcol + 128], view[:, kc, t0:t0 + n]) for kc in range(16)], [sl1, xT])
            K.op("act", lambda e, ps=ps, t0=t0, n=n: e.activation(out=sg[:, t0:t0 + n], in_=ps[:, 0:n], func=AF.Sigmoid, bias=bfh[:, hh:hh + 1]), reads=[ps, bfh], writes=[sg])
            if full:
                ps = PS()
                mm_group(ps[:, 0:n], ps, [(w1[:, kc, 0:128], view[:, kc, t0:t0 + n]) for kc in range(16)], [sl1, xT])
                K.op("act", lambda e, ps=ps, t0=t0, n=n: e.activation(out=qh[:, t0:t0 + n], in_=ps[:, 0:n], func=AF.Silu, bias=bqh[:, hh:hh + 1]), reads=[ps, bqh], writes=[qh])
        N = ntok
        K.op("dve", lambda e: e.tensor_scalar(out=sg[:, 0:N], in0=sg[:, 0:N], scalar1=oml[:, hh:hh + 1], scalar2=lb[:, hh:hh + 1], op0=ALU.mult, op1=ALU.add), reads=[oml, lb], writes=[sg])
        K.op("act", lambda e: e.activation(out=lf[:, 0:N], in_=sg[:, 0:N], func=AF.Ln), reads=[sg], writes=[lf])
        K.op("pool", lambda e: e.tensor_scalar(out=kk[:, 0:N], in0=sg[:, 0:N], scalar1=-1.0, scalar2=1.0, op0=ALU.mult, op1=ALU.add), reads=[sg], writes=[kk])
        bi = load_brow(ci0, 128)
        if not full:
            for t in range(ntile):
                ps = PS()
                mm_group(ps[:, 0:128], ps, [(view[:, kc, t * 128:(t + 1) * 128], w2[:, kc, icol:icol + 128]) for kc in range(16)], [sl2, xT])
                K.op("dve", lambda e, ps=ps, t=t, bi=bi: e.tensor_tensor(out=vtok[:, t * 128:(t + 1) * 128], in0=ps[:, 0:128], in1=bi[:, 0:128], op=ALU.add), reads=[ps, bi], writes=[vtok])
        else:
            bg = load_brow(7176 + hh * 128, 128)
            for t in range(ntile):
                ps = PS()
                mm_group(ps[:, 0:256], ps, [(view[:, kc, t * 128:(t + 1) * 128], w2[:, kc, :]) for kc in range(16)], [sl2, xT])
                K.op("dve", lambda e, ps=ps, t=t, bi=bi: e.tensor_tensor(out=vtok[:, t * 128:(t + 1) * 128], in0=ps[:, 0:128], in1=bi[:, 0:128], op=ALU.add), reads=[ps, bi], writes=[vtok])
                K.op("dve", lambda e, ps=ps, bg=bg: e.tensor_tensor(out=junk[:, :], in0=ps[:, 128:256], in1=bg[:, 0:128], op=ALU.add), reads=[ps, bg], writes=[junk])
                K.op("act", lambda e, t=t: e.activation(out=gg[:, t * 128:(t + 1) * 128], in_=junk[:, :], func=AF.Silu), reads=[junk], writes=[gg])
        K.op("dve", lambda e: e.tensor_tensor_scan(out=bb[:, 0:N], data0=cst[:, C_RM:C_RM + N], data1=lf[:, 0:N], initial=0.0, op0=ALU.mult, op1=ALU.add), reads=[cst, lf], writes=[bb])
        npt = 8
        K.op("dve", lambda e: e.tensor_tensor(out=ex[:, 0:1024].rearrange("p (t s) -> p t s", t=npt), in0=V(bb.t, 127, (128, npt), (0, 128)),
                                              in1=bb[:, 0:1024].rearrange("p (t s) -> p t s", t=npt), op=ALU.subtract), reads=[bb], writes=[ex])
        if N > 1024:
            K.op("dve", lambda e: e.tensor_tensor(out=ex[:, 1024:N].rearrange("p (b l) -> p b l", b=16), in0=V(bb.t, 1024 + 7, (8, 16), (0, 8)),
                                                  in1=bb[:, 1024:N].rearrange("p (b l) -> p b l", b=16), op=ALU.subtract), reads=[bb], writes=[ex])
        K.op("act", lambda e: e.activation(out=ex[:, 0:N], in_=ex[:, 0:N], func=AF.Exp), reads=[ex], writes=[ex])
        K.op("dve", lambda e: e.tensor_tensor(out=kLT[:, 0:N], in0=kk[:, 0:N], in1=ex[:, 0:N], op=ALU.mult), reads=[kk, ex], writes=[kLT])
        K.op("act", lambda e: e.activation(out=eL[:, 0:8], in_=V(bb.t, 127, (128, 8)), func=AF.Exp), reads=[bb], writes=[eL])
        if N > 1024:
            K.op("act", lambda e: e.activation(out=eL[:, 8:24], in_=V(bb.t, 1024 + 7, (8, 16)), func=AF.Exp), reads=[bb], writes=[eL])
        if full:
            nt_all = N // 128
            K.op("dve", lambda e: e.tensor_tensor(out=ex[:, 0:N].rearrange("p (t s) -> p t s", t=nt_all), in0=bb[:, 0:N].rearrange("p (t s) -> p t s", t=nt_all),
                                                  in1=V(bb.t, 63, (128, nt_all), (0, 128)), op=ALU.subtract), reads=[bb, kLT], writes=[ex])
            K.op("act", lambda e: e.activation(out=sg[:, 0:N], in_=ex[:, 0:N], func=AF.Exp), reads=[ex], writes=[sg])
            K.op("dve", lambda e: e.tensor_tensor(out=qb[:, 0:N], in0=qh[:, 0:N], in1=sg[:, 0:N], op=ALU.mult), reads=[qh, sg], writes=[qb])
            K.op("act", lambda e: e.activation(out=sg[:, 0:N], in_=ex[:, 0:N], func=AF.Exp, scale=-1.0), reads=[ex, qb], writes=[sg])
            K.op("dve", lambda e: e.tensor_tensor(out=kb[:, 0:N], in0=kk[:, 0:N], in1=sg[:, 0:N], op=ALU.mult), reads=[kk, sg], writes=[kb])
            K.op("act", lambda e: e.activation(out=sg[:, 0:N], in_=bb[:, 0:N], func=AF.Exp), reads=[bb, kb], writes=[sg])
            K.op("dve", lambda e: e.tensor_tensor(out=qS[:, 0:N], in0=qh[:, 0:N], in1=sg[:, 0:N], op=ALU.mult), reads=[qh, sg], writes=[qS])
            K.op("act", lambda e: e.activation(out=Sb[:, :], in_=Sst[hh][:, :], func=AF.Copy), reads=[Sst[hh]], writes=[Sb])

        def norm_out(po, t):
            K.op("act", lambda e, po=po: e.activation(out=junk[:, :], in_=po[:, 0:128], func=AF.Square, accum_out=st6[:, 0:1]), reads=[po], writes=[junk, st6])
            K.op("act", lambda e: e.activation(out=st6[:, 1:2], in_=st6[:, 0:1], func=AF.Sqrt, scale=1.0 / 128.0, bias=EPS), reads=[st6], writes=[st6])
            K.op("dve", lambda e: e.reciprocal(out=st6[:, 2:3], in_=st6[:, 1:2]), reads=[st6], writes=[st6])
            oo = ogo[ogst["i"] % 2]
            ogst["i"] += 1
            K.op("dve", lambda e, po=po, oo=oo: e.scalar_tensor_tensor(out=oo[:, :], in0=po[:, 0:128], scalar=st6[:, 2:3], in1=gg[:, t * 128:(t + 1) * 128], op0=ALU.mult, op1=ALU.mult),
                 reads=[po, st6, gg], writes=[oo])
            K.dma("sp", og_scr[t * 128:(t + 1) * 128, hh * 128:(hh + 1) * 128], oo[:, :], reads=[oo], writes=[og_scr_b])

        def make_kLtok(t):
            pt = PS()
            ptb = pt[:, 0:64].bitcast(BF16)
            K.op("pe", lambda e, ptb=ptb: e.transpose(ptb, kLT[:, t * 128:(t + 1) * 128], identB[:, :]), reads=[kLT, identB], writes=[pt])
            K.op("act", lambda e, ptb=ptb: e.activation(out=kLtok[:, :], in_=ptb, func=AF.Copy), reads=[pt], writes=[kLtok])

        for t in range(8):
            sl_t = slice(t * 128, (t + 1) * 128)
            if full:
                pA = PS()
                K.op("pe", lambda e, pA=pA, sl_t=sl_t: e.matmul(pA[:, 0:128], kb[:, sl_t], qb[:, sl_t], start=True, stop=True), reads=[kb, qb], writes=[pA])
                K.op("dve", lambda e, pA=pA: e.tensor_tensor(out=ATb[:, :], in0=pA[:, 0:128], in1=cs(C_MP), op=ALU.mult), reads=[pA, cst], writes=[ATb])
                po = PS()
                mm_group(po[:, 0:128], po, [(ATb[:, :], vtok[:, sl_t]), (qS[:, sl_t], Sb[:, :])], [ATb, vtok, qS, Sb])
                norm_out(po, t)
            make_kLtok(t)
            pc = PS()
            K.op("pe", lambda e, pc=pc, sl_t=sl_t: e.matmul(pc[:, 0:128], kLtok[:, :], vtok[:, sl_t], start=True, stop=True), reads=[kLtok, vtok], writes=[pc])
            K.op("dve", lambda e, pc=pc, t=t: e.scalar_tensor_tensor(out=Sst[hh][:, :], in0=Sst[hh][:, :], scalar=eL[:, t:t + 1], in1=pc[:, 0:128], op0=ALU.mult, op1=ALU.add),
                 reads=[pc, eL], writes=[Sst[hh]])
            if full:
                K.op("act", lambda e: e.activation(out=Sb[:, :], in_=Sst[hh][:, :], func=AF.Copy), reads=[Sst[hh]], writes=[Sb])
        if not full:
            K.op("dve", lambda e: e.tensor_scalar(out=Sst[hh][:, :], in0=Sst[hh][:, :], scalar1=flag[:, 0:1], scalar2=None, op0=ALU.mult), reads=[flag], writes=[Sst[hh]])
            return
        K.dma("sp", O["Sp"][hh], Sst[hh][:, :], reads=[Sst[hh]])
        if cut <= 7:
            return
        t = 8
        sl_t = slice(TP, NTOK)
        K.op("act", lambda e: e.activation(out=S0b[:, :], in_=S0f[:, :], func=AF.Copy), reads=[S0f], writes=[S0b])
        K.op("pool", lambda e: e.tensor_copy(out=V(QzH.t, 0, (136, 16), (1, 8)), in_=qS[:, sl_t].rearrange("p (b l) -> p b l", b=16)), reads=[qS], writes=[QzH])
        pA = PS()
        K.op("pe", lambda e, pA=pA: e.matmul(pA[:, 0:128], kb[:, sl_t], qb[:, sl_t], start=True, stop=True), reads=[kb, qb], writes=[pA])
        K.op("dve", lambda e, pA=pA: e.tensor_tensor(out=ATb[:, :], in0=pA[:, 0:128], in1=cs(C_MS), op=ALU.mult), reads=[pA, cst], writes=[ATb])
        po = PS()
        mm_group(po[:, 0:128], po, [(ATb[:, :], vtok[:, t * 128:(t + 1) * 128])] + [(QzH[:, b * 128:(b + 1) * 128], S0b[:, b * 128:(b + 1) * 128]) for b in range(16)],
                 [ATb, vtok, QzH, S0b])
        norm_out(po, t)
        make_kLtok(t)
        for g in range(4):
            vz = Vz[g % 2]
            K.op("dve", lambda e, g=g, vz=vz: e.tensor_tensor(out=vz[:, :].rearrange("p (b v) -> p b v", b=4), in0=V(vtok.t, t * 128, (0, 4), (1, 128)),
                                                               in1=V(cst.t, C_SEQM + g * 4, (1, 4), (0, 128)), op=ALU.mult), reads=[vtok, cst], writes=[vz])
            pc = PS()
            K.op("pe", lambda e, pc=pc, vz=vz: e.matmul(pc[:, :], kLtok[:, :], vz[:, :], start=True, stop=True), reads=[kLtok, vz], writes=[pc])
            K.op("dve", lambda e, g=g: e.tensor_tensor(out=S0f[:, g * 512:(g + 1) * 512].rearrange("p (b v) -> p b v", b=4), in0=S0f[:, g * 512:(g + 1) * 512].rearrange("p (b v) -> p b v", b=4),
                                                       in1=V(eL.t, 8 + g * 4, (1, 4), (0, 128)), op=ALU.mult), reads=[eL, S0b], writes=[S0f])
            K.op("dve", lambda e, pc=pc, g=g: e.tensor_tensor(out=S0f[:, g * 512:(g + 1) * 512], in0=S0f[:, g * 512:(g + 1) * 512], in1=pc[:, :], op=ALU.add), reads=[pc], writes=[S0f])
        K.dma("sp", O["Ss"][:, hh].rearrange("b c v -> c b v"), S0f[:, :].rearrange("p (b v) -> p b v", b=16), reads=[S0f])

    if stage >= 1:
        build_xT([(I["xpre"], 8)], TP)
        K.op("dve", lambda e: e.tensor_copy(out=xhalo[:, :].rearrange("p (k j) -> p k j", k=16), in_=xTv(TP)[:, :, TP - 3:TP]), reads=[xT], writes=[xhalo])
        phase_A_mlstm()
        K.barrier()
        K.op("pool", lambda e: e.memset(QzH[:, :], 0.0), writes=[QzH])
        for hh in range(8):
            hgrn_head(hh, xTv(TP), TP, False)
        K.barrier()
    if stage >= 2:
        build_xT([(I["xp"], 8), (I["xs"], 1)], NTOK)
        phase_B_mlstm()
        K.barrier()
        K.op("pool", lambda e: e.memset(QzH[:, :], 0.0), writes=[QzH])
        if cut > 6:
            for hh in range(8):
                hgrn_head(hh, xTv(NTOK), NTOK, True)
        K.barrier()

    r_scr = nc.dram_tensor("r_scr", [NTOK, D], F32, kind="Internal").ap()
    h_scr = nc.dram_tensor("h_scr", [NTOK, D], F32, kind="Internal").ap()
    r_scr_b = Buf("r_scr")
    h_scr_b = Buf("h_scr")

    def xrows(t):
        return I["xp"][t * 128:(t + 1) * 128, :] if t < 8 else I["xs"]

    def layer_norm_tiles(g_in, b_in_, dstT_buf, dstT_view, tilebufs, lng, lnb, lnst, out_dram=None, out_fn=None, inplace=False):
        K.dma("sp", lng[:, :], g_in[0:1, :].partition_broadcast(128), writes=[lng])
        K.dma("sp", lnb[:, :], b_in_[0:1, :].partition_broadcast(128), writes=[lnb])
        if not inplace:
            K.dma("sp", tilebufs[0][:, :], r_scr[0:128, :], reads=[r_scr_b], writes=[tilebufs[0]])
        for t in range(NT):
            if inplace:
                rt = tilebufs[t]
            else:
                rt = tilebufs[t % 2]
                if t + 1 < NT:
                    K.dma("sp", tilebufs[(t + 1) % 2][:, :], r_scr[(t + 1) * 128:(t + 2) * 128, :], reads=[r_scr_b], writes=[tilebufs[(t + 1) % 2]])
            for c in range(4):
                K.op("dve", lambda e, c=c, rt=rt: e.bn_stats(out=lnst[:, c * 6:(c + 1) * 6], in_=rt[:, c * 512:(c + 1) * 512]), reads=[rt], writes=[lnst])
            K.op("dve", lambda e: e.bn_aggr(out=lnst[:, 24:26], in_=lnst[:, 0:24]), reads=[lnst], writes=[lnst])
            K.op("act", lambda e: e.activation(out=lnst[:, 26:27], in_=lnst[:, 25:26], func=AF.Sqrt, bias=EPS), reads=[lnst], writes=[lnst])
            K.op("dve", lambda e: e.reciprocal(out=lnst[:, 27:28], in_=lnst[:, 26:27]), reads=[lnst], writes=[lnst])
            K.op("dve", lambda e, rt=rt: e.tensor_scalar(out=rt[:, :], in0=rt[:, :], scalar1=lnst[:, 24:25], scalar2=lnst[:, 27:28], op0=ALU.subtract, op1=ALU.mult),
                 reads=[lnst], writes=[rt])
            K.op("pool", lambda e, rt=rt: e.tensor_tensor(out=rt[:, :], in0=rt[:, :], in1=lng[:, :], op=ALU.mult), reads=[lng], writes=[rt])
            K.op("dve", lambda e, rt=rt: e.tensor_tensor(out=rt[:, :], in0=rt[:, :], in1=lnb[:, :], op=ALU.add), reads=[lnb], writes=[rt])
            if out_fn is not None:
                K.dma("sp", out_fn(t), rt[:, :], reads=[rt])
            if out_dram is not None:
                K.dma("sp", out_dram[t * 128:(t + 1) * 128, :], rt[:, :], reads=[rt], writes=[h_scr_b])
            if dstT_view is not None:
                transpose_tile_f32(rt, lambda kc, rt=rt: rt[:, kc * 128:(kc + 1) * 128], dstT_buf, dstT_view, t * 128)

    def out_proj_residual(lhsT_buf, lhsT_view, nkc, w_dram, res_fn, res_bufs, xr, ro):
        items = [(cblk, t) for cblk in range(8) for t in range(NT)]
        nb = len(xr)

        def issue_load(i):
            cblk, t = items[i]
            x_ = xr[i % nb]
            K.dma("sp", x_[:, :], res_fn(t)[:, cblk * 256:(cblk + 1) * 256], reads=res_bufs, writes=[x_])

        for i in range(min(nb - 1, len(items))):
            issue_load(i)
        sl = w = None
        for i, (cblk, t) in enumerate(items):
            if t == 0:
                sl, w = wload([w_dram[:, cblk * 256:(cblk + 1) * 256]])
            if i + nb - 1 < len(items):
                issue_load(i + nb - 1)
            ps = PS()
            mm_group(ps[:, 0:256], ps, [(lhsT_view[:, kc, t * 128:(t + 1) * 128], w[:, kc, :]) for kc in range(nkc)], [lhsT_buf, sl])
            x_ = xr[i % nb]
            r_ = ro[i % nb]
            K.op("dve", lambda e, ps=ps, x_=x_, r_=r_: e.scalar_tensor_tensor(out=r_[:, :], in0=x_[:, :], scalar=ALPHA, in1=ps[:, 0:256], op0=ALU.mult, op1=ALU.add),
                 reads=[ps, x_], writes=[r_])
            K.dma("sp", r_scr[t * 128:(t + 1) * 128, cblk * 256:(cblk + 1) * 256], r_[:, :], reads=[r_], writes=[r_scr_b])

    if stage >= 3:
        A.release(m_mix)
        hmT = A.alloc("hmT", 8 * NTOK, BF16)
        ogT = A.alloc("ogT", 8 * NTOK, BF16)
        mT = A.alloc("mT", 16 * NTOK, BF16)
        tl = [A.alloc(f"tl{i}", 1024, BF16) for i in range(2)]
        sgm = A.alloc("sgm", 512, F32)
        sgh = A.alloc("sgh", 512, F32)
        mm1 = sgm
        mm2 = sgh
        xr = [A.alloc(f"xr{i}", 256, F32) for i in range(3)]
        ro = [A.alloc(f"ro{i}", 256, F32) for i in range(3)]
        i_tl = 0
        for (scr, scr_b, dstT, goff) in ((hm_scr, hm_scr_b, hmT, 0), (og_scr, og_scr_b, ogT, 8)):
            for t in range(NT):
                tb = tl[i_tl % 2]
                i_tl += 1
                K.dma("sp", tb[:, :], scr[t * 128:(t + 1) * 128, :], reads=[scr_b], writes=[tb])
                for g in range(2):
                    ps = PS()
                    psb = ps[:, 0:256].bitcast(BF16)
                    for j in range(4):
                        kc = g * 4 + j
                        K.op("pe", lambda e, psb=psb, j=j, kc=kc, tb=tb: e.transpose(psb[:, j * 128:(j + 1) * 128], tb[:, kc * 128:(kc + 1) * 128], identB[:, :]),
                             reads=[tb, identB], writes=[ps])
                    for j in range(4):
                        kc = g * 4 + j
                        K.op("act", lambda e, psb=psb, j=j, kc=kc, dstT=dstT, goff=goff, t=t: e.activation(
                            out=dstT[:, kc * NTOK + t * 128:kc * NTOK + (t + 1) * 128], in_=psb[:, j * 128:(j + 1) * 128], func=AF.Copy, scale=gnT[:, goff + kc:goff + kc + 1]),
                            reads=[ps, gnT], writes=[dstT])
        viewX = xTv(NTOK)
        hmTv = hmT[:, :].rearrange("p (k t) -> p k t", k=8)
        ogTv = ogT[:, :].rearrange("p (k t) -> p k t", k=8)
        mTv = mT[:, :].rearrange("p (k t) -> p k t", k=16)
        for pr in range(8):
            slgm, wgm = wload([w_in[:, 8200 + pr * 256:8200 + (pr + 1) * 256]])
            slgh, wgh = wload([w_in[:, 10248 + pr * 256:10248 + (pr + 1) * 256]])
            slbm, wbm = wload([I["w_bm"][:, pr * 256:(pr + 1) * 256]])
            slbh, wbh = wload([I["w_bh"][:, pr * 256:(pr + 1) * 256]])
            for c2 in range(2):
                cb = pr * 2 + c2
                cs_ = slice(c2 * 128, (c2 + 1) * 128)
                for (t0, n) in ((0, 512), (512, 512), (1024, 128)):
                    pgm = PS()
                    mm_group(pgm[:, 0:n], pgm, [(wgm[:, kc, cs_], viewX[:, kc, t0:t0 + n]) for kc in range(16)], [slgm, xT])
                    K.op("act", lambda e, pgm=pgm, n=n, cb=cb: e.activation(out=sgm[:, 0:n], in_=pgm[:, 0:n], func=AF.Sigmoid, bias=bgm[:, cb:cb + 1]), reads=[pgm, bgm], writes=[sgm])
                    pgh = PS()
                    mm_group(pgh[:, 0:n], pgh, [(wgh[:, kc, cs_], viewX[:, kc, t0:t0 + n]) for kc in range(16)], [slgh, xT])
                    K.op("act", lambda e, pgh=pgh, n=n, cb=cb: e.activation(out=sgh[:, 0:n], in_=pgh[:, 0:n], func=AF.Sigmoid, bias=bgh[:, cb:cb + 1]), reads=[pgh, bgh], writes=[sgh])
                    ppm = PS()
                    mm_group(ppm[:, 0:n], ppm, [(wbm[:, kc, cs_], hmTv[:, kc, t0:t0 + n]) for kc in range(8)], [slbm, hmT])
                    K.op("dve", lambda e, ppm=ppm, n=n: e.tensor_tensor(out=mm1[:, 0:n], in0=sgm[:, 0:n], in1=ppm[:, 0:n], op=ALU.mult), reads=[ppm, sgm], writes=[mm1])
                    pph = PS()
                    mm_group(pph[:, 0:n], pph, [(wbh[:, kc, cs_], ogTv[:, kc, t0:t0 + n]) for kc in range(8)], [slbh, ogT])
                    K.op("dve", lambda e, pph=pph, n=n: e.tensor_tensor(out=mm2[:, 0:n], in0=sgh[:, 0:n], in1=pph[:, 0:n], op=ALU.mult), reads=[pph, sgh], writes=[mm2])
                    K.op("pool", lambda e, n=n, cb=cb, t0=t0: e.tensor_tensor(out=mTv[:, cb, t0:t0 + n], in0=mm1[:, 0:n], in1=mm2[:, 0:n], op=ALU.add), reads=[mm1, mm2], writes=[mT])
        out_proj_residual(mT, mTv, 16, I["w_out"], xrows, [], xr, ro)
        K.barrier()
        m_ln = A.mark()
        A.release(m_mix)
        tilebufs = [A.alloc(f"tilebuf{i}", D, F32) for i in range(2)]
        lng = A.alloc("lng", D, F32)
        lnb = A.alloc("lnb", D, F32)
        lnst = A.alloc("lnst", 32, F32)
        dbg = None
        if stage == 3:
            dbg = lambda t: (O["y_p"][t * 128:(t + 1) * 128, :] if t < 8 else O["y_s"])
        layer_norm_tiles(I["ln1_g"], I["ln1_b"], xT, xTv(NTOK), tilebufs, lng, lnb, lnst, out_dram=h_scr, out_fn=dbg)
        K.barrier()
        m_post_ln = A.mark()

    if stage >= 4:
        XS = 512.0 ** -0.5
        A.release(m_mix)
        oT = A.alloc("oT", 16 * NTOK, BF16)
        qTh = A.alloc("qTh", 4 * NTOK, BF16)
        memT = A.alloc("memT", 16 * 256, BF16)
        membf = A.alloc("membf", D, BF16)
        KTh = A.alloc("KTh", 4 * 256, BF16)
        Vh = A.alloc("Vh", 2 * 512, BF16)
        kvout = [A.alloc(f"kvout{i}", 256, F32) for i in range(2)]
        Kb = [A.alloc(f"Kb{i}", 2 * 512, BF16) for i in range(2)]
        KTb = [A.alloc(f"KTb{i}", 4 * 256, BF16) for i in range(2)]
        Vb = [A.alloc(f"Vb{i}", 2 * 512, BF16) for i in range(2)]
        scx = A.alloc("scx", 256, F32)
        pn = A.alloc("pn", 256, BF16)
        pT = A.alloc("pT", 256, BF16)
        sTs = A.alloc("sTs", 256, F32)
        xst = A.alloc("xst", 8, F32)
        xr = [A.alloc(f"xr{i}", 256, F32) for i in range(2)]
        ro = [A.alloc(f"ro{i}", 256, F32) for i in range(2)]
        h1Tv = xTv(NTOK)
        oTv = oT[:, :].rearrange("p (k t) -> p k t", k=16)
        qTv = qTh[:, :].rearrange("p (k t) -> p k t", k=4)
        memTv = memT[:, :].rearrange("p (k m) -> p k m", k=16)
        KThv = KTh[:, :].rearrange("p (k m) -> p k m", k=4)
        kvi = 0
        for mt in range(2):
            for hf in range(2):
                K.dma("pool", membf[:, hf * 1024:(hf + 1) * 1024], I["mem"][mt * 128:(mt + 1) * 128, hf * 1024:(hf + 1) * 1024], writes=[membf])
            if cut <= 10:
                continue
            for g in range(2):
                ps = PS()
                psb = ps[:, :].bitcast(BF16)
                for j in range(8):
                    kc = g * 8 + j
                    K.op("pe", lambda e, psb=psb, j=j, kc=kc: e.transpose(psb[:, j * 128:(j + 1) * 128], membf[:, kc * 128:(kc + 1) * 128], identB[:, :]),
                         reads=[membf, identB], writes=[ps])
                K.op("act", lambda e, psb=psb, g=g, mt=mt: e.activation(out=memTv[:, g * 8:(g + 1) * 8, mt * 128:(mt + 1) * 128], in_=psb.rearrange("p (j m) -> p j m", j=8), func=AF.Copy),
                     reads=[ps], writes=[memT])

        def softmax_to_pT(ps_s):
            K.op("dve", lambda e: e.tensor_reduce(out=xst[:, 0:1], in_=ps_s[:, 0:256], axis=AX.X, op=ALU.max), reads=[ps_s], writes=[xst])
            K.op("dve", lambda e: e.tensor_scalar(out=xst[:, 1:2], in0=xst[:, 0:1], scalar1=-XS, scalar2=None, op0=ALU.mult), reads=[xst], writes=[xst])
            K.op("act", lambda e: e.activation(out=scx[:, :], in_=ps_s[:, 0:256], func=AF.Exp, scale=XS, bias=xst[:, 1:2], accum_out=xst[:, 2:3]), reads=[ps_s, xst], writes=[scx, xst])
            K.op("dve", lambda e: e.reciprocal(out=xst[:, 3:4], in_=xst[:, 2:3]), reads=[xst], writes=[xst])
            K.op("dve", lambda e: e.tensor_scalar(out=pn[:, :], in0=scx[:, :], scalar1=xst[:, 3:4], scalar2=None, op0=ALU.mult), reads=[scx, xst], writes=[pn])
            pt = PS()
            ptb = pt[:, 0:128].bitcast(BF16)
            for mt in range(2):
                K.op("pe", lambda e, ptb=ptb, mt=mt: e.transpose(ptb[:, mt * 128:(mt + 1) * 128], pn[:, mt * 128:(mt + 1) * 128], identB[:, :]), reads=[pn, identB], writes=[pt])
            K.op("act", lambda e, ptb=ptb: e.activation(out=pT[:, :], in_=ptb, func=AF.Copy), reads=[pt], writes=[pT])

        for h in range(4):
            if cut <= 11:
                continue
            for half in range(2):
                slq, wq = wload([I["xa_wq"][:, h * 512 + half * 256:h * 512 + (half + 1) * 256]])
                for c2 in range(2):
                    c = half * 2 + c2
                    for (t0, n) in ((0, 512), (512, 512), (1024, 128)):
                        ps = PS()
                        mm_group(ps[:, 0:n], ps, [(wq[:, kc, c2 * 128:(c2 + 1) * 128], h1Tv[:, kc, t0:t0 + n]) for kc in range(16)], [slq, xT])
                        K.op("act", lambda e, ps=ps, c=c, t0=t0, n=n: e.activation(out=qTv[:, c, t0:t0 + n], in_=ps[:, 0:n], func=AF.Copy), reads=[ps], writes=[qTh])
            for half in range(2):
                slk, wk = wload([I["xa_wk"][:, h * 512 + half * 256:h * 512 + (half + 1) * 256]])
                slv, wv_ = wload([I["xa_wv"][:, h * 512 + half * 256:h * 512 + (half + 1) * 256]])
                c0 = h * 512 + half * 256
                for mt in range(2):
                    ms = slice(mt * 128, (mt + 1) * 128)
                    ps = PS()
                    mm_group(ps[:, 0:256], ps, [(memTv[:, kc, ms], wk[:, kc, :]) for kc in range(16)], [memT, slk])
                    ko = kvout[kvi % 2]
                    kvi += 1
                    K.op("act", lambda e, ps=ps, ko=ko: e.activation(out=ko[:, :], in_=ps[:, 0:256], func=AF.Copy), reads=[ps], writes=[ko])
                    K.dma("sp", O["mk"][ms, c0:c0 + 256], ko[:, :], reads=[ko])
                    ps = PS()
                    mm_group(ps[:, 0:256], ps, [(memTv[:, kc, ms], wv_[:, kc, :]) for kc in range(16)], [memT, slv])
                    ko = kvout[kvi % 2]
                    kvi += 1
                    K.op("act", lambda e, ps=ps, ko=ko: e.activation(out=ko[:, :], in_=ps[:, 0:256], func=AF.Copy), reads=[ps], writes=[ko])
                    K.dma("sp", O["mv"][ms, c0:c0 + 256], ko[:, :], reads=[ko])
                    K.op("dve", lambda e, ps=ps, mt=mt, half=half: e.tensor_copy(out=Vh[:, mt * 512 + half * 256:mt * 512 + (half + 1) * 256], in_=ps[:, 0:256]), reads=[ps], writes=[Vh])
                for c2 in range(2):
                    ps = PS()
                    mm_group(ps[:, 0:256], ps, [(wk[:, kc, c2 * 128:(c2 + 1) * 128], memTv[:, kc, :]) for kc in range(16)], [memT, slk])
                    K.op("act", lambda e, ps=ps, dc=half * 2 + c2: e.activation(out=KThv[:, dc, :], in_=ps[:, 0:256], func=AF.Copy), reads=[ps], writes=[KTh])
            for t in range(8):
                if cut <= 12:
                    continue
                ts_ = slice(t * 128, (t + 1) * 128)
                ps_s = PS()
                mm_group(ps_s[:, 0:256], ps_s, [(qTv[:, dc, ts_], KThv[:, dc, :]) for dc in range(4)], [qTh, KTh])
                softmax_to_pT(ps_s)
                ps_o = PS()
                for dc in range(4):
                    for mt in range(2):
                        K.op("pe", lambda e, ps_o=ps_o, dc=dc, mt=mt: e.matmul(ps_o[:, dc * 128:(dc + 1) * 128], Vh[:, mt * 512 + dc * 128:mt * 512 + (dc + 1) * 128],
                                                                            pT[:, mt * 128:(mt + 1) * 128], start=(mt == 0), stop=(mt == 1)), reads=[Vh, pT], writes=[ps_o])
                K.op("act", lambda e, ps_o=ps_o, ts_=ts_, h=h: e.activation(out=oTv[:, h * 4:(h + 1) * 4, ts_], in_=ps_o[:, :].rearrange("p (c t) -> p c t", c=4), func=AF.Copy),
                     reads=[ps_o], writes=[oT])
            if cut <= 13:
                continue
            ps_sT = psum[6]
            for b in range(16):
                kb_ = Kb[b % 2]
                ktb = KTb[b % 2]
                K.dma("pool", kb_[:, :].rearrange("p (mt c) -> p mt c", mt=2), I["ck"][b, :, h * 512:(h + 1) * 512].rearrange("(mt p) c -> p mt c", p=128), writes=[kb_])
                ps = PS()
                psb = ps[:, :].bitcast(BF16)
                for dc in range(4):
                    for mt in range(2):
                        K.op("pe", lambda e, psb=psb, dc=dc, mt=mt, kb_=kb_: e.transpose(psb[:, dc * 256 + mt * 128:dc * 256 + (mt + 1) * 128],
                                                                                     kb_[:, mt * 512 + dc * 128:mt * 512 + (dc + 1) * 128], identB[:, :]),
                             reads=[kb_, identB], writes=[ps])
                K.op("act", lambda e, psb=psb, ktb=ktb: e.activation(out=ktb[:, :], in_=psb, func=AF.Copy), reads=[ps], writes=[ktb])
                for mt in range(2):
                    for dc in range(4):
                        K.op("pe", lambda e, mt=mt, dc=dc, b=b, ktb=ktb: e.matmul(ps_sT[:, mt * 128 + b * 8:mt * 128 + (b + 1) * 8], ktb[:, dc * 256 + mt * 128:dc * 256 + (mt + 1) * 128],
                                                                               qTv[:, dc, TP + b * 8:TP + (b + 1) * 8], start=(dc == 0), stop=(dc == 3)),
                             reads=[ktb, qTh], writes=[ps_sT])
            K.op("act", lambda e: e.activation(out=sTs[:, :], in_=ps_sT[:, 0:256], func=AF.Copy), reads=[ps_sT], writes=[sTs])
            ps_s = PS()
            for mt in range(2):
                K.op("pe", lambda e, ps_s=ps_s, mt=mt: e.transpose(ps_s[:, mt * 128:(mt + 1) * 128], sTs[:, mt * 128:(mt + 1) * 128], ident), reads=[sTs, cst], writes=[ps_s])
            softmax_to_pT(ps_s)
            ps_oS = psum[7]
            for b in range(16):
                vb_ = Vb[b % 2]
                K.dma("pool", vb_[:, :].rearrange("p (mt c) -> p mt c", mt=2), I["cv"][b, :, h * 512:(h + 1) * 512].rearrange("(mt p) c -> p mt c", p=128), writes=[vb_])
                for dc in range(4):
                    for mt in range(2):
                        K.op("pe", lambda e, dc=dc, mt=mt, b=b, vb_=vb_: e.matmul(ps_oS[:, dc * 128 + b * 8:dc * 128 + (b + 1) * 8], vb_[:, mt * 512 + dc * 128:mt * 512 + (dc + 1) * 128],
                                                                               pT[:, mt * 128 + b * 8:mt * 128 + (b + 1) * 8], start=(mt == 0), stop=(mt == 1)),
                             reads=[vb_, pT], writes=[ps_oS])
            K.op("act", lambda e, h=h: e.activation(out=oTv[:, h * 4:(h + 1) * 4, TP:NTOK], in_=ps_oS[:, :].rearrange("p (c t) -> p c t", c=4), func=AF.Copy), reads=[ps_oS], writes=[oT])
        out_proj_residual(oT, oTv, 16, I["xa_wo"], lambda t: h_scr[t * 128:(t + 1) * 128, :], [h_scr_b], xr, ro)
        K.barrier()
        A.release(m_mix)
        tilebufs = [A.alloc(f"tilebufb{i}", D, F32) for i in range(2)]
        lng = A.alloc("lngb", D, F32)
        lnb = A.alloc("lnbb", D, F32)
        lnst = A.alloc("lnstb", 32, F32)
        dbg = None
        if stage == 4:
            dbg = lambda t: (O["y_p"][t * 128:(t + 1) * 128, :] if t < 8 else O["y_s"])
        layer_norm_tiles(I["ln2_g"], I["ln2_b"], xT, xTv(NTOK), tilebufs, lng, lnb, lnst, out_dram=h_scr, out_fn=dbg)
        K.barrier()

    if stage >= 5:
        A.release(m_mix)
        h2Tv = xTv(NTOK)
        acc_t = [A.alloc(f"acc{t}", D, F32) for t in range(NT)]
        m_after_acc = A.mark()
        hid = A.alloc("hid", 4 * NTOK, BF16)
        hidv = hid[:, :].rearrange("p (f t) -> p f t", f=4)
        cbc = A.alloc("cbc", NTOK, F32)
        cTf = cbc
        cThi = A.alloc("cThi", NTOK, BF16)
        cTlo = A.alloc("cTlo", NTOK, BF16)
        sgt = A.alloc("sgt", 512, F32)
        ut = A.alloc("ut", 512, F32)
        comb = A.alloc("comb", NT * 32, F32)
        rbrow = A.alloc("rbrow", 36, F32)
        rs_ = A.alloc("rs_", 128, F32)
        sel = [A.alloc(f"sel{i}", 128, BF16) for i in range(2)]
        for t in range(NT):
            K.dma("sp", acc_t[t][:, :], h_scr[t * 128:(t + 1) * 128, :], reads=[h_scr_b], writes=[acc_t[t]])
            K.op("pool", lambda e, t=t: e.tensor_scalar(out=acc_t[t][:, :], in0=acc_t[t][:, :], scalar1=ALPHA, scalar2=None, op0=ALU.mult), writes=[acc_t[t]])
        K.dma("sp", rbrow[:, :], I["rb"][0:1, :].partition_broadcast(128), writes=[rbrow])
        slr, wr = wload([I["rw"]])
        lg, oh, lge, t8, mk1, mk2 = rs_[:, 0:36], rs_[:, 36:40], rs_[:, 40:48], rs_[:, 48:80], rs_[:, 80:88], rs_[:, 88:96]
        sc = lambda i: rs_[:, 96 + i:97 + i]
        for t in range(NT):
            ps = PS()
            mm_group(ps[:, 0:36], ps, [(h2Tv[:, kc, t * 128:(t + 1) * 128], wr[:, kc, :]) for kc in range(16)], [xT, slr])
            K.op("dve", lambda e, ps=ps: e.tensor_tensor(out=lg, in0=ps[:, 0:36], in1=rbrow[:, :], op=ALU.add), reads=[ps, rbrow], writes=[rs_])
            K.op("dve", lambda e: e.tensor_reduce(out=sc(0), in_=lg[:, 0:4], axis=AX.X, op=ALU.max), reads=[rs_], writes=[rs_])
            K.op("dve", lambda e: e.tensor_scalar(out=oh, in0=lg[:, 0:4], scalar1=sc(0), scalar2=None, op0=ALU.is_equal), reads=[rs_], writes=[rs_])
            K.op("dve", lambda e: e.tensor_scalar(out=sc(1), in0=sc(0), scalar1=-1.0, scalar2=None, op0=ALU.mult), reads=[rs_], writes=[rs_])
            K.op("act", lambda e: e.activation(out=t8[:, 0:4], in_=lg[:, 0:4], func=AF.Exp, bias=sc(1), accum_out=sc(2)), reads=[rs_], writes=[rs_])
            K.op("dve", lambda e: e.reciprocal(out=sc(3), in_=sc(2)), reads=[rs_], writes=[rs_])
            K.op("dve", lambda e: e.tensor_tensor(out=t8.rearrange("p (g e) -> p g e", g=4), in0=lg[:, 4:36].rearrange("p (g e) -> p g e", g=4),
                                                  in1=V(rs_.t, 36, (1, 4), (0, 8)), op=ALU.mult), reads=[rs_], writes=[rs_])
            K.op("dve", lambda e: e.tensor_reduce(out=lge, in_=V(rs_.t, 48, (1, 8), (8, 4)), axis=AX.X, op=ALU.add), reads=[rs_], writes=[rs_])
            K.op("dve", lambda e: e.tensor_reduce(out=sc(4), in_=lge, axis=AX.X, op=ALU.max), reads=[rs_], writes=[rs_])
            K.op("dve", lambda e: e.tensor_scalar(out=mk1, in0=lge, scalar1=sc(4), scalar2=None, op0=ALU.is_equal), reads=[rs_], writes=[rs_])
            K.op("dve", lambda e: e.scalar_tensor_tensor(out=t8[:, 0:8], in0=mk1, scalar=NEGBIG, in1=lge, op0=ALU.mult, op1=ALU.add), reads=[rs_], writes=[rs_])
            K.op("dve", lambda e: e.tensor_reduce(out=sc(5), in_=t8[:, 0:8], axis=AX.X, op=ALU.max), reads=[rs_], writes=[rs_])
            K.op("dve", lambda e: e.tensor_scalar(out=mk2, in0=t8[:, 0:8], scalar1=sc(5), scalar2=None, op0=ALU.is_equal), reads=[rs_], writes=[rs_])
            K.op("dve", lambda e: e.tensor_tensor(out=sc(6), in0=sc(5), in1=sc(4), op=ALU.subtract), reads=[rs_], writes=[rs_])
            K.op("act", lambda e: e.activation(out=sc(7), in_=sc(6), func=AF.Exp), reads=[rs_], writes=[rs_])
            K.op("dve", lambda e: e.tensor_scalar(out=sc(8), in0=sc(7), scalar1=1.0, scalar2=None, op0=ALU.add), reads=[rs_], writes=[rs_])
            K.op("dve", lambda e: e.reciprocal(out=sc(9), in_=sc(8)), reads=[rs_], writes=[rs_])
            K.op("dve", lambda e: e.tensor_tensor(out=sc(10), in0=sc(9), in1=sc(3), op=ALU.mult), reads=[rs_], writes=[rs_])
            K.op("dve", lambda e: e.tensor_tensor(out=sc(11), in0=sc(3), in1=sc(10), op=ALU.subtract), reads=[rs_], writes=[rs_])
            K.op("dve", lambda e: e.tensor_scalar(out=mk1, in0=mk1, scalar1=sc(10), scalar2=None, op0=ALU.mult), reads=[rs_], writes=[rs_])
            K.op("dve", lambda e: e.scalar_tensor_tensor(out=mk1, in0=mk2, scalar=sc(11), in1=mk1, op0=ALU.mult, op1=ALU.add), reads=[rs_], writes=[rs_])
            K.op("dve", lambda e, t=t: e.tensor_tensor(out=comb[:, t * 32:(t + 1) * 32].rearrange("p (g e) -> p g e", g=4), in0=V(rs_.t, 36, (1, 4), (0, 8)),
                                                       in1=V(rs_.t, 80, (0, 4), (1, 8)), op=ALU.mult), reads=[rs_], writes=[comb])
        for g in range(3):
            ps = PS()
            nt_ = 4 if g < 2 else 1
            for j in range(nt_):
                t = g * 4 + j
                K.op("pe", lambda e, ps=ps, j=j, t=t: e.transpose(ps[0:32, j * 128:(j + 1) * 128], comb[:, t * 32:(t + 1) * 32], ident), reads=[comb, cst], writes=[ps])
            K.op("act", lambda e, ps=ps, g=g, nt_=nt_: e.activation(out=cTf[0:32, g * 512:g * 512 + nt_ * 128], in_=ps[0:32, 0:nt_ * 128], func=AF.Copy), reads=[ps], writes=[cTf])
        K.op("dve", lambda e: e.tensor_copy(out=cThi[0:32, :], in_=cTf[0:32, :]), reads=[cTf], writes=[cThi])
        K.op("dve", lambda e: e.tensor_tensor(out=cTlo[0:32, :], in0=cTf[0:32, :], in1=cThi[0:32, :], op=ALU.subtract), reads=[cTf, cThi], writes=[cTlo])
        TB3 = ((0, 512), (512, 512), (1024, 128))
        for xe in range(32):
            se = sel[xe % 2]
            K.op("pool", lambda e, se=se, xe=xe: e.tensor_scalar(out=se[0:32, :], in0=cst[0:32, C_ONES:C_ONES + 128], scalar1=cst[0:32, C_ID + xe:C_ID + xe + 1], scalar2=None, op0=ALU.mult),
                 reads=[cst], writes=[se])
            for (t0, n) in TB3:
                ps = PS()
                mm_group(ps[:, 0:n], ps, [(se[0:32, :], cThi[0:32, t0:t0 + n]), (se[0:32, :], cTlo[0:32, t0:t0 + n])], [se, cThi, cTlo])
                K.op("act", lambda e, ps=ps, t0=t0, n=n: e.activation(out=cbc[:, t0:t0 + n], in_=ps[:, 0:n], func=AF.Copy), reads=[ps], writes=[cbc])
            for fp in range(2):
                slg, wg = wload([I["e_wg"][xe][:, fp * 256:(fp + 1) * 256]])
                slu, wu = wload([I["e_wu"][xe][:, fp * 256:(fp + 1) * 256]])
                for c2 in range(2):
                    fb = fp * 2 + c2
                    cs_ = slice(c2 * 128, (c2 + 1) * 128)
                    for (t0, n) in TB3:
                        pg = PS()
                        mm_group(pg[:, 0:n], pg, [(wg[:, kc, cs_], h2Tv[:, kc, t0:t0 + n]) for kc in range(16)], [slg, xT])
                        K.op("act", lambda e, pg=pg, n=n: e.activation(out=sgt[:, 0:n], in_=pg[:, 0:n], func=AF.Silu), reads=[pg], writes=[sgt])
                        pu = PS()
                        mm_group(pu[:, 0:n], pu, [(wu[:, kc, cs_], h2Tv[:, kc, t0:t0 + n]) for kc in range(16)], [slu, xT])
                        K.op("dve", lambda e, pu=pu, n=n, t0=t0: e.tensor_tensor(out=ut[:, 0:n], in0=pu[:, 0:n], in1=cbc[:, t0:t0 + n], op=ALU.mult), reads=[pu, cbc], writes=[ut])
                        K.op("pool", lambda e, n=n, t0=t0, fb=fb: e.tensor_tensor(out=hidv[:, fb, t0:t0 + n], in0=sgt[:, 0:n], in1=ut[:, 0:n], op=ALU.mult), reads=[sgt, ut], writes=[hid])
            for dh in range(2):
                sld, wd = wload([I["e_wd"][xe][:, dh * 1024:(dh + 1) * 1024]])
                for t in range(NT):
                    for db in range(2):
                        ps = PS()
                        mm_group(ps[:, :], ps, [(hidv[:, fc, t * 128:(t + 1) * 128], wd[:, fc, db * 512:(db + 1) * 512]) for fc in range(4)], [hid, sld])
                        c0 = dh * 1024 + db * 512
                        K.op("dve", lambda e, ps=ps, t=t, c0=c0: e.tensor_tensor(out=acc_t[t][:, c0:c0 + 512], in0=acc_t[t][:, c0:c0 + 512], in1=ps[:, :], op=ALU.add),
                             reads=[ps], writes=[acc_t[t]])
        K.barrier()
        A.release(m_after_acc)
        lng = A.alloc("lngc", D, F32)
        lnb = A.alloc("lnbc", D, F32)
        lnst = A.alloc("lnstc", 32, F32)
        layer_norm_tiles(I["ln3_g"], I["ln3_b"], None, None, acc_t, lng, lnb, lnst, out_dram=None,
                         out_fn=lambda t: (O["y_p"][t * 128:(t + 1) * 128, :] if t < 8 else O["y_s"]), inplace=True)

    def emit_prompt_state_outputs():
        for h in range(4):
            K.dma("sp", O["Cp"][h].rearrange("(dc p) v -> p dc v", p=128), V(Cst[h].t, 0, (257, 2), (1, 256)), reads=[Cst[h]])
            K.dma("sp", O["np_"][h].rearrange("(dc p) -> p dc", p=128), V(Cst[h].t, 256, (257, 2)), reads=[Cst[h]])
        K.dma("sp", O["mp"], mstate[0:1, :], reads=[mstate])
        for hh in range(8):
            K.dma("sp", O["Sp"][hh], Sst[hh][:, :], reads=[Sst[hh]])

    if stage == 1:
        emit_prompt_state_outputs()

    nc._K = K
    nc._A = A
    nc._I = I
    nc._O = O
    with nc.allow_non_contiguous_dma(reason="small strided parameter / state layouts"):
        K.finalize()
    return nc


def prep_core_inputs(inp, c, shared):
    j, hh = c // 2, c % 2
    sl = slice(16 * c, 16 * c + 16)
    m = dict(shared)
    m["xp"] = np.ascontiguousarray(inp["x_prompt"][j, hh * TP:(hh + 1) * TP])
    m["xpre"] = np.ascontiguousarray(inp["x_prompt"][j, 0:TP])
    m["xs"] = np.ascontiguousarray(inp["x_sample"][sl].reshape(128, D))
    m["mem"] = np.ascontiguousarray(inp["mem_prompt"][j])
    m["ck"] = np.ascontiguousarray(inp["cache_mem_k"][0, sl].reshape(16, 256, D))
    m["cv"] = np.ascontiguousarray(inp["cache_mem_v"][0, sl].reshape(16, 256, D))
    m["C0"] = np.ascontiguousarray(inp["state_mlstm_C"][0, sl])
    m["n0"] = np.ascontiguousarray(inp["state_mlstm_n"][0, sl])
    m["m0"] = np.ascontiguousarray(inp["state_mlstm_m"][0, sl])
    m["conv0"] = np.ascontiguousarray(inp["state_mlstm_conv"][0, sl].reshape(48, D))
    m["S0"] = np.ascontiguousarray(inp["state_hgrn_S"][0, sl])
    m["flag"] = np.full((128, 1), float(hh), np.float32)
    return m


def prep_shared(inp):
    s = {"consts": host_consts()}
    for k in ["w_in", "conv_w", "lb_logits", "w_bm", "w_bh", "w_out", "xa_wq", "xa_wk", "xa_wv", "xa_wo", "e_wg", "e_wu", "e_wd"]:
        a = inp[k]
        s[k] = np.ascontiguousarray(a[0] if k != "lb_logits" else a)
    for k in ["b_in", "mlstm_gn", "hgrn_gn", "ln1_g", "ln1_b", "ln2_g", "ln2_b", "ln3_g", "ln3_b"]:
        s[k] = np.ascontiguousarray(inp[k].reshape(1, -1))
    s["rw"] = np.ascontiguousarray(np.concatenate([inp["r1_w"][0], inp["r2_w"][0].transpose(1, 0, 2).reshape(D, 32)], axis=1))
    s["rb"] = np.ascontiguousarray(np.concatenate([inp["r1_b"][0].reshape(-1), inp["r2_b"][0].reshape(-1)]).reshape(1, 36))
    return s


def assemble(res):
    f = np.float32
    y_p = np.zeros((4, 2048, D), f)
    y_s = np.zeros((128, 8, D), f)
    mk = np.zeros((1, 4, 256, 4, 512), f)
    mv = np.zeros((1, 4, 256, 4, 512), f)
    C_p = np.zeros((1, 4, 4, 256, 256), f)
    n_p = np.zeros((1, 4, 4, 256), f)
    m_p = np.zeros((1, 4, 4), f)
    conv_p = np.zeros((1, 4, 3, D), f)
    S_p = np.zeros((1, 4, 8, 128, 128), f)
    C_s = np.zeros((1, 128, 4, 256, 256), f)
    n_s = np.zeros((1, 128, 4, 256), f)
    m_s = np.zeros((1, 128, 4), f)
    conv_s = np.zeros((1, 128, 3, D), f)
    S_s = np.zeros((1, 128, 8, 128, 128), f)
    for c, r in res.items():
        j, hh = c // 2, c % 2
        sl = slice(16 * c, 16 * c + 16)
        y_p[j, hh * TP:(hh + 1) * TP] = r["y_p"]
        y_s[sl] = r["y_s"].reshape(16, 8, D)
        if hh == 0:
            mk[0, j] = r["mk"].reshape(256, 4, 512)
            mv[0, j] = r["mv"].reshape(256, 4, 512)
        else:
            C_p[0, j] = r["Cp"]
            n_p[0, j] = r["np_"]
            m_p[0, j] = r["mp"].reshape(4)
            conv_p[0, j] = r["convp"]
            S_p[0, j] = r["Sp"]
        C_s[0, sl] = r["Cs"]
        n_s[0, sl] = r["ns"]
        m_s[0, sl] = r["ms"]
        conv_s[0, sl] = r["convs"]
        S_s[0, sl] = r["Ss"]
    return (y_p, y_s, mk, mv, C_p, n_p, m_p, conv_p, S_p, C_s, n_s, m_s, conv_s, S_s)


def kernel(**inputs):
    inp = {k: np.asarray(v) for k, v in inputs.items()}
    shared = prep_shared(inp)
    in_maps = [prep_core_inputs(inp, c, shared) for c in range(NCORES)]
    nc = build_program()
    res = run_bass_kernel_spmd(nc, in_maps, core_ids=list(range(NCORES)))
    return assemble({c: res.results[c] for c in range(NCORES)})
```

```python
import math
import numpy as np
import concourse.bass as bass
import concourse.mybir as mybir
from concourse.bass_utils import run_bass_kernel_spmd

F32 = mybir.dt.float32
BF16 = mybir.dt.bfloat16
ALU = mybir.AluOpType
AF = mybir.ActivationFunctionType
AX = mybir.AxisListType

D = 2048
NIN = 12296
NCORES = 8
TP = 1024
NTOK = 1152
NT = 9
ALPHA = 2.0 ** 0.25
EPS = 1e-5
LN16 = math.log(16.0)
NEGBIG = -1.0e30


class Buf:
    def __init__(self, name, t=None, parent=None):
        self.name = name
        self.t = t
        self.parent = parent
        self.kids = {}
        self.last_w = None
        self.readers = []

    def sub(self, key):
        if key not in self.kids:
            self.kids[key] = Buf(f"{self.name}.{key}", self.t, self)
        return self.kids[key]

    def __getitem__(self, idx):
        return self.t[idx]

    def related(self):
        out = [self]
        p = self.parent
        while p is not None:
            out.append(p)
            p = p.parent
        st = list(self.kids.values())
        while st:
            k = st.pop()
            out.append(k)
            st.extend(k.kids.values())
        return out


class Op:
    __slots__ = ("eng", "fn", "deps", "needs_inc", "count", "is_dma", "sem", "val")

    def __init__(self, eng, fn):
        self.eng = eng
        self.fn = fn
        self.deps = []
        self.needs_inc = False
        self.count = None
        self.is_dma = False
        self.sem = None
        self.val = None


class Sched:
    ENGS = ("pe", "act", "dve", "pool", "sp")

    def __init__(self, nc, n_dma_sems=16, same_engine_sync=True):
        self.nc = nc
        self.ops = {e: [] for e in self.ENGS}
        self.esem = {e: nc.alloc_semaphore(f"s_{e}") for e in self.ENGS}
        self.dsem = {q: [nc.alloc_semaphore(f"d_{q}{i}") for i in range(n_dma_sems)] for q in ("sp", "pool")}
        self.dcount = {q: 0 for q in self.dsem}
        self.dlast = {q: [None] * n_dma_sems for q in self.dsem}
        self.same_engine_sync = same_engine_sync
        self.all_dmas = []

    def _add_dep(self, op, dep):
        if dep is None or dep is op:
            return
        if not dep.is_dma and dep.eng == op.eng and not op.is_dma:
            if op.eng == "pe" or not self.same_engine_sync:
                return
        if dep in op.deps:
            return
        op.deps.append(dep)
        if not dep.is_dma:
            dep.needs_inc = True

    def _track(self, op, reads, writes):
        for b in reads:
            for r in b.related():
                self._add_dep(op, r.last_w)
            if getattr(b, "excl", False):
                for rd in b.readers:
                    if rd.eng != op.eng:
                        self._add_dep(op, rd)
        for b in writes:
            for r in b.related():
                self._add_dep(op, r.last_w)
                for rd in r.readers:
                    self._add_dep(op, rd)
        for b in reads:
            b.readers.append(op)
        for b in writes:
            b.last_w = op
            b.readers = []

    def op(self, eng, fn, reads=(), writes=()):
        o = Op(eng, fn)
        self._track(o, list(reads), list(writes))
        self.ops[eng].append(o)
        return o

    def dma(self, q, out, in_, reads=(), writes=(), **kw):
        o = Op(q, lambda e: e.dma_start(out=out, in_=in_, **kw))
        o.is_dma = True
        k = self.dcount[q]
        ns = len(self.dsem[q])
        slot = k % ns
        o.sem = self.dsem[q][slot]
        o.val = 16 * (k // ns + 1)
        prev = self.dlast[q][slot]
        if prev is not None:
            o.deps.append(prev)
        self.dlast[q][slot] = o
        self.dcount[q] = k + 1
        self._track(o, list(reads), list(writes))
        self.ops[q].append(o)
        self.all_dmas.append(o)
        return o

    def barrier(self):
        lasts = []
        for e in self.ENGS:
            for o in reversed(self.ops[e]):
                if not o.is_dma and o.fn is not None:
                    lasts.append(o)
                    break
        dm = [d for q in self.dlast for d in self.dlast[q] if d is not None]
        for e in self.ENGS:
            o = Op(e, None)
            for l in lasts:
                if l.eng != e:
                    o.deps.append(l)
                    l.needs_inc = True
            o.deps.extend(dm)
            self.ops[e].append(o)

    def finalize(self):
        fin = Op("sp", None)
        for q in self.dlast:
            for d in self.dlast[q]:
                if d is not None:
                    fin.deps.append(d)
        self.ops["sp"].append(fin)
        for e in self.ENGS:
            c = 0
            for o in self.ops[e]:
                if (not o.is_dma) and o.needs_inc:
                    c += 1
                    o.count = c
        nc = self.nc
        esem = self.esem
        with nc.Block() as block:
            regs = {"pe": block.tensor, "act": block.scalar, "dve": block.vector, "pool": block.gpsimd, "sp": block.sync}
            for e in self.ENGS:
                ops = self.ops[e]

                def body(h, ops=ops, e=e):
                    waited = {}
                    pending = None
                    for o in ops:
                        for d in o.deps:
                            if d.is_dma:
                                s, v = d.sem, d.val
                            else:
                                s, v = esem[d.eng], d.count
                            key = id(s)
                            if waited.get(key, 0) >= v:
                                continue
                            waited[key] = v
                            h.wait_ge(s, v)
                        if o.fn is None:
                            continue
                        ins = o.fn(h)
                        if o.is_dma:
                            ins.then_inc(o.sem, 16)
                        elif o.needs_inc:
                            ins.then_inc(esem[e], 1)

                regs[e](body)


def V(t, off, *dims, npart=128, p0=0):
    n = t.shape[1]
    return bass.AP(tensor=t, offset=p0 * n + off, ap=[[n, npart]] + [[s, c] for s, c in dims])


class Arena:
    def __init__(self, nc, base=16384, limit=196608):
        self.nc = nc
        self.off = base
        self.limit = limit
        self.n = 0

    def alloc(self, name, nelem, dt):
        sz = nelem * (4 if dt == F32 else 2)
        self.off = (self.off + 63) // 64 * 64
        assert self.off + sz <= self.limit, f"SBUF overflow at {name}: {self.off + sz}"
        self.n += 1
        t = self.nc.alloc_sbuf_tensor_at(f"{name}_{self.n}", [128, nelem], dt, offset=self.off)
        self.off += sz
        return Buf(name, t)

    def mark(self):
        return self.off

    def release(self, m):
        self.off = m


def host_consts():
    c = np.zeros((128, 1056 + NTOK), np.float32)
    idx = np.arange(128)
    c[:, 0:128] = np.eye(128)
    s = idx[:, None]
    t = idx[None, :]
    c[:, 128:256] = (s <= t)
    c[:, 256:384] = np.where(t <= s, 0.0, NEGBIG)
    c[:, 384:512] = (s == 127)
    same = (s // 8) == (t // 8)
    c[:, 512:640] = same & (s <= t)
    c[:, 640:768] = np.where(same & (t <= s), 0.0, NEGBIG)
    c[:, 768:896] = (s == 8 * (t // 8) + 7)
    c[:, 896:912] = (idx[:, None] // 8) == np.arange(16)[None, :]
    c[:, 912:928] = idx[:, None] == 8 * np.arange(16)[None, :]
    c[:, 928:1056] = 1.0
    rm = np.ones(NTOK, np.float32)
    rm[0:1024:128] = 0.0
    rm[1024:NTOK:8] = 0.0
    c[:, 1056:] = rm[None, :]
    return c


C_ID, C_MP, C_NEGP, C_LASTP, C_MS, C_NEGS, C_LASTS, C_SEQM, C_SEQ1, C_ONES, C_RM = 0, 128, 256, 384, 512, 640, 768, 896, 912, 928, 1056


def build_program(stage=99, same_engine_sync=True, cut=99):
    nc = bass.Bass("TRN2", target_bir_lowering=False)

    def din(name, shape):
        return nc.dram_tensor(name, list(shape), F32, kind="ExternalInput").ap()

    def dout(name, shape):
        return nc.dram_tensor(name, list(shape), F32, kind="ExternalOutput").ap()

    I = {}
    for name, shape in [
        ("xp", (TP, D)), ("xpre", (TP, D)), ("xs", (128, D)), ("mem", (256, D)),
        ("ck", (16, 256, D)), ("cv", (16, 256, D)), ("C0", (16, 4, 256, 256)), ("n0", (16, 4, 256)), ("m0", (16, 4)),
        ("conv0", (48, D)), ("S0", (16, 8, 128, 128)), ("flag", (128, 1)), ("consts", (128, 1056 + NTOK)),
        ("w_in", (D, NIN)), ("b_in", (1, NIN)), ("conv_w", (4, D)), ("mlstm_gn", (1, 1024)), ("lb_logits", (2, 1024)),
        ("hgrn_gn", (1, 1024)), ("w_bm", (1024, D)), ("w_bh", (1024, D)), ("w_out", (D, D)),
        ("ln1_g", (1, D)), ("ln1_b", (1, D)), ("xa_wq", (D, D)), ("xa_wk", (D, D)), ("xa_wv", (D, D)), ("xa_wo", (D, D)),
        ("ln2_g", (1, D)), ("ln2_b", (1, D)), ("rw", (D, 36)), ("rb", (1, 36)),
        ("e_wg", (32, D, 512)), ("e_wu", (32, D, 512)), ("e_wd", (32, 512, D)), ("ln3_g", (1, D)), ("ln3_b", (1, D)),
    ]:
        I[name] = din(name, shape)
    O = {}
    for name, shape in [
        ("y_p", (TP, D)), ("y_s", (128, D)), ("mk", (256, D)), ("mv", (256, D)),
        ("Cp", (4, 256, 256)), ("np_", (4, 256)), ("mp", (1, 4)), ("convp", (3, D)), ("Sp", (8, 128, 128)),
        ("Cs", (16, 4, 256, 256)), ("ns", (16, 4, 256)), ("ms", (16, 4)), ("convs", (16, 3, D)), ("Ss", (16, 8, 128, 128)),
    ]:
        O[name] = dout(name, shape)

    K = Sched(nc, same_engine_sync=same_engine_sync)
    A = Arena(nc)
    psum = [Buf(f"ps{i}", nc.alloc_psum_tensor(f"ps{i}", [128, 512], F32)) for i in range(8)]
    for b_ in psum:
        b_.excl = True
    pstate = {"i": 0}

    def PS():
        b = psum[pstate["i"] % 6]
        pstate["i"] += 1
        return b


    cst = A.alloc("cst", 1056 + NTOK, F32)
    K.dma("sp", cst[:, :], I["consts"], writes=[cst])
    identB = A.alloc("identB", 128, BF16)
    K.op("dve", lambda e: e.tensor_copy(out=identB[:, :], in_=cst[:, C_ID:C_ID + 128]), reads=[cst], writes=[identB])
    flag = A.alloc("flag", 1, F32)
    K.dma("sp", flag[:, :], I["flag"], writes=[flag])

    def cs(c0, n=128):
        return cst[:, c0:c0 + n]

    ident = cs(C_ID)
    ones = cs(C_ONES)

    NSLOT = 4
    wslots = [A.alloc(f"wslot{i}", 4096, BF16) for i in range(NSLOT)]
    wstate = {"i": 0}

    def wload(parts):
        sl = wslots[wstate["i"] % NSLOT]
        wstate["i"] += 1
        kc = parts[0].shape[0] // 128
        tot = sum(p.shape[1] for p in parts)
        assert kc * tot <= 4096
        view = sl[:, 0:kc * tot].rearrange("p (k c) -> p k c", k=kc)
        c0 = 0
        for p in parts:
            n = p.shape[1]
            K.dma("pool", view[:, :, c0:c0 + n], p.rearrange("(k p) c -> p k c", p=128), writes=[sl])
            c0 += n
        return sl, view

    w_in = I["w_in"]
    b_in = I["b_in"]

    def fm_bias(name, c0, nblk):
        b = A.alloc(name, nblk, F32)
        K.dma("sp", b[:, :], b_in[0, c0:c0 + nblk * 128].rearrange("(b p) -> p b", p=128), writes=[b])
        return b

    bqk = fm_bias("bqk", 0, 16)
    bqh = fm_bias("bqh", 4104, 8)
    bfh = fm_bias("bfh", 5128, 8)
    bgm = fm_bias("bgm", 8200, 16)
    bgh = fm_bias("bgh", 10248, 16)
    cw = A.alloc("cw", 64, F32)
    for j in range(4):
        K.dma("sp", V(cw.t, j, (4, 16)), I["conv_w"][j, :].rearrange("(c p) -> p c", p=128), writes=[cw])
    lbt = A.alloc("lbt", 16, F32)
    for r in range(2):
        K.dma("sp", lbt[:, r * 8:(r + 1) * 8], I["lb_logits"][r, :].rearrange("(h p) -> p h", p=128), writes=[lbt])
    lb = A.alloc("lb", 8, F32)
    oml = A.alloc("oml", 8, F32)
    K.op("dve", lambda e: e.tensor_tensor(out=lb[:, :], in0=lbt[:, 0:8], in1=lbt[:, 8:16], op=ALU.subtract), reads=[lbt], writes=[lb])
    K.op("act", lambda e: e.activation(out=lb[:, :], in_=lb[:, :], func=AF.Sigmoid), reads=[lb], writes=[lb])
    K.op("dve", lambda e: e.tensor_scalar(out=oml[:, :], in0=lb[:, :], scalar1=-1.0, scalar2=1.0, op0=ALU.mult, op1=ALU.add), reads=[lb], writes=[oml])
    bgate = A.alloc("bgate", 8, F32)
    K.dma("sp", bgate[:, :], b_in[0:1, 4096:4104].partition_broadcast(128), writes=[bgate])
    gnT = A.alloc("gnT", 16, F32)
    K.dma("sp", gnT[:, 0:8], I["mlstm_gn"][0, :].rearrange("(k p) -> p k", p=128), writes=[gnT])
    K.dma("sp", gnT[:, 8:16], I["hgrn_gn"][0, :].rearrange("(k p) -> p k", p=128), writes=[gnT])
    hm_scr = nc.dram_tensor("hm_scr", [NTOK, 1024], BF16, kind="Internal").ap()
    og_scr = nc.dram_tensor("og_scr", [NTOK, 1024], BF16, kind="Internal").ap()
    hm_scr_b = Buf("hm_scr")
    og_scr_b = Buf("og_scr")

    xT = A.alloc("xT", 16 * NTOK, BF16)
    xtile = []
    xst = {"i": 0}

    def xTv(ntok):
        return xT[:, 0:16 * ntok].rearrange("p (k t) -> p k t", k=16)

    def transpose_tile_f32(src_buf, src_ap_fn, dstT_buf, dst_view, tok0):
        for g in range(4):
            ps = PS()
            for j in range(4):
                kc = g * 4 + j
                K.op("pe", lambda e, ps=ps, j=j, kc=kc: e.transpose(ps[:, j * 128:(j + 1) * 128], src_ap_fn(kc), ident),
                     reads=[src_buf, cst], writes=[ps])
            eng = "act" if g % 2 == 0 else "dve"
            outv = dst_view[:, g * 4:(g + 1) * 4, tok0:tok0 + 128]
            inv = ps[:, :].rearrange("p (j t) -> p j t", j=4)
            if eng == "act":
                K.op("act", lambda e, outv=outv, inv=inv: e.activation(out=outv, in_=inv, func=AF.Copy), reads=[ps], writes=[dstT_buf])
            else:
                K.op("dve", lambda e, outv=outv, inv=inv: e.tensor_copy(out=outv, in_=inv), reads=[ps], writes=[dstT_buf])

    def build_xT(srcs, ntok):
        view = xTv(ntok)
        t = 0
        for src, ntile in srcs:
            for i in range(ntile):
                xb = xtile[xst["i"] % 2]
                xst["i"] += 1
                K.dma("sp", xb[:, :], src[i * 128:(i + 1) * 128, :], writes=[xb])
                transpose_tile_f32(xb, lambda kc, xb=xb: xb[:, kc * 128:(kc + 1) * 128], xT, view, t * 128)
                t += 1
        return view

    def mm_group(ps_ap, ps_buf, pairs, reads):
        n = len(pairs)
        for i, (l, r) in enumerate(pairs):
            K.op("pe", lambda e, l=l, r=r, i=i: e.matmul(ps_ap, l, r, start=(i == 0), stop=(i == n - 1)), reads=reads, writes=[ps_buf])

    m_mix = A.mark()

    Cst = [A.alloc(f"Cst{h}", 2 * 257, F32) for h in range(4)]
    Sst = [A.alloc(f"Sst{h}", 128, F32) for h in range(8)]
    mstate = A.alloc("mstate", 4, F32)
    xhalo = A.alloc("xhalo", 48, BF16)
    for h in range(4):
        K.op("pool", lambda e, h=h: e.memset(Cst[h][:, :], 0.0), writes=[Cst[h]])
    for h in range(8):
        K.op("pool", lambda e, h=h: e.memset(Sst[h][:, :], 0.0), writes=[Sst[h]])
    K.op("pool", lambda e: e.memset(mstate[:, :], 0.0), writes=[mstate])


    sm = A.alloc("gate_small", 160, F32)
    Dg = A.alloc("Dg", 512, F32)
    Dm = A.alloc("Dm", 512, F32)

    def smv(i, n=4):
        return sm[:, i:i + n]

    def gate_tile(gt_ap, Mc, NEGc, LASTc, mprev_ap, mprev_buf, out, need_P):
        gi = gt_ap[:, 0:4]
        gf = gt_ap[:, 4:8]
        gb = out["gbuf"]
        e1, nlf, g, Fv, mx, iw, mt, negm, d1, L8, d2, t1 = (smv(0), smv(4), smv(8), smv(12), smv(16), smv(20), smv(24), smv(28), smv(32), smv(36, 8), smv(44), smv(48))
        mf = smv(52, 8)
        K.op("act", lambda e: e.activation(out=e1, in_=gf, func=AF.Exp, scale=-1.0), reads=[gb], writes=[sm])
        K.op("act", lambda e: e.activation(out=nlf, in_=e1, func=AF.Ln, bias=1.0), reads=[sm], writes=[sm])
        ps = PS()
        K.op("pe", lambda e, ps=ps: e.matmul(ps[:, 0:4], cs(Mc), nlf, start=True, stop=True), reads=[sm, cst], writes=[ps])
        K.op("dve", lambda e, ps=ps: e.tensor_tensor(out=g, in0=gi, in1=ps[:, 0:4], op=ALU.add), reads=[ps, gb], writes=[sm])
        K.op("dve", lambda e, ps=ps: e.tensor_scalar(out=Fv, in0=ps[:, 0:4], scalar1=-1.0, scalar2=None, op0=ALU.mult), reads=[ps], writes=[sm])
        K.op("dve", lambda e: e.tensor_tensor(out=iw, in0=Fv, in1=mprev_ap, op=ALU.add), reads=[sm, mprev_buf], writes=[sm])
        for h in range(4):
            K.op("dve", lambda e, h=h: e.tensor_scalar(out=Dg[:, h * 128:(h + 1) * 128], in0=ident, scalar1=g[:, h:h + 1], scalar2=None, op0=ALU.mult),
                 reads=[sm, cst], writes=[Dg])
        ps2 = PS()
        K.op("pe", lambda e, ps2=ps2: e.matmul(ps2[:, :], ones, Dg[:, :], start=True, stop=True), reads=[Dg, cst], writes=[ps2])
        for h in range(4):
            K.op("dve", lambda e, h=h, ps2=ps2: e.scalar_tensor_tensor(out=Dm[:, h * 128:(h + 1) * 128], in0=ps2[:, h * 128:(h + 1) * 128], scalar=Fv[:, h:h + 1],
                                                                 in1=cs(NEGc), op0=ALU.add, op1=ALU.add), reads=[ps2, sm, cst], writes=[Dm])
        K.op("dve", lambda e: e.tensor_reduce(out=mx, in_=Dm[:, :].rearrange("p (h s) -> p h s", h=4), axis=AX.X, op=ALU.max), reads=[Dm], writes=[sm])
        K.op("dve", lambda e: e.tensor_tensor(out=mf[:, 0:4], in0=iw, in1=mx, op=ALU.max), reads=[sm], writes=[sm])
        K.op("dve", lambda e: e.tensor_copy(out=mf[:, 4:8], in_=Fv), reads=[sm], writes=[sm])
        mtv = mf[:, 0:4]
        K.op("dve", lambda e: e.tensor_scalar(out=negm, in0=mtv, scalar1=-1.0, scalar2=None, op0=ALU.mult), reads=[sm], writes=[sm])
        if need_P:
            for h in range(4):
                K.op("act", lambda e, h=h: e.activation(out=out["P"][:, h, :], in_=Dm[:, h * 128:(h + 1) * 128], func=AF.Exp, bias=negm[:, h:h + 1]),
                     reads=[Dm, sm], writes=[out["Pbuf"]])
            K.op("dve", lambda e: e.tensor_tensor(out=d1, in0=iw, in1=mtv, op=ALU.subtract), reads=[sm], writes=[sm])
            K.op("act", lambda e: e.activation(out=out["a0"], in_=d1, func=AF.Exp), reads=[sm], writes=[out["gsb"]])
            K.op("act", lambda e: e.activation(out=out["floor"], in_=negm, func=AF.Exp, bias=LN16), reads=[sm], writes=[out["gsb"]])
        ps3 = PS()
        K.op("pe", lambda e, ps3=ps3: e.matmul(ps3[:, 0:8], cs(LASTc), mf, start=True, stop=True), reads=[sm, cst], writes=[ps3])
        K.op("act", lambda e, ps3=ps3: e.activation(out=L8, in_=ps3[:, 0:8], func=AF.Copy), reads=[ps3], writes=[sm])
        K.op("dve", lambda e: e.tensor_tensor(out=d2, in0=L8[:, 4:8], in1=L8[:, 0:4], op=ALU.subtract), reads=[sm], writes=[sm])
        K.op("dve", lambda e: e.tensor_tensor(out=t1, in0=g, in1=d2, op=ALU.add), reads=[sm], writes=[sm])
        K.op("act", lambda e: e.activation(out=out["wL"], in_=t1, func=AF.Exp), reads=[sm], writes=[out["gsb"]])
        K.op("dve", lambda e: e.tensor_tensor(out=d1, in0=d2, in1=mprev_ap, op=ALU.add), reads=[sm, mprev_buf], writes=[sm])
        K.op("act", lambda e: e.activation(out=out["decay"], in_=d1, func=AF.Exp), reads=[sm], writes=[out["gsb"]])
        K.op("dve", lambda e: e.tensor_copy(out=out["mlast"], in_=L8[:, 0:4]), reads=[sm], writes=[out["mlast_buf"]])

    def gates_proj(view, ntile, gt):
        sl, wv = wload([w_in[:, 4096:4104]])
        for t in range(ntile):
            ps = PS()
            mm_group(ps[:, 0:8], ps, [(view[:, kc, t * 128:(t + 1) * 128], wv[:, kc, :]) for kc in range(16)], [xT, sl])
            K.op("dve", lambda e, ps=ps, t=t: e.tensor_tensor(out=gt[:, t * 8:(t + 1) * 8], in0=ps[:, 0:8], in1=bgate[:, :], op=ALU.add),
                 reads=[ps, bgate], writes=[gt])

    brow = [A.alloc(f"brow{i}", 256, F32) for i in range(2)]
    browst = {"i": 0}
    st6 = A.alloc("st6", 16, F32)
    hmo = [A.alloc(f"hmo{i}", 256, BF16) for i in range(2)]
    hmost = {"i": 0}
    convo = A.alloc("convo", 48, F32)
    convso = A.alloc("convso", 16 * 48, F32)
    c0T = A.alloc("c0T", 16 * 48, F32)
    cacc = A.alloc("cacc", NTOK, F32)
    m_head = A.mark()
    xtile.append(A.alloc("xtile0", D, F32))
    xtile.append(xtile[0])
    cvb = A.alloc("cvb", 3 + TP, F32)
    cvs = A.alloc("cvs", 16 * 11, F32)
    qT = A.alloc("qT", 2 * NTOK, BF16)
    kT = A.alloc("kT", 2 * NTOK, BF16)
    vaug = A.alloc("vaug", NT * 257, BF16)
    sigo = A.alloc("sigo", NT * 256, BF16)
    gt = A.alloc("gt", NT * 8, F32)
    Pm = A.alloc("Pm", NT * 512, BF16)
    gsb = A.alloc("gsb", NT * 16, F32)
    mprev_s = A.alloc("mprev_s", 4, F32)
    mprev_tok = A.alloc("mprev_tok", 4, F32)
    mlast_smp = A.alloc("mlast_smp", 4, F32)
    dbc = A.alloc("dbc", 64, F32)
    rhsd = A.alloc("rhsd", 64, F32)
    Cb = A.alloc("Cb", 2 * 257, BF16)
    Sc = A.alloc("Sc", 128, BF16)
    ScT = A.alloc("ScT", 128, BF16)
    ktok = A.alloc("ktok", 256, BF16)
    wvt = A.alloc("wvt", 257, BF16)
    tmpi = A.alloc("tmpi", 257, F32)
    num = A.alloc("num", 257, F32)
    hm = A.alloc("hm", 256, F32)
    tmpo = hm
    C0f = [A.alloc(f"C0f{i}", 2 * 257, F32) for i in range(2)]
    C0b = [A.alloc(f"C0b{i}", 2 * 257, BF16) for i in range(2)]
    Cnew = [A.alloc(f"Cnew{i}", 2 * 257, F32) for i in range(2)]
    wvz = [A.alloc(f"wvz{i}", 257, BF16) for i in range(2)]
    Qz = A.alloc("Qz", 2 * 16 * 128, BF16)
    m_end_mlstm = A.mark()

    def load_brow(c0, n):
        b = brow[browst["i"] % 2]
        browst["i"] += 1
        K.dma("sp", b[:, 0:n], b_in[0:1, c0:c0 + n].partition_broadcast(128), writes=[b])
        return b

    def gates_out(t):
        o = t * 16
        return dict(a0=gsb[:, o:o + 4], floor=gsb[:, o + 4:o + 8], wL=gsb[:, o + 8:o + 12], decay=gsb[:, o + 12:o + 16], gsb=gsb.sub(t),
                    P=Pm[:, t * 512:(t + 1) * 512].rearrange("p (h s) -> p h s", h=4), Pbuf=Pm.sub(t), gbuf=gt)

    def conv_silu(cvbuf, T, cb, dst_ap, dst_buf, seg=None):
        if seg is None:
            src = lambda j: cvbuf[:, j:j + T]
            acc = cacc[:, 0:T]
        else:
            nb, L = seg
            src = lambda j: V(cvbuf.t, j, (3 + L, nb), (1, L))
            acc = cacc[:, 0:nb * L].rearrange("p (b l) -> p b l", b=nb)
        K.op("dve", lambda e: e.tensor_scalar(out=acc, in0=src(0), scalar1=cw[:, cb * 4:cb * 4 + 1], scalar2=None, op0=ALU.mult), reads=[cvbuf, cw], writes=[cacc])
        for j in range(1, 4):
            K.op("dve", lambda e, j=j: e.scalar_tensor_tensor(out=acc, in0=src(j), scalar=cw[:, cb * 4 + j:cb * 4 + j + 1], in1=acc, op0=ALU.mult, op1=ALU.add),
                 reads=[cvbuf, cw, cacc], writes=[cacc])
        K.op("act", lambda e: e.activation(out=dst_ap, in_=acc, func=AF.Silu), reads=[cacc], writes=[dst_buf])

    def make_ktok(kT_ap_fn):
        ps = PS()
        psb = ps[:, 0:128].bitcast(BF16)
        for dc in range(2):
            K.op("pe", lambda e, dc=dc, psb=psb: e.transpose(psb[:, dc * 128:(dc + 1) * 128], kT_ap_fn(dc), identB[:, :]), reads=[kT, identB], writes=[ps])
        K.op("act", lambda e, psb=psb: e.activation(out=ktok[:, :], in_=psb, func=AF.Copy), reads=[ps], writes=[ktok])

    def mlstm_state_update(h, t, wL_ap, decay_ap):
        make_ktok(lambda dc: kT[:, dc * NTOK + t * 128:dc * NTOK + (t + 1) * 128])
        K.op("dve", lambda e: e.tensor_scalar(out=wvt[:, :], in0=vaug[:, t * 257:(t + 1) * 257], scalar1=wL_ap, scalar2=None, op0=ALU.mult), reads=[vaug, gsb], writes=[wvt])
        for dc in range(2):
            pc = PS()
            K.op("pe", lambda e, pc=pc, dc=dc: e.matmul(pc[:, 0:257], ktok[:, dc * 128:(dc + 1) * 128], wvt[:, :], start=True, stop=True), reads=[ktok, wvt], writes=[pc])
            cv_ = Cst[h][:, dc * 257:(dc + 1) * 257]
            K.op("dve", lambda e, pc=pc, cv_=cv_: e.scalar_tensor_tensor(out=cv_, in0=cv_, scalar=decay_ap, in1=pc[:, 0:257], op0=ALU.mult, op1=ALU.add),
                 reads=[pc, gsb], writes=[Cst[h]])

    def head_v_proj(view, ntile, h, slv, wv_):
        bv = load_brow(2048 + h * 256, 256)
        K.op("pool", lambda e: e.memset(V(vaug.t, 256, (257, NT), (1, 1)), 1.0), writes=[vaug])
        for t in range(ntile):
            ps = PS()
            mm_group(ps[:, 0:256], ps, [(view[:, kc, t * 128:(t + 1) * 128], wv_[:, kc, :]) for kc in range(16)], [slv, xT])
            K.op("dve", lambda e, ps=ps, t=t, bv=bv: e.tensor_tensor(out=vaug[:, t * 257:t * 257 + 256], in0=ps[:, 0:256], in1=bv[:, 0:256], op=ALU.add),
                 reads=[ps, bv], writes=[vaug])

    def phase_A_mlstm():
        viewA = xTv(TP)
        gates_proj(viewA, 8, gt)
        for t in range(8):
            og = gates_out(t)
            og["mlast"] = mstate[:, :]
            og["mlast_buf"] = mstate
            K.op("dve", lambda e: e.tensor_copy(out=mprev_s[:, :], in_=mstate[:, :]), reads=[mstate], writes=[mprev_s])
            gate_tile(gt[:, t * 8:(t + 1) * 8], C_MP, C_NEGP, C_LASTP, mprev_s[:, :], mprev_s, og, need_P=False)
        K.op("pool", lambda e: e.memset(cvb[:, 0:3], 0.0), writes=[cvb])
        for h in range(4):
            slk, wk = wload([w_in[:, 1024 + h * 256:1024 + (h + 1) * 256]])
            slv, wv_ = wload([w_in[:, 2048 + h * 256:2048 + (h + 1) * 256]])
            for cbk in range(2):
                cb = 8 + h * 2 + cbk
                for tb in range(2):
                    ps = PS()
                    mm_group(ps[:, :], ps, [(wk[:, kc, cbk * 128:(cbk + 1) * 128], viewA[:, kc, tb * 512:(tb + 1) * 512]) for kc in range(16)], [slk, xT])
                    K.op("act", lambda e, ps=ps, tb=tb, cb=cb: e.activation(out=cvb[:, 3 + tb * 512:3 + (tb + 1) * 512], in_=ps[:, :], func=AF.Identity, bias=bqk[:, cb:cb + 1]),
                         reads=[ps, bqk], writes=[cvb])
                conv_silu(cvb, TP, cb, kT[:, cbk * NTOK:cbk * NTOK + TP], kT)
            head_v_proj(viewA, 8, h, slv, wv_)
            for t in range(8):
                og = gates_out(t)
                mlstm_state_update(h, t, og["wL"][:, h:h + 1], og["decay"][:, h:h + 1])
            K.op("dve", lambda e, h=h: e.tensor_scalar(out=Cst[h][:, :], in0=Cst[h][:, :], scalar1=flag[:, 0:1], scalar2=None, op0=ALU.mult), reads=[flag], writes=[Cst[h]])
        K.op("dve", lambda e: e.tensor_scalar(out=mstate[:, :], in0=mstate[:, :], scalar1=flag[:, 0:1], scalar2=None, op0=ALU.mult), reads=[flag], writes=[mstate])

    def mlstm_tile_full(h, t, sample):
        og = gates_out(t)
        qs = lambda dc: qT[:, dc * NTOK + t * 128:dc * NTOK + (t + 1) * 128]
        ks = lambda dc: kT[:, dc * NTOK + t * 128:dc * NTOK + (t + 1) * 128]
        ps = PS()
        mm_group(ps[:, 0:128], ps, [(qs(dc), ks(dc)) for dc in range(2)], [qT, kT])
        K.op("dve", lambda e, ps=ps: e.tensor_tensor(out=Sc[:, :], in0=ps[:, 0:128], in1=og["P"][:, h, :], op=ALU.mult), reads=[ps, og["Pbuf"]], writes=[Sc])
        pt = PS()
        ptb = pt[:, 0:64].bitcast(BF16)
        K.op("pe", lambda e, ptb=ptb: e.transpose(ptb, Sc[:, :], identB[:, :]), reads=[Sc, identB], writes=[pt])
        K.op("act", lambda e, ptb=ptb: e.activation(out=ScT[:, :], in_=ptb, func=AF.Copy), reads=[pt], writes=[ScT])
        pa = psum[7] if sample else PS()
        K.op("pe", lambda e, pa=pa: e.matmul(pa[:, 0:257], ScT[:, :], vaug[:, t * 257:(t + 1) * 257], start=True, stop=True), reads=[ScT, vaug], writes=[pa])
        pb = psum[6] if sample else PS()
        if not sample:
            mm_group(pb[:, 0:257], pb, [(qs(dc), Cb[:, dc * 257:(dc + 1) * 257]) for dc in range(2)], [qT, Cb])
        else:
            for dc in range(2):
                K.op("pool", lambda e, dc=dc: e.tensor_copy(out=V(Qz.t, dc * 2048, (136, 16), (1, 8)),
                                                           in_=qT[:, dc * NTOK + TP:dc * NTOK + NTOK].rearrange("p (b l) -> p b l", b=16)), reads=[qT], writes=[Qz])
            make_ktok(ks)
            def load_c0(b):
                cf_ = C0f[b % 2]
                K.dma("sp", V(cf_.t, 0, (257, 2), (1, 256)), I["C0"][b, h].rearrange("(dc p) v -> p dc v", p=128), writes=[cf_])
                K.dma("sp", V(cf_.t, 256, (257, 2)), I["n0"][b, h].rearrange("(dc p) -> p dc", p=128), writes=[cf_])

            load_c0(0)
            for b in range(16):
                cf = C0f[b % 2]
                cbf = C0b[b % 2]
                cn = Cnew[b % 2]
                wz = wvz[b % 2]
                if b + 1 < 16:
                    load_c0(b + 1)
                K.op("act", lambda e, cf=cf, cbf=cbf: e.activation(out=cbf[:, :], in_=cf[:, :], func=AF.Copy), reads=[cf], writes=[cbf])
                for dc in range(2):
                    first = (b == 0 and dc == 0)
                    last = (b == 15 and dc == 1)
                    K.op("pe", lambda e, pb=pb, dc=dc, b=b, cbf=cbf, first=first, last=last: e.matmul(
                        pb[:, 0:257], Qz[:, dc * 2048 + b * 128:dc * 2048 + (b + 1) * 128], cbf[:, dc * 257:(dc + 1) * 257], start=first, stop=last),
                        reads=[Qz, cbf], writes=[pb])
                K.op("dve", lambda e, wz=wz, b=b: e.tensor_scalar(out=wz[:, :], in0=vaug[:, t * 257:(t + 1) * 257], scalar1=og["wL"][:, h:h + 1],
                                                                    scalar2=cst[:, C_SEQM + b:C_SEQM + b + 1], op0=ALU.mult, op1=ALU.mult), reads=[vaug, gsb, cst], writes=[wz])
                for dc in range(2):
                    pc = PS()
                    K.op("pe", lambda e, pc=pc, dc=dc, wz=wz: e.matmul(pc[:, 0:257], ktok[:, dc * 128:(dc + 1) * 128], wz[:, :], start=True, stop=True), reads=[ktok, wz], writes=[pc])
                    K.op("dve", lambda e, pc=pc, dc=dc, cf=cf, cn=cn, b=b: e.scalar_tensor_tensor(
                        out=cn[:, dc * 257:(dc + 1) * 257], in0=cf[:, dc * 257:(dc + 1) * 257], scalar=dbc[:, h * 16 + b:h * 16 + b + 1], in1=pc[:, 0:257],
                        op0=ALU.mult, op1=ALU.add), reads=[pc, cf, dbc], writes=[cn])
                K.dma("sp", O["Cs"][b, h].rearrange("(dc p) v -> p dc v", p=128), V(cn.t, 0, (257, 2), (1, 256)), reads=[cn])
                K.dma("sp", O["ns"][b, h].rearrange("(dc p) -> p dc", p=128), V(cn.t, 256, (257, 2)), reads=[cn])
        K.op("act", lambda e, pb=pb: e.activation(out=tmpi[:, :], in_=pb[:, 0:257], func=AF.Copy, scale=og["a0"][:, h:h + 1]), reads=[pb, gsb], writes=[tmpi])
        K.op("dve", lambda e, pa=pa: e.tensor_tensor(out=num[:, :], in0=tmpi[:, :], in1=pa[:, 0:257], op=ALU.add), reads=[pa, tmpi], writes=[num])
        K.op("dve", lambda e: e.tensor_scalar(out=st6[:, 12:13], in0=num[:, 256:257], scalar1=-1.0, scalar2=None, op0=ALU.mult), reads=[num], writes=[st6])
        K.op("dve", lambda e: e.scalar_tensor_tensor(out=st6[:, 0:1], in0=num[:, 256:257], scalar=og["floor"][:, h:h + 1], in1=st6[:, 12:13], op0=ALU.max, op1=ALU.max),
             reads=[num, gsb, st6], writes=[st6])
        K.op("dve", lambda e: e.reciprocal(out=st6[:, 1:2], in_=st6[:, 0:1]), reads=[st6], writes=[st6])
        K.op("dve", lambda e: e.scalar_tensor_tensor(out=hm[:, :], in0=num[:, 0:256], scalar=st6[:, 1:2], in1=sigo[:, t * 256:(t + 1) * 256], op0=ALU.mult, op1=ALU.mult),
             reads=[num, st6, sigo], writes=[hm])
        K.op("dve", lambda e: e.bn_stats(out=st6[:, 2:8], in_=hm[:, :]), reads=[hm], writes=[st6])
        K.op("dve", lambda e: e.bn_aggr(out=st6[:, 8:10], in_=st6[:, 2:8]), reads=[st6], writes=[st6])
        K.op("act", lambda e: e.activation(out=st6[:, 10:11], in_=st6[:, 9:10], func=AF.Sqrt, bias=EPS), reads=[st6], writes=[st6])
        K.op("dve", lambda e: e.reciprocal(out=st6[:, 11:12], in_=st6[:, 10:11]), reads=[st6], writes=[st6])
        ho = hmo[hmost["i"] % 2]
        hmost["i"] += 1
        K.op("dve", lambda e, ho=ho: e.tensor_scalar(out=ho[:, :], in0=hm[:, :], scalar1=st6[:, 8:9], scalar2=st6[:, 11:12], op0=ALU.subtract, op1=ALU.mult),
             reads=[hm, st6], writes=[ho])
        K.dma("sp", hm_scr[t * 128:(t + 1) * 128, h * 256:(h + 1) * 256], ho[:, :], reads=[ho], writes=[hm_scr_b])
        if not sample:
            mlstm_state_update(h, t, og["wL"][:, h:h + 1], og["decay"][:, h:h + 1])
            K.op("act", lambda e: e.activation(out=Cb[:, :], in_=Cst[h][:, :], func=AF.Copy), reads=[Cst[h]], writes=[Cb])

    def phase_B_mlstm():
        viewB = xTv(NTOK)
        cvin = xtile[0]
        K.dma("sp", cvin[0:48, :], I["conv0"], writes=[cvin])
        for g in range(4):
            ps = PS()
            for j in range(4):
                cb = g * 4 + j
                K.op("pe", lambda e, ps=ps, j=j, cb=cb: e.transpose(ps[:, j * 48:(j + 1) * 48], cvin[0:48, cb * 128:(cb + 1) * 128], cst[0:48, C_ID:C_ID + 48]),
                     reads=[cvin, cst], writes=[ps])
            K.op("act", lambda e, ps=ps, g=g: e.activation(out=c0T[:, g * 192:(g + 1) * 192], in_=ps[:, 0:192], func=AF.Copy), reads=[ps], writes=[c0T])
        if cut <= 1:
            return
        gates_proj(viewB, 9, gt)
        for t in range(8):
            og = gates_out(t)
            og["mlast"] = mstate[:, :]
            og["mlast_buf"] = mstate
            K.op("dve", lambda e: e.tensor_copy(out=mprev_s[:, :], in_=mstate[:, :]), reads=[mstate], writes=[mprev_s])
            gate_tile(gt[:, t * 8:(t + 1) * 8], C_MP, C_NEGP, C_LASTP, mprev_s[:, :], mprev_s, og, need_P=True)
        K.dma("sp", O["mp"], mstate[0:1, :], reads=[mstate])
        if cut <= 2:
            return
        K.dma("sp", mprev_tok[:, :], bass.AP(tensor=I["m0"].tensor, offset=0, ap=[[4, 16], [0, 8], [1, 4]]), writes=[mprev_tok])
        og = gates_out(8)
        og["mlast"] = mlast_smp[:, :]
        og["mlast_buf"] = mlast_smp
        gate_tile(gt[:, 64:72], C_MS, C_NEGS, C_LASTS, mprev_tok[:, :], mprev_tok, og, need_P=True)
        K.dma("sp", O["ms"], bass.AP(tensor=mlast_smp.t, offset=7 * 4, ap=[[32, 16], [1, 4]]), reads=[mlast_smp])
        for h in range(4):
            K.op("dve", lambda e, h=h: e.tensor_scalar(out=rhsd[:, h * 16:(h + 1) * 16], in0=cst[:, C_SEQ1:C_SEQ1 + 16], scalar1=og["decay"][:, h:h + 1], scalar2=None, op0=ALU.mult),
                 reads=[cst, gsb], writes=[rhsd])
        ps = PS()
        K.op("pe", lambda e, ps=ps: e.matmul(ps[:, 0:64], ones, rhsd[:, :], start=True, stop=True), reads=[rhsd, cst], writes=[ps])
        K.op("act", lambda e, ps=ps: e.activation(out=dbc[:, :], in_=ps[:, 0:64], func=AF.Copy), reads=[ps], writes=[dbc])
        K.op("pool", lambda e: e.memset(Qz[:, :], 0.0), writes=[Qz])
        xh = xhalo[:, :].rearrange("p (k j) -> p k j", k=16)
        if cut <= 3:
            return
        for h in range(4):
            slq, wq = wload([w_in[:, h * 256:(h + 1) * 256]])
            slk, wk = wload([w_in[:, 1024 + h * 256:1024 + (h + 1) * 256]])
            slv, wv_ = wload([w_in[:, 2048 + h * 256:2048 + (h + 1) * 256]])
            slo, wo_ = wload([w_in[:, 3072 + h * 256:3072 + (h + 1) * 256]])
            for (sl, wt, cb0, dstT) in ((slq, wq, h * 2, qT), (slk, wk, 8 + h * 2, kT)):
                for cbk in range(2):
                    cb = cb0 + cbk
                    lw = lambda kc, wt=wt, cbk=cbk: wt[:, kc, cbk * 128:(cbk + 1) * 128]
                    ps = PS()
                    mm_group(ps[:, 0:3], ps, [(lw(kc), xh[:, kc, :]) for kc in range(16)], [sl, xhalo])
                    K.op("act", lambda e, ps=ps, cb=cb: e.activation(out=cvb[:, 0:3], in_=ps[:, 0:3], func=AF.Identity, bias=bqk[:, cb:cb + 1]), reads=[ps, bqk], writes=[cvb])
                    K.op("dve", lambda e: e.tensor_scalar(out=cvb[:, 0:3], in0=cvb[:, 0:3], scalar1=flag[:, 0:1], scalar2=None, op0=ALU.mult), reads=[flag], writes=[cvb])
                    for tb in range(2):
                        ps = PS()
                        mm_group(ps[:, :], ps, [(lw(kc), viewB[:, kc, tb * 512:(tb + 1) * 512]) for kc in range(16)], [sl, xT])
                        K.op("act", lambda e, ps=ps, tb=tb, cb=cb: e.activation(out=cvb[:, 3 + tb * 512:3 + (tb + 1) * 512], in_=ps[:, :], func=AF.Identity, bias=bqk[:, cb:cb + 1]),
                             reads=[ps, bqk], writes=[cvb])
                    ps = PS()
                    mm_group(ps[:, 0:128], ps, [(lw(kc), viewB[:, kc, TP:NTOK]) for kc in range(16)], [sl, xT])
                    K.op("act", lambda e, ps=ps, cb=cb: e.activation(out=V(cvs.t, 3, (11, 16), (1, 8)), in_=ps[:, 0:128].rearrange("p (b l) -> p b l", b=16), func=AF.Identity,
                                                                     bias=bqk[:, cb:cb + 1]), reads=[ps, bqk], writes=[cvs])
                    K.op("dve", lambda e, cb=cb: e.tensor_copy(out=V(cvs.t, 0, (11, 16), (1, 3)), in_=c0T[:, cb * 48:(cb + 1) * 48].rearrange("p (b j) -> p b j", b=16)),
                         reads=[c0T], writes=[cvs])
                    conv_silu(cvb, TP, cb, dstT[:, cbk * NTOK:cbk * NTOK + TP], dstT)
                    conv_silu(cvs, 128, cb, dstT[:, cbk * NTOK + TP:cbk * NTOK + NTOK].rearrange("p (b l) -> p b l", b=16), dstT, seg=(16, 8))
                    K.op("pool", lambda e, cb=cb: e.tensor_copy(out=convo[:, cb * 3:(cb + 1) * 3], in_=cvb[:, TP:TP + 3]), reads=[cvb], writes=[convo])
                    K.op("pool", lambda e, cb=cb: e.tensor_copy(out=convso[:, cb * 48:(cb + 1) * 48].rearrange("p (b j) -> p b j", b=16), in_=V(cvs.t, 8, (11, 16), (1, 3))),
                         reads=[cvs], writes=[convso])
            head_v_proj(viewB, 9, h, slv, wv_)
            bo = load_brow(3072 + h * 256, 256)
            for t in range(9):
                ps = PS()
                mm_group(ps[:, 0:256], ps, [(viewB[:, kc, t * 128:(t + 1) * 128], wo_[:, kc, :]) for kc in range(16)], [slo, xT])
                K.op("dve", lambda e, ps=ps, bo=bo: e.tensor_tensor(out=tmpo[:, :], in0=ps[:, 0:256], in1=bo[:, 0:256], op=ALU.add), reads=[ps, bo], writes=[tmpo])
                K.op("act", lambda e, t=t: e.activation(out=sigo[:, t * 256:(t + 1) * 256], in_=tmpo[:, :], func=AF.Sigmoid), reads=[tmpo], writes=[sigo])
            K.op("act", lambda e, h=h: e.activation(out=Cb[:, :], in_=Cst[h][:, :], func=AF.Copy), reads=[Cst[h]], writes=[Cb])
            if cut <= 4:
                continue
            for t in range(8):
                mlstm_tile_full(h, t, False)
            K.dma("sp", O["Cp"][h].rearrange("(dc p) v -> p dc v", p=128), V(Cst[h].t, 0, (257, 2), (1, 256)), reads=[Cst[h]])
            K.dma("sp", O["np_"][h].rearrange("(dc p) -> p dc", p=128), V(Cst[h].t, 256, (257, 2)), reads=[Cst[h]])
            if cut <= 5:
                continue
            mlstm_tile_full(h, 8, True)
        for cb in range(16):
            K.dma("sp", O["convp"][:, cb * 128:(cb + 1) * 128].rearrange("j p -> p j"), convo[:, cb * 3:(cb + 1) * 3], reads=[convo])
            K.dma("sp", O["convs"][:, :, cb * 128:(cb + 1) * 128].rearrange("b j p -> p b j"), convso[:, cb * 48:(cb + 1) * 48].rearrange("p (b j) -> p b j", b=16), reads=[convso])

    A.release(m_head)
    sg = A.alloc("sg", NTOK, F32)
    lf = A.alloc("lf", NTOK, F32)
    kk = A.alloc("kk", NTOK, F32)
    bb = A.alloc("bb", NTOK, F32)
    qh = A.alloc("qh", NTOK, F32)
    ex = lf
    qb = A.alloc("qb", NTOK, BF16)
    kb = A.alloc("kb", NTOK, BF16)
    qS = A.alloc("qS", NTOK, BF16)
    kLT = A.alloc("kLT", NTOK, BF16)
    vtok = A.alloc("vtok", NT * 128, BF16)
    gg = A.alloc("gg", NT * 128, BF16)
    eL = A.alloc("eL", 24, F32)
    Sb = A.alloc("Sb", 128, BF16)
    ATb = A.alloc("ATb", 128, BF16)
    kLtok = A.alloc("kLtok", 128, BF16)
    ogo = [A.alloc(f"ogo{i}", 128, BF16) for i in range(2)]
    ogst = {"i": 0}
    junk = A.alloc("junk", 128, F32)
    S0f = A.alloc("S0f", 2048, F32)
    S0b = A.alloc("S0b", 2048, BF16)
    Vz = [A.alloc(f"Vz{i}", 512, BF16) for i in range(2)]
    QzH = A.alloc("QzH", 2048, BF16)
    m_end_hgrn = A.mark()

    def hgrn_head(hh, view, ntok, full):
        ntile = ntok // 128
        tbs = [(0, 512), (512, 512)] + ([(1024, 128)] if ntok > 1024 else [])
        if full and cut > 7:
            K.dma("sp", S0f[:, :].rearrange("p (b v) -> p b v", b=16), I["S0"][:, hh].rearrange("b c v -> c b v"), writes=[S0f])
        cf0 = 5128 + hh * 128
        ci0 = 6152 + hh * 128
        if full:
            sl1, w1 = wload([w_in[:, 4104 + hh * 128:4104 + (hh + 1) * 128], w_in[:, cf0:cf0 + 128]])
            sl2, w2 = wload([w_in[:, ci0:ci0 + 128], w_in[:, 7176 + hh * 128:7176 + (hh + 1) * 128]])
            fcol, icol = 128, 0
        else:
            sl1, w1 = wload([w_in[:, cf0:cf0 + 128], w_in[:, ci0:ci0 + 128]])
            sl2, w2 = sl1, w1
            fcol, icol = 0, 128
        for (t0, n) in tbs:
            ps = PS()
            mm_group(ps[:, 0:n], ps, [(w1[:, kc, fcol:fcol + 128], view[:, kc, t0:t0 + n]) for kc in range(16)], [sl1, xT])
            K.op("act", lambda e, ps=ps, t0=t0, n=n: e.activation(out=sg[:, t0:t0 + n], in_=ps[:, 0:n], func=AF.Sigmoid, bias=bfh[:, hh:hh + 1]), reads=[ps, bfh], writes=[sg])
            if full:
                ps = PS()
                mm_group(ps[:, 0:n], ps, [(w1[:, kc, 0:128], view[:, kc, t0:t0 + n]) for kc in range(16)], [sl1, xT])
                K.op("act", lambda e, ps=ps, t0=t0, n=n: e.activation(out=qh[:, t0:t0 + n], in_=ps[:, 0:n], func=AF.Silu, bias=bqh[:, hh:hh + 1]), reads=[ps, bqh], writes=[qh])
        N = ntok
        K.op("dve", lambda e: e.tensor_scalar(out=sg[:, 0:N], in0=sg[:, 0:N], scalar1=oml[:, hh:hh + 1], scalar2=lb[:, hh:hh + 1], op0=ALU.mult, op1=ALU.add), reads=[oml, lb], writes=[sg])
        K.op("act", lambda e: e.activation(out=lf[:, 0:N], in_=sg[:, 0:N], func=AF.Ln), reads=[sg], writes=[lf])
        K.op("dve", lambda e: e.tensor_scalar(out=kk[:, 0:N], in0=sg[:, 0:N], scalar1=-1.0, scalar2=1.0, op0=ALU.mult, op1=ALU.add), reads=[sg], writes=[kk])
        bi = load_brow(ci0, 128)
        if not full:
            for t in range(ntile):
                ps = PS()
                mm_group(ps[:, 0:128], ps, [(view[:, kc, t * 128:(t + 1) * 128], w2[:, kc, icol:icol + 128]) for kc in range(16)], [sl2, xT])
                K.op("dve", lambda e, ps=ps, t=t, bi=bi: e.tensor_tensor(out=vtok[:, t * 128:(t + 1) * 128], in0=ps[:, 0:128], in1=bi[:, 0:128], op=ALU.add), reads=[ps, bi], writes=[vtok])
        else:
            bg = load_brow(7176 + hh * 128, 128)
            for t in range(ntile):
                ps = PS()
                mm_group(ps[:, 0:256], ps, [(view[:, kc, t * 128:(t + 1) * 128], w2[:, kc, :]) for kc in range(16)], [sl2, xT])
                K.op("dve", lambda e, ps=ps, t=t, bi=bi: e.tensor_tensor(out=vtok[:, t * 128:(t + 1) * 128], in0=ps[:, 0:128], in1=bi[:, 0:128], op=ALU.add), reads=[ps, bi], writes=[vtok])
                K.op("dve", lambda e, ps=ps, bg=bg: e.tensor_tensor(out=junk[:, :], in0=ps[:, 128:256], in1=bg[:, 0:128], op=ALU.add), reads=[ps, bg], writes=[junk])
                K.op("act", lambda e, t=t: e.activation(out=gg[:, t * 128:(t + 1) * 128], in_=junk[:, :], func=AF.Silu), reads=[junk], writes=[gg])
        K.op("dve", lambda e: e.tensor_tensor_scan(out=bb[:, 0:N], data0=cst[:, C_RM:C_RM + N], data1=lf[:, 0:N], initial=0.0, op0=ALU.mult, op1=ALU.add), reads=[cst, lf], writes=[bb])
        npt = 8
        K.op("dve", lambda e: e.tensor_tensor(out=ex[:, 0:1024].rearrange("p (t s) -> p t s", t=npt), in0=V(bb.t, 127, (128, npt), (0, 128)),
                                              in1=bb[:, 0:1024].rearrange("p (t s) -> p t s", t=npt), op=ALU.subtract), reads=[bb], writes=[ex])
        if N > 1024:
            K.op("dve", lambda e: e.tensor_tensor(out=ex[:, 1024:N].rearrange("p (b l) -> p b l", b=16), in0=V(bb.t, 1024 + 7, (8, 16), (0, 8)),
                                                  in1=bb[:, 1024:N].rearrange("p (b l) -> p b l", b=16), op=ALU.subtract), reads=[bb], writes=[ex])
        K.op("act", lambda e: e.activation(out=ex[:, 0:N], in_=ex[:, 0:N], func=AF.Exp), reads=[ex], writes=[ex])
        K.op("dve", lambda e: e.tensor_tensor(out=kLT[:, 0:N], in0=kk[:, 0:N], in1=ex[:, 0:N], op=ALU.mult), reads=[kk, ex], writes=[kLT])
        K.op("act", lambda e: e.activation(out=eL[:, 0:8], in_=V(bb.t, 127, (128, 8)), func=AF.Exp), reads=[bb], writes=[eL])
        if N > 1024:
            K.op("act", lambda e: e.activation(out=eL[:, 8:24], in_=V(bb.t, 1024 + 7, (8, 16)), func=AF.Exp), reads=[bb], writes=[eL])
        if full:
            nt_all = N // 128
            K.op("dve", lambda e: e.tensor_tensor(out=ex[:, 0:N].rearrange("p (t s) -> p t s", t=nt_all), in0=bb[:, 0:N].rearrange("p (t s) -> p t s", t=nt_all),
                                                  in1=V(bb.t, 63, (128, nt_all), (0, 128)), op=ALU.subtract), reads=[bb, kLT], writes=[ex])
            K.op("act", lambda e: e.activation(out=sg[:, 0:N], in_=ex[:, 0:N], func=AF.Exp), reads=[ex], writes=[sg])
            K.op("dve", lambda e: e.tensor_tensor(out=qb[:, 0:N], in0=qh[:, 0:N], in1=sg[:, 0:N], op=ALU.mult), reads=[qh, sg], writes=[qb])
            K.op("act", lambda e: e.activation(out=sg[:, 0:N], in_=ex[:, 0:N], func=AF.Exp, scale=-1.0), reads=[ex, qb], writes=[sg])
            K.op("dve", lambda e: e.tensor_tensor(out=kb[:, 0:N], in0=kk[:, 0:N], in1=sg[:, 0:N], op=ALU.mult), reads=[kk, sg], writes=[kb])
            K.op("act", lambda e: e.activation(out=sg[:, 0:N], in_=bb[:, 0:N], func=AF.Exp), reads=[bb, kb], writes=[sg])
            K.op("dve", lambda e: e.tensor_tensor(out=qS[:, 0:N], in0=qh[:, 0:N], in1=sg[:, 0:N], op=ALU.mult), reads=[qh, sg], writes=[qS])
            K.op("act", lambda e: e.activation(out=Sb[:, :], in_=Sst[hh][:, :], func=AF.Copy), reads=[Sst[hh]], writes=[Sb])

        def norm_out(po, t):
            K.op("act", lambda e, po=po: e.activation(out=junk[:, :], in_=po[:, 0:128], func=AF.Square, accum_out=st6[:, 0:1]), reads=[po], writes=[junk, st6])
            K.op("act", lambda e: e.activation(out=st6[:, 1:2], in_=st6[:, 0:1], func=AF.Sqrt, scale=1.0 / 128.0, bias=EPS), reads=[st6], writes=[st6])
            K.op("dve", lambda e: e.reciprocal(out=st6[:, 2:3], in_=st6[:, 1:2]), reads=[st6], writes=[st6])
            oo = ogo[ogst["i"] % 2]
            ogst["i"] += 1
            K.op("dve", lambda e, po=po, oo=oo: e.scalar_tensor_tensor(out=oo[:, :], in0=po[:, 0:128], scalar=st6[:, 2:3], in1=gg[:, t * 128:(t + 1) * 128], op0=ALU.mult, op1=ALU.mult),
                 reads=[po, st6, gg], writes=[oo])
            K.dma("sp", og_scr[t * 128:(t + 1) * 128, hh * 128:(hh + 1) * 128], oo[:, :], reads=[oo], writes=[og_scr_b])

        def make_kLtok(t):
            pt = PS()
            ptb = pt[:, 0:64].bitcast(BF16)
            K.op("pe", lambda e, ptb=ptb: e.transpose(ptb, kLT[:, t * 128:(t + 1) * 128], identB[:, :]), reads=[kLT, identB], writes=[pt])
            K.op("act", lambda e, ptb=ptb: e.activation(out=kLtok[:, :], in_=ptb, func=AF.Copy), reads=[pt], writes=[kLtok])

        for t in range(8):
            sl_t = slice(t * 128, (t + 1) * 128)
            if full:
                pA = PS()
                K.op("pe", lambda e, pA=pA, sl_t=sl_t: e.matmul(pA[:, 0:128], kb[:, sl_t], qb[:, sl_t], start=True, stop=True), reads=[kb, qb], writes=[pA])
                K.op("dve", lambda e, pA=pA: e.tensor_tensor(out=ATb[:, :], in0=pA[:, 0:128], in1=cs(C_MP), op=ALU.mult), reads=[pA, cst], writes=[ATb])
                po = PS()
                mm_group(po[:, 0:128], po, [(ATb[:, :], vtok[:, sl_t]), (qS[:, sl_t], Sb[:, :])], [ATb, vtok, qS, Sb])
                norm_out(po, t)
            make_kLtok(t)
            pc = PS()
            K.op("pe", lambda e, pc=pc, sl_t=sl_t: e.matmul(pc[:, 0:128], kLtok[:, :], vtok[:, sl_t], start=True, stop=True), reads=[kLtok, vtok], writes=[pc])
            K.op("dve", lambda e, pc=pc, t=t: e.scalar_tensor_tensor(out=Sst[hh][:, :], in0=Sst[hh][:, :], scalar=eL[:, t:t + 1], in1=pc[:, 0:128], op0=ALU.mult, op1=ALU.add),
                 reads=[pc, eL], writes=[Sst[hh]])
            if full:
                K.op("act", lambda e: e.activation(out=Sb[:, :], in_=Sst[hh][:, :], func=AF.Copy), reads=[Sst[hh]], writes=[Sb])
        if not full:
            K.op("dve", lambda e: e.tensor_scalar(out=Sst[hh][:, :], in0=Sst[hh][:, :], scalar1=flag[:, 0:1], scalar2=None, op0=ALU.mult), reads=[flag], writes=[Sst[hh]])
            return
        K.dma("sp", O["Sp"][hh], Sst[hh][:, :], reads=[Sst[hh]])
        if cut <= 7:
            return
        t = 8
        sl_t = slice(TP, NTOK)
        K.op("act", lambda e: e.activation(out=S0b[:, :], in_=S0f[:, :], func=AF.Copy), reads=[S0f], writes=[S0b])
        K.op("pool", lambda e: e.tensor_copy(out=V(QzH.t, 0, (136, 16), (1, 8)), in_=qS[:, sl_t].rearrange("p (b l) -> p b l", b=16)), reads=[qS], writes=[QzH])
        pA = PS()
        K.op("pe", lambda e, pA=pA: e.matmul(pA[:, 0:128], kb[:, sl_t], qb[:, sl_t], start=True, stop=True), reads=[kb, qb], writes=[pA])
        K.op("dve", lambda e, pA=pA: e.tensor_tensor(out=ATb[:, :], in0=pA[:, 0:128], in1=cs(C_MS), op=ALU.mult), reads=[pA, cst], writes=[ATb])
        po = PS()
        mm_group(po[:, 0:128], po, [(ATb[:, :], vtok[:, t * 128:(t + 1) * 128])] + [(QzH[:, b * 128:(b + 1) * 128], S0b[:, b * 128:(b + 1) * 128]) for b in range(16)],
                 [ATb, vtok, QzH, S0b])
        norm_out(po, t)
        make_kLtok(t)
        for g in range(4):
            vz = Vz[g % 2]
            K.op("dve", lambda e, g=g, vz=vz: e.tensor_tensor(out=vz[:, :].rearrange("p (b v) -> p b v", b=4), in0=V(vtok.t, t * 128, (0, 4), (1, 128)),
                                                               in1=V(cst.t, C_SEQM + g * 4, (1, 4), (0, 128)), op=ALU.mult), reads=[vtok, cst], writes=[vz])
            pc = PS()
            K.op("pe", lambda e, pc=pc, vz=vz: e.matmul(pc[:, :], kLtok[:, :], vz[:, :], start=True, stop=True), reads=[kLtok, vz], writes=[pc])
            K.op("dve", lambda e, g=g: e.tensor_tensor(out=S0f[:, g * 512:(g + 1) * 512].rearrange("p (b v) -> p b v", b=4), in0=S0f[:, g * 512:(g + 1) * 512].rearrange("p (b v) -> p b v", b=4),
                                                       in1=V(eL.t, 8 + g * 4, (1, 4), (0, 128)), op=ALU.mult), reads=[eL, S0b], writes=[S0f])
            K.op("dve", lambda e, pc=pc, g=g: e.tensor_tensor(out=S0f[:, g * 512:(g + 1) * 512], in0=S0f[:, g * 512:(g + 1) * 512], in1=pc[:, :], op=ALU.add), reads=[pc], writes=[S0f])
        K.dma("sp", O["Ss"][:, hh].rearrange("b c v -> c b v"), S0f[:, :].rearrange("p (b v) -> p b v", b=16), reads=[S0f])

    if stage >= 1:
        build_xT([(I["xpre"], 8)], TP)
        K.op("dve", lambda e: e.tensor_copy(out=xhalo[:, :].rearrange("p (k j) -> p k j", k=16), in_=xTv(TP)[:, :, TP - 3:TP]), reads=[xT], writes=[xhalo])
        phase_A_mlstm()
        K.barrier()
        K.op("pool", lambda e: e.memset(QzH[:, :], 0.0), writes=[QzH])
        for hh in range(8):
            hgrn_head(hh, xTv(TP), TP, False)
        K.barrier()
    if stage >= 2:
        build_xT([(I["xp"], 8), (I["xs"], 1)], NTOK)
        phase_B_mlstm()
        K.barrier()
        K.op("pool", lambda e: e.memset(QzH[:, :], 0.0), writes=[QzH])
        if cut > 6:
            for hh in range(8):
                hgrn_head(hh, xTv(NTOK), NTOK, True)
        K.barrier()

    r_scr = nc.dram_tensor("r_scr", [NTOK, D], F32, kind="Internal").ap()
    h_scr = nc.dram_tensor("h_scr", [NTOK, D], F32, kind="Internal").ap()
    r_scr_b = Buf("r_scr")
    h_scr_b = Buf("h_scr")

    def xrows(t):
        return I["xp"][t * 128:(t + 1) * 128, :] if t < 8 else I["xs"]

    def layer_norm_tiles(g_in, b_in_, dstT_buf, dstT_view, tilebufs, lng, lnb, lnst, out_dram=None, out_fn=None, inplace=False):
        K.dma("sp", lng[:, :], g_in[0:1, :].partition_broadcast(128), writes=[lng])
        K.dma("sp", lnb[:, :], b_in_[0:1, :].partition_broadcast(128), writes=[lnb])
        if not inplace:
            K.dma("sp", tilebufs[0][:, :], r_scr[0:128, :], reads=[r_scr_b], writes=[tilebufs[0]])
        for t in range(NT):
            if inplace:
                rt = tilebufs[t]
            else:
                rt = tilebufs[t % 2]
                if t + 1 < NT:
                    K.dma("sp", tilebufs[(t + 1) % 2][:, :], r_scr[(t + 1) * 128:(t + 2) * 128, :], reads=[r_scr_b], writes=[tilebufs[(t + 1) % 2]])
            for c in range(4):
                K.op("dve", lambda e, c=c, rt=rt: e.bn_stats(out=lnst[:, c * 6:(c + 1) * 6], in_=rt[:, c * 512:(c + 1) * 512]), reads=[rt], writes=[lnst])
            K.op("dve", lambda e: e.bn_aggr(out=lnst[:, 24:26], in_=lnst[:, 0:24]), reads=[lnst], writes=[lnst])
            K.op("act", lambda e: e.activation(out=lnst[:, 26:27], in_=lnst[:, 25:26], func=AF.Sqrt, bias=EPS), reads=[lnst], writes=[lnst])
            K.op("dve", lambda e: e.reciprocal(out=lnst[:, 27:28], in_=lnst[:, 26:27]), reads=[lnst], writes=[lnst])
            K.op("dve", lambda e: e.tensor_scalar(out=lnst[:, 28:29], in0=lnst[:, 24:25], scalar1=-1.0, scalar2=lnst[:, 27:28], op0=ALU.mult, op1=ALU.mult), reads=[lnst], writes=[lnst])
            K.op("act", lambda e, rt=rt: e.activation(out=rt[:, :], in_=rt[:, :], func=AF.Identity, scale=lnst[:, 27:28], bias=lnst[:, 28:29]), reads=[lnst], writes=[rt])
            K.op("dve", lambda e, rt=rt: e.tensor_tensor(out=rt[:, :], in0=rt[:, :], in1=lng[:, :], op=ALU.mult), reads=[lng], writes=[rt])
            K.op("dve", lambda e, rt=rt: e.tensor_tensor(out=rt[:, :], in0=rt[:, :], in1=lnb[:, :], op=ALU.add), reads=[lnb], writes=[rt])
            if out_fn is not None:
                K.dma("sp", out_fn(t), rt[:, :], reads=[rt])
            if out_dram is not None:
                K.dma("sp", out_dram[t * 128:(t + 1) * 128, :], rt[:, :], reads=[rt], writes=[h_scr_b])
            if dstT_view is not None:
                transpose_tile_f32(rt, lambda kc, rt=rt: rt[:, kc * 128:(kc + 1) * 128], dstT_buf, dstT_view, t * 128)

    def out_proj_residual(lhsT_buf, lhsT_view, nkc, w_dram, res_fn, res_bufs, xr, ro):
        items = [(cblk, t) for cblk in range(8) for t in range(NT)]
        nb = len(xr)

        def issue_load(i):
            cblk, t = items[i]
            x_ = xr[i % nb]
            K.dma("sp", x_[:, :], res_fn(t)[:, cblk * 256:(cblk + 1) * 256], reads=res_bufs, writes=[x_])

        for i in range(min(nb - 1, len(items))):
            issue_load(i)
        sl = w = None
        for i, (cblk, t) in enumerate(items):
            if t == 0:
                sl, w = wload([w_dram[:, cblk * 256:(cblk + 1) * 256]])
            if i + nb - 1 < len(items):
                issue_load(i + nb - 1)
            ps = PS()
            mm_group(ps[:, 0:256], ps, [(lhsT_view[:, kc, t * 128:(t + 1) * 128], w[:, kc, :]) for kc in range(nkc)], [lhsT_buf, sl])
            x_ = xr[i % nb]
            r_ = ro[i % nb]
            K.op("dve", lambda e, ps=ps, x_=x_, r_=r_: e.scalar_tensor_tensor(out=r_[:, :], in0=x_[:, :], scalar=ALPHA, in1=ps[:, 0:256], op0=ALU.mult, op1=ALU.add),
                 reads=[ps, x_], writes=[r_])
            K.dma("sp", r_scr[t * 128:(t + 1) * 128, cblk * 256:(cblk + 1) * 256], r_[:, :], reads=[r_], writes=[r_scr_b])

    if stage >= 3:
        A.release(m_mix)
        hmT = A.alloc("hmT", 8 * NTOK, BF16)
        ogT = A.alloc("ogT", 8 * NTOK, BF16)
        mT = A.alloc("mT", 16 * NTOK, BF16)
        tl = [A.alloc(f"tl{i}", 1024, BF16) for i in range(2)]
        sgm = A.alloc("sgm", 512, F32)
        sgh = A.alloc("sgh", 512, F32)
        mm1 = sgm
        mm2 = sgh
        xr = [A.alloc(f"xr{i}", 256, F32) for i in range(3)]
        ro = [A.alloc(f"ro{i}", 256, F32) for i in range(3)]
        i_tl = 0
        for (scr, scr_b, dstT, goff) in ((hm_scr, hm_scr_b, hmT, 0), (og_scr, og_scr_b, ogT, 8)):
            for t in range(NT):
                tb = tl[i_tl % 2]
                i_tl += 1
                K.dma("sp", tb[:, :], scr[t * 128:(t + 1) * 128, :], reads=[scr_b], writes=[tb])
                for g in range(2):
                    ps = PS()
                    psb = ps[:, 0:256].bitcast(BF16)
                    for j in range(4):
                        kc = g * 4 + j
                        K.op("pe", lambda e, psb=psb, j=j, kc=kc, tb=tb: e.transpose(psb[:, j * 128:(j + 1) * 128], tb[:, kc * 128:(kc + 1) * 128], identB[:, :]),
                             reads=[tb, identB], writes=[ps])
                    for j in range(4):
                        kc = g * 4 + j
                        K.op("act", lambda e, psb=psb, j=j, kc=kc, dstT=dstT, goff=goff, t=t: e.activation(
                            out=dstT[:, kc * NTOK + t * 128:kc * NTOK + (t + 1) * 128], in_=psb[:, j * 128:(j + 1) * 128], func=AF.Copy, scale=gnT[:, goff + kc:goff + kc + 1]),
                            reads=[ps, gnT], writes=[dstT])
        viewX = xTv(NTOK)
        hmTv = hmT[:, :].rearrange("p (k t) -> p k t", k=8)
        ogTv = ogT[:, :].rearrange("p (k t) -> p k t", k=8)
        mTv = mT[:, :].rearrange("p (k t) -> p k t", k=16)
        for pr in range(8):
            slgm, wgm = wload([w_in[:, 8200 + pr * 256:8200 + (pr + 1) * 256]])
            slgh, wgh = wload([w_in[:, 10248 + pr * 256:10248 + (pr + 1) * 256]])
            slbm, wbm = wload([I["w_bm"][:, pr * 256:(pr + 1) * 256]])
            slbh, wbh = wload([I["w_bh"][:, pr * 256:(pr + 1) * 256]])
            for c2 in range(2):
                cb = pr * 2 + c2
                cs_ = slice(c2 * 128, (c2 + 1) * 128)
                for (t0, n) in ((0, 512), (512, 512), (1024, 128)):
                    pgm = PS()
                    mm_group(pgm[:, 0:n], pgm, [(wgm[:, kc, cs_], viewX[:, kc, t0:t0 + n]) for kc in range(16)], [slgm, xT])
                    K.op("act", lambda e, pgm=pgm, n=n, cb=cb: e.activation(out=sgm[:, 0:n], in_=pgm[:, 0:n], func=AF.Sigmoid, bias=bgm[:, cb:cb + 1]), reads=[pgm, bgm], writes=[sgm])
                    pgh = PS()
                    mm_group(pgh[:, 0:n], pgh, [(wgh[:, kc, cs_], viewX[:, kc, t0:t0 + n]) for kc in range(16)], [slgh, xT])
                    K.op("act", lambda e, pgh=pgh, n=n, cb=cb: e.activation(out=sgh[:, 0:n], in_=pgh[:, 0:n], func=AF.Sigmoid, bias=bgh[:, cb:cb + 1]), reads=[pgh, bgh], writes=[sgh])
                    ppm = PS()
                    mm_group(ppm[:, 0:n], ppm, [(wbm[:, kc, cs_], hmTv[:, kc, t0:t0 + n]) for kc in range(8)], [slbm, hmT])
                    K.op("dve", lambda e, ppm=ppm, n=n: e.tensor_tensor(out=mm1[:, 0:n], in0=sgm[:, 0:n], in1=ppm[:, 0:n], op=ALU.mult), reads=[ppm, sgm], writes=[mm1])
                    pph = PS()
                    mm_group(pph[:, 0:n], pph, [(wbh[:, kc, cs_], ogTv[:, kc, t0:t0 + n]) for kc in range(8)], [slbh, ogT])
                    K.op("dve", lambda e, pph=pph, n=n: e.tensor_tensor(out=mm2[:, 0:n], in0=sgh[:, 0:n], in1=pph[:, 0:n], op=ALU.mult), reads=[pph, sgh], writes=[mm2])
                    K.op("dve", lambda e, n=n, cb=cb, t0=t0: e.tensor_tensor(out=mTv[:, cb, t0:t0 + n], in0=mm1[:, 0:n], in1=mm2[:, 0:n], op=ALU.add), reads=[mm1, mm2], writes=[mT])
        out_proj_residual(mT, mTv, 16, I["w_out"], xrows, [], xr, ro)
        K.barrier()
        m_ln = A.mark()
        A.release(m_mix)
        tilebufs = [A.alloc(f"tilebuf{i}", D, F32) for i in range(2)]
        lng = A.alloc("lng", D, F32)
        lnb = A.alloc("lnb", D, F32)
        lnst = A.alloc("lnst", 32, F32)
        dbg = None
        if stage == 3:
            dbg = lambda t: (O["y_p"][t * 128:(t + 1) * 128, :] if t < 8 else O["y_s"])
        layer_norm_tiles(I["ln1_g"], I["ln1_b"], xT, xTv(NTOK), tilebufs, lng, lnb, lnst, out_dram=h_scr, out_fn=dbg)
        K.barrier()
        m_post_ln = A.mark()

    if stage >= 4:
        XS = 512.0 ** -0.5
        A.release(m_mix)
        oT = A.alloc("oT", 16 * NTOK, BF16)
        qTh = A.alloc("qTh", 4 * NTOK, BF16)
        memT = A.alloc("memT", 16 * 256, BF16)
        membf = A.alloc("membf", D, BF16)
        KTh = A.alloc("KTh", 4 * 256, BF16)
        Vh = A.alloc("Vh", 2 * 512, BF16)
        kvout = [A.alloc(f"kvout{i}", 256, F32) for i in range(2)]
        Kb = [A.alloc(f"Kb{i}", 2 * 512, BF16) for i in range(2)]
        KTb = [A.alloc(f"KTb{i}", 4 * 256, BF16) for i in range(2)]
        Vb = [A.alloc(f"Vb{i}", 2 * 512, BF16) for i in range(2)]
        scx = A.alloc("scx", 256, F32)
        pn = A.alloc("pn", 256, BF16)
        pT = A.alloc("pT", 256, BF16)
        sTs = A.alloc("sTs", 256, F32)
        xst = A.alloc("xst", 8, F32)
        xr = [A.alloc(f"xr{i}", 256, F32) for i in range(2)]
        ro = [A.alloc(f"ro{i}", 256, F32) for i in range(2)]
        h1Tv = xTv(NTOK)
        oTv = oT[:, :].rearrange("p (k t) -> p k t", k=16)
        qTv = qTh[:, :].rearrange("p (k t) -> p k t", k=4)
        memTv = memT[:, :].rearrange("p (k m) -> p k m", k=16)
        KThv = KTh[:, :].rearrange("p (k m) -> p k m", k=4)
        kvi = 0
        for mt in range(2):
            for hf in range(2):
                K.dma("pool", membf[:, hf * 1024:(hf + 1) * 1024], I["mem"][mt * 128:(mt + 1) * 128, hf * 1024:(hf + 1) * 1024], writes=[membf])
            if cut <= 10:
                continue
            for g in range(2):
                ps = PS()
                psb = ps[:, :].bitcast(BF16)
                for j in range(8):
                    kc = g * 8 + j
                    K.op("pe", lambda e, psb=psb, j=j, kc=kc: e.transpose(psb[:, j * 128:(j + 1) * 128], membf[:, kc * 128:(kc + 1) * 128], identB[:, :]),
                         reads=[membf, identB], writes=[ps])
                K.op("act", lambda e, psb=psb, g=g, mt=mt: e.activation(out=memTv[:, g * 8:(g + 1) * 8, mt * 128:(mt + 1) * 128], in_=psb.rearrange("p (j m) -> p j m", j=8), func=AF.Copy),
                     reads=[ps], writes=[memT])

        def softmax_to_pT(ps_s):
            K.op("dve", lambda e: e.tensor_reduce(out=xst[:, 0:1], in_=ps_s[:, 0:256], axis=AX.X, op=ALU.max), reads=[ps_s], writes=[xst])
            K.op("dve", lambda e: e.tensor_scalar(out=xst[:, 1:2], in0=xst[:, 0:1], scalar1=-XS, scalar2=None, op0=ALU.mult), reads=[xst], writes=[xst])
            K.op("act", lambda e: e.activation(out=scx[:, :], in_=ps_s[:, 0:256], func=AF.Exp, scale=XS, bias=xst[:, 1:2], accum_out=xst[:, 2:3]), reads=[ps_s, xst], writes=[scx, xst])
            K.op("dve", lambda e: e.reciprocal(out=xst[:, 3:4], in_=xst[:, 2:3]), reads=[xst], writes=[xst])
            K.op("dve", lambda e: e.tensor_scalar(out=pn[:, :], in0=scx[:, :], scalar1=xst[:, 3:4], scalar2=None, op0=ALU.mult), reads=[scx, xst], writes=[pn])
            pt = PS()
            ptb = pt[:, 0:128].bitcast(BF16)
            for mt in range(2):
                K.op("pe", lambda e, ptb=ptb, mt=mt: e.transpose(ptb[:, mt * 128:(mt + 1) * 128], pn[:, mt * 128:(mt + 1) * 128], identB[:, :]), reads=[pn, identB], writes=[pt])
            K.op("act", lambda e, ptb=ptb: e.activation(out=pT[:, :], in_=ptb, func=AF.Copy), reads=[pt], writes=[pT])

        for h in range(4):
            if cut <= 11:
                continue
            for half in range(2):
                slq, wq = wload([I["xa_wq"][:, h * 512 + half * 256:h * 512 + (half + 1) * 256]])
                for c2 in range(2):
                    c = half * 2 + c2
                    for (t0, n) in ((0, 512), (512, 512), (1024, 128)):
                        ps = PS()
                        mm_group(ps[:, 0:n], ps, [(wq[:, kc, c2 * 128:(c2 + 1) * 128], h1Tv[:, kc, t0:t0 + n]) for kc in range(16)], [slq, xT])
                        K.op("act", lambda e, ps=ps, c=c, t0=t0, n=n: e.activation(out=qTv[:, c, t0:t0 + n], in_=ps[:, 0:n], func=AF.Copy), reads=[ps], writes=[qTh])
            for half in range(2):
                slk, wk = wload([I["xa_wk"][:, h * 512 + half * 256:h * 512 + (half + 1) * 256]])
                slv, wv_ = wload([I["xa_wv"][:, h * 512 + half * 256:h * 512 + (half + 1) * 256]])
                c0 = h * 512 + half * 256
                for mt in range(2):
                    ms = slice(mt * 128, (mt + 1) * 128)
                    ps = PS()
                    mm_group(ps[:, 0:256], ps, [(memTv[:, kc, ms], wk[:, kc, :]) for kc in range(16)], [memT, slk])
                    ko = kvout[kvi % 2]
                    kvi += 1
                    K.op("act", lambda e, ps=ps, ko=ko: e.activation(out=ko[:, :], in_=ps[:, 0:256], func=AF.Copy), reads=[ps], writes=[ko])
                    K.dma("sp", O["mk"][ms, c0:c0 + 256], ko[:, :], reads=[ko])
                    ps = PS()
                    mm_group(ps[:, 0:256], ps, [(memTv[:, kc, ms], wv_[:, kc, :]) for kc in range(16)], [memT, slv])
                    ko = kvout[kvi % 2]
                    kvi += 1
                    K.op("act", lambda e, ps=ps, ko=ko: e.activation(out=ko[:, :], in_=ps[:, 0:256], func=AF.Copy), reads=[ps], writes=[ko])
                    K.dma("sp", O["mv"][ms, c0:c0 + 256], ko[:, :], reads=[ko])
                    K.op("dve", lambda e, ps=ps, mt=mt, half=half: e.tensor_copy(out=Vh[:, mt * 512 + half * 256:mt * 512 + (half + 1) * 256], in_=ps[:, 0:256]), reads=[ps], writes=[Vh])
                for c2 in range(2):
                    ps = PS()
                    mm_group(ps[:, 0:256], ps, [(wk[:, kc, c2 * 128:(c2 + 1) * 128], memTv[:, kc, :]) for kc in range(16)], [memT, slk])
                    K.op("act", lambda e, ps=ps, dc=half * 2 + c2: e.activation(out=KThv[:, dc, :], in_=ps[:, 0:256], func=AF.Copy), reads=[ps], writes=[KTh])
            for t in range(8):
                if cut <= 12:
                    continue
                ts_ = slice(t * 128, (t + 1) * 128)
                ps_s = PS()
                mm_group(ps_s[:, 0:256], ps_s, [(qTv[:, dc, ts_], KThv[:, dc, :]) for dc in range(4)], [qTh, KTh])
                softmax_to_pT(ps_s)
                ps_o = PS()
                for dc in range(4):
                    for mt in range(2):
                        K.op("pe", lambda e, ps_o=ps_o, dc=dc, mt=mt: e.matmul(ps_o[:, dc * 128:(dc + 1) * 128], Vh[:, mt * 512 + dc * 128:mt * 512 + (dc + 1) * 128],
                                                                            pT[:, mt * 128:(mt + 1) * 128], start=(mt == 0), stop=(mt == 1)), reads=[Vh, pT], writes=[ps_o])
                K.op("act", lambda e, ps_o=ps_o, ts_=ts_, h=h: e.activation(out=oTv[:, h * 4:(h + 1) * 4, ts_], in_=ps_o[:, :].rearrange("p (c t) -> p c t", c=4), func=AF.Copy),
                     reads=[ps_o], writes=[oT])
            if cut <= 13:
                continue
            ps_sT = psum[6]
            for b in range(16):
                kb_ = Kb[b % 2]
                ktb = KTb[b % 2]
                K.dma("pool", kb_[:, :].rearrange("p (mt c) -> p mt c", mt=2), I["ck"][b, :, h * 512:(h + 1) * 512].rearrange("(mt p) c -> p mt c", p=128), writes=[kb_])
                ps = PS()
                psb = ps[:, :].bitcast(BF16)
                for dc in range(4):
                    for mt in range(2):
                        K.op("pe", lambda e, psb=psb, dc=dc, mt=mt, kb_=kb_: e.transpose(psb[:, dc * 256 + mt * 128:dc * 256 + (mt + 1) * 128],
                                                                                     kb_[:, mt * 512 + dc * 128:mt * 512 + (dc + 1) * 128], identB[:, :]),
                             reads=[kb_, identB], writes=[ps])
                K.op("act", lambda e, psb=psb, ktb=ktb: e.activation(out=ktb[:, :], in_=psb, func=AF.Copy), reads=[ps], writes=[ktb])
                for mt in range(2):
                    for dc in range(4):
                        K.op("pe", lambda e, mt=mt, dc=dc, b=b, ktb=ktb: e.matmul(ps_sT[:, mt * 128 + b * 8:mt * 128 + (b + 1) * 8], ktb[:, dc * 256 + mt * 128:dc * 256 + (mt + 1) * 128],
                                                                               qTv[:, dc, TP + b * 8:TP + (b + 1) * 8], start=(dc == 0), stop=(dc == 3)),
                             reads=[ktb, qTh], writes=[ps_sT])
            K.op("act", lambda e: e.activation(out=sTs[:, :], in_=ps_sT[:, 0:256], func=AF.Copy), reads=[ps_sT], writes=[sTs])
            ps_s = PS()
            for mt in range(2):
                K.op("pe", lambda e, ps_s=ps_s, mt=mt: e.transpose(ps_s[:, mt * 128:(mt + 1) * 128], sTs[:, mt * 128:(mt + 1) * 128], ident), reads=[sTs, cst], writes=[ps_s])
            softmax_to_pT(ps_s)
            ps_oS = psum[7]
            for b in range(16):
                vb_ = Vb[b % 2]
                K.dma("pool", vb_[:, :].rearrange("p (mt c) -> p mt c", mt=2), I["cv"][b, :, h * 512:(h + 1) * 512].rearrange("(mt p) c -> p mt c", p=128), writes=[vb_])
                for dc in range(4):
                    for mt in range(2):
                        K.op("pe", lambda e, dc=dc, mt=mt, b=b, vb_=vb_: e.matmul(ps_oS[:, dc * 128 + b * 8:dc * 128 + (b + 1) * 8], vb_[:, mt * 512 + dc * 128:mt * 512 + (dc + 1) * 128],
                                                                               pT[:, mt * 128 + b * 8:mt * 128 + (b + 1) * 8], start=(mt == 0), stop=(mt == 1)),
                             reads=[vb_, pT], writes=[ps_oS])
            K.op("act", lambda e, h=h: e.activation(out=oTv[:, h * 4:(h + 1) * 4, TP:NTOK], in_=ps_oS[:, :].rearrange("p (c t) -> p c t", c=4), func=AF.Copy), reads=[ps_oS], writes=[oT])
        out_proj_residual(oT, oTv, 16, I["xa_wo"], lambda t: h_scr[t * 128:(t + 1) * 128, :], [h_scr_b], xr, ro)
        K.barrier()
        A.release(m_mix)
        tilebufs = [A.alloc(f"tilebufb{i}", D, F32) for i in range(2)]
        lng = A.alloc("lngb", D, F32)
        lnb = A.alloc("lnbb", D, F32)
        lnst = A.alloc("lnstb", 32, F32)
        dbg = None
        if stage == 4:
            dbg = lambda t: (O["y_p"][t * 128:(t + 1) * 128, :] if t < 8 else O["y_s"])
        layer_norm_tiles(I["ln2_g"], I["ln2_b"], xT, xTv(NTOK), tilebufs, lng, lnb, lnst, out_dram=h_scr, out_fn=dbg)
        K.barrier()

    if stage >= 5:
        A.release(m_mix)
        h2Tv = xTv(NTOK)
        acc_t = [A.alloc(f"acc{t}", D, F32) for t in range(NT)]
        m_after_acc = A.mark()
        hid = A.alloc("hid", 4 * NTOK, BF16)
        hidv = hid[:, :].rearrange("p (f t) -> p f t", f=4)
        cbc = A.alloc("cbc", NTOK, F32)
        cTf = cbc
        cThi = A.alloc("cThi", NTOK, BF16)
        cTlo = A.alloc("cTlo", NTOK, BF16)
        sgt = A.alloc("sgt", 512, F32)
        ut = A.alloc("ut", 512, F32)
        comb = A.alloc("comb", NT * 32, F32)
        rbrow = A.alloc("rbrow", 36, F32)
        rs_ = A.alloc("rs_", 128, F32)
        sel = [A.alloc(f"sel{i}", 128, BF16) for i in range(2)]
        for t in range(NT):
            K.dma("sp", acc_t[t][:, :], h_scr[t * 128:(t + 1) * 128, :], reads=[h_scr_b], writes=[acc_t[t]])
            K.op("act", lambda e, t=t: e.activation(out=acc_t[t][:, :], in_=acc_t[t][:, :], func=AF.Copy, scale=ALPHA), writes=[acc_t[t]])
        K.dma("sp", rbrow[:, :], I["rb"][0:1, :].partition_broadcast(128), writes=[rbrow])
        slr, wr = wload([I["rw"]])
        lg, oh, lge, t8, mk1, mk2 = rs_[:, 0:36], rs_[:, 36:40], rs_[:, 40:48], rs_[:, 48:80], rs_[:, 80:88], rs_[:, 88:96]
        sc = lambda i: rs_[:, 96 + i:97 + i]
        for t in range(NT):
            ps = PS()
            mm_group(ps[:, 0:36], ps, [(h2Tv[:, kc, t * 128:(t + 1) * 128], wr[:, kc, :]) for kc in range(16)], [xT, slr])
            K.op("dve", lambda e, ps=ps: e.tensor_tensor(out=lg, in0=ps[:, 0:36], in1=rbrow[:, :], op=ALU.add), reads=[ps, rbrow], writes=[rs_])
            K.op("dve", lambda e: e.tensor_reduce(out=sc(0), in_=lg[:, 0:4], axis=AX.X, op=ALU.max), reads=[rs_], writes=[rs_])
            K.op("dve", lambda e: e.tensor_scalar(out=oh, in0=lg[:, 0:4], scalar1=sc(0), scalar2=None, op0=ALU.is_equal), reads=[rs_], writes=[rs_])
            K.op("dve", lambda e: e.tensor_scalar(out=sc(1), in0=sc(0), scalar1=-1.0, scalar2=None, op0=ALU.mult), reads=[rs_], writes=[rs_])
            K.op("act", lambda e: e.activation(out=t8[:, 0:4], in_=lg[:, 0:4], func=AF.Exp, bias=sc(1), accum_out=sc(2)), reads=[rs_], writes=[rs_])
            K.op("dve", lambda e: e.reciprocal(out=sc(3), in_=sc(2)), reads=[rs_], writes=[rs_])
            K.op("dve", lambda e: e.tensor_tensor(out=t8.rearrange("p (g e) -> p g e", g=4), in0=lg[:, 4:36].rearrange("p (g e) -> p g e", g=4),
                                                  in1=V(rs_.t, 36, (1, 4), (0, 8)), op=ALU.mult), reads=[rs_], writes=[rs_])
            K.op("dve", lambda e: e.tensor_reduce(out=lge, in_=V(rs_.t, 48, (1, 8), (8, 4)), axis=AX.X, op=ALU.add), reads=[rs_], writes=[rs_])
            K.op("dve", lambda e: e.tensor_reduce(out=sc(4), in_=lge, axis=AX.X, op=ALU.max), reads=[rs_], writes=[rs_])
            K.op("dve", lambda e: e.tensor_scalar(out=mk1, in0=lge, scalar1=sc(4), scalar2=None, op0=ALU.is_equal), reads=[rs_], writes=[rs_])
            K.op("dve", lambda e: e.scalar_tensor_tensor(out=t8[:, 0:8], in0=mk1, scalar=NEGBIG, in1=lge, op0=ALU.mult, op1=ALU.add), reads=[rs_], writes=[rs_])
            K.op("dve", lambda e: e.tensor_reduce(out=sc(5), in_=t8[:, 0:8], axis=AX.X, op=ALU.max), reads=[rs_], writes=[rs_])
            K.op("dve", lambda e: e.tensor_scalar(out=mk2, in0=t8[:, 0:8], scalar1=sc(5), scalar2=None, op0=ALU.is_equal), reads=[rs_], writes=[rs_])
            K.op("dve", lambda e: e.tensor_tensor(out=sc(6), in0=sc(5), in1=sc(4), op=ALU.subtract), reads=[rs_], writes=[rs_])
            K.op("act", lambda e: e.activation(out=sc(7), in_=sc(6), func=AF.Exp), reads=[rs_], writes=[rs_])
            K.op("dve", lambda e: e.tensor_scalar(out=sc(8), in0=sc(7), scalar1=1.0, scalar2=None, op0=ALU.add), reads=[rs_], writes=[rs_])
            K.op("dve", lambda e: e.reciprocal(out=sc(9), in_=sc(8)), reads=[rs_], writes=[rs_])
            K.op("dve", lambda e: e.tensor_tensor(out=sc(10), in0=sc(9), in1=sc(3), op=ALU.mult), reads=[rs_], writes=[rs_])
            K.op("dve", lambda e: e.tensor_tensor(out=sc(11), in0=sc(3), in1=sc(10), op=ALU.subtract), reads=[rs_], writes=[rs_])
            K.op("dve", lambda e: e.tensor_scalar(out=mk1, in0=mk1, scalar1=sc(10), scalar2=None, op0=ALU.mult), reads=[rs_], writes=[rs_])
            K.op("dve", lambda e: e.scalar_tensor_tensor(out=mk1, in0=mk2, scalar=sc(11), in1=mk1, op0=ALU.mult, op1=ALU.add), reads=[rs_], writes=[rs_])
            K.op("dve", lambda e, t=t: e.tensor_tensor(out=comb[:, t * 32:(t + 1) * 32].rearrange("p (g e) -> p g e", g=4), in0=V(rs_.t, 36, (1, 4), (0, 8)),
                                                       in1=V(rs_.t, 80, (0, 4), (1, 8)), op=ALU.mult), reads=[rs_], writes=[comb])
        for g in range(3):
            ps = PS()
            nt_ = 4 if g < 2 else 1
            for j in range(nt_):
                t = g * 4 + j
                K.op("pe", lambda e, ps=ps, j=j, t=t: e.transpose(ps[0:32, j * 128:(j + 1) * 128], comb[:, t * 32:(t + 1) * 32], ident), reads=[comb, cst], writes=[ps])
            K.op("act", lambda e, ps=ps, g=g, nt_=nt_: e.activation(out=cTf[0:32, g * 512:g * 512 + nt_ * 128], in_=ps[0:32, 0:nt_ * 128], func=AF.Copy), reads=[ps], writes=[cTf])
        K.op("dve", lambda e: e.tensor_copy(out=cThi[0:32, :], in_=cTf[0:32, :]), reads=[cTf], writes=[cThi])
        K.op("dve", lambda e: e.tensor_tensor(out=cTlo[0:32, :], in0=cTf[0:32, :], in1=cThi[0:32, :], op=ALU.subtract), reads=[cTf, cThi], writes=[cTlo])
        TB3 = ((0, 512), (512, 512), (1024, 128))
        for xe in range(32):
            se = sel[xe % 2]
            K.op("pool", lambda e, se=se, xe=xe: e.tensor_scalar(out=se[0:32, :], in0=cst[0:32, C_ONES:C_ONES + 128], scalar1=cst[0:32, C_ID + xe:C_ID + xe + 1], scalar2=None, op0=ALU.mult),
                 reads=[cst], writes=[se])
            for (t0, n) in TB3:
                ps = PS()
                mm_group(ps[:, 0:n], ps, [(se[0:32, :], cThi[0:32, t0:t0 + n]), (se[0:32, :], cTlo[0:32, t0:t0 + n])], [se, cThi, cTlo])
                K.op("act", lambda e, ps=ps, t0=t0, n=n: e.activation(out=cbc[:, t0:t0 + n], in_=ps[:, 0:n], func=AF.Copy), reads=[ps], writes=[cbc])
            for fp in range(2):
                slg, wg = wload([I["e_wg"][xe][:, fp * 256:(fp + 1) * 256]])
                slu, wu = wload([I["e_wu"][xe][:, fp * 256:(fp + 1) * 256]])
                for c2 in range(2):
                    fb = fp * 2 + c2
                    cs_ = slice(c2 * 128, (c2 + 1) * 128)
                    for (t0, n) in TB3:
                        pg = PS()
                        mm_group(pg[:, 0:n], pg, [(wg[:, kc, cs_], h2Tv[:, kc, t0:t0 + n]) for kc in range(16)], [slg, xT])
                        K.op("act", lambda e, pg=pg, n=n: e.activation(out=sgt[:, 0:n], in_=pg[:, 0:n], func=AF.Silu), reads=[pg], writes=[sgt])
                        pu = PS()
                        mm_group(pu[:, 0:n], pu, [(wu[:, kc, cs_], h2Tv[:, kc, t0:t0 + n]) for kc in range(16)], [slu, xT])
                        K.op("dve", lambda e, pu=pu, n=n, t0=t0: e.tensor_tensor(out=ut[:, 0:n], in0=pu[:, 0:n], in1=cbc[:, t0:t0 + n], op=ALU.mult), reads=[pu, cbc], writes=[ut])
                        K.op("dve", lambda e, n=n, t0=t0, fb=fb: e.tensor_tensor(out=hidv[:, fb, t0:t0 + n], in0=sgt[:, 0:n], in1=ut[:, 0:n], op=ALU.mult), reads=[sgt, ut], writes=[hid])
            for dh in range(2):
                sld, wd = wload([I["e_wd"][xe][:, dh * 1024:(dh + 1) * 1024]])
                for t in range(NT):
                    for db in range(2):
                        ps = PS()
                        mm_group(ps[:, :], ps, [(hidv[:, fc, t * 128:(t + 1) * 128], wd[:, fc, db * 512:(db + 1) * 512]) for fc in range(4)], [hid, sld])
                        c0 = dh * 1024 + db * 512
                        K.op("dve", lambda e, ps=ps, t=t, c0=c0: e.tensor_tensor(out=acc_t[t][:, c0:c0 + 512], in0=acc_t[t][:, c0:c0 + 512], in1=ps[:, :], op=ALU.add),
                             reads=[ps], writes=[acc_t[t]])
        K.barrier()
        A.release(m_after_acc)
        lng = A.alloc("lngc", D, F32)
        lnb = A.alloc("lnbc", D, F32)
        lnst = A.alloc("lnstc", 32, F32)
        layer_norm_tiles(I["ln3_g"], I["ln3_b"], None, None, acc_t, lng, lnb, lnst, out_dram=None,
                         out_fn=lambda t: (O["y_p"][t * 128:(t + 1) * 128, :] if t < 8 else O["y_s"]), inplace=True)

    def emit_prompt_state_outputs():
        for h in range(4):
            K.dma("sp", O["Cp"][h].rearrange("(dc p) v -> p dc v", p=128), V(Cst[h].t, 0, (257, 2), (1, 256)), reads=[Cst[h]])
            K.dma("sp", O["np_"][h].rearrange("(dc p) -> p dc", p=128), V(Cst[h].t, 256, (257, 2)), reads=[Cst[h]])
        K.dma("sp", O["mp"], mstate[0:1, :], reads=[mstate])
        for hh in range(8):
            K.dma("sp", O["Sp"][hh], Sst[hh][:, :], reads=[Sst[hh]])

    if stage == 1:
        emit_prompt_state_outputs()

    nc._K = K
    nc._A = A
    nc._I = I
    nc._O = O
    with nc.allow_non_contiguous_dma(reason="small strided parameter / state layouts"):
        K.finalize()
    return nc


def prep_core_inputs(inp, c, shared):
    j, hh = c // 2, c % 2
    sl = slice(16 * c, 16 * c + 16)
    m = dict(shared)
    m["xp"] = np.ascontiguousarray(inp["x_prompt"][j, hh * TP:(hh + 1) * TP])
    m["xpre"] = np.ascontiguousarray(inp["x_prompt"][j, 0:TP])
    m["xs"] = np.ascontiguousarray(inp["x_sample"][sl].reshape(128, D))
    m["mem"] = np.ascontiguousarray(inp["mem_prompt"][j])
    m["ck"] = np.ascontiguousarray(inp["cache_mem_k"][0, sl].reshape(16, 256, D))
    m["cv"] = np.ascontiguousarray(inp["cache_mem_v"][0, sl].reshape(16, 256, D))
    m["C0"] = np.ascontiguousarray(inp["state_mlstm_C"][0, sl])
    m["n0"] = np.ascontiguousarray(inp["state_mlstm_n"][0, sl])
    m["m0"] = np.ascontiguousarray(inp["state_mlstm_m"][0, sl])
    m["conv0"] = np.ascontiguousarray(inp["state_mlstm_conv"][0, sl].reshape(48, D))
    m["S0"] = np.ascontiguousarray(inp["state_hgrn_S"][0, sl])
    m["flag"] = np.full((128, 1), float(hh), np.float32)
    return m


def prep_shared(inp):
    s = {"consts": host_consts()}
    for k in ["w_in", "conv_w", "lb_logits", "w_bm", "w_bh", "w_out", "xa_wq", "xa_wk", "xa_wv", "xa_wo", "e_wg", "e_wu", "e_wd"]:
        a = inp[k]
        s[k] = np.ascontiguousarray(a[0] if k != "lb_logits" else a)
    for k in ["b_in", "mlstm_gn", "hgrn_gn", "ln1_g", "ln1_b", "ln2_g", "ln2_b", "ln3_g", "ln3_b"]:
        s[k] = np.ascontiguousarray(inp[k].reshape(1, -1))
    s["rw"] = np.ascontiguousarray(np.concatenate([inp["r1_w"][0], inp["r2_w"][0].transpose(1, 0, 2).reshape(D, 32)], axis=1))
    s["rb"] = np.ascontiguousarray(np.concatenate([inp["r1_b"][0].reshape(-1), inp["r2_b"][0].reshape(-1)]).reshape(1, 36))
    return s


def assemble(res):
    f = np.float32
    y_p = np.zeros((4, 2048, D), f)
    y_s = np.zeros((128, 8, D), f)
    mk = np.zeros((1, 4, 256, 4, 512), f)
    mv = np.zeros((1, 4, 256, 4, 512), f)
    C_p = np.zeros((1, 4, 4, 256, 256), f)
    n_p = np.zeros((1, 4, 4, 256), f)
    m_p = np.zeros((1, 4, 4), f)
    conv_p = np.zeros((1, 4, 3, D), f)
    S_p = np.zeros((1, 4, 8, 128, 128), f)
    C_s = np.zeros((1, 128, 4, 256, 256), f)
    n_s = np.zeros((1, 128, 4, 256), f)
    m_s = np.zeros((1, 128, 4), f)
    conv_s = np.zeros((1, 128, 3, D), f)
    S_s = np.zeros((1, 128, 8, 128, 128), f)
    for c, r in res.items():
        j, hh = c // 2, c % 2
        sl = slice(16 * c, 16 * c + 16)
        y_p[j, hh * TP:(hh + 1) * TP] = r["y_p"]
        y_s[sl] = r["y_s"].reshape(16, 8, D)
        if hh == 0:
            mk[0, j] = r["mk"].reshape(256, 4, 512)
            mv[0, j] = r["mv"].reshape(256, 4, 512)
        else:
            C_p[0, j] = r["Cp"]
            n_p[0, j] = r["np_"]
            m_p[0, j] = r["mp"].reshape(4)
            conv_p[0, j] = r["convp"]
            S_p[0, j] = r["Sp"]
        C_s[0, sl] = r["Cs"]
        n_s[0, sl] = r["ns"]
        m_s[0, sl] = r["ms"]
        conv_s[0, sl] = r["convs"]
        S_s[0, sl] = r["Ss"]
    return (y_p, y_s, mk, mv, C_p, n_p, m_p, conv_p, S_p, C_s, n_s, m_s, conv_s, S_s)


def kernel(**inputs):
    inp = {k: np.asarray(v) for k, v in inputs.items()}
    shared = prep_shared(inp)
    in_maps = [prep_core_inputs(inp, c, shared) for c in range(NCORES)]
    nc = build_program()
    res = run_bass_kernel_spmd(nc, in_maps, core_ids=list(range(NCORES)))
    return assemble({c: res.results[c] for c in range(NCORES)})
```

```python
import math
import numpy as np
import concourse.bass as bass
import concourse.mybir as mybir
from concourse.bass_utils import run_bass_kernel_spmd

F32 = mybir.dt.float32
BF16 = mybir.dt.bfloat16
ALU = mybir.AluOpType
AF = mybir.ActivationFunctionType
AX = mybir.AxisListType

D = 2048
NIN = 12296
NCORES = 8
TP = 1024
NTOK = 1152
NT = 9
ALPHA = 2.0 ** 0.25
EPS = 1e-5
LN16 = math.log(16.0)
NEGBIG = -1.0e30


class Buf:
    def __init__(self, name, t=None, parent=None):
        self.name = name
        self.t = t
        self.parent = parent
        self.kids = {}
        self.last_w = None
        self.readers = []

    def sub(self, key):
        if key not in self.kids:
            self.kids[key] = Buf(f"{self.name}.{key}", self.t, self)
        return self.kids[key]

    def __getitem__(self, idx):
        return self.t[idx]

    def related(self):
        out = [self]
        p = self.parent
        while p is not None:
            out.append(p)
            p = p.parent
        st = list(self.kids.values())
        while st:
            k = st.pop()
            out.append(k)
            st.extend(k.kids.values())
        return out


class Op:
    __slots__ = ("eng", "fn", "deps", "needs_inc", "count", "is_dma", "sem", "val")

    def __init__(self, eng, fn):
        self.eng = eng
        self.fn = fn
        self.deps = []
        self.needs_inc = False
        self.count = None
        self.is_dma = False
        self.sem = None
        self.val = None


class Sched:
    ENGS = ("pe", "act", "dve", "pool", "sp")

    def __init__(self, nc, n_dma_sems=16, same_engine_sync=True):
        self.nc = nc
        self.ops = {e: [] for e in self.ENGS}
        self.esem = {e: nc.alloc_semaphore(f"s_{e}") for e in self.ENGS}
        self.dsem = {q: [nc.alloc_semaphore(f"d_{q}{i}") for i in range(n_dma_sems)] for q in ("sp", "pool")}
        self.dcount = {q: 0 for q in self.dsem}
        self.dlast = {q: [None] * n_dma_sems for q in self.dsem}
        self.same_engine_sync = same_engine_sync
        self.all_dmas = []

    def _add_dep(self, op, dep):
        if dep is None or dep is op:
            return
        if not dep.is_dma and dep.eng == op.eng and not op.is_dma:
            if op.eng == "pe" or not self.same_engine_sync:
                return
        if dep in op.deps:
            return
        op.deps.append(dep)
        if not dep.is_dma:
            dep.needs_inc = True

    def _track(self, op, reads, writes):
        for b in reads:
            for r in b.related():
                self._add_dep(op, r.last_w)
            if getattr(b, "excl", False):
                for rd in b.readers:
                    if rd.eng != op.eng:
                        self._add_dep(op, rd)
        for b in writes:
            for r in b.related():
                self._add_dep(op, r.last_w)
                for rd in r.readers:
                    self._add_dep(op, rd)
        for b in reads:
            b.readers.append(op)
        for b in writes:
            b.last_w = op
            b.readers = []

    def op(self, eng, fn, reads=(), writes=()):
        o = Op(eng, fn)
        self._track(o, list(reads), list(writes))
        self.ops[eng].append(o)
        return o

    def dma(self, q, out, in_, reads=(), writes=(), **kw):
        o = Op(q, lambda e: e.dma_start(out=out, in_=in_, **kw))
        o.is_dma = True
        k = self.dcount[q]
        ns = len(self.dsem[q])
        slot = k % ns
        o.sem = self.dsem[q][slot]
        o.val = 16 * (k // ns + 1)
        prev = self.dlast[q][slot]
        if prev is not None:
            o.deps.append(prev)
        self.dlast[q][slot] = o
        self.dcount[q] = k + 1
        self._track(o, list(reads), list(writes))
        self.ops[q].append(o)
        self.all_dmas.append(o)
        return o

    def barrier(self):
        lasts = []
        for e in self.ENGS:
            for o in reversed(self.ops[e]):
                if not o.is_dma and o.fn is not None:
                    lasts.append(o)
                    break
        dm = [d for q in self.dlast for d in self.dlast[q] if d is not None]
        for e in self.ENGS:
            o = Op(e, None)
            for l in lasts:
                if l.eng != e:
                    o.deps.append(l)
                    l.needs_inc = True
            o.deps.extend(dm)
            self.ops[e].append(o)

    def finalize(self):
        fin = Op("sp", None)
        for q in self.dlast:
            for d in self.dlast[q]:
                if d is not None:
                    fin.deps.append(d)
        self.ops["sp"].append(fin)
        for e in self.ENGS:
            c = 0
            for o in self.ops[e]:
                if (not o.is_dma) and o.needs_inc:
                    c += 1
                    o.count = c
        nc = self.nc
        esem = self.esem
        with nc.Block() as block:
            regs = {"pe": block.tensor, "act": block.scalar, "dve": block.vector, "pool": block.gpsimd, "sp": block.sync}
            for e in self.ENGS:
                ops = self.ops[e]

                def body(h, ops=ops, e=e):
                    waited = {}
                    pending = None
                    for o in ops:
                        for d in o.deps:
                            if d.is_dma:
                                s, v = d.sem, d.val
                            else:
                                s, v = esem[d.eng], d.count
                            key = id(s)
                            if waited.get(key, 0) >= v:
                                continue
                            waited[key] = v
                            h.wait_ge(s, v)
                        if o.fn is None:
                            continue
                        ins = o.fn(h)
                        if o.is_dma:
                            ins.then_inc(o.sem, 16)
                        elif o.needs_inc:
                            ins.then_inc(esem[e], 1)

                regs[e](body)


def V(t, off, *dims, npart=128, p0=0):
    n = t.shape[1]
    return bass.AP(tensor=t, offset=p0 * n + off, ap=[[n, npart]] + [[s, c] for s, c in dims])


class Arena:
    def __init__(self, nc, base=16384, limit=196608):
        self.nc = nc
        self.off = base
        self.limit = limit
        self.n = 0

    def alloc(self, name, nelem, dt):
        sz = nelem * (4 if dt == F32 else 2)
        self.off = (self.off + 63) // 64 * 64
        assert self.off + sz <= self.limit, f"SBUF overflow at {name}: {self.off + sz}"
        self.n += 1
        t = self.nc.alloc_sbuf_tensor_at(f"{name}_{self.n}", [128, nelem], dt, offset=self.off)
        self.off += sz
        return Buf(name, t)

    def mark(self):
        return self.off

    def release(self, m):
        self.off = m


def host_consts():
    c = np.zeros((128, 1056 + NTOK), np.float32)
    idx = np.arange(128)
    c[:, 0:128] = np.eye(128)
    s = idx[:, None]
    t = idx[None, :]
    c[:, 128:256] = (s <= t)
    c[:, 256:384] = np.where(t <= s, 0.0, NEGBIG)
    c[:, 384:512] = (s == 127)
    same = (s // 8) == (t // 8)
    c[:, 512:640] = same & (s <= t)
    c[:, 640:768] = np.where(same & (t <= s), 0.0, NEGBIG)
    c[:, 768:896] = (s == 8 * (t // 8) + 7)
    c[:, 896:912] = (idx[:, None] // 8) == np.arange(16)[None, :]
    c[:, 912:928] = idx[:, None] == 8 * np.arange(16)[None, :]
    c[:, 928:1056] = 1.0
    rm = np.ones(NTOK, np.float32)
    rm[0:1024:128] = 0.0
    rm[1024:NTOK:8] = 0.0
    c[:, 1056:] = rm[None, :]
    return c


C_ID, C_MP, C_NEGP, C_LASTP, C_MS, C_NEGS, C_LASTS, C_SEQM, C_SEQ1, C_ONES, C_RM = 0, 128, 256, 384, 512, 640, 768, 896, 912, 928, 1056


def build_program(stage=99, same_engine_sync=True, cut=99):
    nc = bass.Bass("TRN2", target_bir_lowering=False)

    def din(name, shape):
        return nc.dram_tensor(name, list(shape), F32, kind="ExternalInput").ap()

    def dout(name, shape):
        return nc.dram_tensor(name, list(shape), F32, kind="ExternalOutput").ap()

    I = {}
    for name, shape in [
        ("xp", (TP, D)), ("xpre", (TP, D)), ("xs", (128, D)), ("mem", (256, D)),
        ("ck", (16, 256, D)), ("cv", (16, 256, D)), ("C0", (16, 4, 256, 256)), ("n0", (16, 4, 256)), ("m0", (16, 4)),
        ("conv0", (48, D)), ("S0", (16, 8, 128, 128)), ("flag", (128, 1)), ("consts", (128, 1056 + NTOK)),
        ("w_in", (D, NIN)), ("b_in", (1, NIN)), ("conv_w", (4, D)), ("mlstm_gn", (1, 1024)), ("lb_logits", (2, 1024)),
        ("hgrn_gn", (1, 1024)), ("w_bm", (1024, D)), ("w_bh", (1024, D)), ("w_out", (D, D)),
        ("ln1_g", (1, D)), ("ln1_b", (1, D)), ("xa_wq", (D, D)), ("xa_wk", (D, D)), ("xa_wv", (D, D)), ("xa_wo", (D, D)),
        ("ln2_g", (1, D)), ("ln2_b", (1, D)), ("rw", (D, 36)), ("rb", (1, 36)),
        ("e_wg", (32, D, 512)), ("e_wu", (32, D, 512)), ("e_wd", (32, 512, D)), ("ln3_g", (1, D)), ("ln3_b", (1, D)),
    ]:
        I[name] = din(name, shape)
    O = {}
    for name, shape in [
        ("y_p", (TP, D)), ("y_s", (128, D)), ("mk", (256, D)), ("mv", (256, D)),
        ("Cp", (4, 256, 256)), ("np_", (4, 256)), ("mp", (1, 4)), ("convp", (3, D)), ("Sp", (8, 128, 128)),
        ("Cs", (16, 4, 256, 256)), ("ns", (16, 4, 256)), ("ms", (16, 4)), ("convs", (16, 3, D)), ("Ss", (16, 8, 128, 128)),
    ]:
        O[name] = dout(name, shape)

    K = Sched(nc, same_engine_sync=same_engine_sync)
    A = Arena(nc)
    psum = [Buf(f"ps{i}", nc.alloc_psum_tensor(f"ps{i}", [128, 512], F32)) for i in range(8)]
    for b_ in psum:
        b_.excl = True
    pstate = {"i": 0}

    def PS():
        b = psum[pstate["i"] % 6]
        pstate["i"] += 1
        return b


    cst = A.alloc("cst", 1056 + NTOK, F32)
    K.dma("sp", cst[:, :], I["consts"], writes=[cst])
    identB = A.alloc("identB", 128, BF16)
    K.op("dve", lambda e: e.tensor_copy(out=identB[:, :], in_=cst[:, C_ID:C_ID + 128]), reads=[cst], writes=[identB])
    flag = A.alloc("flag", 1, F32)
    K.dma("sp", flag[:, :], I["flag"], writes=[flag])

    def cs(c0, n=128):
        return cst[:, c0:c0 + n]

    ident = cs(C_ID)
    ones = cs(C_ONES)

    NSLOT = 4
    wslots = [A.alloc(f"wslot{i}", 4096, BF16) for i in range(NSLOT)]
    wstate = {"i": 0}

    def wload(parts):
        sl = wslots[wstate["i"] % NSLOT]
        wstate["i"] += 1
        kc = parts[0].shape[0] // 128
        tot = sum(p.shape[1] for p in parts)
        assert kc * tot <= 4096
        view = sl[:, 0:kc * tot].rearrange("p (k c) -> p k c", k=kc)
        c0 = 0
        for p in parts:
            n = p.shape[1]
            K.dma("pool", view[:, :, c0:c0 + n], p.rearrange("(k p) c -> p k c", p=128), writes=[sl])
            c0 += n
        return sl, view

    w_in = I["w_in"]
    b_in = I["b_in"]

    def fm_bias(name, c0, nblk):
        b = A.alloc(name, nblk, F32)
        K.dma("sp", b[:, :], b_in[0, c0:c0 + nblk * 128].rearrange("(b p) -> p b", p=128), writes=[b])
        return b

    bqk = fm_bias("bqk", 0, 16)
    bqh = fm_bias("bqh", 4104, 8)
    bfh = fm_bias("bfh", 5128, 8)
    bgm = fm_bias("bgm", 8200, 16)
    bgh = fm_bias("bgh", 10248, 16)
    cw = A.alloc("cw", 64, F32)
    for j in range(4):
        K.dma("sp", V(cw.t, j, (4, 16)), I["conv_w"][j, :].rearrange("(c p) -> p c", p=128), writes=[cw])
    lbt = A.alloc("lbt", 16, F32)
    for r in range(2):
        K.dma("sp", lbt[:, r * 8:(r + 1) * 8], I["lb_logits"][r, :].rearrange("(h p) -> p h", p=128), writes=[lbt])
    lb = A.alloc("lb", 8, F32)
    oml = A.alloc("oml", 8, F32)
    K.op("dve", lambda e: e.tensor_tensor(out=lb[:, :], in0=lbt[:, 0:8], in1=lbt[:, 8:16], op=ALU.subtract), reads=[lbt], writes=[lb])
    K.op("act", lambda e: e.activation(out=lb[:, :], in_=lb[:, :], func=AF.Sigmoid), reads=[lb], writes=[lb])
    K.op("dve", lambda e: e.tensor_scalar(out=oml[:, :], in0=lb[:, :], scalar1=-1.0, scalar2=1.0, op0=ALU.mult, op1=ALU.add), reads=[lb], writes=[oml])
    bgate = A.alloc("bgate", 8, F32)
    K.dma("sp", bgate[:, :], b_in[0:1, 4096:4104].partition_broadcast(128), writes=[bgate])
    gnT = A.alloc("gnT", 16, F32)
    K.dma("sp", gnT[:, 0:8], I["mlstm_gn"][0, :].rearrange("(k p) -> p k", p=128), writes=[gnT])
    K.dma("sp", gnT[:, 8:16], I["hgrn_gn"][0, :].rearrange("(k p) -> p k", p=128), writes=[gnT])
    hm_scr = nc.dram_tensor("hm_scr", [NTOK, 1024], BF16, kind="Internal").ap()
    og_scr = nc.dram_tensor("og_scr", [NTOK, 1024], BF16, kind="Internal").ap()
    hm_scr_b = Buf("hm_scr")
    og_scr_b = Buf("og_scr")

    xT = A.alloc("xT", 16 * NTOK, BF16)
    xtile = []
    xst = {"i": 0}

    def xTv(ntok):
        return xT[:, 0:16 * ntok].rearrange("p (k t) -> p k t", k=16)

    def transpose_tile_f32(src_buf, src_ap_fn, dstT_buf, dst_view, tok0):
        for g in range(4):
            ps = PS()
            for j in range(4):
                kc = g * 4 + j
                K.op("pe", lambda e, ps=ps, j=j, kc=kc: e.transpose(ps[:, j * 128:(j + 1) * 128], src_ap_fn(kc), ident),
                     reads=[src_buf, cst], writes=[ps])
            eng = "act" if g % 2 == 0 else "dve"
            outv = dst_view[:, g * 4:(g + 1) * 4, tok0:tok0 + 128]
            inv = ps[:, :].rearrange("p (j t) -> p j t", j=4)
            if eng == "act":
                K.op("act", lambda e, outv=outv, inv=inv: e.activation(out=outv, in_=inv, func=AF.Copy), reads=[ps], writes=[dstT_buf])
            else:
                K.op("dve", lambda e, outv=outv, inv=inv: e.tensor_copy(out=outv, in_=inv), reads=[ps], writes=[dstT_buf])

    def build_xT(srcs, ntok):
        view = xTv(ntok)
        t = 0
        for src, ntile in srcs:
            for i in range(ntile):
                xb = xtile[xst["i"] % 2]
                xst["i"] += 1
                K.dma("sp", xb[:, :], src[i * 128:(i + 1) * 128, :], writes=[xb])
                transpose_tile_f32(xb, lambda kc, xb=xb: xb[:, kc * 128:(kc + 1) * 128], xT, view, t * 128)
                t += 1
        return view

    def mm_group(ps_ap, ps_buf, pairs, reads):
        n = len(pairs)
        for i, (l, r) in enumerate(pairs):
            K.op("pe", lambda e, l=l, r=r, i=i: e.matmul(ps_ap, l, r, start=(i == 0), stop=(i == n - 1)), reads=reads, writes=[ps_buf])

    m_mix = A.mark()

    Cst = [A.alloc(f"Cst{h}", 2 * 257, F32) for h in range(4)]
    Sst = [A.alloc(f"Sst{h}", 128, F32) for h in range(8)]
    mstate = A.alloc("mstate", 4, F32)
    xhalo = A.alloc("xhalo", 48, BF16)
    for h in range(4):
        K.op("pool", lambda e, h=h: e.memset(Cst[h][:, :], 0.0), writes=[Cst[h]])
    for h in range(8):
        K.op("pool", lambda e, h=h: e.memset(Sst[h][:, :], 0.0), writes=[Sst[h]])
    K.op("pool", lambda e: e.memset(mstate[:, :], 0.0), writes=[mstate])


    sm = A.alloc("gate_small", 160, F32)
    Dg = A.alloc("Dg", 512, F32)
    Dm = A.alloc("Dm", 512, F32)

    def smv(i, n=4):
        return sm[:, i:i + n]

    def gate_tile(gt_ap, Mc, NEGc, LASTc, mprev_ap, mprev_buf, out, need_P):
        gi = gt_ap[:, 0:4]
        gf = gt_ap[:, 4:8]
        gb = out["gbuf"]
        e1, nlf, g, Fv, mx, iw, mt, negm, d1, L8, d2, t1 = (smv(0), smv(4), smv(8), smv(12), smv(16), smv(20), smv(24), smv(28), smv(32), smv(36, 8), smv(44), smv(48))
        mf = smv(52, 8)
        K.op("act", lambda e: e.activation(out=e1, in_=gf, func=AF.Exp, scale=-1.0), reads=[gb], writes=[sm])
        K.op("act", lambda e: e.activation(out=nlf, in_=e1, func=AF.Ln, bias=1.0), reads=[sm], writes=[sm])
        ps = PS()
        K.op("pe", lambda e, ps=ps: e.matmul(ps[:, 0:4], cs(Mc), nlf, start=True, stop=True), reads=[sm, cst], writes=[ps])
        K.op("dve", lambda e, ps=ps: e.tensor_tensor(out=g, in0=gi, in1=ps[:, 0:4], op=ALU.add), reads=[ps, gb], writes=[sm])
        K.op("dve", lambda e, ps=ps: e.tensor_scalar(out=Fv, in0=ps[:, 0:4], scalar1=-1.0, scalar2=None, op0=ALU.mult), reads=[ps], writes=[sm])
        K.op("dve", lambda e: e.tensor_tensor(out=iw, in0=Fv, in1=mprev_ap, op=ALU.add), reads=[sm, mprev_buf], writes=[sm])
        for h in range(4):
            K.op("dve", lambda e, h=h: e.tensor_scalar(out=Dg[:, h * 128:(h + 1) * 128], in0=ident, scalar1=g[:, h:h + 1], scalar2=None, op0=ALU.mult),
                 reads=[sm, cst], writes=[Dg])
        ps2 = PS()
        K.op("pe", lambda e, ps2=ps2: e.matmul(ps2[:, :], ones, Dg[:, :], start=True, stop=True), reads=[Dg, cst], writes=[ps2])
        for h in range(4):
            K.op("dve", lambda e, h=h, ps2=ps2: e.scalar_tensor_tensor(out=Dm[:, h * 128:(h + 1) * 128], in0=ps2[:, h * 128:(h + 1) * 128], scalar=Fv[:, h:h + 1],
                                                                 in1=cs(NEGc), op0=ALU.add, op1=ALU.add), reads=[ps2, sm, cst], writes=[Dm])
        K.op("dve", lambda e: e.tensor_reduce(out=mx, in_=Dm[:, :].rearrange("p (h s) -> p h s", h=4), axis=AX.X, op=ALU.max), reads=[Dm], writes=[sm])
        K.op("dve", lambda e: e.tensor_tensor(out=mf[:, 0:4], in0=iw, in1=mx, op=ALU.max), reads=[sm], writes=[sm])
        K.op("dve", lambda e: e.tensor_copy(out=mf[:, 4:8], in_=Fv), reads=[sm], writes=[sm])
        mtv = mf[:, 0:4]
        K.op("dve", lambda e: e.tensor_scalar(out=negm, in0=mtv, scalar1=-1.0, scalar2=None, op0=ALU.mult), reads=[sm], writes=[sm])
        if need_P:
            for h in range(4):
                K.op("act", lambda e, h=h: e.activation(out=out["P"][:, h, :], in_=Dm[:, h * 128:(h + 1) * 128], func=AF.Exp, bias=negm[:, h:h + 1]),
                     reads=[Dm, sm], writes=[out["Pbuf"]])
            K.op("dve", lambda e: e.tensor_tensor(out=d1, in0=iw, in1=mtv, op=ALU.subtract), reads=[sm], writes=[sm])
            K.op("act", lambda e: e.activation(out=out["a0"], in_=d1, func=AF.Exp), reads=[sm], writes=[out["gsb"]])
            K.op("act", lambda e: e.activation(out=out["floor"], in_=negm, func=AF.Exp, bias=LN16), reads=[sm], writes=[out["gsb"]])
        ps3 = PS()
        K.op("pe", lambda e, ps3=ps3: e.matmul(ps3[:, 0:8], cs(LASTc), mf, start=True, stop=True), reads=[sm, cst], writes=[ps3])
        K.op("act", lambda e, ps3=ps3: e.activation(out=L8, in_=ps3[:, 0:8], func=AF.Copy), reads=[ps3], writes=[sm])
        K.op("dve", lambda e: e.tensor_tensor(out=d2, in0=L8[:, 4:8], in1=L8[:, 0:4], op=ALU.subtract), reads=[sm], writes=[sm])
        K.op("dve", lambda e: e.tensor_tensor(out=t1, in0=g, in1=d2, op=ALU.add), reads=[sm], writes=[sm])
        K.op("act", lambda e: e.activation(out=out["wL"], in_=t1, func=AF.Exp), reads=[sm], writes=[out["gsb"]])
        K.op("dve", lambda e: e.tensor_tensor(out=d1, in0=d2, in1=mprev_ap, op=ALU.add), reads=[sm, mprev_buf], writes=[sm])
        K.op("act", lambda e: e.activation(out=out["decay"], in_=d1, func=AF.Exp), reads=[sm], writes=[out["gsb"]])
        K.op("dve", lambda e: e.tensor_copy(out=out["mlast"], in_=L8[:, 0:4]), reads=[sm], writes=[out["mlast_buf"]])

    def gates_proj(view, ntile, gt):
        sl, wv = wload([w_in[:, 4096:4104]])
        for t in range(ntile):
            ps = PS()
            mm_group(ps[:, 0:8], ps, [(view[:, kc, t * 128:(t + 1) * 128], wv[:, kc, :]) for kc in range(16)], [xT, sl])
            K.op("dve", lambda e, ps=ps, t=t: e.tensor_tensor(out=gt[:, t * 8:(t + 1) * 8], in0=ps[:, 0:8], in1=bgate[:, :], op=ALU.add),
                 reads=[ps, bgate], writes=[gt])

    brow = [A.alloc(f"brow{i}", 256, F32) for i in range(2)]
    browst = {"i": 0}
    st6 = A.alloc("st6", 16, F32)
    hmo = [A.alloc(f"hmo{i}", 256, BF16) for i in range(2)]
    hmost = {"i": 0}
    convo = A.alloc("convo", 48, F32)
    convso = A.alloc("convso", 16 * 48, F32)
    c0T = A.alloc("c0T", 16 * 48, F32)
    cacc = A.alloc("cacc", NTOK, F32)
    m_head = A.mark()
    xtile.append(A.alloc("xtile0", D, F32))
    xtile.append(xtile[0])
    cvb = A.alloc("cvb", 3 + TP, F32)
    cvs = A.alloc("cvs", 16 * 11, F32)
    qT = A.alloc("qT", 2 * NTOK, BF16)
    kT = A.alloc("kT", 2 * NTOK, BF16)
    vaug = A.alloc("vaug", NT * 257, BF16)
    sigo = A.alloc("sigo", NT * 256, BF16)
    gt = A.alloc("gt", NT * 8, F32)
    Pm = A.alloc("Pm", NT * 512, BF16)
    gsb = A.alloc("gsb", NT * 16, F32)
    mprev_s = A.alloc("mprev_s", 4, F32)
    mprev_tok = A.alloc("mprev_tok", 4, F32)
    mlast_smp = A.alloc("mlast_smp", 4, F32)
    dbc = A.alloc("dbc", 64, F32)
    rhsd = A.alloc("rhsd", 64, F32)
    Cb = A.alloc("Cb", 2 * 257, BF16)
    Sc = A.alloc("Sc", 128, BF16)
    ScT = A.alloc("ScT", 128, BF16)
    ktok = A.alloc("ktok", 256, BF16)
    wvt = A.alloc("wvt", 257, BF16)
    tmpi = A.alloc("tmpi", 257, F32)
    num = A.alloc("num", 257, F32)
    hm = A.alloc("hm", 256, F32)
    tmpo = hm
    C0f = [A.alloc(f"C0f{i}", 2 * 257, F32) for i in range(2)]
    C0b = [A.alloc(f"C0b{i}", 2 * 257, BF16) for i in range(2)]
    Cnew = [A.alloc(f"Cnew{i}", 2 * 257, F32) for i in range(2)]
    wvz = [A.alloc(f"wvz{i}", 257, BF16) for i in range(2)]
    Qz = A.alloc("Qz", 2 * 16 * 128, BF16)
    m_end_mlstm = A.mark()

    def load_brow(c0, n):
        b = brow[browst["i"] % 2]
        browst["i"] += 1
        K.dma("sp", b[:, 0:n], b_in[0:1, c0:c0 + n].partition_broadcast(128), writes=[b])
        return b

    def gates_out(t):
        o = t * 16
        return dict(a0=gsb[:, o:o + 4], floor=gsb[:, o + 4:o + 8], wL=gsb[:, o + 8:o + 12], decay=gsb[:, o + 12:o + 16], gsb=gsb.sub(t),
                    P=Pm[:, t * 512:(t + 1) * 512].rearrange("p (h s) -> p h s", h=4), Pbuf=Pm.sub(t), gbuf=gt)

    def conv_silu(cvbuf, T, cb, dst_ap, dst_buf, seg=None):
        if seg is None:
            src = lambda j: cvbuf[:, j:j + T]
            acc = cacc[:, 0:T]
        else:
            nb, L = seg
            src = lambda j: V(cvbuf.t, j, (3 + L, nb), (1, L))
            acc = cacc[:, 0:nb * L].rearrange("p (b l) -> p b l", b=nb)
        K.op("dve", lambda e: e.tensor_scalar(out=acc, in0=src(0), scalar1=cw[:, cb * 4:cb * 4 + 1], scalar2=None, op0=ALU.mult), reads=[cvbuf, cw], writes=[cacc])
        for j in range(1, 4):
            K.op("dve", lambda e, j=j: e.scalar_tensor_tensor(out=acc, in0=src(j), scalar=cw[:, cb * 4 + j:cb * 4 + j + 1], in1=acc, op0=ALU.mult, op1=ALU.add),
                 reads=[cvbuf, cw, cacc], writes=[cacc])
        K.op("act", lambda e: e.activation(out=dst_ap, in_=acc, func=AF.Silu), reads=[cacc], writes=[dst_buf])

    def make_ktok(kT_ap_fn):
        ps = PS()
        psb = ps[:, 0:128].bitcast(BF16)
        for dc in range(2):
            K.op("pe", lambda e, dc=dc, psb=psb: e.transpose(psb[:, dc * 128:(dc + 1) * 128], kT_ap_fn(dc), identB[:, :]), reads=[kT, identB], writes=[ps])
        K.op("act", lambda e, psb=psb: e.activation(out=ktok[:, :], in_=psb, func=AF.Copy), reads=[ps], writes=[ktok])

    def mlstm_state_update(h, t, wL_ap, decay_ap):
        make_ktok(lambda dc: kT[:, dc * NTOK + t * 128:dc * NTOK + (t + 1) * 128])
        K.op("dve", lambda e: e.tensor_scalar(out=wvt[:, :], in0=vaug[:, t * 257:(t + 1) * 257], scalar1=wL_ap, scalar2=None, op0=ALU.mult), reads=[vaug, gsb], writes=[wvt])
        for dc in range(2):
            pc = PS()
            K.op("pe", lambda e, pc=pc, dc=dc: e.matmul(pc[:, 0:257], ktok[:, dc * 128:(dc + 1) * 128], wvt[:, :], start=True, stop=True), reads=[ktok, wvt], writes=[pc])
            cv_ = Cst[h][:, dc * 257:(dc + 1) * 257]
            K.op("dve", lambda e, pc=pc, cv_=cv_: e.scalar_tensor_tensor(out=cv_, in0=cv_, scalar=decay_ap, in1=pc[:, 0:257], op0=ALU.mult, op1=ALU.add),
                 reads=[pc, gsb], writes=[Cst[h]])

    def head_v_proj(view, ntile, h, slv, wv_):
        bv = load_brow(2048 + h * 256, 256)
        K.op("pool", lambda e: e.memset(V(vaug.t, 256, (257, NT), (1, 1)), 1.0), writes=[vaug])
        for t in range(ntile):
            ps = PS()
            mm_group(ps[:, 0:256], ps, [(view[:, kc, t * 128:(t + 1) * 128], wv_[:, kc, :]) for kc in range(16)], [slv, xT])
            K.op("dve", lambda e, ps=ps, t=t, bv=bv: e.tensor_tensor(out=vaug[:, t * 257:t * 257 + 256], in0=ps[:, 0:256], in1=bv[:, 0:256], op=ALU.add),
                 reads=[ps, bv], writes=[vaug])

    def phase_A_mlstm():
        viewA = xTv(TP)
        gates_proj(viewA, 8, gt)
        for t in range(8):
            og = gates_out(t)
            og["mlast"] = mstate[:, :]
            og["mlast_buf"] = mstate
            K.op("dve", lambda e: e.tensor_copy(out=mprev_s[:, :], in_=mstate[:, :]), reads=[mstate], writes=[mprev_s])
            gate_tile(gt[:, t * 8:(t + 1) * 8], C_MP, C_NEGP, C_LASTP, mprev_s[:, :], mprev_s, og, need_P=False)
        K.op("pool", lambda e: e.memset(cvb[:, 0:3], 0.0), writes=[cvb])
        for h in range(4):
            slk, wk = wload([w_in[:, 1024 + h * 256:1024 + (h + 1) * 256]])
            slv, wv_ = wload([w_in[:, 2048 + h * 256:2048 + (h + 1) * 256]])
            for cbk in range(2):
                cb = 8 + h * 2 + cbk
                for tb in range(2):
                    ps = PS()
                    mm_group(ps[:, :], ps, [(wk[:, kc, cbk * 128:(cbk + 1) * 128], viewA[:, kc, tb * 512:(tb + 1) * 512]) for kc in range(16)], [slk, xT])
                    K.op("act", lambda e, ps=ps, tb=tb, cb=cb: e.activation(out=cvb[:, 3 + tb * 512:3 + (tb + 1) * 512], in_=ps[:, :], func=AF.Identity, bias=bqk[:, cb:cb + 1]),
                         reads=[ps, bqk], writes=[cvb])
                conv_silu(cvb, TP, cb, kT[:, cbk * NTOK:cbk * NTOK + TP], kT)
            head_v_proj(viewA, 8, h, slv, wv_)
            for t in range(8):
                og = gates_out(t)
                mlstm_state_update(h, t, og["wL"][:, h:h + 1], og["decay"][:, h:h + 1])
            K.op("dve", lambda e, h=h: e.tensor_scalar(out=Cst[h][:, :], in0=Cst[h][:, :], scalar1=flag[:, 0:1], scalar2=None, op0=ALU.mult), reads=[flag], writes=[Cst[h]])
        K.op("dve", lambda e: e.tensor_scalar(out=mstate[:, :], in0=mstate[:, :], scalar1=flag[:, 0:1], scalar2=None, op0=ALU.mult), reads=[flag], writes=[mstate])

    def mlstm_tile_full(h, t, sample):
        og = gates_out(t)
        qs = lambda dc: qT[:, dc * NTOK + t * 128:dc * NTOK + (t + 1) * 128]
        ks = lambda dc: kT[:, dc * NTOK + t * 128:dc * NTOK + (t + 1) * 128]
        ps = PS()
        mm_group(ps[:, 0:128], ps, [(qs(dc), ks(dc)) for dc in range(2)], [qT, kT])
        K.op("dve", lambda e, ps=ps: e.tensor_tensor(out=Sc[:, :], in0=ps[:, 0:128], in1=og["P"][:, h, :], op=ALU.mult), reads=[ps, og["Pbuf"]], writes=[Sc])
        pt = PS()
        ptb = pt[:, 0:64].bitcast(BF16)
        K.op("pe", lambda e, ptb=ptb: e.transpose(ptb, Sc[:, :], identB[:, :]), reads=[Sc, identB], writes=[pt])
        K.op("act", lambda e, ptb=ptb: e.activation(out=ScT[:, :], in_=ptb, func=AF.Copy), reads=[pt], writes=[ScT])
        pa = psum[7] if sample else PS()
        K.op("pe", lambda e, pa=pa: e.matmul(pa[:, 0:257], ScT[:, :], vaug[:, t * 257:(t + 1) * 257], start=True, stop=True), reads=[ScT, vaug], writes=[pa])
        pb = psum[6] if sample else PS()
        if not sample:
            mm_group(pb[:, 0:257], pb, [(qs(dc), Cb[:, dc * 257:(dc + 1) * 257]) for dc in range(2)], [qT, Cb])
        else:
            for dc in range(2):
                K.op("pool", lambda e, dc=dc: e.tensor_copy(out=V(Qz.t, dc * 2048, (136, 16), (1, 8)),
                                                           in_=qT[:, dc * NTOK + TP:dc * NTOK + NTOK].rearrange("p (b l) -> p b l", b=16)), reads=[qT], writes=[Qz])
            make_ktok(ks)
            def load_c0(b):
                cf_ = C0f[b % 2]
                K.dma("sp", V(cf_.t, 0, (257, 2), (1, 256)), I["C0"][b, h].rearrange("(dc p) v -> p dc v", p=128), writes=[cf_])
                K.dma("sp", V(cf_.t, 256, (257, 2)), I["n0"][b, h].rearrange("(dc p) -> p dc", p=128), writes=[cf_])

            load_c0(0)
            for b in range(16):
                cf = C0f[b % 2]
                cbf = C0b[b % 2]
                cn = Cnew[b % 2]
                wz = wvz[b % 2]
                if b + 1 < 16:
                    load_c0(b + 1)
                K.op("act", lambda e, cf=cf, cbf=cbf: e.activation(out=cbf[:, :], in_=cf[:, :], func=AF.Copy), reads=[cf], writes=[cbf])
                for dc in range(2):
                    first = (b == 0 and dc == 0)
                    last = (b == 15 and dc == 1)
                    K.op("pe", lambda e, pb=pb, dc=dc, b=b, cbf=cbf, first=first, last=last: e.matmul(
                        pb[:, 0:257], Qz[:, dc * 2048 + b * 128:dc * 2048 + (b + 1) * 128], cbf[:, dc * 257:(dc + 1) * 257], start=first, stop=last),
                        reads=[Qz, cbf], writes=[pb])
                K.op("dve", lambda e, wz=wz, b=b: e.tensor_scalar(out=wz[:, :], in0=vaug[:, t * 257:(t + 1) * 257], scalar1=og["wL"][:, h:h + 1],
                                                                    scalar2=cst[:, C_SEQM + b:C_SEQM + b + 1], op0=ALU.mult, op1=ALU.mult), reads=[vaug, gsb, cst], writes=[wz])
                for dc in range(2):
                    pc = PS()
                    K.op("pe", lambda e, pc=pc, dc=dc, wz=wz: e.matmul(pc[:, 0:257], ktok[:, dc * 128:(dc + 1) * 128], wz[:, :], start=True, stop=True), reads=[ktok, wz], writes=[pc])
                    K.op("dve", lambda e, pc=pc, dc=dc, cf=cf, cn=cn, b=b: e.scalar_tensor_tensor(
                        out=cn[:, dc * 257:(dc + 1) * 257], in0=cf[:, dc * 257:(dc + 1) * 257], scalar=dbc[:, h * 16 + b:h * 16 + b + 1], in1=pc[:, 0:257],
                        op0=ALU.mult, op1=ALU.add), reads=[pc, cf, dbc], writes=[cn])
                K.dma("sp", O["Cs"][b, h].rearrange("(dc p) v -> p dc v", p=128), V(cn.t, 0, (257, 2), (1, 256)), reads=[cn])
                K.dma("sp", O["ns"][b, h].rearrange("(dc p) -> p dc", p=128), V(cn.t, 256, (257, 2)), reads=[cn])
        K.op("act", lambda e, pb=pb: e.activation(out=tmpi[:, :], in_=pb[:, 0:257], func=AF.Copy, scale=og["a0"][:, h:h + 1]), reads=[pb, gsb], writes=[tmpi])
        K.op("dve", lambda e, pa=pa: e.tensor_tensor(out=num[:, :], in0=tmpi[:, :], in1=pa[:, 0:257], op=ALU.add), reads=[pa, tmpi], writes=[num])
        K.op("dve", lambda e: e.tensor_scalar(out=st6[:, 12:13], in0=num[:, 256:257], scalar1=-1.0, scalar2=None, op0=ALU.mult), reads=[num], writes=[st6])
        K.op("dve", lambda e: e.scalar_tensor_tensor(out=st6[:, 0:1], in0=num[:, 256:257], scalar=og["floor"][:, h:h + 1], in1=st6[:, 12:13], op0=ALU.max, op1=ALU.max),
             reads=[num, gsb, st6], writes=[st6])
        K.op("dve", lambda e: e.reciprocal(out=st6[:, 1:2], in_=st6[:, 0:1]), reads=[st6], writes=[st6])
        K.op("dve", lambda e: e.scalar_tensor_tensor(out=hm[:, :], in0=num[:, 0:256], scalar=st6[:, 1:2], in1=sigo[:, t * 256:(t + 1) * 256], op0=ALU.mult, op1=ALU.mult),
             reads=[num, st6, sigo], writes=[hm])
        K.op("dve", lambda e: e.bn_stats(out=st6[:, 2:8], in_=hm[:, :]), reads=[hm], writes=[st6])
        K.op("dve", lambda e: e.bn_aggr(out=st6[:, 8:10], in_=st6[:, 2:8]), reads=[st6], writes=[st6])
        K.op("act", lambda e: e.activation(out=st6[:, 10:11], in_=st6[:, 9:10], func=AF.Sqrt, bias=EPS), reads=[st6], writes=[st6])
        K.op("dve", lambda e: e.reciprocal(out=st6[:, 11:12], in_=st6[:, 10:11]), reads=[st6], writes=[st6])
        ho = hmo[hmost["i"] % 2]
        hmost["i"] += 1
        K.op("dve", lambda e, ho=ho: e.tensor_scalar(out=ho[:, :], in0=hm[:, :], scalar1=st6[:, 8:9], scalar2=st6[:, 11:12], op0=ALU.subtract, op1=ALU.mult),
             reads=[hm, st6], writes=[ho])
        K.dma("sp", hm_scr[t * 128:(t + 1) * 128, h * 256:(h + 1) * 256], ho[:, :], reads=[ho], writes=[hm_scr_b])
        if not sample:
            mlstm_state_update(h, t, og["wL"][:, h:h + 1], og["decay"][:, h:h + 1])
            K.op("act", lambda e: e.activation(out=Cb[:, :], in_=Cst[h][:, :], func=AF.Copy), reads=[Cst[h]], writes=[Cb])

    def phase_B_mlstm():
        viewB = xTv(NTOK)
        cvin = xtile[0]
        K.dma("sp", cvin[0:48, :], I["conv0"], writes=[cvin])
        for g in range(4):
            ps = PS()
            for j in range(4):
                cb = g * 4 + j
                K.op("pe", lambda e, ps=ps, j=j, cb=cb: e.transpose(ps[:, j * 48:(j + 1) * 48], cvin[0:48, cb * 128:(cb + 1) * 128], cst[0:48, C_ID:C_ID + 48]),
                     reads=[cvin, cst], writes=[ps])
            K.op("act", lambda e, ps=ps, g=g: e.activation(out=c0T[:, g * 192:(g + 1) * 192], in_=ps[:, 0:192], func=AF.Copy), reads=[ps], writes=[c0T])
        if cut <= 1:
            return
        gates_proj(viewB, 9, gt)
        for t in range(8):
            og = gates_out(t)
            og["mlast"] = mstate[:, :]
            og["mlast_buf"] = mstate
            K.op("dve", lambda e: e.tensor_copy(out=mprev_s[:, :], in_=mstate[:, :]), reads=[mstate], writes=[mprev_s])
            gate_tile(gt[:, t * 8:(t + 1) * 8], C_MP, C_NEGP, C_LASTP, mprev_s[:, :], mprev_s, og, need_P=True)
        K.dma("sp", O["mp"], mstate[0:1, :], reads=[mstate])
        if cut <= 2:
            return
        K.dma("sp", mprev_tok[:, :], bass.AP(tensor=I["m0"].tensor, offset=0, ap=[[4, 16], [0, 8], [1, 4]]), writes=[mprev_tok])
        og = gates_out(8)
        og["mlast"] = mlast_smp[:, :]
        og["mlast_buf"] = mlast_smp
        gate_tile(gt[:, 64:72], C_MS, C_NEGS, C_LASTS, mprev_tok[:, :], mprev_tok, og, need_P=True)
        K.dma("sp", O["ms"], bass.AP(tensor=mlast_smp.t, offset=7 * 4, ap=[[32, 16], [1, 4]]), reads=[mlast_smp])
        for h in range(4):
            K.op("dve", lambda e, h=h: e.tensor_scalar(out=rhsd[:, h * 16:(h + 1) * 16], in0=cst[:, C_SEQ1:C_SEQ1 + 16], scalar1=og["decay"][:, h:h + 1], scalar2=None, op0=ALU.mult),
                 reads=[cst, gsb], writes=[rhsd])
        ps = PS()
        K.op("pe", lambda e, ps=ps: e.matmul(ps[:, 0:64], ones, rhsd[:, :], start=True, stop=True), reads=[rhsd, cst], writes=[ps])
        K.op("act", lambda e, ps=ps: e.activation(out=dbc[:, :], in_=ps[:, 0:64], func=AF.Copy), reads=[ps], writes=[dbc])
        K.op("pool", lambda e: e.memset(Qz[:, :], 0.0), writes=[Qz])
        xh = xhalo[:, :].rearrange("p (k j) -> p k j", k=16)
        if cut <= 3:
            return
        for h in range(4):
            slq, wq = wload([w_in[:, h * 256:(h + 1) * 256]])
            slk, wk = wload([w_in[:, 1024 + h * 256:1024 + (h + 1) * 256]])
            slv, wv_ = wload([w_in[:, 2048 + h * 256:2048 + (h + 1) * 256]])
            slo, wo_ = wload([w_in[:, 3072 + h * 256:3072 + (h + 1) * 256]])
            for (sl, wt, cb0, dstT) in ((slq, wq, h * 2, qT), (slk, wk, 8 + h * 2, kT)):
                for cbk in range(2):
                    cb = cb0 + cbk
                    lw = lambda kc, wt=wt, cbk=cbk: wt[:, kc, cbk * 128:(cbk + 1) * 128]
                    ps = PS()
                    mm_group(ps[:, 0:3], ps, [(lw(kc), xh[:, kc, :]) for kc in range(16)], [sl, xhalo])
                    K.op("act", lambda e, ps=ps, cb=cb: e.activation(out=cvb[:, 0:3], in_=ps[:, 0:3], func=AF.Identity, bias=bqk[:, cb:cb + 1]), reads=[ps, bqk], writes=[cvb])
                    K.op("dve", lambda e: e.tensor_scalar(out=cvb[:, 0:3], in0=cvb[:, 0:3], scalar1=flag[:, 0:1], scalar2=None, op0=ALU.mult), reads=[flag], writes=[cvb])
                    for tb in range(2):
                        ps = PS()
                        mm_group(ps[:, :], ps, [(lw(kc), viewB[:, kc, tb * 512:(tb + 1) * 512]) for kc in range(16)], [sl, xT])
                        K.op("act", lambda e, ps=ps, tb=tb, cb=cb: e.activation(out=cvb[:, 3 + tb * 512:3 + (tb + 1) * 512], in_=ps[:, :], func=AF.Identity, bias=bqk[:, cb:cb + 1]),
                             reads=[ps, bqk], writes=[cvb])
                    ps = PS()
                    mm_group(ps[:, 0:128], ps, [(lw(kc), viewB[:, kc, TP:NTOK]) for kc in range(16)], [sl, xT])
                    K.op("act", lambda e, ps=ps, cb=cb: e.activation(out=V(cvs.t, 3, (11, 16), (1, 8)), in_=ps[:, 0:128].rearrange("p (b l) -> p b l", b=16), func=AF.Identity,
                                                                     bias=bqk[:, cb:cb + 1]), reads=[ps, bqk], writes=[cvs])
                    K.op("dve", lambda e, cb=cb: e.tensor_copy(out=V(cvs.t, 0, (11, 16), (1, 3)), in_=c0T[:, cb * 48:(cb + 1) * 48].rearrange("p (b j) -> p b j", b=16)),
                         reads=[c0T], writes=[cvs])
                    conv_silu(cvb, TP, cb, dstT[:, cbk * NTOK:cbk * NTOK + TP], dstT)
                    conv_silu(cvs, 128, cb, dstT[:, cbk * NTOK + TP:cbk * NTOK + NTOK].rearrange("p (b l) -> p b l", b=16), dstT, seg=(16, 8))
                    K.op("pool", lambda e, cb=cb: e.tensor_copy(out=convo[:, cb * 3:(cb + 1) * 3], in_=cvb[:, TP:TP + 3]), reads=[cvb], writes=[convo])
                    K.op("pool", lambda e, cb=cb: e.tensor_copy(out=convso[:, cb * 48:(cb + 1) * 48].rearrange("p (b j) -> p b j", b=16), in_=V(cvs.t, 8, (11, 16), (1, 3))),
                         reads=[cvs], writes=[convso])
            head_v_proj(viewB, 9, h, slv, wv_)
            bo = load_brow(3072 + h * 256, 256)
            for t in range(9):
                ps = PS()
                mm_group(ps[:, 0:256], ps, [(viewB[:, kc, t * 128:(t + 1) * 128], wo_[:, kc, :]) for kc in range(16)], [slo, xT])
                K.op("dve", lambda e, ps=ps, bo=bo: e.tensor_tensor(out=tmpo[:, :], in0=ps[:, 0:256], in1=bo[:, 0:256], op=ALU.add), reads=[ps, bo], writes=[tmpo])
                K.op("act", lambda e, t=t: e.activation(out=sigo[:, t * 256:(t + 1) * 256], in_=tmpo[:, :], func=AF.Sigmoid), reads=[tmpo], writes=[sigo])
            K.op("act", lambda e, h=h: e.activation(out=Cb[:, :], in_=Cst[h][:, :], func=AF.Copy), reads=[Cst[h]], writes=[Cb])
            if cut <= 4:
                continue
            for t in range(8):
                mlstm_tile_full(h, t, False)
            K.dma("sp", O["Cp"][h].rearrange("(dc p) v -> p dc v", p=128), V(Cst[h].t, 0, (257, 2), (1, 256)), reads=[Cst[h]])
            K.dma("sp", O["np_"][h].rearrange("(dc p) -> p dc", p=128), V(Cst[h].t, 256, (257, 2)), reads=[Cst[h]])
            if cut <= 5:
                continue
            mlstm_tile_full(h, 8, True)
        cvo = xtile[0]
        for (srcb, w_, nrow, dst) in ((convso, 48, 48, O["convs"].rearrange("b j d -> (b j) d")), (convo, 3, 3, O["convp"])):
            for g in range(4):
                ps = PS()
                for j in range(4):
                    cb = g * 4 + j
                    K.op("pe", lambda e, ps=ps, j=j, cb=cb, srcb=srcb, w_=w_, nrow=nrow: e.transpose(ps[0:nrow, j * 128:(j + 1) * 128], srcb[:, cb * w_:(cb + 1) * w_], ident),
                         reads=[srcb, cst], writes=[ps])
                K.op("act", lambda e, ps=ps, g=g, nrow=nrow: e.activation(out=cvo[0:nrow, g * 512:(g + 1) * 512], in_=ps[0:nrow, :], func=AF.Copy), reads=[ps], writes=[cvo])
            K.dma("sp", dst, cvo[0:nrow, :], reads=[cvo])

    A.release(m_head)
    sg = A.alloc("sg", NTOK, F32)
    lf = A.alloc("lf", NTOK, F32)
    kk = A.alloc("kk", NTOK, F32)
    bb = A.alloc("bb", NTOK, F32)
    qh = A.alloc("qh", NTOK, F32)
    ex = lf
    qb = A.alloc("qb", NTOK, BF16)
    kb = A.alloc("kb", NTOK, BF16)
    qS = A.alloc("qS", NTOK, BF16)
    kLT = A.alloc("kLT", NTOK, BF16)
    vtok = A.alloc("vtok", NT * 128, BF16)
    gg = A.alloc("gg", NT * 128, BF16)
    eL = A.alloc("eL", 24, F32)
    Sb = A.alloc("Sb", 128, BF16)
    ATb = A.alloc("ATb", 128, BF16)
    kLtok = A.alloc("kLtok", 128, BF16)
    ogo = [A.alloc(f"ogo{i}", 128, BF16) for i in range(2)]
    ogst = {"i": 0}
    junk = A.alloc("junk", 128, F32)
    S0f = A.alloc("S0f", 2048, F32)
    S0b = A.alloc("S0b", 2048, BF16)
    Vz = [A.alloc(f"Vz{i}", 512, BF16) for i in range(2)]
    QzH = A.alloc("QzH", 2048, BF16)
    m_end_hgrn = A.mark()

    def hgrn_head(hh, view, ntok, full):
        ntile = ntok // 128
        tbs = [(0, 512), (512, 512)] + ([(1024, 128)] if ntok > 1024 else [])
        if full and cut > 7:
            K.dma("sp", S0f[:, :].rearrange("p (b v) -> p b v", b=16), I["S0"][:, hh].rearrange("b c v -> c b v"), writes=[S0f])
        cf0 = 5128 + hh * 128
        ci0 = 6152 + hh * 128
        if full:
            sl1, w1 = wload([w_in[:, 4104 + hh * 128:4104 + (hh + 1) * 128], w_in[:, cf0:cf0 + 128]])
            sl2, w2 = wload([w_in[:, ci0:ci0 + 128], w_in[:, 7176 + hh * 128:7176 + (hh + 1) * 128]])
            fcol, icol = 128, 0
        else:
            sl1, w1 = wload([w_in[:, cf0:cf0 + 128], w_in[:, ci0:ci0 + 128]])
            sl2, w2 = sl1, w1
            fcol, icol = 0, 128
        for (t0, n) in tbs:
            ps = PS()
            mm_group(ps[:, 0:n], ps, [(w1[:, kc, fcol:fcol + 128], view[:, kc, t0:t0 + n]) for kc in range(16)], [sl1, xT])
            K.op("act", lambda e, ps=ps, t0=t0, n=n: e.activation(out=sg[:, t0:t0 + n], in_=ps[:, 0:n], func=AF.Sigmoid, bias=bfh[:, hh:hh + 1]), reads=[ps, bfh], writes=[sg])
            if full:
                ps = PS()
                mm_group(ps[:, 0:n], ps, [(w1[:, kc, 0:128], view[:, kc, t0:t0 + n]) for kc in range(16)], [sl1, xT])
                K.op("act", lambda e, ps=ps, t0=t0, n=n: e.activation(out=qh[:, t0:t0 + n], in_=ps[:, 0:n], func=AF.Silu, bias=bqh[:, hh:hh + 1]), reads=[ps, bqh], writes=[qh])
        N = ntok
        K.op("dve", lambda e: e.tensor_scalar(out=sg[:, 0:N], in0=sg[:, 0:N], scalar1=oml[:, hh:hh + 1], scalar2=lb[:, hh:hh + 1], op0=ALU.mult, op1=ALU.add), reads=[oml, lb], writes=[sg])
        K.op("act", lambda e: e.activation(out=lf[:, 0:N], in_=sg[:, 0:N], func=AF.Ln), reads=[sg], writes=[lf])
        K.op("dve", lambda e: e.tensor_scalar(out=kk[:, 0:N], in0=sg[:, 0:N], scalar1=-1.0, scalar2=1.0, op0=ALU.mult, op1=ALU.add), reads=[sg], writes=[kk])
        bi = load_brow(ci0, 128)
        if not full:
            for t in range(ntile):
                ps = PS()
                mm_group(ps[:, 0:128], ps, [(view[:, kc, t * 128:(t + 1) * 128], w2[:, kc, icol:icol + 128]) for kc in range(16)], [sl2, xT])
                K.op("dve", lambda e, ps=ps, t=t, bi=bi: e.tensor_tensor(out=vtok[:, t * 128:(t + 1) * 128], in0=ps[:, 0:128], in1=bi[:, 0:128], op=ALU.add), reads=[ps, bi], writes=[vtok])
        else:
            bg = load_brow(7176 + hh * 128, 128)
            for t in range(ntile):
                ps = PS()
                mm_group(ps[:, 0:256], ps, [(view[:, kc, t * 128:(t + 1) * 128], w2[:, kc, :]) for kc in range(16)], [sl2, xT])
                K.op("dve", lambda e, ps=ps, t=t, bi=bi: e.tensor_tensor(out=vtok[:, t * 128:(t + 1) * 128], in0=ps[:, 0:128], in1=bi[:, 0:128], op=ALU.add), reads=[ps, bi], writes=[vtok])
                K.op("dve", lambda e, ps=ps, bg=bg: e.tensor_tensor(out=junk[:, :], in0=ps[:, 128:256], in1=bg[:, 0:128], op=ALU.add), reads=[ps, bg], writes=[junk])
                K.op("act", lambda e, t=t: e.activation(out=gg[:, t * 128:(t + 1) * 128], in_=junk[:, :], func=AF.Silu), reads=[junk], writes=[gg])
        K.op("dve", lambda e: e.tensor_tensor_scan(out=bb[:, 0:N], data0=cst[:, C_RM:C_RM + N], data1=lf[:, 0:N], initial=0.0, op0=ALU.mult, op1=ALU.add), reads=[cst, lf], writes=[bb])
        npt = 8
        K.op("dve", lambda e: e.tensor_tensor(out=ex[:, 0:1024].rearrange("p (t s) -> p t s", t=npt), in0=V(bb.t, 127, (128, npt), (0, 128)),
                                              in1=bb[:, 0:1024].rearrange("p (t s) -> p t s", t=npt), op=ALU.subtract), reads=[bb], writes=[ex])
        if N > 1024:
            K.op("dve", lambda e: e.tensor_tensor(out=ex[:, 1024:N].rearrange("p (b l) -> p b l", b=16), in0=V(bb.t, 1024 + 7, (8, 16), (0, 8)),
                                                  in1=bb[:, 1024:N].rearrange("p (b l) -> p b l", b=16), op=ALU.subtract), reads=[bb], writes=[ex])
        K.op("act", lambda e: e.activation(out=ex[:, 0:N], in_=ex[:, 0:N], func=AF.Exp), reads=[ex], writes=[ex])
        K.op("dve", lambda e: e.tensor_tensor(out=kLT[:, 0:N], in0=kk[:, 0:N], in1=ex[:, 0:N], op=ALU.mult), reads=[kk, ex], writes=[kLT])
        K.op("act", lambda e: e.activation(out=eL[:, 0:8], in_=V(bb.t, 127, (128, 8)), func=AF.Exp), reads=[bb], writes=[eL])
        if N > 1024:
            K.op("act", lambda e: e.activation(out=eL[:, 8:24], in_=V(bb.t, 1024 + 7, (8, 16)), func=AF.Exp), reads=[bb], writes=[eL])
        if full:
            nt_all = N // 128
            K.op("dve", lambda e: e.tensor_tensor(out=ex[:, 0:N].rearrange("p (t s) -> p t s", t=nt_all), in0=bb[:, 0:N].rearrange("p (t s) -> p t s", t=nt_all),
                                                  in1=V(bb.t, 63, (128, nt_all), (0, 128)), op=ALU.subtract), reads=[bb, kLT], writes=[ex])
            K.op("act", lambda e: e.activation(out=sg[:, 0:N], in_=ex[:, 0:N], func=AF.Exp), reads=[ex], writes=[sg])
            K.op("dve", lambda e: e.tensor_tensor(out=qb[:, 0:N], in0=qh[:, 0:N], in1=sg[:, 0:N], op=ALU.mult), reads=[qh, sg], writes=[qb])
            K.op("act", lambda e: e.activation(out=sg[:, 0:N], in_=ex[:, 0:N], func=AF.Exp, scale=-1.0), reads=[ex, qb], writes=[sg])
            K.op("dve", lambda e: e.tensor_tensor(out=kb[:, 0:N], in0=kk[:, 0:N], in1=sg[:, 0:N], op=ALU.mult), reads=[kk, sg], writes=[kb])
            K.op("act", lambda e: e.activation(out=sg[:, 0:N], in_=bb[:, 0:N], func=AF.Exp), reads=[bb, kb], writes=[sg])
            K.op("dve", lambda e: e.tensor_tensor(out=qS[:, 0:N], in0=qh[:, 0:N], in1=sg[:, 0:N], op=ALU.mult), reads=[qh, sg], writes=[qS])
            K.op("act", lambda e: e.activation(out=Sb[:, :], in_=Sst[hh][:, :], func=AF.Copy), reads=[Sst[hh]], writes=[Sb])

        def norm_out(po, t):
            K.op("act", lambda e, po=po: e.activation(out=junk[:, :], in_=po[:, 0:128], func=AF.Square, accum_out=st6[:, 0:1]), reads=[po], writes=[junk, st6])
            K.op("act", lambda e: e.activation(out=st6[:, 1:2], in_=st6[:, 0:1], func=AF.Sqrt, scale=1.0 / 128.0, bias=EPS), reads=[st6], writes=[st6])
            K.op("dve", lambda e: e.reciprocal(out=st6[:, 2:3], in_=st6[:, 1:2]), reads=[st6], writes=[st6])
            oo = ogo[ogst["i"] % 2]
            ogst["i"] += 1
            K.op("dve", lambda e, po=po, oo=oo: e.scalar_tensor_tensor(out=oo[:, :], in0=po[:, 0:128], scalar=st6[:, 2:3], in1=gg[:, t * 128:(t + 1) * 128], op0=ALU.mult, op1=ALU.mult),
                 reads=[po, st6, gg], writes=[oo])
            K.dma("sp", og_scr[t * 128:(t + 1) * 128, hh * 128:(hh + 1) * 128], oo[:, :], reads=[oo], writes=[og_scr_b])

        def make_kLtok(t):
            pt = PS()
            ptb = pt[:, 0:64].bitcast(BF16)
            K.op("pe", lambda e, ptb=ptb: e.transpose(ptb, kLT[:, t * 128:(t + 1) * 128], identB[:, :]), reads=[kLT, identB], writes=[pt])
            K.op("act", lambda e, ptb=ptb: e.activation(out=kLtok[:, :], in_=ptb, func=AF.Copy), reads=[pt], writes=[kLtok])

        for t in range(8):
            sl_t = slice(t * 128, (t + 1) * 128)
            if full:
                pA = PS()
                K.op("pe", lambda e, pA=pA, sl_t=sl_t: e.matmul(pA[:, 0:128], kb[:, sl_t], qb[:, sl_t], start=True, stop=True), reads=[kb, qb], writes=[pA])
                K.op("dve", lambda e, pA=pA: e.tensor_tensor(out=ATb[:, :], in0=pA[:, 0:128], in1=cs(C_MP), op=ALU.mult), reads=[pA, cst], writes=[ATb])
                po = PS()
                mm_group(po[:, 0:128], po, [(ATb[:, :], vtok[:, sl_t]), (qS[:, sl_t], Sb[:, :])], [ATb, vtok, qS, Sb])
                norm_out(po, t)
            make_kLtok(t)
            pc = PS()
            K.op("pe", lambda e, pc=pc, sl_t=sl_t: e.matmul(pc[:, 0:128], kLtok[:, :], vtok[:, sl_t], start=True, stop=True), reads=[kLtok, vtok], writes=[pc])
            K.op("dve", lambda e, pc=pc, t=t: e.scalar_tensor_tensor(out=Sst[hh][:, :], in0=Sst[hh][:, :], scalar=eL[:, t:t + 1], in1=pc[:, 0:128], op0=ALU.mult, op1=ALU.add),
                 reads=[pc, eL], writes=[Sst[hh]])
            if full:
                K.op("act", lambda e: e.activation(out=Sb[:, :], in_=Sst[hh][:, :], func=AF.Copy), reads=[Sst[hh]], writes=[Sb])
        if not full:
            K.op("dve", lambda e: e.tensor_scalar(out=Sst[hh][:, :], in0=Sst[hh][:, :], scalar1=flag[:, 0:1], scalar2=None, op0=ALU.mult), reads=[flag], writes=[Sst[hh]])
            return
        K.dma("sp", O["Sp"][hh], Sst[hh][:, :], reads=[Sst[hh]])
        if cut <= 7:
            return
        t = 8
        sl_t = slice(TP, NTOK)
        K.op("act", lambda e: e.activation(out=S0b[:, :], in_=S0f[:, :], func=AF.Copy), reads=[S0f], writes=[S0b])
        K.op("pool", lambda e: e.tensor_copy(out=V(QzH.t, 0, (136, 16), (1, 8)), in_=qS[:, sl_t].rearrange("p (b l) -> p b l", b=16)), reads=[qS], writes=[QzH])
        pA = PS()
        K.op("pe", lambda e, pA=pA: e.matmul(pA[:, 0:128], kb[:, sl_t], qb[:, sl_t], start=True, stop=True), reads=[kb, qb], writes=[pA])
        K.op("dve", lambda e, pA=pA: e.tensor_tensor(out=ATb[:, :], in0=pA[:, 0:128], in1=cs(C_MS), op=ALU.mult), reads=[pA, cst], writes=[ATb])
        po = PS()
        mm_group(po[:, 0:128], po, [(ATb[:, :], vtok[:, t * 128:(t + 1) * 128])] + [(QzH[:, b * 128:(b + 1) * 128], S0b[:, b * 128:(b + 1) * 128]) for b in range(16)],
                 [ATb, vtok, QzH, S0b])
        norm_out(po, t)
        make_kLtok(t)
        for g in range(4):
            vz = Vz[g % 2]
            K.op("dve", lambda e, g=g, vz=vz: e.tensor_tensor(out=vz[:, :].rearrange("p (b v) -> p b v", b=4), in0=V(vtok.t, t * 128, (0, 4), (1, 128)),
                                                               in1=V(cst.t, C_SEQM + g * 4, (1, 4), (0, 128)), op=ALU.mult), reads=[vtok, cst], writes=[vz])
            pc = PS()
            K.op("pe", lambda e, pc=pc, vz=vz: e.matmul(pc[:, :], kLtok[:, :], vz[:, :], start=True, stop=True), reads=[kLtok, vz], writes=[pc])
            K.op("dve", lambda e, g=g: e.tensor_tensor(out=S0f[:, g * 512:(g + 1) * 512].rearrange("p (b v) -> p b v", b=4), in0=S0f[:, g * 512:(g + 1) * 512].rearrange("p (b v) -> p b v", b=4),
                                                       in1=V(eL.t, 8 + g * 4, (1, 4), (0, 128)), op=ALU.mult), reads=[eL, S0b], writes=[S0f])
            K.op("dve", lambda e, pc=pc, g=g: e.tensor_tensor(out=S0f[:, g * 512:(g + 1) * 512], in0=S0f[:, g * 512:(g + 1) * 512], in1=pc[:, :], op=ALU.add), reads=[pc], writes=[S0f])
        K.dma("sp", O["Ss"][:, hh].rearrange("b c v -> c b v"), S0f[:, :].rearrange("p (b v) -> p b v", b=16), reads=[S0f])

    if stage >= 1:
        build_xT([(I["xpre"], 8)], TP)
        K.op("dve", lambda e: e.tensor_copy(out=xhalo[:, :].rearrange("p (k j) -> p k j", k=16), in_=xTv(TP)[:, :, TP - 3:TP]), reads=[xT], writes=[xhalo])
        phase_A_mlstm()
        K.barrier()
        K.op("pool", lambda e: e.memset(QzH[:, :], 0.0), writes=[QzH])
        for hh in range(8):
            hgrn_head(hh, xTv(TP), TP, False)
        K.barrier()
    if stage >= 2:
        build_xT([(I["xp"], 8), (I["xs"], 1)], NTOK)
        phase_B_mlstm()
        K.barrier()
        K.op("pool", lambda e: e.memset(QzH[:, :], 0.0), writes=[QzH])
        if cut > 6:
            for hh in range(8):
                hgrn_head(hh, xTv(NTOK), NTOK, True)
        K.barrier()

    r_scr = nc.dram_tensor("r_scr", [NTOK, D], F32, kind="Internal").ap()
    h_scr = nc.dram_tensor("h_scr", [NTOK, D], F32, kind="Internal").ap()
    r_scr_b = Buf("r_scr")
    h_scr_b = Buf("h_scr")

    def xrows(t):
        return I["xp"][t * 128:(t + 1) * 128, :] if t < 8 else I["xs"]

    def layer_norm_tiles(g_in, b_in_, dstT_buf, dstT_view, tilebufs, lng, lnb, lnst, out_dram=None, out_fn=None, inplace=False):
        K.dma("sp", lng[:, :], g_in[0:1, :].partition_broadcast(128), writes=[lng])
        K.dma("sp", lnb[:, :], b_in_[0:1, :].partition_broadcast(128), writes=[lnb])
        if not inplace:
            K.dma("sp", tilebufs[0][:, :], r_scr[0:128, :], reads=[r_scr_b], writes=[tilebufs[0]])
        for t in range(NT):
            if inplace:
                rt = tilebufs[t]
            else:
                rt = tilebufs[t % 2]
                if t + 1 < NT:
                    K.dma("sp", tilebufs[(t + 1) % 2][:, :], r_scr[(t + 1) * 128:(t + 2) * 128, :], reads=[r_scr_b], writes=[tilebufs[(t + 1) % 2]])
            for c in range(4):
                K.op("dve", lambda e, c=c, rt=rt: e.bn_stats(out=lnst[:, c * 6:(c + 1) * 6], in_=rt[:, c * 512:(c + 1) * 512]), reads=[rt], writes=[lnst])
            K.op("dve", lambda e: e.bn_aggr(out=lnst[:, 24:26], in_=lnst[:, 0:24]), reads=[lnst], writes=[lnst])
            K.op("act", lambda e: e.activation(out=lnst[:, 26:27], in_=lnst[:, 25:26], func=AF.Sqrt, bias=EPS), reads=[lnst], writes=[lnst])
            K.op("dve", lambda e: e.reciprocal(out=lnst[:, 27:28], in_=lnst[:, 26:27]), reads=[lnst], writes=[lnst])
            K.op("dve", lambda e: e.tensor_scalar(out=lnst[:, 28:29], in0=lnst[:, 24:25], scalar1=-1.0, scalar2=lnst[:, 27:28], op0=ALU.mult, op1=ALU.mult), reads=[lnst], writes=[lnst])
            K.op("act", lambda e, rt=rt: e.activation(out=rt[:, :], in_=rt[:, :], func=AF.Identity, scale=lnst[:, 27:28], bias=lnst[:, 28:29]), reads=[lnst], writes=[rt])
            K.op("dve", lambda e, rt=rt: e.tensor_tensor(out=rt[:, :], in0=rt[:, :], in1=lng[:, :], op=ALU.mult), reads=[lng], writes=[rt])
            K.op("dve", lambda e, rt=rt: e.tensor_tensor(out=rt[:, :], in0=rt[:, :], in1=lnb[:, :], op=ALU.add), reads=[lnb], writes=[rt])
            if out_fn is not None:
                K.dma("sp", out_fn(t), rt[:, :], reads=[rt])
            if out_dram is not None:
                K.dma("sp", out_dram[t * 128:(t + 1) * 128, :], rt[:, :], reads=[rt], writes=[h_scr_b])
            if dstT_view is not None:
                transpose_tile_f32(rt, lambda kc, rt=rt: rt[:, kc * 128:(kc + 1) * 128], dstT_buf, dstT_view, t * 128)

    def out_proj_residual(lhsT_buf, lhsT_view, nkc, w_dram, res_fn, res_bufs, xr, ro):
        items = [(cblk, t) for cblk in range(8) for t in range(NT)]
        nb = len(xr)

        def issue_load(i):
            cblk, t = items[i]
            x_ = xr[i % nb]
            K.dma("sp", x_[:, :], res_fn(t)[:, cblk * 256:(cblk + 1) * 256], reads=res_bufs, writes=[x_])

        for i in range(min(nb - 1, len(items))):
            issue_load(i)
        sl = w = None
        for i, (cblk, t) in enumerate(items):
            if t == 0:
                sl, w = wload([w_dram[:, cblk * 256:(cblk + 1) * 256]])
            if i + nb - 1 < len(items):
                issue_load(i + nb - 1)
            ps = PS()
            mm_group(ps[:, 0:256], ps, [(lhsT_view[:, kc, t * 128:(t + 1) * 128], w[:, kc, :]) for kc in range(nkc)], [lhsT_buf, sl])
            x_ = xr[i % nb]
            r_ = ro[i % nb]
            K.op("dve", lambda e, ps=ps, x_=x_, r_=r_: e.scalar_tensor_tensor(out=r_[:, :], in0=x_[:, :], scalar=ALPHA, in1=ps[:, 0:256], op0=ALU.mult, op1=ALU.add),
                 reads=[ps, x_], writes=[r_])
            K.dma("sp", r_scr[t * 128:(t + 1) * 128, cblk * 256:(cblk + 1) * 256], r_[:, :], reads=[r_], writes=[r_scr_b])

    if stage >= 3:
        A.release(m_mix)
        hmT = A.alloc("hmT", 8 * NTOK, BF16)
        ogT = A.alloc("ogT", 8 * NTOK, BF16)
        mT = A.alloc("mT", 16 * NTOK, BF16)
        tl = [A.alloc(f"tl{i}", 1024, BF16) for i in range(2)]
        sgm = A.alloc("sgm", 512, F32)
        sgh = A.alloc("sgh", 512, F32)
        mm1 = sgm
        mm2 = sgh
        xr = [A.alloc(f"xr{i}", 256, F32) for i in range(3)]
        ro = [A.alloc(f"ro{i}", 256, F32) for i in range(3)]
        i_tl = 0
        for (scr, scr_b, dstT, goff) in ((hm_scr, hm_scr_b, hmT, 0), (og_scr, og_scr_b, ogT, 8)):
            for t in range(NT):
                tb = tl[i_tl % 2]
                i_tl += 1
                K.dma("sp", tb[:, :], scr[t * 128:(t + 1) * 128, :], reads=[scr_b], writes=[tb])
                for g in range(2):
                    ps = PS()
                    psb = ps[:, 0:256].bitcast(BF16)
                    for j in range(4):
                        kc = g * 4 + j
                        K.op("pe", lambda e, psb=psb, j=j, kc=kc, tb=tb: e.transpose(psb[:, j * 128:(j + 1) * 128], tb[:, kc * 128:(kc + 1) * 128], identB[:, :]),
                             reads=[tb, identB], writes=[ps])
                    for j in range(4):
                        kc = g * 4 + j
                        K.op("act", lambda e, psb=psb, j=j, kc=kc, dstT=dstT, goff=goff, t=t: e.activation(
                            out=dstT[:, kc * NTOK + t * 128:kc * NTOK + (t + 1) * 128], in_=psb[:, j * 128:(j + 1) * 128], func=AF.Copy, scale=gnT[:, goff + kc:goff + kc + 1]),
                            reads=[ps, gnT], writes=[dstT])
        viewX = xTv(NTOK)
        hmTv = hmT[:, :].rearrange("p (k t) -> p k t", k=8)
        ogTv = ogT[:, :].rearrange("p (k t) -> p k t", k=8)
        mTv = mT[:, :].rearrange("p (k t) -> p k t", k=16)
        for pr in range(8):
            slgm, wgm = wload([w_in[:, 8200 + pr * 256:8200 + (pr + 1) * 256]])
            slgh, wgh = wload([w_in[:, 10248 + pr * 256:10248 + (pr + 1) * 256]])
            slbm, wbm = wload([I["w_bm"][:, pr * 256:(pr + 1) * 256]])
            slbh, wbh = wload([I["w_bh"][:, pr * 256:(pr + 1) * 256]])
            for c2 in range(2):
                cb = pr * 2 + c2
                cs_ = slice(c2 * 128, (c2 + 1) * 128)
                for (t0, n) in ((0, 512), (512, 512), (1024, 128)):
                    pgm = PS()
                    mm_group(pgm[:, 0:n], pgm, [(wgm[:, kc, cs_], viewX[:, kc, t0:t0 + n]) for kc in range(16)], [slgm, xT])
                    K.op("act", lambda e, pgm=pgm, n=n, cb=cb: e.activation(out=sgm[:, 0:n], in_=pgm[:, 0:n], func=AF.Sigmoid, bias=bgm[:, cb:cb + 1]), reads=[pgm, bgm], writes=[sgm])
                    pgh = PS()
                    mm_group(pgh[:, 0:n], pgh, [(wgh[:, kc, cs_], viewX[:, kc, t0:t0 + n]) for kc in range(16)], [slgh, xT])
                    K.op("act", lambda e, pgh=pgh, n=n, cb=cb: e.activation(out=sgh[:, 0:n], in_=pgh[:, 0:n], func=AF.Sigmoid, bias=bgh[:, cb:cb + 1]), reads=[pgh, bgh], writes=[sgh])
                    ppm = PS()
                    mm_group(ppm[:, 0:n], ppm, [(wbm[:, kc, cs_], hmTv[:, kc, t0:t0 + n]) for kc in range(8)], [slbm, hmT])
                    K.op("dve", lambda e, ppm=ppm, n=n: e.tensor_tensor(out=mm1[:, 0:n], in0=sgm[:, 0:n], in1=ppm[:, 0:n], op=ALU.mult), reads=[ppm, sgm], writes=[mm1])
                    pph = PS()
                    mm_group(pph[:, 0:n], pph, [(wbh[:, kc, cs_], ogTv[:, kc, t0:t0 + n]) for kc in range(8)], [slbh, ogT])
                    K.op("dve", lambda e, pph=pph, n=n: e.tensor_tensor(out=mm2[:, 0:n], in0=sgh[:, 0:n], in1=pph[:, 0:n], op=ALU.mult), reads=[pph, sgh], writes=[mm2])
                    K.op("dve", lambda e, n=n, cb=cb, t0=t0: e.tensor_tensor(out=mTv[:, cb, t0:t0 + n], in0=mm1[:, 0:n], in1=mm2[:, 0:n], op=ALU.add), reads=[mm1, mm2], writes=[mT])
        out_proj_residual(mT, mTv, 16, I["w_out"], xrows, [], xr, ro)
        K.barrier()
        m_ln = A.mark()
        A.release(m_mix)
        tilebufs = [A.alloc(f"tilebuf{i}", D, F32) for i in range(2)]
        lng = A.alloc("lng", D, F32)
        lnb = A.alloc("lnb", D, F32)
        lnst = A.alloc("lnst", 32, F32)
        dbg = None
        if stage == 3:
            dbg = lambda t: (O["y_p"][t * 128:(t + 1) * 128, :] if t < 8 else O["y_s"])
        layer_norm_tiles(I["ln1_g"], I["ln1_b"], xT, xTv(NTOK), tilebufs, lng, lnb, lnst, out_dram=h_scr, out_fn=dbg)
        K.barrier()
        m_post_ln = A.mark()

    if stage >= 4:
        XS = 512.0 ** -0.5
        A.release(m_mix)
        oT = A.alloc("oT", 16 * NTOK, BF16)
        qTh = A.alloc("qTh", 4 * NTOK, BF16)
        memT = A.alloc("memT", 16 * 256, BF16)
        membf = A.alloc("membf", D, BF16)
        KTh = A.alloc("KTh", 4 * 256, BF16)
        Vh = A.alloc("Vh", 2 * 512, BF16)
        kvout = [A.alloc(f"kvout{i}", 256, F32) for i in range(2)]
        Kb = [A.alloc(f"Kb{i}", 2 * 512, BF16) for i in range(2)]
        KTb = [A.alloc(f"KTb{i}", 4 * 256, BF16) for i in range(2)]
        Vb = [A.alloc(f"Vb{i}", 2 * 512, BF16) for i in range(2)]
        scx = A.alloc("scx", 256, F32)
        pn = A.alloc("pn", 256, BF16)
        pT = A.alloc("pT", 256, BF16)
        sTs = A.alloc("sTs", 256, F32)
        xst = A.alloc("xst", 8, F32)
        xr = [A.alloc(f"xr{i}", 256, F32) for i in range(2)]
        ro = [A.alloc(f"ro{i}", 256, F32) for i in range(2)]
        h1Tv = xTv(NTOK)
        oTv = oT[:, :].rearrange("p (k t) -> p k t", k=16)
        qTv = qTh[:, :].rearrange("p (k t) -> p k t", k=4)
        memTv = memT[:, :].rearrange("p (k m) -> p k m", k=16)
        KThv = KTh[:, :].rearrange("p (k m) -> p k m", k=4)
        kvi = 0
        for mt in range(2):
            for hf in range(2):
                K.dma("pool", membf[:, hf * 1024:(hf + 1) * 1024], I["mem"][mt * 128:(mt + 1) * 128, hf * 1024:(hf + 1) * 1024], writes=[membf])
            if cut <= 10:
                continue
            for g in range(2):
                ps = PS()
                psb = ps[:, :].bitcast(BF16)
                for j in range(8):
                    kc = g * 8 + j
                    K.op("pe", lambda e, psb=psb, j=j, kc=kc: e.transpose(psb[:, j * 128:(j + 1) * 128], membf[:, kc * 128:(kc + 1) * 128], identB[:, :]),
                         reads=[membf, identB], writes=[ps])
                K.op("act", lambda e, psb=psb, g=g, mt=mt: e.activation(out=memTv[:, g * 8:(g + 1) * 8, mt * 128:(mt + 1) * 128], in_=psb.rearrange("p (j m) -> p j m", j=8), func=AF.Copy),
                     reads=[ps], writes=[memT])

        def softmax_to_pT(ps_s):
            K.op("dve", lambda e: e.tensor_reduce(out=xst[:, 0:1], in_=ps_s[:, 0:256], axis=AX.X, op=ALU.max), reads=[ps_s], writes=[xst])
            K.op("dve", lambda e: e.tensor_scalar(out=xst[:, 1:2], in0=xst[:, 0:1], scalar1=-XS, scalar2=None, op0=ALU.mult), reads=[xst], writes=[xst])
            K.op("act", lambda e: e.activation(out=scx[:, :], in_=ps_s[:, 0:256], func=AF.Exp, scale=XS, bias=xst[:, 1:2], accum_out=xst[:, 2:3]), reads=[ps_s, xst], writes=[scx, xst])
            K.op("dve", lambda e: e.reciprocal(out=xst[:, 3:4], in_=xst[:, 2:3]), reads=[xst], writes=[xst])
            K.op("dve", lambda e: e.tensor_scalar(out=pn[:, :], in0=scx[:, :], scalar1=xst[:, 3:4], scalar2=None, op0=ALU.mult), reads=[scx, xst], writes=[pn])
            pt = PS()
            ptb = pt[:, 0:128].bitcast(BF16)
            for mt in range(2):
                K.op("pe", lambda e, ptb=ptb, mt=mt: e.transpose(ptb[:, mt * 128:(mt + 1) * 128], pn[:, mt * 128:(mt + 1) * 128], identB[:, :]), reads=[pn, identB], writes=[pt])
            K.op("act", lambda e, ptb=ptb: e.activation(out=pT[:, :], in_=ptb, func=AF.Copy), reads=[pt], writes=[pT])

        for h in range(4):
            if cut <= 11:
                continue
            for half in range(2):
                slq, wq = wload([I["xa_wq"][:, h * 512 + half * 256:h * 512 + (half + 1) * 256]])
                for c2 in range(2):
                    c = half * 2 + c2
                    for (t0, n) in ((0, 512), (512, 512), (1024, 128)):
                        ps = PS()
                        mm_group(ps[:, 0:n], ps, [(wq[:, kc, c2 * 128:(c2 + 1) * 128], h1Tv[:, kc, t0:t0 + n]) for kc in range(16)], [slq, xT])
                        K.op("act", lambda e, ps=ps, c=c, t0=t0, n=n: e.activation(out=qTv[:, c, t0:t0 + n], in_=ps[:, 0:n], func=AF.Copy), reads=[ps], writes=[qTh])
            for half in range(2):
                slk, wk = wload([I["xa_wk"][:, h * 512 + half * 256:h * 512 + (half + 1) * 256]])
                slv, wv_ = wload([I["xa_wv"][:, h * 512 + half * 256:h * 512 + (half + 1) * 256]])
                c0 = h * 512 + half * 256
                for mt in range(2):
                    ms = slice(mt * 128, (mt + 1) * 128)
                    ps = PS()
                    mm_group(ps[:, 0:256], ps, [(memTv[:, kc, ms], wk[:, kc, :]) for kc in range(16)], [memT, slk])
                    ko = kvout[kvi % 2]
                    kvi += 1
                    K.op("act", lambda e, ps=ps, ko=ko: e.activation(out=ko[:, :], in_=ps[:, 0:256], func=AF.Copy), reads=[ps], writes=[ko])
                    K.dma("sp", O["mk"][ms, c0:c0 + 256], ko[:, :], reads=[ko])
                    ps = PS()
                    mm_group(ps[:, 0:256], ps, [(memTv[:, kc, ms], wv_[:, kc, :]) for kc in range(16)], [memT, slv])
                    ko = kvout[kvi % 2]
                    kvi += 1
                    K.op("act", lambda e, ps=ps, ko=ko: e.activation(out=ko[:, :], in_=ps[:, 0:256], func=AF.Copy), reads=[ps], writes=[ko])
                    K.dma("sp", O["mv"][ms, c0:c0 + 256], ko[:, :], reads=[ko])
                    K.op("dve", lambda e, ps=ps, mt=mt, half=half: e.tensor_copy(out=Vh[:, mt * 512 + half * 256:mt * 512 + (half + 1) * 256], in_=ps[:, 0:256]), reads=[ps], writes=[Vh])
                for c2 in range(2):
                    ps = PS()
                    mm_group(ps[:, 0:256], ps, [(wk[:, kc, c2 * 128:(c2 + 1) * 128], memTv[:, kc, :]) for kc in range(16)], [memT, slk])
                    K.op("act", lambda e, ps=ps, dc=half * 2 + c2: e.activation(out=KThv[:, dc, :], in_=ps[:, 0:256], func=AF.Copy), reads=[ps], writes=[KTh])
            for t in range(8):
                if cut <= 12:
                    continue
                ts_ = slice(t * 128, (t + 1) * 128)
                ps_s = PS()
                mm_group(ps_s[:, 0:256], ps_s, [(qTv[:, dc, ts_], KThv[:, dc, :]) for dc in range(4)], [qTh, KTh])
                softmax_to_pT(ps_s)
                ps_o = PS()
                for dc in range(4):
                    for mt in range(2):
                        K.op("pe", lambda e, ps_o=ps_o, dc=dc, mt=mt: e.matmul(ps_o[:, dc * 128:(dc + 1) * 128], Vh[:, mt * 512 + dc * 128:mt * 512 + (dc + 1) * 128],
                                                                            pT[:, mt * 128:(mt + 1) * 128], start=(mt == 0), stop=(mt == 1)), reads=[Vh, pT], writes=[ps_o])
                K.op("act", lambda e, ps_o=ps_o, ts_=ts_, h=h: e.activation(out=oTv[:, h * 4:(h + 1) * 4, ts_], in_=ps_o[:, :].rearrange("p (c t) -> p c t", c=4), func=AF.Copy),
                     reads=[ps_o], writes=[oT])
            if cut <= 13:
                continue
            ps_sT = psum[6]
            for b in range(16):
                kb_ = Kb[b % 2]
                ktb = KTb[b % 2]
                K.dma("pool", kb_[:, :].rearrange("p (mt c) -> p mt c", mt=2), I["ck"][b, :, h * 512:(h + 1) * 512].rearrange("(mt p) c -> p mt c", p=128), writes=[kb_])
                ps = PS()
                psb = ps[:, :].bitcast(BF16)
                for dc in range(4):
                    for mt in range(2):
                        K.op("pe", lambda e, psb=psb, dc=dc, mt=mt, kb_=kb_: e.transpose(psb[:, dc * 256 + mt * 128:dc * 256 + (mt + 1) * 128],
                                                                                     kb_[:, mt * 512 + dc * 128:mt * 512 + (dc + 1) * 128], identB[:, :]),
                             reads=[kb_, identB], writes=[ps])
                K.op("act", lambda e, psb=psb, ktb=ktb: e.activation(out=ktb[:, :], in_=psb, func=AF.Copy), reads=[ps], writes=[ktb])
                for mt in range(2):
                    for dc in range(4):
                        K.op("pe", lambda e, mt=mt, dc=dc, b=b, ktb=ktb: e.matmul(ps_sT[:, mt * 128 + b * 8:mt * 128 + (b + 1) * 8], ktb[:, dc * 256 + mt * 128:dc * 256 + (mt + 1) * 128],
                                                                               qTv[:, dc, TP + b * 8:TP + (b + 1) * 8], start=(dc == 0), stop=(dc == 3)),
                             reads=[ktb, qTh], writes=[ps_sT])
            K.op("act", lambda e: e.activation(out=sTs[:, :], in_=ps_sT[:, 0:256], func=AF.Copy), reads=[ps_sT], writes=[sTs])
            ps_s = PS()
            for mt in range(2):
                K.op("pe", lambda e, ps_s=ps_s, mt=mt: e.transpose(ps_s[:, mt * 128:(mt + 1) * 128], sTs[:, mt * 128:(mt + 1) * 128], ident), reads=[sTs, cst], writes=[ps_s])
            softmax_to_pT(ps_s)
            ps_oS = psum[7]
            for b in range(16):
                vb_ = Vb[b % 2]
                K.dma("pool", vb_[:, :].rearrange("p (mt c) -> p mt c", mt=2), I["cv"][b, :, h * 512:(h + 1) * 512].rearrange("(mt p) c -> p mt c", p=128), writes=[vb_])
                for dc in range(4):
                    for mt in range(2):
                        K.op("pe", lambda e, dc=dc, mt=mt, b=b, vb_=vb_: e.matmul(ps_oS[:, dc * 128 + b * 8:dc * 128 + (b + 1) * 8], vb_[:, mt * 512 + dc * 128:mt * 512 + (dc + 1) * 128],
                                                                               pT[:, mt * 128 + b * 8:mt * 128 + (b + 1) * 8], start=(mt == 0), stop=(mt == 1)),
                             reads=[vb_, pT], writes=[ps_oS])
            K.op("act", lambda e, h=h: e.activation(out=oTv[:, h * 4:(h + 1) * 4, TP:NTOK], in_=ps_oS[:, :].rearrange("p (c t) -> p c t", c=4), func=AF.Copy), reads=[ps_oS], writes=[oT])
        out_proj_residual(oT, oTv, 16, I["xa_wo"], lambda t: h_scr[t * 128:(t + 1) * 128, :], [h_scr_b], xr, ro)
        K.barrier()
        A.release(m_mix)
        tilebufs = [A.alloc(f"tilebufb{i}", D, F32) for i in range(2)]
        lng = A.alloc("lngb", D, F32)
        lnb = A.alloc("lnbb", D, F32)
        lnst = A.alloc("lnstb", 32, F32)
        dbg = None
        if stage == 4:
            dbg = lambda t: (O["y_p"][t * 128:(t + 1) * 128, :] if t < 8 else O["y_s"])
        layer_norm_tiles(I["ln2_g"], I["ln2_b"], xT, xTv(NTOK), tilebufs, lng, lnb, lnst, out_dram=h_scr, out_fn=dbg)
        K.barrier()

    if stage >= 5:
        A.release(m_mix)
        h2Tv = xTv(NTOK)
        acc_t = [A.alloc(f"acc{t}", D, F32) for t in range(NT)]
        m_after_acc = A.mark()
        hid = A.alloc("hid", 4 * NTOK, BF16)
        hidv = hid[:, :].rearrange("p (f t) -> p f t", f=4)
        cbc = A.alloc("cbc", NTOK, F32)
        cTf = cbc
        cThi = A.alloc("cThi", NTOK, BF16)
        cTlo = A.alloc("cTlo", NTOK, BF16)
        sgt = A.alloc("sgt", 512, F32)
        ut = A.alloc("ut", 512, F32)
        comb = A.alloc("comb", NT * 32, F32)
        rbrow = A.alloc("rbrow", 36, F32)
        rs_ = A.alloc("rs_", 128, F32)
        sel = [A.alloc(f"sel{i}", 128, BF16) for i in range(2)]
        for t in range(NT):
            K.dma("sp", acc_t[t][:, :], h_scr[t * 128:(t + 1) * 128, :], reads=[h_scr_b], writes=[acc_t[t]])
            K.op("act", lambda e, t=t: e.activation(out=acc_t[t][:, :], in_=acc_t[t][:, :], func=AF.Copy, scale=ALPHA), writes=[acc_t[t]])
        K.dma("sp", rbrow[:, :], I["rb"][0:1, :].partition_broadcast(128), writes=[rbrow])
        slr, wr = wload([I["rw"]])
        lg, oh, lge, t8, mk1, mk2 = rs_[:, 0:36], rs_[:, 36:40], rs_[:, 40:48], rs_[:, 48:80], rs_[:, 80:88], rs_[:, 88:96]
        sc = lambda i: rs_[:, 96 + i:97 + i]
        for t in range(NT):
            ps = PS()
            mm_group(ps[:, 0:36], ps, [(h2Tv[:, kc, t * 128:(t + 1) * 128], wr[:, kc, :]) for kc in range(16)], [xT, slr])
            K.op("dve", lambda e, ps=ps: e.tensor_tensor(out=lg, in0=ps[:, 0:36], in1=rbrow[:, :], op=ALU.add), reads=[ps, rbrow], writes=[rs_])
            K.op("dve", lambda e: e.tensor_reduce(out=sc(0), in_=lg[:, 0:4], axis=AX.X, op=ALU.max), reads=[rs_], writes=[rs_])
            K.op("dve", lambda e: e.tensor_scalar(out=oh, in0=lg[:, 0:4], scalar1=sc(0), scalar2=None, op0=ALU.is_equal), reads=[rs_], writes=[rs_])
            K.op("dve", lambda e: e.tensor_scalar(out=sc(1), in0=sc(0), scalar1=-1.0, scalar2=None, op0=ALU.mult), reads=[rs_], writes=[rs_])
            K.op("act", lambda e: e.activation(out=t8[:, 0:4], in_=lg[:, 0:4], func=AF.Exp, bias=sc(1), accum_out=sc(2)), reads=[rs_], writes=[rs_])
            K.op("dve", lambda e: e.reciprocal(out=sc(3), in_=sc(2)), reads=[rs_], writes=[rs_])
            K.op("dve", lambda e: e.tensor_tensor(out=t8.rearrange("p (g e) -> p g e", g=4), in0=lg[:, 4:36].rearrange("p (g e) -> p g e", g=4),
                                                  in1=V(rs_.t, 36, (1, 4), (0, 8)), op=ALU.mult), reads=[rs_], writes=[rs_])
            K.op("dve", lambda e: e.tensor_reduce(out=lge, in_=V(rs_.t, 48, (1, 8), (8, 4)), axis=AX.X, op=ALU.add), reads=[rs_], writes=[rs_])
            K.op("dve", lambda e: e.tensor_reduce(out=sc(4), in_=lge, axis=AX.X, op=ALU.max), reads=[rs_], writes=[rs_])
            K.op("dve", lambda e: e.tensor_scalar(out=mk1, in0=lge, scalar1=sc(4), scalar2=None, op0=ALU.is_equal), reads=[rs_], writes=[rs_])
            K.op("dve", lambda e: e.scalar_tensor_tensor(out=t8[:, 0:8], in0=mk1, scalar=NEGBIG, in1=lge, op0=ALU.mult, op1=ALU.add), reads=[rs_], writes=[rs_])
            K.op("dve", lambda e: e.tensor_reduce(out=sc(5), in_=t8[:, 0:8], axis=AX.X, op=ALU.max), reads=[rs_], writes=[rs_])
            K.op("dve", lambda e: e.tensor_scalar(out=mk2, in0=t8[:, 0:8], scalar1=sc(5), scalar2=None, op0=ALU.is_equal), reads=[rs_], writes=[rs_])
            K.op("dve", lambda e: e.tensor_tensor(out=sc(6), in0=sc(5), in1=sc(4), op=ALU.subtract), reads=[rs_], writes=[rs_])
            K.op("act", lambda e: e.activation(out=sc(7), in_=sc(6), func=AF.Exp), reads=[rs_], writes=[rs_])
            K.op("dve", lambda e: e.tensor_scalar(out=sc(8), in0=sc(7), scalar1=1.0, scalar2=None, op0=ALU.add), reads=[rs_], writes=[rs_])
            K.op("dve", lambda e: e.reciprocal(out=sc(9), in_=sc(8)), reads=[rs_], writes=[rs_])
            K.op("dve", lambda e: e.tensor_tensor(out=sc(10), in0=sc(9), in1=sc(3), op=ALU.mult), reads=[rs_], writes=[rs_])
            K.op("dve", lambda e: e.tensor_tensor(out=sc(11), in0=sc(3), in1=sc(10), op=ALU.subtract), reads=[rs_], writes=[rs_])
            K.op("dve", lambda e: e.tensor_scalar(out=mk1, in0=mk1, scalar1=sc(10), scalar2=None, op0=ALU.mult), reads=[rs_], writes=[rs_])
            K.op("dve", lambda e: e.scalar_tensor_tensor(out=mk1, in0=mk2, scalar=sc(11), in1=mk1, op0=ALU.mult, op1=ALU.add), reads=[rs_], writes=[rs_])
            K.op("dve", lambda e, t=t: e.tensor_tensor(out=comb[:, t * 32:(t + 1) * 32].rearrange("p (g e) -> p g e", g=4), in0=V(rs_.t, 36, (1, 4), (0, 8)),
                                                       in1=V(rs_.t, 80, (0, 4), (1, 8)), op=ALU.mult), reads=[rs_], writes=[comb])
        for g in range(3):
            ps = PS()
            nt_ = 4 if g < 2 else 1
            for j in range(nt_):
                t = g * 4 + j
                K.op("pe", lambda e, ps=ps, j=j, t=t: e.transpose(ps[0:32, j * 128:(j + 1) * 128], comb[:, t * 32:(t + 1) * 32], ident), reads=[comb, cst], writes=[ps])
            K.op("act", lambda e, ps=ps, g=g, nt_=nt_: e.activation(out=cTf[0:32, g * 512:g * 512 + nt_ * 128], in_=ps[0:32, 0:nt_ * 128], func=AF.Copy), reads=[ps], writes=[cTf])
        K.op("dve", lambda e: e.tensor_copy(out=cThi[0:32, :], in_=cTf[0:32, :]), reads=[cTf], writes=[cThi])
        K.op("dve", lambda e: e.tensor_tensor(out=cTlo[0:32, :], in0=cTf[0:32, :], in1=cThi[0:32, :], op=ALU.subtract), reads=[cTf, cThi], writes=[cTlo])
        TB3 = ((0, 512), (512, 512), (1024, 128))
        for xe in range(32):
            se = sel[xe % 2]
            K.op("pool", lambda e, se=se, xe=xe: e.tensor_scalar(out=se[0:32, :], in0=cst[0:32, C_ONES:C_ONES + 128], scalar1=cst[0:32, C_ID + xe:C_ID + xe + 1], scalar2=None, op0=ALU.mult),
                 reads=[cst], writes=[se])
            for (t0, n) in TB3:
                ps = PS()
                mm_group(ps[:, 0:n], ps, [(se[0:32, :], cThi[0:32, t0:t0 + n]), (se[0:32, :], cTlo[0:32, t0:t0 + n])], [se, cThi, cTlo])
                K.op("act", lambda e, ps=ps, t0=t0, n=n: e.activation(out=cbc[:, t0:t0 + n], in_=ps[:, 0:n], func=AF.Copy), reads=[ps], writes=[cbc])
            for fp in range(2):
                slg, wg = wload([I["e_wg"][xe][:, fp * 256:(fp + 1) * 256]])
                slu, wu = wload([I["e_wu"][xe][:, fp * 256:(fp + 1) * 256]])
                for c2 in range(2):
                    fb = fp * 2 + c2
                    cs_ = slice(c2 * 128, (c2 + 1) * 128)
                    for (t0, n) in TB3:
                        pg = PS()
                        mm_group(pg[:, 0:n], pg, [(wg[:, kc, cs_], h2Tv[:, kc, t0:t0 + n]) for kc in range(16)], [slg, xT])
                        K.op("act", lambda e, pg=pg, n=n: e.activation(out=sgt[:, 0:n], in_=pg[:, 0:n], func=AF.Silu), reads=[pg], writes=[sgt])
                        pu = PS()
                        mm_group(pu[:, 0:n], pu, [(wu[:, kc, cs_], h2Tv[:, kc, t0:t0 + n]) for kc in range(16)], [slu, xT])
                        K.op("dve", lambda e, pu=pu, n=n, t0=t0: e.tensor_tensor(out=ut[:, 0:n], in0=pu[:, 0:n], in1=cbc[:, t0:t0 + n], op=ALU.mult), reads=[pu, cbc], writes=[ut])
                        K.op("dve", lambda e, n=n, t0=t0, fb=fb: e.tensor_tensor(out=hidv[:, fb, t0:t0 + n], in0=sgt[:, 0:n], in1=ut[:, 0:n], op=ALU.mult), reads=[sgt, ut], writes=[hid])
            for dh in range(2):
                sld, wd = wload([I["e_wd"][xe][:, dh * 1024:(dh + 1) * 1024]])
                for t in range(NT):
                    for db in range(2):
                        ps = PS()
                        mm_group(ps[:, :], ps, [(hidv[:, fc, t * 128:(t + 1) * 128], wd[:, fc, db * 512:(db + 1) * 512]) for fc in range(4)], [hid, sld])
                        c0 = dh * 1024 + db * 512
                        K.op("dve", lambda e, ps=ps, t=t, c0=c0: e.tensor_tensor(out=acc_t[t][:, c0:c0 + 512], in0=acc_t[t][:, c0:c0 + 512], in1=ps[:, :], op=ALU.add),
                             reads=[ps], writes=[acc_t[t]])
        K.barrier()
        A.release(m_after_acc)
        lng = A.alloc("lngc", D, F32)
        lnb = A.alloc("lnbc", D, F32)
        lnst = A.alloc("lnstc", 32, F32)
        layer_norm_tiles(I["ln3_g"], I["ln3_b"], None, None, acc_t, lng, lnb, lnst, out_dram=None,
                         out_fn=lambda t: (O["y_p"][t * 128:(t + 1) * 128, :] if t < 8 else O["y_s"]), inplace=True)

    def emit_prompt_state_outputs():
        for h in range(4):
            K.dma("sp", O["Cp"][h].rearrange("(dc p) v -> p dc v", p=128), V(Cst[h].t, 0, (257, 2), (1, 256)), reads=[Cst[h]])
            K.dma("sp", O["np_"][h].rearrange("(dc p) -> p dc", p=128), V(Cst[h].t, 256, (257, 2)), reads=[Cst[h]])
        K.dma("sp", O["mp"], mstate[0:1, :], reads=[mstate])
        for hh in range(8):
            K.dma("sp", O["Sp"][hh], Sst[hh][:, :], reads=[Sst[hh]])

    if stage == 1:
        emit_prompt_state_outputs()

    nc._K = K
    nc._A = A
    nc._I = I
    nc._O = O
    with nc.allow_non_contiguous_dma(reason="small strided parameter / state layouts"):
        K.finalize()
    return nc


def prep_core_inputs(inp, c, shared):
    j, hh = c // 2, c % 2
    sl = slice(16 * c, 16 * c + 16)
    m = dict(shared)
    m["xp"] = np.ascontiguousarray(inp["x_prompt"][j, hh * TP:(hh + 1) * TP])
    m["xpre"] = np.ascontiguousarray(inp["x_prompt"][j, 0:TP])
    m["xs"] = np.ascontiguousarray(inp["x_sample"][sl].reshape(128, D))
    m["mem"] = np.ascontiguousarray(inp["mem_prompt"][j])
    m["ck"] = np.ascontiguousarray(inp["cache_mem_k"][0, sl].reshape(16, 256, D))
    m["cv"] = np.ascontiguousarray(inp["cache_mem_v"][0, sl].reshape(16, 256, D))
    m["C0"] = np.ascontiguousarray(inp["state_mlstm_C"][0, sl])
    m["n0"] = np.ascontiguousarray(inp["state_mlstm_n"][0, sl])
    m["m0"] = np.ascontiguousarray(inp["state_mlstm_m"][0, sl])
    m["conv0"] = np.ascontiguousarray(inp["state_mlstm_conv"][0, sl].reshape(48, D))
    m["S0"] = np.ascontiguousarray(inp["state_hgrn_S"][0, sl])
    m["flag"] = np.full((128, 1), float(hh), np.float32)
    return m


def prep_shared(inp):
    s = {"consts": host_consts()}
    for k in ["w_in", "conv_w", "lb_logits", "w_bm", "w_bh", "w_out", "xa_wq", "xa_wk", "xa_wv", "xa_wo", "e_wg", "e_wu", "e_wd"]:
        a = inp[k]
        s[k] = np.ascontiguousarray(a[0] if k != "lb_logits" else a)
    for k in ["b_in", "mlstm_gn", "hgrn_gn", "ln1_g", "ln1_b", "ln2_g", "ln2_b", "ln3_g", "ln3_b"]:
        s[k] = np.ascontiguousarray(inp[k].reshape(1, -1))
    s["rw"] = np.ascontiguousarray(np.concatenate([inp["r1_w"][0], inp["r2_w"][0].transpose(1, 0, 2).reshape(D, 32)], axis=1))
    s["rb"] = np.ascontiguousarray(np.concatenate([inp["r1_b"][0].reshape(-1), inp["r2_b"][0].reshape(-1)]).reshape(1, 36))
    return s


def assemble(res):
    f = np.float32
    y_p = np.zeros((4, 2048, D), f)
    y_s = np.zeros((128, 8, D), f)
    mk = np.zeros((1, 4, 256, 4, 512), f)
    mv = np.zeros((1, 4, 256, 4, 512), f)
    C_p = np.zeros((1, 4, 4, 256, 256), f)
    n_p = np.zeros((1, 4, 4, 256), f)
    m_p = np.zeros((1, 4, 4), f)
    conv_p = np.zeros((1, 4, 3, D), f)
    S_p = np.zeros((1, 4, 8, 128, 128), f)
    C_s = np.zeros((1, 128, 4, 256, 256), f)
    n_s = np.zeros((1, 128, 4, 256), f)
    m_s = np.zeros((1, 128, 4), f)
    conv_s = np.zeros((1, 128, 3, D), f)
    S_s = np.zeros((1, 128, 8, 128, 128), f)
    for c, r in res.items():
        j, hh = c // 2, c % 2
        sl = slice(16 * c, 16 * c + 16)
        y_p[j, hh * TP:(hh + 1) * TP] = r["y_p"]
        y_s[sl] = r["y_s"].reshape(16, 8, D)
        if hh == 0:
            mk[0, j] = r["mk"].reshape(256, 4, 512)
            mv[0, j] = r["mv"].reshape(256, 4, 512)
        else:
            C_p[0, j] = r["Cp"]
            n_p[0, j] = r["np_"]
            m_p[0, j] = r["mp"].reshape(4)
            conv_p[0, j] = r["convp"]
            S_p[0, j] = r["Sp"]
        C_s[0, sl] = r["Cs"]
        n_s[0, sl] = r["ns"]
        m_s[0, sl] = r["ms"]
        conv_s[0, sl] = r["convs"]
        S_s[0, sl] = r["Ss"]
    return (y_p, y_s, mk, mv, C_p, n_p, m_p, conv_p, S_p, C_s, n_s, m_s, conv_s, S_s)


def kernel(**inputs):
    inp = {k: np.asarray(v) for k, v in inputs.items()}
    shared = prep_shared(inp)
    in_maps = [prep_core_inputs(inp, c, shared) for c in range(NCORES)]
    nc = build_program()
    res = run_bass_kernel_spmd(nc, in_maps, core_ids=list(range(NCORES)))
    return assemble({c: res.results[c] for c in range(NCORES)})
```

```python
import math
import numpy as np
import concourse.bass as bass
import concourse.mybir as mybir
from concourse.bass_utils import run_bass_kernel_spmd

F32 = mybir.dt.float32
BF16 = mybir.dt.bfloat16
ALU = mybir.AluOpType
AF = mybir.ActivationFunctionType
AX = mybir.AxisListType

D = 2048
NIN = 12296
NCORES = 8
TP = 1024
NTOK = 1152
NT = 9
ALPHA = 2.0 ** 0.25
EPS = 1e-5
LN16 = math.log(16.0)
NEGBIG = -1.0e30


class Buf:
    def __init__(self, name, t=None, parent=None):
        self.name = name
        self.t = t
        self.parent = parent
        self.kids = {}
        self.last_w = None
        self.readers = []

    def sub(self, key):
        if key not in self.kids:
            self.kids[key] = Buf(f"{self.name}.{key}", self.t, self)
        return self.kids[key]

    def __getitem__(self, idx):
        return self.t[idx]

    def related(self):
        out = [self]
        p = self.parent
        while p is not None:
            out.append(p)
            p = p.parent
        st = list(self.kids.values())
        while st:
            k = st.pop()
            out.append(k)
            st.extend(k.kids.values())
        return out


class Op:
    __slots__ = ("eng", "fn", "deps", "needs_inc", "count", "is_dma", "sem", "val")

    def __init__(self, eng, fn):
        self.eng = eng
        self.fn = fn
        self.deps = []
        self.needs_inc = False
        self.count = None
        self.is_dma = False
        self.sem = None
        self.val = None


class Sched:
    ENGS = ("pe", "act", "dve", "pool", "sp")

    def __init__(self, nc, n_dma_sems=16, same_engine_sync=True):
        self.nc = nc
        self.ops = {e: [] for e in self.ENGS}
        self.esem = {e: nc.alloc_semaphore(f"s_{e}") for e in self.ENGS}
        self.dsem = {q: [nc.alloc_semaphore(f"d_{q}{i}") for i in range(n_dma_sems)] for q in ("sp", "pool")}
        self.dcount = {q: 0 for q in self.dsem}
        self.dlast = {q: [None] * n_dma_sems for q in self.dsem}
        self.same_engine_sync = same_engine_sync
        self.all_dmas = []

    def _add_dep(self, op, dep):
        if dep is None or dep is op:
            return
        if not dep.is_dma and dep.eng == op.eng and not op.is_dma:
            if op.eng == "pe" or not self.same_engine_sync:
                return
        if dep in op.deps:
            return
        op.deps.append(dep)
        if not dep.is_dma:
            dep.needs_inc = True

    def _track(self, op, reads, writes):
        for b in reads:
            for r in b.related():
                self._add_dep(op, r.last_w)
            if getattr(b, "excl", False):
                for rd in b.readers:
                    if rd.eng != op.eng:
                        self._add_dep(op, rd)
        for b in writes:
            for r in b.related():
                self._add_dep(op, r.last_w)
                for rd in r.readers:
                    self._add_dep(op, rd)
        for b in reads:
            b.readers.append(op)
        for b in writes:
            b.last_w = op
            b.readers = []

    def op(self, eng, fn, reads=(), writes=()):
        o = Op(eng, fn)
        self._track(o, list(reads), list(writes))
        self.ops[eng].append(o)
        return o

    def dma(self, q, out, in_, reads=(), writes=(), **kw):
        o = Op(q, lambda e: e.dma_start(out=out, in_=in_, **kw))
        o.is_dma = True
        k = self.dcount[q]
        ns = len(self.dsem[q])
        slot = k % ns
        o.sem = self.dsem[q][slot]
        o.val = 16 * (k // ns + 1)
        prev = self.dlast[q][slot]
        if prev is not None:
            o.deps.append(prev)
        self.dlast[q][slot] = o
        self.dcount[q] = k + 1
        self._track(o, list(reads), list(writes))
        self.ops[q].append(o)
        self.all_dmas.append(o)
        return o

    def barrier(self):
        lasts = []
        for e in self.ENGS:
            for o in reversed(self.ops[e]):
                if not o.is_dma and o.fn is not None:
                    lasts.append(o)
                    break
        dm = [d for q in self.dlast for d in self.dlast[q] if d is not None]
        for e in self.ENGS:
            o = Op(e, None)
            for l in lasts:
                if l.eng != e:
                    o.deps.append(l)
                    l.needs_inc = True
            o.deps.extend(dm)
            self.ops[e].append(o)

    def finalize(self):
        fin = Op("sp", None)
        for q in self.dlast:
            for d in self.dlast[q]:
                if d is not None:
                    fin.deps.append(d)
        self.ops["sp"].append(fin)
        for e in self.ENGS:
            c = 0
            for o in self.ops[e]:
                if (not o.is_dma) and o.needs_inc:
                    c += 1
                    o.count = c
        nc = self.nc
        esem = self.esem
        with nc.Block() as block:
            regs = {"pe": block.tensor, "act": block.scalar, "dve": block.vector, "pool": block.gpsimd, "sp": block.sync}
            for e in self.ENGS:
                ops = self.ops[e]

                def body(h, ops=ops, e=e):
                    waited = {}
                    pending = None
                    for o in ops:
                        for d in o.deps:
                            if d.is_dma:
                                s, v = d.sem, d.val
                            else:
                                s, v = esem[d.eng], d.count
                            key = id(s)
                            if waited.get(key, 0) >= v:
                                continue
                            waited[key] = v
                            h.wait_ge(s, v)
                        if o.fn is None:
                            continue
                        ins = o.fn(h)
                        if o.is_dma:
                            ins.then_inc(o.sem, 16)
                        elif o.needs_inc:
                            ins.then_inc(esem[e], 1)

                regs[e](body)


def V(t, off, *dims, npart=128, p0=0):
    n = t.shape[1]
    return bass.AP(tensor=t, offset=p0 * n + off, ap=[[n, npart]] + [[s, c] for s, c in dims])


class Arena:
    def __init__(self, nc, base=16384, limit=196608):
        self.nc = nc
        self.off = base
        self.limit = limit
        self.n = 0

    def alloc(self, name, nelem, dt):
        sz = nelem * (4 if dt == F32 else 2)
        self.off = (self.off + 63) // 64 * 64
        assert self.off + sz <= self.limit, f"SBUF overflow at {name}: {self.off + sz}"
        self.n += 1
        t = self.nc.alloc_sbuf_tensor_at(f"{name}_{self.n}", [128, nelem], dt, offset=self.off)
        self.off += sz
        return Buf(name, t)

    def mark(self):
        return self.off

    def release(self, m):
        self.off = m


def host_consts():
    c = np.zeros((128, 1056 + NTOK), np.float32)
    idx = np.arange(128)
    c[:, 0:128] = np.eye(128)
    s = idx[:, None]
    t = idx[None, :]
    c[:, 128:256] = (s <= t)
    c[:, 256:384] = np.where(t <= s, 0.0, NEGBIG)
    c[:, 384:512] = (s == 127)
    same = (s // 8) == (t // 8)
    c[:, 512:640] = same & (s <= t)
    c[:, 640:768] = np.where(same & (t <= s), 0.0, NEGBIG)
    c[:, 768:896] = (s == 8 * (t // 8) + 7)
    c[:, 896:912] = (idx[:, None] // 8) == np.arange(16)[None, :]
    c[:, 912:928] = idx[:, None] == 8 * np.arange(16)[None, :]
    c[:, 928:1056] = 1.0
    rm = np.ones(NTOK, np.float32)
    rm[0:1024:128] = 0.0
    rm[1024:NTOK:8] = 0.0
    c[:, 1056:] = rm[None, :]
    return c


C_ID, C_MP, C_NEGP, C_LASTP, C_MS, C_NEGS, C_LASTS, C_SEQM, C_SEQ1, C_ONES, C_RM = 0, 128, 256, 384, 512, 640, 768, 896, 912, 928, 1056


def build_program(stage=99, same_engine_sync=True, cut=99):
    nc = bass.Bass("TRN2", target_bir_lowering=False)

    def din(name, shape):
        return nc.dram_tensor(name, list(shape), F32, kind="ExternalInput").ap()

    def dout(name, shape):
        return nc.dram_tensor(name, list(shape), F32, kind="ExternalOutput").ap()

    I = {}
    for name, shape in [
        ("xp", (TP, D)), ("xpre", (TP, D)), ("xs", (128, D)), ("mem", (256, D)),
        ("ck", (16, 256, D)), ("cv", (16, 256, D)), ("C0", (16, 4, 256, 256)), ("n0", (16, 4, 256)), ("m0", (16, 4)),
        ("conv0", (48, D)), ("S0", (16, 8, 128, 128)), ("flag", (128, 1)), ("consts", (128, 1056 + NTOK)),
        ("w_in", (D, NIN)), ("b_in", (1, NIN)), ("conv_w", (4, D)), ("mlstm_gn", (1, 1024)), ("lb_logits", (2, 1024)),
        ("hgrn_gn", (1, 1024)), ("w_bm", (1024, D)), ("w_bh", (1024, D)), ("w_out", (D, D)),
        ("ln1_g", (1, D)), ("ln1_b", (1, D)), ("xa_wq", (D, D)), ("xa_wk", (D, D)), ("xa_wv", (D, D)), ("xa_wo", (D, D)),
        ("ln2_g", (1, D)), ("ln2_b", (1, D)), ("rw", (D, 36)), ("rb", (1, 36)),
        ("e_wg", (32, D, 512)), ("e_wu", (32, D, 512)), ("e_wd", (32, 512, D)), ("ln3_g", (1, D)), ("ln3_b", (1, D)),
    ]:
        I[name] = din(name, shape)
    O = {}
    for name, shape in [
        ("y_p", (TP, D)), ("y_s", (128, D)), ("mk", (256, D)), ("mv", (256, D)),
        ("Cp", (4, 256, 256)), ("np_", (4, 256)), ("mp", (1, 4)), ("convp", (3, D)), ("Sp", (8, 128, 128)),
        ("Cs", (16, 4, 256, 256)), ("ns", (16, 4, 256)), ("ms", (16, 4)), ("convs", (16, 3, D)), ("Ss", (16, 8, 128, 128)),
    ]:
        O[name] = dout(name, shape)

    K = Sched(nc, same_engine_sync=same_engine_sync)
    A = Arena(nc)
    psum = [Buf(f"ps{i}", nc.alloc_psum_tensor(f"ps{i}", [128, 512], F32)) for i in range(8)]
    for b_ in psum:
        b_.excl = True
    pstate = {"i": 0}

    def PS():
        b = psum[pstate["i"] % 6]
        pstate["i"] += 1
        return b


    cst = A.alloc("cst", 1056 + NTOK, F32)
    K.dma("sp", cst[:, :], I["consts"], writes=[cst])
    identB = A.alloc("identB", 128, BF16)
    K.op("dve", lambda e: e.tensor_copy(out=identB[:, :], in_=cst[:, C_ID:C_ID + 128]), reads=[cst], writes=[identB])
    flag = A.alloc("flag", 1, F32)
    K.dma("sp", flag[:, :], I["flag"], writes=[flag])

    def cs(c0, n=128):
        return cst[:, c0:c0 + n]

    ident = cs(C_ID)
    ones = cs(C_ONES)

    NSLOT = 4
    wslots = [A.alloc(f"wslot{i}", 4096, BF16) for i in range(NSLOT)]
    wstate = {"i": 0}

    def wload(parts):
        sl = wslots[wstate["i"] % NSLOT]
        wstate["i"] += 1
        kc = parts[0].shape[0] // 128
        tot = sum(p.shape[1] for p in parts)
        assert kc * tot <= 4096
        view = sl[:, 0:kc * tot].rearrange("p (k c) -> p k c", k=kc)
        c0 = 0
        for p in parts:
            n = p.shape[1]
            K.dma("pool", view[:, :, c0:c0 + n], p.rearrange("(k p) c -> p k c", p=128), writes=[sl])
            c0 += n
        return sl, view

    deferred = []
    w_in = I["w_in"]
    b_in = I["b_in"]

    def fm_bias(name, c0, nblk):
        b = A.alloc(name, nblk, F32)
        deferred.append(lambda j=locals().get('j'), r=locals().get('r'), b=locals().get('b'), c0=locals().get('c0'), nblk=locals().get('nblk'): K.dma("sp", b[:, :], b_in[0, c0:c0 + nblk * 128].rearrange("(b p) -> p b", p=128), writes=[b]))
        return b

    bqk = fm_bias("bqk", 0, 16)
    bqh = fm_bias("bqh", 4104, 8)
    bfh = fm_bias("bfh", 5128, 8)
    bgm = fm_bias("bgm", 8200, 16)
    bgh = fm_bias("bgh", 10248, 16)
    cw = A.alloc("cw", 64, F32)
    for j in range(4):
        deferred.append(lambda j=locals().get('j'), r=locals().get('r'), b=locals().get('b'), c0=locals().get('c0'), nblk=locals().get('nblk'): K.dma("sp", V(cw.t, j, (4, 16)), I["conv_w"][j, :].rearrange("(c p) -> p c", p=128), writes=[cw]))
    lbt = A.alloc("lbt", 16, F32)
    for r in range(2):
        deferred.append(lambda j=locals().get('j'), r=locals().get('r'), b=locals().get('b'), c0=locals().get('c0'), nblk=locals().get('nblk'): K.dma("sp", lbt[:, r * 8:(r + 1) * 8], I["lb_logits"][r, :].rearrange("(h p) -> p h", p=128), writes=[lbt]))
    lb = A.alloc("lb", 8, F32)
    oml = A.alloc("oml", 8, F32)
    deferred.append(lambda j=locals().get('j'), r=locals().get('r'), b=locals().get('b'), c0=locals().get('c0'), nblk=locals().get('nblk'): K.op("dve", lambda e: e.tensor_tensor(out=lb[:, :], in0=lbt[:, 0:8], in1=lbt[:, 8:16], op=ALU.subtract), reads=[lbt], writes=[lb]))
    deferred.append(lambda j=locals().get('j'), r=locals().get('r'), b=locals().get('b'), c0=locals().get('c0'), nblk=locals().get('nblk'): K.op("act", lambda e: e.activation(out=lb[:, :], in_=lb[:, :], func=AF.Sigmoid), reads=[lb], writes=[lb]))
    deferred.append(lambda j=locals().get('j'), r=locals().get('r'), b=locals().get('b'), c0=locals().get('c0'), nblk=locals().get('nblk'): K.op("dve", lambda e: e.tensor_scalar(out=oml[:, :], in0=lb[:, :], scalar1=-1.0, scalar2=1.0, op0=ALU.mult, op1=ALU.add), reads=[lb], writes=[oml]))
    bgate = A.alloc("bgate", 8, F32)
    deferred.append(lambda j=locals().get('j'), r=locals().get('r'), b=locals().get('b'), c0=locals().get('c0'), nblk=locals().get('nblk'): K.dma("sp", bgate[:, :], b_in[0:1, 4096:4104].partition_broadcast(128), writes=[bgate]))
    gnT = A.alloc("gnT", 16, F32)
    deferred.append(lambda j=locals().get('j'), r=locals().get('r'), b=locals().get('b'), c0=locals().get('c0'), nblk=locals().get('nblk'): K.dma("sp", gnT[:, 0:8], I["mlstm_gn"][0, :].rearrange("(k p) -> p k", p=128), writes=[gnT]))
    deferred.append(lambda j=locals().get('j'), r=locals().get('r'), b=locals().get('b'), c0=locals().get('c0'), nblk=locals().get('nblk'): K.dma("sp", gnT[:, 8:16], I["hgrn_gn"][0, :].rearrange("(k p) -> p k", p=128), writes=[gnT]))
    hm_scr = nc.dram_tensor("hm_scr", [NTOK, 1024], BF16, kind="Internal").ap()
    og_scr = nc.dram_tensor("og_scr", [NTOK, 1024], BF16, kind="Internal").ap()
    hm_scr_b = Buf("hm_scr")
    og_scr_b = Buf("og_scr")

    xT = A.alloc("xT", 16 * NTOK, BF16)
    xtile = []
    xst = {"i": 0}

    def xTv(ntok):
        return xT[:, 0:16 * ntok].rearrange("p (k t) -> p k t", k=16)

    def transpose_tile_f32(src_buf, src_ap_fn, dstT_buf, dst_view, tok0):
        for g in range(4):
            ps = PS()
            for j in range(4):
                kc = g * 4 + j
                K.op("pe", lambda e, ps=ps, j=j, kc=kc: e.transpose(ps[:, j * 128:(j + 1) * 128], src_ap_fn(kc), ident),
                     reads=[src_buf, cst], writes=[ps])
            eng = "act" if g % 2 == 0 else "dve"
            outv = dst_view[:, g * 4:(g + 1) * 4, tok0:tok0 + 128]
            inv = ps[:, :].rearrange("p (j t) -> p j t", j=4)
            if eng == "act":
                K.op("act", lambda e, outv=outv, inv=inv: e.activation(out=outv, in_=inv, func=AF.Copy), reads=[ps], writes=[dstT_buf])
            else:
                K.op("dve", lambda e, outv=outv, inv=inv: e.tensor_copy(out=outv, in_=inv), reads=[ps], writes=[dstT_buf])

    def build_xT(srcs, ntok):
        view = xTv(ntok)
        t = 0
        for src, ntile in srcs:
            for i in range(ntile):
                xb = xtile[xst["i"] % 2]
                xst["i"] += 1
                K.dma("sp", xb[:, :], src[i * 128:(i + 1) * 128, :], writes=[xb])
                transpose_tile_f32(xb, lambda kc, xb=xb: xb[:, kc * 128:(kc + 1) * 128], xT, view, t * 128)
                t += 1
        return view

    def mm_group(ps_ap, ps_buf, pairs, reads):
        n = len(pairs)
        for i, (l, r) in enumerate(pairs):
            K.op("pe", lambda e, l=l, r=r, i=i: e.matmul(ps_ap, l, r, start=(i == 0), stop=(i == n - 1)), reads=reads, writes=[ps_buf])

    m_mix = A.mark()

    Cst = [A.alloc(f"Cst{h}", 2 * 257, F32) for h in range(4)]
    Sst = [A.alloc(f"Sst{h}", 128, F32) for h in range(8)]
    mstate = A.alloc("mstate", 4, F32)
    xhalo = A.alloc("xhalo", 48, BF16)
    for h in range(4):
        K.op("pool", lambda e, h=h: e.memset(Cst[h][:, :], 0.0), writes=[Cst[h]])
    for h in range(8):
        K.op("pool", lambda e, h=h: e.memset(Sst[h][:, :], 0.0), writes=[Sst[h]])
    K.op("pool", lambda e: e.memset(mstate[:, :], 0.0), writes=[mstate])


    sm = A.alloc("gate_small", 160, F32)
    Dg = A.alloc("Dg", 512, F32)
    Dm = A.alloc("Dm", 512, F32)

    def smv(i, n=4):
        return sm[:, i:i + n]

    def gate_tile(gt_ap, Mc, NEGc, LASTc, mprev_ap, mprev_buf, out, need_P):
        gi = gt_ap[:, 0:4]
        gf = gt_ap[:, 4:8]
        gb = out["gbuf"]
        e1, nlf, g, Fv, mx, iw, mt, negm, d1, L8, d2, t1 = (smv(0), smv(4), smv(8), smv(12), smv(16), smv(20), smv(24), smv(28), smv(32), smv(36, 8), smv(44), smv(48))
        mf = smv(52, 8)
        K.op("act", lambda e: e.activation(out=e1, in_=gf, func=AF.Exp, scale=-1.0), reads=[gb], writes=[sm])
        K.op("act", lambda e: e.activation(out=nlf, in_=e1, func=AF.Ln, bias=1.0), reads=[sm], writes=[sm])
        ps = PS()
        K.op("pe", lambda e, ps=ps: e.matmul(ps[:, 0:4], cs(Mc), nlf, start=True, stop=True), reads=[sm, cst], writes=[ps])
        K.op("dve", lambda e, ps=ps: e.tensor_tensor(out=g, in0=gi, in1=ps[:, 0:4], op=ALU.add), reads=[ps, gb], writes=[sm])
        K.op("dve", lambda e, ps=ps: e.tensor_scalar(out=Fv, in0=ps[:, 0:4], scalar1=-1.0, scalar2=None, op0=ALU.mult), reads=[ps], writes=[sm])
        K.op("dve", lambda e: e.tensor_tensor(out=iw, in0=Fv, in1=mprev_ap, op=ALU.add), reads=[sm, mprev_buf], writes=[sm])
        for h in range(4):
            K.op("dve", lambda e, h=h: e.tensor_scalar(out=Dg[:, h * 128:(h + 1) * 128], in0=ident, scalar1=g[:, h:h + 1], scalar2=None, op0=ALU.mult),
                 reads=[sm, cst], writes=[Dg])
        ps2 = PS()
        K.op("pe", lambda e, ps2=ps2: e.matmul(ps2[:, :], ones, Dg[:, :], start=True, stop=True), reads=[Dg, cst], writes=[ps2])
        for h in range(4):
            K.op("dve", lambda e, h=h, ps2=ps2: e.scalar_tensor_tensor(out=Dm[:, h * 128:(h + 1) * 128], in0=ps2[:, h * 128:(h + 1) * 128], scalar=Fv[:, h:h + 1],
                                                                 in1=cs(NEGc), op0=ALU.add, op1=ALU.add), reads=[ps2, sm, cst], writes=[Dm])
        K.op("dve", lambda e: e.tensor_reduce(out=mx, in_=Dm[:, :].rearrange("p (h s) -> p h s", h=4), axis=AX.X, op=ALU.max), reads=[Dm], writes=[sm])
        K.op("dve", lambda e: e.tensor_tensor(out=mf[:, 0:4], in0=iw, in1=mx, op=ALU.max), reads=[sm], writes=[sm])
        K.op("dve", lambda e: e.tensor_copy(out=mf[:, 4:8], in_=Fv), reads=[sm], writes=[sm])
        mtv = mf[:, 0:4]
        K.op("dve", lambda e: e.tensor_scalar(out=negm, in0=mtv, scalar1=-1.0, scalar2=None, op0=ALU.mult), reads=[sm], writes=[sm])
        if need_P:
            for h in range(4):
                K.op("act", lambda e, h=h: e.activation(out=out["P"][:, h, :], in_=Dm[:, h * 128:(h + 1) * 128], func=AF.Exp, bias=negm[:, h:h + 1]),
                     reads=[Dm, sm], writes=[out["Pbuf"]])
            K.op("dve", lambda e: e.tensor_tensor(out=d1, in0=iw, in1=mtv, op=ALU.subtract), reads=[sm], writes=[sm])
            K.op("act", lambda e: e.activation(out=out["a0"], in_=d1, func=AF.Exp), reads=[sm], writes=[out["gsb"]])
            K.op("act", lambda e: e.activation(out=out["floor"], in_=negm, func=AF.Exp, bias=LN16), reads=[sm], writes=[out["gsb"]])
        ps3 = PS()
        K.op("pe", lambda e, ps3=ps3: e.matmul(ps3[:, 0:8], cs(LASTc), mf, start=True, stop=True), reads=[sm, cst], writes=[ps3])
        K.op("act", lambda e, ps3=ps3: e.activation(out=L8, in_=ps3[:, 0:8], func=AF.Copy), reads=[ps3], writes=[sm])
        K.op("dve", lambda e: e.tensor_tensor(out=d2, in0=L8[:, 4:8], in1=L8[:, 0:4], op=ALU.subtract), reads=[sm], writes=[sm])
        K.op("dve", lambda e: e.tensor_tensor(out=t1, in0=g, in1=d2, op=ALU.add), reads=[sm], writes=[sm])
        K.op("act", lambda e: e.activation(out=out["wL"], in_=t1, func=AF.Exp), reads=[sm], writes=[out["gsb"]])
        K.op("dve", lambda e: e.tensor_tensor(out=d1, in0=d2, in1=mprev_ap, op=ALU.add), reads=[sm, mprev_buf], writes=[sm])
        K.op("act", lambda e: e.activation(out=out["decay"], in_=d1, func=AF.Exp), reads=[sm], writes=[out["gsb"]])
        K.op("dve", lambda e: e.tensor_copy(out=out["mlast"], in_=L8[:, 0:4]), reads=[sm], writes=[out["mlast_buf"]])

    def gates_proj(view, ntile, gt):
        sl, wv = wload([w_in[:, 4096:4104]])
        for t in range(ntile):
            ps = PS()
            mm_group(ps[:, 0:8], ps, [(view[:, kc, t * 128:(t + 1) * 128], wv[:, kc, :]) for kc in range(16)], [xT, sl])
            K.op("dve", lambda e, ps=ps, t=t: e.tensor_tensor(out=gt[:, t * 8:(t + 1) * 8], in0=ps[:, 0:8], in1=bgate[:, :], op=ALU.add),
                 reads=[ps, bgate], writes=[gt])

    brow = [A.alloc(f"brow{i}", 256, F32) for i in range(2)]
    browst = {"i": 0}
    st6 = A.alloc("st6", 16, F32)
    hmo = [A.alloc(f"hmo{i}", 256, BF16) for i in range(2)]
    hmost = {"i": 0}
    convo = A.alloc("convo", 48, F32)
    convso = A.alloc("convso", 16 * 48, F32)
    c0T = A.alloc("c0T", 16 * 48, F32)
    cacc = A.alloc("cacc", NTOK, F32)
    m_head = A.mark()
    xtile.append(A.alloc("xtile0", D, F32))
    xtile.append(xtile[0])
    cvb = A.alloc("cvb", 3 + TP, F32)
    cvs = A.alloc("cvs", 16 * 11, F32)
    qT = A.alloc("qT", 2 * NTOK, BF16)
    kT = A.alloc("kT", 2 * NTOK, BF16)
    vaug = A.alloc("vaug", NT * 257, BF16)
    sigo = A.alloc("sigo", NT * 256, BF16)
    gt = A.alloc("gt", NT * 8, F32)
    Pm = A.alloc("Pm", NT * 512, BF16)
    gsb = A.alloc("gsb", NT * 16, F32)
    mprev_s = A.alloc("mprev_s", 4, F32)
    mprev_tok = A.alloc("mprev_tok", 4, F32)
    mlast_smp = A.alloc("mlast_smp", 4, F32)
    dbc = A.alloc("dbc", 64, F32)
    rhsd = A.alloc("rhsd", 64, F32)
    Cb = A.alloc("Cb", 2 * 257, BF16)
    Sc = A.alloc("Sc", 128, BF16)
    ScT = A.alloc("ScT", 128, BF16)
    ktok = A.alloc("ktok", 256, BF16)
    wvt = A.alloc("wvt", 257, BF16)
    tmpi = A.alloc("tmpi", 257, F32)
    num = A.alloc("num", 257, F32)
    hm = A.alloc("hm", 256, F32)
    tmpo = hm
    C0f = [A.alloc(f"C0f{i}", 2 * 257, F32) for i in range(2)]
    C0b = [A.alloc(f"C0b{i}", 2 * 257, BF16) for i in range(2)]
    Cnew = [A.alloc(f"Cnew{i}", 2 * 257, F32) for i in range(2)]
    wvz = [A.alloc(f"wvz{i}", 257, BF16) for i in range(2)]
    Qz = A.alloc("Qz", 2 * 16 * 128, BF16)
    m_end_mlstm = A.mark()

    def load_brow(c0, n):
        b = brow[browst["i"] % 2]
        browst["i"] += 1
        K.dma("sp", b[:, 0:n], b_in[0:1, c0:c0 + n].partition_broadcast(128), writes=[b])
        return b

    def gates_out(t):
        o = t * 16
        return dict(a0=gsb[:, o:o + 4], floor=gsb[:, o + 4:o + 8], wL=gsb[:, o + 8:o + 12], decay=gsb[:, o + 12:o + 16], gsb=gsb.sub(t),
                    P=Pm[:, t * 512:(t + 1) * 512].rearrange("p (h s) -> p h s", h=4), Pbuf=Pm.sub(t), gbuf=gt)

    def conv_silu(cvbuf, T, cb, dst_ap, dst_buf, seg=None):
        if seg is None:
            src = lambda j: cvbuf[:, j:j + T]
            acc = cacc[:, 0:T]
        else:
            nb, L = seg
            src = lambda j: V(cvbuf.t, j, (3 + L, nb), (1, L))
            acc = cacc[:, 0:nb * L].rearrange("p (b l) -> p b l", b=nb)
        K.op("dve", lambda e: e.tensor_scalar(out=acc, in0=src(0), scalar1=cw[:, cb * 4:cb * 4 + 1], scalar2=None, op0=ALU.mult), reads=[cvbuf, cw], writes=[cacc])
        for j in range(1, 4):
            K.op("dve", lambda e, j=j: e.scalar_tensor_tensor(out=acc, in0=src(j), scalar=cw[:, cb * 4 + j:cb * 4 + j + 1], in1=acc, op0=ALU.mult, op1=ALU.add),
                 reads=[cvbuf, cw, cacc], writes=[cacc])
        K.op("act", lambda e: e.activation(out=dst_ap, in_=acc, func=AF.Silu), reads=[cacc], writes=[dst_buf])

    def make_ktok(kT_ap_fn):
        ps = PS()
        psb = ps[:, 0:128].bitcast(BF16)
        for dc in range(2):
            K.op("pe", lambda e, dc=dc, psb=psb: e.transpose(psb[:, dc * 128:(dc + 1) * 128], kT_ap_fn(dc), identB[:, :]), reads=[kT, identB], writes=[ps])
        K.op("act", lambda e, psb=psb: e.activation(out=ktok[:, :], in_=psb, func=AF.Copy), reads=[ps], writes=[ktok])

    def mlstm_state_update(h, t, wL_ap, decay_ap):
        make_ktok(lambda dc: kT[:, dc * NTOK + t * 128:dc * NTOK + (t + 1) * 128])
        K.op("dve", lambda e: e.tensor_scalar(out=wvt[:, :], in0=vaug[:, t * 257:(t + 1) * 257], scalar1=wL_ap, scalar2=None, op0=ALU.mult), reads=[vaug, gsb], writes=[wvt])
        for dc in range(2):
            pc = PS()
            K.op("pe", lambda e, pc=pc, dc=dc: e.matmul(pc[:, 0:257], ktok[:, dc * 128:(dc + 1) * 128], wvt[:, :], start=True, stop=True), reads=[ktok, wvt], writes=[pc])
            cv_ = Cst[h][:, dc * 257:(dc + 1) * 257]
            K.op("dve", lambda e, pc=pc, cv_=cv_: e.scalar_tensor_tensor(out=cv_, in0=cv_, scalar=decay_ap, in1=pc[:, 0:257], op0=ALU.mult, op1=ALU.add),
                 reads=[pc, gsb], writes=[Cst[h]])

    def head_v_proj(view, ntile, h, slv, wv_):
        bv = load_brow(2048 + h * 256, 256)
        K.op("pool", lambda e: e.memset(V(vaug.t, 256, (257, NT), (1, 1)), 1.0), writes=[vaug])
        for t in range(ntile):
            ps = PS()
            mm_group(ps[:, 0:256], ps, [(view[:, kc, t * 128:(t + 1) * 128], wv_[:, kc, :]) for kc in range(16)], [slv, xT])
            K.op("dve", lambda e, ps=ps, t=t, bv=bv: e.tensor_tensor(out=vaug[:, t * 257:t * 257 + 256], in0=ps[:, 0:256], in1=bv[:, 0:256], op=ALU.add),
                 reads=[ps, bv], writes=[vaug])

    def phase_A_mlstm():
        viewA = xTv(TP)
        gates_proj(viewA, 8, gt)
        for t in range(8):
            og = gates_out(t)
            og["mlast"] = mstate[:, :]
            og["mlast_buf"] = mstate
            K.op("dve", lambda e: e.tensor_copy(out=mprev_s[:, :], in_=mstate[:, :]), reads=[mstate], writes=[mprev_s])
            gate_tile(gt[:, t * 8:(t + 1) * 8], C_MP, C_NEGP, C_LASTP, mprev_s[:, :], mprev_s, og, need_P=False)
        K.op("pool", lambda e: e.memset(cvb[:, 0:3], 0.0), writes=[cvb])
        for h in range(4):
            slk, wk = wload([w_in[:, 1024 + h * 256:1024 + (h + 1) * 256]])
            slv, wv_ = wload([w_in[:, 2048 + h * 256:2048 + (h + 1) * 256]])
            for cbk in range(2):
                cb = 8 + h * 2 + cbk
                for tb in range(2):
                    ps = PS()
                    mm_group(ps[:, :], ps, [(wk[:, kc, cbk * 128:(cbk + 1) * 128], viewA[:, kc, tb * 512:(tb + 1) * 512]) for kc in range(16)], [slk, xT])
                    K.op("act", lambda e, ps=ps, tb=tb, cb=cb: e.activation(out=cvb[:, 3 + tb * 512:3 + (tb + 1) * 512], in_=ps[:, :], func=AF.Identity, bias=bqk[:, cb:cb + 1]),
                         reads=[ps, bqk], writes=[cvb])
                conv_silu(cvb, TP, cb, kT[:, cbk * NTOK:cbk * NTOK + TP], kT)
            head_v_proj(viewA, 8, h, slv, wv_)
            for t in range(8):
                og = gates_out(t)
                mlstm_state_update(h, t, og["wL"][:, h:h + 1], og["decay"][:, h:h + 1])
            K.op("dve", lambda e, h=h: e.tensor_scalar(out=Cst[h][:, :], in0=Cst[h][:, :], scalar1=flag[:, 0:1], scalar2=None, op0=ALU.mult), reads=[flag], writes=[Cst[h]])
        K.op("dve", lambda e: e.tensor_scalar(out=mstate[:, :], in0=mstate[:, :], scalar1=flag[:, 0:1], scalar2=None, op0=ALU.mult), reads=[flag], writes=[mstate])

    def mlstm_tile_full(h, t, sample):
        og = gates_out(t)
        qs = lambda dc: qT[:, dc * NTOK + t * 128:dc * NTOK + (t + 1) * 128]
        ks = lambda dc: kT[:, dc * NTOK + t * 128:dc * NTOK + (t + 1) * 128]
        ps = PS()
        mm_group(ps[:, 0:128], ps, [(qs(dc), ks(dc)) for dc in range(2)], [qT, kT])
        K.op("dve", lambda e, ps=ps: e.tensor_tensor(out=Sc[:, :], in0=ps[:, 0:128], in1=og["P"][:, h, :], op=ALU.mult), reads=[ps, og["Pbuf"]], writes=[Sc])
        pt = PS()
        ptb = pt[:, 0:64].bitcast(BF16)
        K.op("pe", lambda e, ptb=ptb: e.transpose(ptb, Sc[:, :], identB[:, :]), reads=[Sc, identB], writes=[pt])
        K.op("act", lambda e, ptb=ptb: e.activation(out=ScT[:, :], in_=ptb, func=AF.Copy), reads=[pt], writes=[ScT])
        pa = psum[7] if sample else PS()
        K.op("pe", lambda e, pa=pa: e.matmul(pa[:, 0:257], ScT[:, :], vaug[:, t * 257:(t + 1) * 257], start=True, stop=True), reads=[ScT, vaug], writes=[pa])
        pb = psum[6] if sample else PS()
        if not sample:
            mm_group(pb[:, 0:257], pb, [(qs(dc), Cb[:, dc * 257:(dc + 1) * 257]) for dc in range(2)], [qT, Cb])
        else:
            for dc in range(2):
                K.op("pool", lambda e, dc=dc: e.tensor_copy(out=V(Qz.t, dc * 2048, (136, 16), (1, 8)),
                                                           in_=qT[:, dc * NTOK + TP:dc * NTOK + NTOK].rearrange("p (b l) -> p b l", b=16)), reads=[qT], writes=[Qz])
            make_ktok(ks)
            def load_c0(b):
                cf_ = C0f[b % 2]
                K.dma("sp", V(cf_.t, 0, (257, 2), (1, 256)), I["C0"][b, h].rearrange("(dc p) v -> p dc v", p=128), writes=[cf_])
                K.dma("sp", V(cf_.t, 256, (257, 2)), I["n0"][b, h].rearrange("(dc p) -> p dc", p=128), writes=[cf_])

            load_c0(0)
            for b in range(16):
                cf = C0f[b % 2]
                cbf = C0b[b % 2]
                cn = Cnew[b % 2]
                wz = wvz[b % 2]
                if b + 1 < 16:
                    load_c0(b + 1)
                K.op("act", lambda e, cf=cf, cbf=cbf: e.activation(out=cbf[:, :], in_=cf[:, :], func=AF.Copy), reads=[cf], writes=[cbf])
                for dc in range(2):
                    first = (b == 0 and dc == 0)
                    last = (b == 15 and dc == 1)
                    K.op("pe", lambda e, pb=pb, dc=dc, b=b, cbf=cbf, first=first, last=last: e.matmul(
                        pb[:, 0:257], Qz[:, dc * 2048 + b * 128:dc * 2048 + (b + 1) * 128], cbf[:, dc * 257:(dc + 1) * 257], start=first, stop=last),
                        reads=[Qz, cbf], writes=[pb])
                K.op("dve", lambda e, wz=wz, b=b: e.tensor_scalar(out=wz[:, :], in0=vaug[:, t * 257:(t + 1) * 257], scalar1=og["wL"][:, h:h + 1],
                                                                    scalar2=cst[:, C_SEQM + b:C_SEQM + b + 1], op0=ALU.mult, op1=ALU.mult), reads=[vaug, gsb, cst], writes=[wz])
                for dc in range(2):
                    pc = PS()
                    K.op("pe", lambda e, pc=pc, dc=dc, wz=wz: e.matmul(pc[:, 0:257], ktok[:, dc * 128:(dc + 1) * 128], wz[:, :], start=True, stop=True), reads=[ktok, wz], writes=[pc])
                    K.op("dve", lambda e, pc=pc, dc=dc, cf=cf, cn=cn, b=b: e.scalar_tensor_tensor(
                        out=cn[:, dc * 257:(dc + 1) * 257], in0=cf[:, dc * 257:(dc + 1) * 257], scalar=dbc[:, h * 16 + b:h * 16 + b + 1], in1=pc[:, 0:257],
                        op0=ALU.mult, op1=ALU.add), reads=[pc, cf, dbc], writes=[cn])
                K.dma("sp", O["Cs"][b, h].rearrange("(dc p) v -> p dc v", p=128), V(cn.t, 0, (257, 2), (1, 256)), reads=[cn])
                K.dma("sp", O["ns"][b, h].rearrange("(dc p) -> p dc", p=128), V(cn.t, 256, (257, 2)), reads=[cn])
        K.op("act", lambda e, pb=pb: e.activation(out=tmpi[:, :], in_=pb[:, 0:257], func=AF.Copy, scale=og["a0"][:, h:h + 1]), reads=[pb, gsb], writes=[tmpi])
        K.op("dve", lambda e, pa=pa: e.tensor_tensor(out=num[:, :], in0=tmpi[:, :], in1=pa[:, 0:257], op=ALU.add), reads=[pa, tmpi], writes=[num])
        K.op("dve", lambda e: e.tensor_scalar(out=st6[:, 12:13], in0=num[:, 256:257], scalar1=-1.0, scalar2=None, op0=ALU.mult), reads=[num], writes=[st6])
        K.op("dve", lambda e: e.scalar_tensor_tensor(out=st6[:, 0:1], in0=num[:, 256:257], scalar=og["floor"][:, h:h + 1], in1=st6[:, 12:13], op0=ALU.max, op1=ALU.max),
             reads=[num, gsb, st6], writes=[st6])
        K.op("dve", lambda e: e.reciprocal(out=st6[:, 1:2], in_=st6[:, 0:1]), reads=[st6], writes=[st6])
        K.op("dve", lambda e: e.scalar_tensor_tensor(out=hm[:, :], in0=num[:, 0:256], scalar=st6[:, 1:2], in1=sigo[:, t * 256:(t + 1) * 256], op0=ALU.mult, op1=ALU.mult),
             reads=[num, st6, sigo], writes=[hm])
        K.op("dve", lambda e: e.bn_stats(out=st6[:, 2:8], in_=hm[:, :]), reads=[hm], writes=[st6])
        K.op("dve", lambda e: e.bn_aggr(out=st6[:, 8:10], in_=st6[:, 2:8]), reads=[st6], writes=[st6])
        K.op("act", lambda e: e.activation(out=st6[:, 10:11], in_=st6[:, 9:10], func=AF.Sqrt, bias=EPS), reads=[st6], writes=[st6])
        K.op("dve", lambda e: e.reciprocal(out=st6[:, 11:12], in_=st6[:, 10:11]), reads=[st6], writes=[st6])
        ho = hmo[hmost["i"] % 2]
        hmost["i"] += 1
        K.op("dve", lambda e, ho=ho: e.tensor_scalar(out=ho[:, :], in0=hm[:, :], scalar1=st6[:, 8:9], scalar2=st6[:, 11:12], op0=ALU.subtract, op1=ALU.mult),
             reads=[hm, st6], writes=[ho])
        K.dma("sp", hm_scr[t * 128:(t + 1) * 128, h * 256:(h + 1) * 256], ho[:, :], reads=[ho], writes=[hm_scr_b])
        if not sample:
            mlstm_state_update(h, t, og["wL"][:, h:h + 1], og["decay"][:, h:h + 1])
            K.op("act", lambda e: e.activation(out=Cb[:, :], in_=Cst[h][:, :], func=AF.Copy), reads=[Cst[h]], writes=[Cb])

    def phase_B_mlstm():
        viewB = xTv(NTOK)
        cvin = xtile[0]
        K.dma("sp", cvin[0:48, :], I["conv0"], writes=[cvin])
        for g in range(4):
            ps = PS()
            for j in range(4):
                cb = g * 4 + j
                K.op("pe", lambda e, ps=ps, j=j, cb=cb: e.transpose(ps[:, j * 48:(j + 1) * 48], cvin[0:48, cb * 128:(cb + 1) * 128], cst[0:48, C_ID:C_ID + 48]),
                     reads=[cvin, cst], writes=[ps])
            K.op("act", lambda e, ps=ps, g=g: e.activation(out=c0T[:, g * 192:(g + 1) * 192], in_=ps[:, 0:192], func=AF.Copy), reads=[ps], writes=[c0T])
        if cut <= 1:
            return
        gates_proj(viewB, 9, gt)
        for t in range(8):
            og = gates_out(t)
            og["mlast"] = mstate[:, :]
            og["mlast_buf"] = mstate
            K.op("dve", lambda e: e.tensor_copy(out=mprev_s[:, :], in_=mstate[:, :]), reads=[mstate], writes=[mprev_s])
            gate_tile(gt[:, t * 8:(t + 1) * 8], C_MP, C_NEGP, C_LASTP, mprev_s[:, :], mprev_s, og, need_P=True)
        K.dma("sp", O["mp"], mstate[0:1, :], reads=[mstate])
        if cut <= 2:
            return
        K.dma("sp", mprev_tok[:, :], bass.AP(tensor=I["m0"].tensor, offset=0, ap=[[4, 16], [0, 8], [1, 4]]), writes=[mprev_tok])
        og = gates_out(8)
        og["mlast"] = mlast_smp[:, :]
        og["mlast_buf"] = mlast_smp
        gate_tile(gt[:, 64:72], C_MS, C_NEGS, C_LASTS, mprev_tok[:, :], mprev_tok, og, need_P=True)
        K.dma("sp", O["ms"], bass.AP(tensor=mlast_smp.t, offset=7 * 4, ap=[[32, 16], [1, 4]]), reads=[mlast_smp])
        for h in range(4):
            K.op("dve", lambda e, h=h: e.tensor_scalar(out=rhsd[:, h * 16:(h + 1) * 16], in0=cst[:, C_SEQ1:C_SEQ1 + 16], scalar1=og["decay"][:, h:h + 1], scalar2=None, op0=ALU.mult),
                 reads=[cst, gsb], writes=[rhsd])
        ps = PS()
        K.op("pe", lambda e, ps=ps: e.matmul(ps[:, 0:64], ones, rhsd[:, :], start=True, stop=True), reads=[rhsd, cst], writes=[ps])
        K.op("act", lambda e, ps=ps: e.activation(out=dbc[:, :], in_=ps[:, 0:64], func=AF.Copy), reads=[ps], writes=[dbc])
        K.op("pool", lambda e: e.memset(Qz[:, :], 0.0), writes=[Qz])
        xh = xhalo[:, :].rearrange("p (k j) -> p k j", k=16)
        if cut <= 3:
            return
        for h in range(4):
            slq, wq = wload([w_in[:, h * 256:(h + 1) * 256]])
            slk, wk = wload([w_in[:, 1024 + h * 256:1024 + (h + 1) * 256]])
            slv, wv_ = wload([w_in[:, 2048 + h * 256:2048 + (h + 1) * 256]])
            slo, wo_ = wload([w_in[:, 3072 + h * 256:3072 + (h + 1) * 256]])
            for (sl, wt, cb0, dstT) in ((slq, wq, h * 2, qT), (slk, wk, 8 + h * 2, kT)):
                for cbk in range(2):
                    cb = cb0 + cbk
                    lw = lambda kc, wt=wt, cbk=cbk: wt[:, kc, cbk * 128:(cbk + 1) * 128]
                    ps = PS()
                    mm_group(ps[:, 0:3], ps, [(lw(kc), xh[:, kc, :]) for kc in range(16)], [sl, xhalo])
                    K.op("act", lambda e, ps=ps, cb=cb: e.activation(out=cvb[:, 0:3], in_=ps[:, 0:3], func=AF.Identity, bias=bqk[:, cb:cb + 1]), reads=[ps, bqk], writes=[cvb])
                    K.op("dve", lambda e: e.tensor_scalar(out=cvb[:, 0:3], in0=cvb[:, 0:3], scalar1=flag[:, 0:1], scalar2=None, op0=ALU.mult), reads=[flag], writes=[cvb])
                    for tb in range(2):
                        ps = PS()
                        mm_group(ps[:, :], ps, [(lw(kc), viewB[:, kc, tb * 512:(tb + 1) * 512]) for kc in range(16)], [sl, xT])
                        K.op("act", lambda e, ps=ps, tb=tb, cb=cb: e.activation(out=cvb[:, 3 + tb * 512:3 + (tb + 1) * 512], in_=ps[:, :], func=AF.Identity, bias=bqk[:, cb:cb + 1]),
                             reads=[ps, bqk], writes=[cvb])
                    ps = PS()
                    mm_group(ps[:, 0:128], ps, [(lw(kc), viewB[:, kc, TP:NTOK]) for kc in range(16)], [sl, xT])
                    K.op("act", lambda e, ps=ps, cb=cb: e.activation(out=V(cvs.t, 3, (11, 16), (1, 8)), in_=ps[:, 0:128].rearrange("p (b l) -> p b l", b=16), func=AF.Identity,
                                                                     bias=bqk[:, cb:cb + 1]), reads=[ps, bqk], writes=[cvs])
                    K.op("dve", lambda e, cb=cb: e.tensor_copy(out=V(cvs.t, 0, (11, 16), (1, 3)), in_=c0T[:, cb * 48:(cb + 1) * 48].rearrange("p (b j) -> p b j", b=16)),
                         reads=[c0T], writes=[cvs])
                    conv_silu(cvb, TP, cb, dstT[:, cbk * NTOK:cbk * NTOK + TP], dstT)
                    conv_silu(cvs, 128, cb, dstT[:, cbk * NTOK + TP:cbk * NTOK + NTOK].rearrange("p (b l) -> p b l", b=16), dstT, seg=(16, 8))
                    K.op("pool", lambda e, cb=cb: e.tensor_copy(out=convo[:, cb * 3:(cb + 1) * 3], in_=cvb[:, TP:TP + 3]), reads=[cvb], writes=[convo])
                    K.op("pool", lambda e, cb=cb: e.tensor_copy(out=convso[:, cb * 48:(cb + 1) * 48].rearrange("p (b j) -> p b j", b=16), in_=V(cvs.t, 8, (11, 16), (1, 3))),
                         reads=[cvs], writes=[convso])
            head_v_proj(viewB, 9, h, slv, wv_)
            bo = load_brow(3072 + h * 256, 256)
            for t in range(9):
                ps = PS()
                mm_group(ps[:, 0:256], ps, [(viewB[:, kc, t * 128:(t + 1) * 128], wo_[:, kc, :]) for kc in range(16)], [slo, xT])
                K.op("dve", lambda e, ps=ps, bo=bo: e.tensor_tensor(out=tmpo[:, :], in0=ps[:, 0:256], in1=bo[:, 0:256], op=ALU.add), reads=[ps, bo], writes=[tmpo])
                K.op("act", lambda e, t=t: e.activation(out=sigo[:, t * 256:(t + 1) * 256], in_=tmpo[:, :], func=AF.Sigmoid), reads=[tmpo], writes=[sigo])
            K.op("act", lambda e, h=h: e.activation(out=Cb[:, :], in_=Cst[h][:, :], func=AF.Copy), reads=[Cst[h]], writes=[Cb])
            if cut <= 4:
                continue
            for t in range(8):
                mlstm_tile_full(h, t, False)
            K.dma("sp", O["Cp"][h].rearrange("(dc p) v -> p dc v", p=128), V(Cst[h].t, 0, (257, 2), (1, 256)), reads=[Cst[h]])
            K.dma("sp", O["np_"][h].rearrange("(dc p) -> p dc", p=128), V(Cst[h].t, 256, (257, 2)), reads=[Cst[h]])
            if cut <= 5:
                continue
            mlstm_tile_full(h, 8, True)
        cvo = xtile[0]
        for (srcb, w_, nrow, dst) in ((convso, 48, 48, O["convs"].rearrange("b j d -> (b j) d")), (convo, 3, 3, O["convp"])):
            for g in range(4):
                ps = PS()
                for j in range(4):
                    cb = g * 4 + j
                    K.op("pe", lambda e, ps=ps, j=j, cb=cb, srcb=srcb, w_=w_, nrow=nrow: e.transpose(ps[0:nrow, j * 128:(j + 1) * 128], srcb[:, cb * w_:(cb + 1) * w_], ident),
                         reads=[srcb, cst], writes=[ps])
                K.op("act", lambda e, ps=ps, g=g, nrow=nrow: e.activation(out=cvo[0:nrow, g * 512:(g + 1) * 512], in_=ps[0:nrow, :], func=AF.Copy), reads=[ps], writes=[cvo])
            K.dma("sp", dst, cvo[0:nrow, :], reads=[cvo])

    A.release(m_head)
    sg = A.alloc("sg", NTOK, F32)
    lf = A.alloc("lf", NTOK, F32)
    kk = A.alloc("kk", NTOK, F32)
    bb = A.alloc("bb", NTOK, F32)
    qh = A.alloc("qh", NTOK, F32)
    ex = lf
    qb = A.alloc("qb", NTOK, BF16)
    kb = A.alloc("kb", NTOK, BF16)
    qS = A.alloc("qS", NTOK, BF16)
    kLT = A.alloc("kLT", NTOK, BF16)
    vtok = A.alloc("vtok", NT * 128, BF16)
    gg = A.alloc("gg", NT * 128, BF16)
    eL = A.alloc("eL", 24, F32)
    Sb = A.alloc("Sb", 128, BF16)
    ATb = A.alloc("ATb", 128, BF16)
    kLtok = A.alloc("kLtok", 128, BF16)
    ogo = [A.alloc(f"ogo{i}", 128, BF16) for i in range(2)]
    ogst = {"i": 0}
    junk = A.alloc("junk", 128, F32)
    S0f = A.alloc("S0f", 2048, F32)
    S0b = A.alloc("S0b", 2048, BF16)
    Vz = [A.alloc(f"Vz{i}", 512, BF16) for i in range(2)]
    QzH = A.alloc("QzH", 2048, BF16)
    m_end_hgrn = A.mark()

    def hgrn_head(hh, view, ntok, full):
        ntile = ntok // 128
        tbs = [(0, 512), (512, 512)] + ([(1024, 128)] if ntok > 1024 else [])
        if full and cut > 7:
            K.dma("sp", S0f[:, :].rearrange("p (b v) -> p b v", b=16), I["S0"][:, hh].rearrange("b c v -> c b v"), writes=[S0f])
        cf0 = 5128 + hh * 128
        ci0 = 6152 + hh * 128
        if full:
            sl1, w1 = wload([w_in[:, 4104 + hh * 128:4104 + (hh + 1) * 128], w_in[:, cf0:cf0 + 128]])
            sl2, w2 = wload([w_in[:, ci0:ci0 + 128], w_in[:, 7176 + hh * 128:7176 + (hh + 1) * 128]])
            fcol, icol = 128, 0
        else:
            sl1, w1 = wload([w_in[:, cf0:cf0 + 128], w_in[:, ci0:ci0 + 128]])
            sl2, w2 = sl1, w1
            fcol, icol = 0, 128
        for (t0, n) in tbs:
            ps = PS()
            mm_group(ps[:, 0:n], ps, [(w1[:, kc, fcol:fcol + 128], view[:, kc, t0:t0 + n]) for kc in range(16)], [sl1, xT])
            K.op("act", lambda e, ps=ps, t0=t0, n=n: e.activation(out=sg[:, t0:t0 + n], in_=ps[:, 0:n], func=AF.Sigmoid, bias=bfh[:, hh:hh + 1]), reads=[ps, bfh], writes=[sg])
            if full:
                ps = PS()
                mm_group(ps[:, 0:n], ps, [(w1[:, kc, 0:128], view[:, kc, t0:t0 + n]) for kc in range(16)], [sl1, xT])
                K.op("act", lambda e, ps=ps, t0=t0, n=n: e.activation(out=qh[:, t0:t0 + n], in_=ps[:, 0:n], func=AF.Silu, bias=bqh[:, hh:hh + 1]), reads=[ps, bqh], writes=[qh])
        N = ntok
        K.op("dve", lambda e: e.tensor_scalar(out=sg[:, 0:N], in0=sg[:, 0:N], scalar1=oml[:, hh:hh + 1], scalar2=lb[:, hh:hh + 1], op0=ALU.mult, op1=ALU.add), reads=[oml, lb], writes=[sg])
        K.op("act", lambda e: e.activation(out=lf[:, 0:N], in_=sg[:, 0:N], func=AF.Ln), reads=[sg], writes=[lf])
        K.op("dve", lambda e: e.tensor_scalar(out=kk[:, 0:N], in0=sg[:, 0:N], scalar1=-1.0, scalar2=1.0, op0=ALU.mult, op1=ALU.add), reads=[sg], writes=[kk])
        bi = load_brow(ci0, 128)
        if not full:
            for t in range(ntile):
                ps = PS()
                mm_group(ps[:, 0:128], ps, [(view[:, kc, t * 128:(t + 1) * 128], w2[:, kc, icol:icol + 128]) for kc in range(16)], [sl2, xT])
                K.op("dve", lambda e, ps=ps, t=t, bi=bi: e.tensor_tensor(out=vtok[:, t * 128:(t + 1) * 128], in0=ps[:, 0:128], in1=bi[:, 0:128], op=ALU.add), reads=[ps, bi], writes=[vtok])
        else:
            bg = load_brow(7176 + hh * 128, 128)
            for t in range(ntile):
                ps = PS()
                mm_group(ps[:, 0:256], ps, [(view[:, kc, t * 128:(t + 1) * 128], w2[:, kc, :]) for kc in range(16)], [sl2, xT])
                K.op("dve", lambda e, ps=ps, t=t, bi=bi: e.tensor_tensor(out=vtok[:, t * 128:(t + 1) * 128], in0=ps[:, 0:128], in1=bi[:, 0:128], op=ALU.add), reads=[ps, bi], writes=[vtok])
                K.op("dve", lambda e, ps=ps, bg=bg: e.tensor_tensor(out=junk[:, :], in0=ps[:, 128:256], in1=bg[:, 0:128], op=ALU.add), reads=[ps, bg], writes=[junk])
                K.op("act", lambda e, t=t: e.activation(out=gg[:, t * 128:(t + 1) * 128], in_=junk[:, :], func=AF.Silu), reads=[junk], writes=[gg])
        K.op("dve", lambda e: e.tensor_tensor_scan(out=bb[:, 0:N], data0=cst[:, C_RM:C_RM + N], data1=lf[:, 0:N], initial=0.0, op0=ALU.mult, op1=ALU.add), reads=[cst, lf], writes=[bb])
        npt = 8
        K.op("dve", lambda e: e.tensor_tensor(out=ex[:, 0:1024].rearrange("p (t s) -> p t s", t=npt), in0=V(bb.t, 127, (128, npt), (0, 128)),
                                              in1=bb[:, 0:1024].rearrange("p (t s) -> p t s", t=npt), op=ALU.subtract), reads=[bb], writes=[ex])
        if N > 1024:
            K.op("dve", lambda e: e.tensor_tensor(out=ex[:, 1024:N].rearrange("p (b l) -> p b l", b=16), in0=V(bb.t, 1024 + 7, (8, 16), (0, 8)),
                                                  in1=bb[:, 1024:N].rearrange("p (b l) -> p b l", b=16), op=ALU.subtract), reads=[bb], writes=[ex])
        K.op("act", lambda e: e.activation(out=ex[:, 0:N], in_=ex[:, 0:N], func=AF.Exp), reads=[ex], writes=[ex])
        K.op("dve", lambda e: e.tensor_tensor(out=kLT[:, 0:N], in0=kk[:, 0:N], in1=ex[:, 0:N], op=ALU.mult), reads=[kk, ex], writes=[kLT])
        K.op("act", lambda e: e.activation(out=eL[:, 0:8], in_=V(bb.t, 127, (128, 8)), func=AF.Exp), reads=[bb], writes=[eL])
        if N > 1024:
            K.op("act", lambda e: e.activation(out=eL[:, 8:24], in_=V(bb.t, 1024 + 7, (8, 16)), func=AF.Exp), reads=[bb], writes=[eL])
        if full:
            nt_all = N // 128
            K.op("dve", lambda e: e.tensor_tensor(out=ex[:, 0:N].rearrange("p (t s) -> p t s", t=nt_all), in0=bb[:, 0:N].rearrange("p (t s) -> p t s", t=nt_all),
                                                  in1=V(bb.t, 63, (128, nt_all), (0, 128)), op=ALU.subtract), reads=[bb, kLT], writes=[ex])
            K.op("act", lambda e: e.activation(out=sg[:, 0:N], in_=ex[:, 0:N], func=AF.Exp), reads=[ex], writes=[sg])
            K.op("dve", lambda e: e.tensor_tensor(out=qb[:, 0:N], in0=qh[:, 0:N], in1=sg[:, 0:N], op=ALU.mult), reads=[qh, sg], writes=[qb])
            K.op("act", lambda e: e.activation(out=sg[:, 0:N], in_=ex[:, 0:N], func=AF.Exp, scale=-1.0), reads=[ex, qb], writes=[sg])
            K.op("dve", lambda e: e.tensor_tensor(out=kb[:, 0:N], in0=kk[:, 0:N], in1=sg[:, 0:N], op=ALU.mult), reads=[kk, sg], writes=[kb])
            K.op("act", lambda e: e.activation(out=sg[:, 0:N], in_=bb[:, 0:N], func=AF.Exp), reads=[bb, kb], writes=[sg])
            K.op("dve", lambda e: e.tensor_tensor(out=qS[:, 0:N], in0=qh[:, 0:N], in1=sg[:, 0:N], op=ALU.mult), reads=[qh, sg], writes=[qS])
            K.op("act", lambda e: e.activation(out=Sb[:, :], in_=Sst[hh][:, :], func=AF.Copy), reads=[Sst[hh]], writes=[Sb])

        def norm_out(po, t):
            K.op("act", lambda e, po=po: e.activation(out=junk[:, :], in_=po[:, 0:128], func=AF.Square, accum_out=st6[:, 0:1]), reads=[po], writes=[junk, st6])
            K.op("act", lambda e: e.activation(out=st6[:, 1:2], in_=st6[:, 0:1], func=AF.Sqrt, scale=1.0 / 128.0, bias=EPS), reads=[st6], writes=[st6])
            K.op("dve", lambda e: e.reciprocal(out=st6[:, 2:3], in_=st6[:, 1:2]), reads=[st6], writes=[st6])
            oo = ogo[ogst["i"] % 2]
            ogst["i"] += 1
            K.op("dve", lambda e, po=po, oo=oo: e.scalar_tensor_tensor(out=oo[:, :], in0=po[:, 0:128], scalar=st6[:, 2:3], in1=gg[:, t * 128:(t + 1) * 128], op0=ALU.mult, op1=ALU.mult),
                 reads=[po, st6, gg], writes=[oo])
            K.dma("sp", og_scr[t * 128:(t + 1) * 128, hh * 128:(hh + 1) * 128], oo[:, :], reads=[oo], writes=[og_scr_b])

        def make_kLtok(t):
            pt = PS()
            ptb = pt[:, 0:64].bitcast(BF16)
            K.op("pe", lambda e, ptb=ptb: e.transpose(ptb, kLT[:, t * 128:(t + 1) * 128], identB[:, :]), reads=[kLT, identB], writes=[pt])
            K.op("act", lambda e, ptb=ptb: e.activation(out=kLtok[:, :], in_=ptb, func=AF.Copy), reads=[pt], writes=[kLtok])

        for t in range(8):
            sl_t = slice(t * 128, (t + 1) * 128)
            if full:
                pA = PS()
                K.op("pe", lambda e, pA=pA, sl_t=sl_t: e.matmul(pA[:, 0:128], kb[:, sl_t], qb[:, sl_t], start=True, stop=True), reads=[kb, qb], writes=[pA])
                K.op("dve", lambda e, pA=pA: e.tensor_tensor(out=ATb[:, :], in0=pA[:, 0:128], in1=cs(C_MP), op=ALU.mult), reads=[pA, cst], writes=[ATb])
                po = PS()
                mm_group(po[:, 0:128], po, [(ATb[:, :], vtok[:, sl_t]), (qS[:, sl_t], Sb[:, :])], [ATb, vtok, qS, Sb])
                norm_out(po, t)
            make_kLtok(t)
            pc = PS()
            K.op("pe", lambda e, pc=pc, sl_t=sl_t: e.matmul(pc[:, 0:128], kLtok[:, :], vtok[:, sl_t], start=True, stop=True), reads=[kLtok, vtok], writes=[pc])
            K.op("dve", lambda e, pc=pc, t=t: e.scalar_tensor_tensor(out=Sst[hh][:, :], in0=Sst[hh][:, :], scalar=eL[:, t:t + 1], in1=pc[:, 0:128], op0=ALU.mult, op1=ALU.add),
                 reads=[pc, eL], writes=[Sst[hh]])
            if full:
                K.op("act", lambda e: e.activation(out=Sb[:, :], in_=Sst[hh][:, :], func=AF.Copy), reads=[Sst[hh]], writes=[Sb])
        if not full:
            K.op("dve", lambda e: e.tensor_scalar(out=Sst[hh][:, :], in0=Sst[hh][:, :], scalar1=flag[:, 0:1], scalar2=None, op0=ALU.mult), reads=[flag], writes=[Sst[hh]])
            return
        K.dma("sp", O["Sp"][hh], Sst[hh][:, :], reads=[Sst[hh]])
        if cut <= 7:
            return
        t = 8
        sl_t = slice(TP, NTOK)
        K.op("act", lambda e: e.activation(out=S0b[:, :], in_=S0f[:, :], func=AF.Copy), reads=[S0f], writes=[S0b])
        K.op("pool", lambda e: e.tensor_copy(out=V(QzH.t, 0, (136, 16), (1, 8)), in_=qS[:, sl_t].rearrange("p (b l) -> p b l", b=16)), reads=[qS], writes=[QzH])
        pA = PS()
        K.op("pe", lambda e, pA=pA: e.matmul(pA[:, 0:128], kb[:, sl_t], qb[:, sl_t], start=True, stop=True), reads=[kb, qb], writes=[pA])
        K.op("dve", lambda e, pA=pA: e.tensor_tensor(out=ATb[:, :], in0=pA[:, 0:128], in1=cs(C_MS), op=ALU.mult), reads=[pA, cst], writes=[ATb])
        po = PS()
        mm_group(po[:, 0:128], po, [(ATb[:, :], vtok[:, t * 128:(t + 1) * 128])] + [(QzH[:, b * 128:(b + 1) * 128], S0b[:, b * 128:(b + 1) * 128]) for b in range(16)],
                 [ATb, vtok, QzH, S0b])
        norm_out(po, t)
        make_kLtok(t)
        for g in range(4):
            vz = Vz[g % 2]
            K.op("dve", lambda e, g=g, vz=vz: e.tensor_tensor(out=vz[:, :].rearrange("p (b v) -> p b v", b=4), in0=V(vtok.t, t * 128, (0, 4), (1, 128)),
                                                               in1=V(cst.t, C_SEQM + g * 4, (1, 4), (0, 128)), op=ALU.mult), reads=[vtok, cst], writes=[vz])
            pc = PS()
            K.op("pe", lambda e, pc=pc, vz=vz: e.matmul(pc[:, :], kLtok[:, :], vz[:, :], start=True, stop=True), reads=[kLtok, vz], writes=[pc])
            K.op("dve", lambda e, g=g: e.tensor_tensor(out=S0f[:, g * 512:(g + 1) * 512].rearrange("p (b v) -> p b v", b=4), in0=S0f[:, g * 512:(g + 1) * 512].rearrange("p (b v) -> p b v", b=4),
                                                       in1=V(eL.t, 8 + g * 4, (1, 4), (0, 128)), op=ALU.mult), reads=[eL, S0b], writes=[S0f])
            K.op("dve", lambda e, pc=pc, g=g: e.tensor_tensor(out=S0f[:, g * 512:(g + 1) * 512], in0=S0f[:, g * 512:(g + 1) * 512], in1=pc[:, :], op=ALU.add), reads=[pc], writes=[S0f])
        K.dma("sp", O["Ss"][:, hh].rearrange("b c v -> c b v"), S0f[:, :].rearrange("p (b v) -> p b v", b=16), reads=[S0f])

    if stage >= 1:
        build_xT([(I["xpre"], 8)], TP)
        for f_ in deferred:
            f_()
        K.op("dve", lambda e: e.tensor_copy(out=xhalo[:, :].rearrange("p (k j) -> p k j", k=16), in_=xTv(TP)[:, :, TP - 3:TP]), reads=[xT], writes=[xhalo])
        phase_A_mlstm()
        K.barrier()
        K.op("pool", lambda e: e.memset(QzH[:, :], 0.0), writes=[QzH])
        for hh in range(8):
            hgrn_head(hh, xTv(TP), TP, False)
        K.barrier()
    if stage >= 2:
        build_xT([(I["xp"], 8), (I["xs"], 1)], NTOK)
        phase_B_mlstm()
        K.barrier()
        K.op("pool", lambda e: e.memset(QzH[:, :], 0.0), writes=[QzH])
        if cut > 6:
            for hh in range(8):
                hgrn_head(hh, xTv(NTOK), NTOK, True)
        K.barrier()

    r_scr = nc.dram_tensor("r_scr", [NTOK, D], F32, kind="Internal").ap()
    h_scr = nc.dram_tensor("h_scr", [NTOK, D], F32, kind="Internal").ap()
    r_scr_b = Buf("r_scr")
    h_scr_b = Buf("h_scr")

    def xrows(t):
        return I["xp"][t * 128:(t + 1) * 128, :] if t < 8 else I["xs"]

    def layer_norm_tiles(g_in, b_in_, dstT_buf, dstT_view, tilebufs, lng, lnb, lnst, out_dram=None, out_fn=None, inplace=False):
        K.dma("sp", lng[:, :], g_in[0:1, :].partition_broadcast(128), writes=[lng])
        K.dma("sp", lnb[:, :], b_in_[0:1, :].partition_broadcast(128), writes=[lnb])
        if not inplace:
            K.dma("sp", tilebufs[0][:, :], r_scr[0:128, :], reads=[r_scr_b], writes=[tilebufs[0]])
        for t in range(NT):
            if inplace:
                rt = tilebufs[t]
            else:
                rt = tilebufs[t % 2]
                if t + 1 < NT:
                    K.dma("sp", tilebufs[(t + 1) % 2][:, :], r_scr[(t + 1) * 128:(t + 2) * 128, :], reads=[r_scr_b], writes=[tilebufs[(t + 1) % 2]])
            for c in range(4):
                K.op("dve", lambda e, c=c, rt=rt: e.bn_stats(out=lnst[:, c * 6:(c + 1) * 6], in_=rt[:, c * 512:(c + 1) * 512]), reads=[rt], writes=[lnst])
            K.op("dve", lambda e: e.bn_aggr(out=lnst[:, 24:26], in_=lnst[:, 0:24]), reads=[lnst], writes=[lnst])
            K.op("act", lambda e: e.activation(out=lnst[:, 26:27], in_=lnst[:, 25:26], func=AF.Sqrt, bias=EPS), reads=[lnst], writes=[lnst])
            K.op("dve", lambda e: e.reciprocal(out=lnst[:, 27:28], in_=lnst[:, 26:27]), reads=[lnst], writes=[lnst])
            K.op("dve", lambda e: e.tensor_scalar(out=lnst[:, 28:29], in0=lnst[:, 24:25], scalar1=-1.0, scalar2=lnst[:, 27:28], op0=ALU.mult, op1=ALU.mult), reads=[lnst], writes=[lnst])
            K.op("act", lambda e, rt=rt: e.activation(out=rt[:, :], in_=rt[:, :], func=AF.Identity, scale=lnst[:, 27:28], bias=lnst[:, 28:29]), reads=[lnst], writes=[rt])
            K.op("dve", lambda e, rt=rt: e.tensor_tensor(out=rt[:, :], in0=rt[:, :], in1=lng[:, :], op=ALU.mult), reads=[lng], writes=[rt])
            K.op("dve", lambda e, rt=rt: e.tensor_tensor(out=rt[:, :], in0=rt[:, :], in1=lnb[:, :], op=ALU.add), reads=[lnb], writes=[rt])
            if out_fn is not None:
                K.dma("sp", out_fn(t), rt[:, :], reads=[rt])
            if out_dram is not None:
                K.dma("sp", out_dram[t * 128:(t + 1) * 128, :], rt[:, :], reads=[rt], writes=[h_scr_b])
            if dstT_view is not None:
                transpose_tile_f32(rt, lambda kc, rt=rt: rt[:, kc * 128:(kc + 1) * 128], dstT_buf, dstT_view, t * 128)

    def out_proj_residual(lhsT_buf, lhsT_view, nkc, w_dram, res_fn, res_bufs, xr, ro):
        items = [(cblk, t) for cblk in range(8) for t in range(NT)]
        nb = len(xr)

        def issue_load(i):
            cblk, t = items[i]
            x_ = xr[i % nb]
            K.dma("sp", x_[:, :], res_fn(t)[:, cblk * 256:(cblk + 1) * 256], reads=res_bufs, writes=[x_])

        for i in range(min(nb - 1, len(items))):
            issue_load(i)
        sl = w = None
        for i, (cblk, t) in enumerate(items):
            if t == 0:
                sl, w = wload([w_dram[:, cblk * 256:(cblk + 1) * 256]])
            if i + nb - 1 < len(items):
                issue_load(i + nb - 1)
            ps = PS()
            mm_group(ps[:, 0:256], ps, [(lhsT_view[:, kc, t * 128:(t + 1) * 128], w[:, kc, :]) for kc in range(nkc)], [lhsT_buf, sl])
            x_ = xr[i % nb]
            r_ = ro[i % nb]
            K.op("dve", lambda e, ps=ps, x_=x_, r_=r_: e.scalar_tensor_tensor(out=r_[:, :], in0=x_[:, :], scalar=ALPHA, in1=ps[:, 0:256], op0=ALU.mult, op1=ALU.add),
                 reads=[ps, x_], writes=[r_])
            K.dma("sp", r_scr[t * 128:(t + 1) * 128, cblk * 256:(cblk + 1) * 256], r_[:, :], reads=[r_], writes=[r_scr_b])

    if stage >= 3:
        A.release(m_mix)
        hmT = A.alloc("hmT", 8 * NTOK, BF16)
        ogT = A.alloc("ogT", 8 * NTOK, BF16)
        mT = A.alloc("mT", 16 * NTOK, BF16)
        tl = [A.alloc(f"tl{i}", 1024, BF16) for i in range(2)]
        sgm = A.alloc("sgm", 512, F32)
        sgh = A.alloc("sgh", 512, F32)
        mm1 = sgm
        mm2 = sgh
        xr = [A.alloc(f"xr{i}", 256, F32) for i in range(3)]
        ro = [A.alloc(f"ro{i}", 256, F32) for i in range(3)]
        i_tl = 0
        for (scr, scr_b, dstT, goff) in ((hm_scr, hm_scr_b, hmT, 0), (og_scr, og_scr_b, ogT, 8)):
            for t in range(NT):
                tb = tl[i_tl % 2]
                i_tl += 1
                K.dma("sp", tb[:, :], scr[t * 128:(t + 1) * 128, :], reads=[scr_b], writes=[tb])
                for g in range(2):
                    ps = PS()
                    psb = ps[:, 0:256].bitcast(BF16)
                    for j in range(4):
                        kc = g * 4 + j
                        K.op("pe", lambda e, psb=psb, j=j, kc=kc, tb=tb: e.transpose(psb[:, j * 128:(j + 1) * 128], tb[:, kc * 128:(kc + 1) * 128], identB[:, :]),
                             reads=[tb, identB], writes=[ps])
                    for j in range(4):
                        kc = g * 4 + j
                        K.op("act", lambda e, psb=psb, j=j, kc=kc, dstT=dstT, goff=goff, t=t: e.activation(
                            out=dstT[:, kc * NTOK + t * 128:kc * NTOK + (t + 1) * 128], in_=psb[:, j * 128:(j + 1) * 128], func=AF.Copy, scale=gnT[:, goff + kc:goff + kc + 1]),
                            reads=[ps, gnT], writes=[dstT])
        viewX = xTv(NTOK)
        hmTv = hmT[:, :].rearrange("p (k t) -> p k t", k=8)
        ogTv = ogT[:, :].rearrange("p (k t) -> p k t", k=8)
        mTv = mT[:, :].rearrange("p (k t) -> p k t", k=16)
        for pr in range(8):
            slgm, wgm = wload([w_in[:, 8200 + pr * 256:8200 + (pr + 1) * 256]])
            slgh, wgh = wload([w_in[:, 10248 + pr * 256:10248 + (pr + 1) * 256]])
            slbm, wbm = wload([I["w_bm"][:, pr * 256:(pr + 1) * 256]])
            slbh, wbh = wload([I["w_bh"][:, pr * 256:(pr + 1) * 256]])
            for c2 in range(2):
                cb = pr * 2 + c2
                cs_ = slice(c2 * 128, (c2 + 1) * 128)
                for (t0, n) in ((0, 512), (512, 512), (1024, 128)):
                    pgm = PS()
                    mm_group(pgm[:, 0:n], pgm, [(wgm[:, kc, cs_], viewX[:, kc, t0:t0 + n]) for kc in range(16)], [slgm, xT])
                    K.op("act", lambda e, pgm=pgm, n=n, cb=cb: e.activation(out=sgm[:, 0:n], in_=pgm[:, 0:n], func=AF.Sigmoid, bias=bgm[:, cb:cb + 1]), reads=[pgm, bgm], writes=[sgm])
                    pgh = PS()
                    mm_group(pgh[:, 0:n], pgh, [(wgh[:, kc, cs_], viewX[:, kc, t0:t0 + n]) for kc in range(16)], [slgh, xT])
                    K.op("act", lambda e, pgh=pgh, n=n, cb=cb: e.activation(out=sgh[:, 0:n], in_=pgh[:, 0:n], func=AF.Sigmoid, bias=bgh[:, cb:cb + 1]), reads=[pgh, bgh], writes=[sgh])
                    ppm = PS()
                    mm_group(ppm[:, 0:n], ppm, [(wbm[:, kc, cs_], hmTv[:, kc, t0:t0 + n]) for kc in range(8)], [slbm, hmT])
                    K.op("dve", lambda e, ppm=ppm, n=n: e.tensor_tensor(out=mm1[:, 0:n], in0=sgm[:, 0:n], in1=ppm[:, 0:n], op=ALU.mult), reads=[ppm, sgm], writes=[mm1])
                    pph = PS()
                    mm_group(pph[:, 0:n], pph, [(wbh[:, kc, cs_], ogTv[:, kc, t0:t0 + n]) for kc in range(8)], [slbh, ogT])
                    K.op("dve", lambda e, pph=pph, n=n: e.tensor_tensor(out=mm2[:, 0:n], in0=sgh[:, 0:n], in1=pph[:, 0:n], op=ALU.mult), reads=[pph, sgh], writes=[mm2])
                    K.op("dve", lambda e, n=n, cb=cb, t0=t0: e.tensor_tensor(out=mTv[:, cb, t0:t0 + n], in0=mm1[:, 0:n], in1=mm2[:, 0:n], op=ALU.add), reads=[mm1, mm2], writes=[mT])
        out_proj_residual(mT, mTv, 16, I["w_out"], xrows, [], xr, ro)
        K.barrier()
        m_ln = A.mark()
        A.release(m_mix)
        tilebufs = [A.alloc(f"tilebuf{i}", D, F32) for i in range(2)]
        lng = A.alloc("lng", D, F32)
        lnb = A.alloc("lnb", D, F32)
        lnst = A.alloc("lnst", 32, F32)
        dbg = None
        if stage == 3:
            dbg = lambda t: (O["y_p"][t * 128:(t + 1) * 128, :] if t < 8 else O["y_s"])
        layer_norm_tiles(I["ln1_g"], I["ln1_b"], xT, xTv(NTOK), tilebufs, lng, lnb, lnst, out_dram=h_scr, out_fn=dbg)
        K.barrier()
        m_post_ln = A.mark()

    if stage >= 4:
        XS = 512.0 ** -0.5
        A.release(m_mix)
        oT = A.alloc("oT", 16 * NTOK, BF16)
        qTh = A.alloc("qTh", 4 * NTOK, BF16)
        memT = A.alloc("memT", 16 * 256, BF16)
        membf = A.alloc("membf", D, BF16)
        KTh = A.alloc("KTh", 4 * 256, BF16)
        Vh = A.alloc("Vh", 2 * 512, BF16)
        kvout = [A.alloc(f"kvout{i}", 256, F32) for i in range(2)]
        Kb = [A.alloc(f"Kb{i}", 2 * 512, BF16) for i in range(3)]
        KTb = [A.alloc(f"KTb{i}", 4 * 256, BF16) for i in range(3)]
        Vb = [A.alloc(f"Vb{i}", 2 * 512, BF16) for i in range(3)]
        scx = A.alloc("scx", 256, F32)
        pn = A.alloc("pn", 256, BF16)
        pT = A.alloc("pT", 256, BF16)
        sTs = A.alloc("sTs", 256, F32)
        xst = A.alloc("xst", 8, F32)
        xr = [A.alloc(f"xr{i}", 256, F32) for i in range(2)]
        ro = [A.alloc(f"ro{i}", 256, F32) for i in range(2)]
        h1Tv = xTv(NTOK)
        oTv = oT[:, :].rearrange("p (k t) -> p k t", k=16)
        qTv = qTh[:, :].rearrange("p (k t) -> p k t", k=4)
        memTv = memT[:, :].rearrange("p (k m) -> p k m", k=16)
        KThv = KTh[:, :].rearrange("p (k m) -> p k m", k=4)
        kvi = 0
        for mt in range(2):
            for hf in range(2):
                K.dma("pool", membf[:, hf * 1024:(hf + 1) * 1024], I["mem"][mt * 128:(mt + 1) * 128, hf * 1024:(hf + 1) * 1024], writes=[membf])
            if cut <= 10:
                continue
            for g in range(2):
                ps = PS()
                psb = ps[:, :].bitcast(BF16)
                for j in range(8):
                    kc = g * 8 + j
                    K.op("pe", lambda e, psb=psb, j=j, kc=kc: e.transpose(psb[:, j * 128:(j + 1) * 128], membf[:, kc * 128:(kc + 1) * 128], identB[:, :]),
                         reads=[membf, identB], writes=[ps])
                K.op("act", lambda e, psb=psb, g=g, mt=mt: e.activation(out=memTv[:, g * 8:(g + 1) * 8, mt * 128:(mt + 1) * 128], in_=psb.rearrange("p (j m) -> p j m", j=8), func=AF.Copy),
                     reads=[ps], writes=[memT])

        def softmax_to_pT(ps_s):
            K.op("dve", lambda e: e.tensor_reduce(out=xst[:, 0:1], in_=ps_s[:, 0:256], axis=AX.X, op=ALU.max), reads=[ps_s], writes=[xst])
            K.op("dve", lambda e: e.tensor_scalar(out=xst[:, 1:2], in0=xst[:, 0:1], scalar1=-XS, scalar2=None, op0=ALU.mult), reads=[xst], writes=[xst])
            K.op("act", lambda e: e.activation(out=scx[:, :], in_=ps_s[:, 0:256], func=AF.Exp, scale=XS, bias=xst[:, 1:2], accum_out=xst[:, 2:3]), reads=[ps_s, xst], writes=[scx, xst])
            K.op("dve", lambda e: e.reciprocal(out=xst[:, 3:4], in_=xst[:, 2:3]), reads=[xst], writes=[xst])
            K.op("dve", lambda e: e.tensor_scalar(out=pn[:, :], in0=scx[:, :], scalar1=xst[:, 3:4], scalar2=None, op0=ALU.mult), reads=[scx, xst], writes=[pn])
            pt = PS()
            ptb = pt[:, 0:128].bitcast(BF16)
            for mt in range(2):
                K.op("pe", lambda e, ptb=ptb, mt=mt: e.transpose(ptb[:, mt * 128:(mt + 1) * 128], pn[:, mt * 128:(mt + 1) * 128], identB[:, :]), reads=[pn, identB], writes=[pt])
            K.op("act", lambda e, ptb=ptb: e.activation(out=pT[:, :], in_=ptb, func=AF.Copy), reads=[pt], writes=[pT])

        for h in range(4):
            if cut <= 11:
                continue
            for half in range(2):
                slq, wq = wload([I["xa_wq"][:, h * 512 + half * 256:h * 512 + (half + 1) * 256]])
                for c2 in range(2):
                    c = half * 2 + c2
                    for (t0, n) in ((0, 512), (512, 512), (1024, 128)):
                        ps = PS()
                        mm_group(ps[:, 0:n], ps, [(wq[:, kc, c2 * 128:(c2 + 1) * 128], h1Tv[:, kc, t0:t0 + n]) for kc in range(16)], [slq, xT])
                        K.op("act", lambda e, ps=ps, c=c, t0=t0, n=n: e.activation(out=qTv[:, c, t0:t0 + n], in_=ps[:, 0:n], func=AF.Copy), reads=[ps], writes=[qTh])
            for half in range(2):
                slk, wk = wload([I["xa_wk"][:, h * 512 + half * 256:h * 512 + (half + 1) * 256]])
                slv, wv_ = wload([I["xa_wv"][:, h * 512 + half * 256:h * 512 + (half + 1) * 256]])
                c0 = h * 512 + half * 256
                for mt in range(2):
                    ms = slice(mt * 128, (mt + 1) * 128)
                    ps = PS()
                    mm_group(ps[:, 0:256], ps, [(memTv[:, kc, ms], wk[:, kc, :]) for kc in range(16)], [memT, slk])
                    ko = kvout[kvi % 2]
                    kvi += 1
                    K.op("act", lambda e, ps=ps, ko=ko: e.activation(out=ko[:, :], in_=ps[:, 0:256], func=AF.Copy), reads=[ps], writes=[ko])
                    K.dma("sp", O["mk"][ms, c0:c0 + 256], ko[:, :], reads=[ko])
                    ps = PS()
                    mm_group(ps[:, 0:256], ps, [(memTv[:, kc, ms], wv_[:, kc, :]) for kc in range(16)], [memT, slv])
                    ko = kvout[kvi % 2]
                    kvi += 1
                    K.op("act", lambda e, ps=ps, ko=ko: e.activation(out=ko[:, :], in_=ps[:, 0:256], func=AF.Copy), reads=[ps], writes=[ko])
                    K.dma("sp", O["mv"][ms, c0:c0 + 256], ko[:, :], reads=[ko])
                    K.op("dve", lambda e, ps=ps, mt=mt, half=half: e.tensor_copy(out=Vh[:, mt * 512 + half * 256:mt * 512 + (half + 1) * 256], in_=ps[:, 0:256]), reads=[ps], writes=[Vh])
                for c2 in range(2):
                    ps = PS()
                    mm_group(ps[:, 0:256], ps, [(wk[:, kc, c2 * 128:(c2 + 1) * 128], memTv[:, kc, :]) for kc in range(16)], [memT, slk])
                    K.op("act", lambda e, ps=ps, dc=half * 2 + c2: e.activation(out=KThv[:, dc, :], in_=ps[:, 0:256], func=AF.Copy), reads=[ps], writes=[KTh])
            for t in range(8):
                if cut <= 12:
                    continue
                ts_ = slice(t * 128, (t + 1) * 128)
                ps_s = PS()
                mm_group(ps_s[:, 0:256], ps_s, [(qTv[:, dc, ts_], KThv[:, dc, :]) for dc in range(4)], [qTh, KTh])
                softmax_to_pT(ps_s)
                ps_o = PS()
                for dc in range(4):
                    for mt in range(2):
                        K.op("pe", lambda e, ps_o=ps_o, dc=dc, mt=mt: e.matmul(ps_o[:, dc * 128:(dc + 1) * 128], Vh[:, mt * 512 + dc * 128:mt * 512 + (dc + 1) * 128],
                                                                            pT[:, mt * 128:(mt + 1) * 128], start=(mt == 0), stop=(mt == 1)), reads=[Vh, pT], writes=[ps_o])
                K.op("act", lambda e, ps_o=ps_o, ts_=ts_, h=h: e.activation(out=oTv[:, h * 4:(h + 1) * 4, ts_], in_=ps_o[:, :].rearrange("p (c t) -> p c t", c=4), func=AF.Copy),
                     reads=[ps_o], writes=[oT])
            if cut <= 13:
                continue
            ps_sT = psum[6]
            for b in range(16):
                kb_ = Kb[b % 3]
                ktb = KTb[b % 3]
                K.dma("pool", kb_[:, :].rearrange("p (mt c) -> p mt c", mt=2), I["ck"][b, :, h * 512:(h + 1) * 512].rearrange("(mt p) c -> p mt c", p=128), writes=[kb_])
                ps = PS()
                psb = ps[:, :].bitcast(BF16)
                for dc in range(4):
                    for mt in range(2):
                        K.op("pe", lambda e, psb=psb, dc=dc, mt=mt, kb_=kb_: e.transpose(psb[:, dc * 256 + mt * 128:dc * 256 + (mt + 1) * 128],
                                                                                     kb_[:, mt * 512 + dc * 128:mt * 512 + (dc + 1) * 128], identB[:, :]),
                             reads=[kb_, identB], writes=[ps])
                K.op("act", lambda e, psb=psb, ktb=ktb: e.activation(out=ktb[:, :], in_=psb, func=AF.Copy), reads=[ps], writes=[ktb])
                for mt in range(2):
                    for dc in range(4):
                        K.op("pe", lambda e, mt=mt, dc=dc, b=b, ktb=ktb: e.matmul(ps_sT[:, mt * 128 + b * 8:mt * 128 + (b + 1) * 8], ktb[:, dc * 256 + mt * 128:dc * 256 + (mt + 1) * 128],
                                                                               qTv[:, dc, TP + b * 8:TP + (b + 1) * 8], start=(dc == 0), stop=(dc == 3)),
                             reads=[ktb, qTh], writes=[ps_sT])
            K.op("act", lambda e: e.activation(out=sTs[:, :], in_=ps_sT[:, 0:256], func=AF.Copy), reads=[ps_sT], writes=[sTs])
            ps_s = PS()
            for mt in range(2):
                K.op("pe", lambda e, ps_s=ps_s, mt=mt: e.transpose(ps_s[:, mt * 128:(mt + 1) * 128], sTs[:, mt * 128:(mt + 1) * 128], ident), reads=[sTs, cst], writes=[ps_s])
            softmax_to_pT(ps_s)
            ps_oS = psum[7]
            for b in range(16):
                vb_ = Vb[b % 3]
                K.dma("pool", vb_[:, :].rearrange("p (mt c) -> p mt c", mt=2), I["cv"][b, :, h * 512:(h + 1) * 512].rearrange("(mt p) c -> p mt c", p=128), writes=[vb_])
                for dc in range(4):
                    for mt in range(2):
                        K.op("pe", lambda e, dc=dc, mt=mt, b=b, vb_=vb_: e.matmul(ps_oS[:, dc * 128 + b * 8:dc * 128 + (b + 1) * 8], vb_[:, mt * 512 + dc * 128:mt * 512 + (dc + 1) * 128],
                                                                               pT[:, mt * 128 + b * 8:mt * 128 + (b + 1) * 8], start=(mt == 0), stop=(mt == 1)),
                             reads=[vb_, pT], writes=[ps_oS])
            K.op("act", lambda e, h=h: e.activation(out=oTv[:, h * 4:(h + 1) * 4, TP:NTOK], in_=ps_oS[:, :].rearrange("p (c t) -> p c t", c=4), func=AF.Copy), reads=[ps_oS], writes=[oT])
        out_proj_residual(oT, oTv, 16, I["xa_wo"], lambda t: h_scr[t * 128:(t + 1) * 128, :], [h_scr_b], xr, ro)
        K.barrier()
        A.release(m_mix)
        tilebufs = [A.alloc(f"tilebufb{i}", D, F32) for i in range(2)]
        lng = A.alloc("lngb", D, F32)
        lnb = A.alloc("lnbb", D, F32)
        lnst = A.alloc("lnstb", 32, F32)
        dbg = None
        if stage == 4:
            dbg = lambda t: (O["y_p"][t * 128:(t + 1) * 128, :] if t < 8 else O["y_s"])
        layer_norm_tiles(I["ln2_g"], I["ln2_b"], xT, xTv(NTOK), tilebufs, lng, lnb, lnst, out_dram=h_scr, out_fn=dbg)
        K.barrier()

    if stage >= 5:
        A.release(m_mix)
        h2Tv = xTv(NTOK)
        acc_t = [A.alloc(f"acc{t}", D, F32) for t in range(NT)]
        m_after_acc = A.mark()
        hid = A.alloc("hid", 4 * NTOK, BF16)
        hidv = hid[:, :].rearrange("p (f t) -> p f t", f=4)
        cbc = A.alloc("cbc", NTOK, F32)
        cTf = cbc
        cThi = A.alloc("cThi", NTOK, BF16)
        cTlo = A.alloc("cTlo", NTOK, BF16)
        sgt = A.alloc("sgt", 512, F32)
        ut = A.alloc("ut", 512, F32)
        comb = A.alloc("comb", NT * 32, F32)
        rbrow = A.alloc("rbrow", 36, F32)
        rs_ = A.alloc("rs_", 128, F32)
        sel = [A.alloc(f"sel{i}", 128, BF16) for i in range(2)]
        for t in range(NT):
            K.dma("sp", acc_t[t][:, :], h_scr[t * 128:(t + 1) * 128, :], reads=[h_scr_b], writes=[acc_t[t]])
            K.op("act", lambda e, t=t: e.activation(out=acc_t[t][:, :], in_=acc_t[t][:, :], func=AF.Copy, scale=ALPHA), writes=[acc_t[t]])
        K.dma("sp", rbrow[:, :], I["rb"][0:1, :].partition_broadcast(128), writes=[rbrow])
        slr, wr = wload([I["rw"]])
        lg, oh, lge, t8, mk1, mk2 = rs_[:, 0:36], rs_[:, 36:40], rs_[:, 40:48], rs_[:, 48:80], rs_[:, 80:88], rs_[:, 88:96]
        sc = lambda i: rs_[:, 96 + i:97 + i]
        for t in range(NT):
            ps = PS()
            mm_group(ps[:, 0:36], ps, [(h2Tv[:, kc, t * 128:(t + 1) * 128], wr[:, kc, :]) for kc in range(16)], [xT, slr])
            K.op("dve", lambda e, ps=ps: e.tensor_tensor(out=lg, in0=ps[:, 0:36], in1=rbrow[:, :], op=ALU.add), reads=[ps, rbrow], writes=[rs_])
            K.op("dve", lambda e: e.tensor_reduce(out=sc(0), in_=lg[:, 0:4], axis=AX.X, op=ALU.max), reads=[rs_], writes=[rs_])
            K.op("dve", lambda e: e.tensor_scalar(out=oh, in0=lg[:, 0:4], scalar1=sc(0), scalar2=None, op0=ALU.is_equal), reads=[rs_], writes=[rs_])
            K.op("dve", lambda e: e.tensor_scalar(out=sc(1), in0=sc(0), scalar1=-1.0, scalar2=None, op0=ALU.mult), reads=[rs_], writes=[rs_])
            K.op("act", lambda e: e.activation(out=t8[:, 0:4], in_=lg[:, 0:4], func=AF.Exp, bias=sc(1), accum_out=sc(2)), reads=[rs_], writes=[rs_])
            K.op("dve", lambda e: e.reciprocal(out=sc(3), in_=sc(2)), reads=[rs_], writes=[rs_])
            K.op("dve", lambda e: e.tensor_tensor(out=t8.rearrange("p (g e) -> p g e", g=4), in0=lg[:, 4:36].rearrange("p (g e) -> p g e", g=4),
                                                  in1=V(rs_.t, 36, (1, 4), (0, 8)), op=ALU.mult), reads=[rs_], writes=[rs_])
            K.op("dve", lambda e: e.tensor_reduce(out=lge, in_=V(rs_.t, 48, (1, 8), (8, 4)), axis=AX.X, op=ALU.add), reads=[rs_], writes=[rs_])
            K.op("dve", lambda e: e.tensor_reduce(out=sc(4), in_=lge, axis=AX.X, op=ALU.max), reads=[rs_], writes=[rs_])
            K.op("dve", lambda e: e.tensor_scalar(out=mk1, in0=lge, scalar1=sc(4), scalar2=None, op0=ALU.is_equal), reads=[rs_], writes=[rs_])
            K.op("dve", lambda e: e.scalar_tensor_tensor(out=t8[:, 0:8], in0=mk1, scalar=NEGBIG, in1=lge, op0=ALU.mult, op1=ALU.add), reads=[rs_], writes=[rs_])
            K.op("dve", lambda e: e.tensor_reduce(out=sc(5), in_=t8[:, 0:8], axis=AX.X, op=ALU.max), reads=[rs_], writes=[rs_])
            K.op("dve", lambda e: e.tensor_scalar(out=mk2, in0=t8[:, 0:8], scalar1=sc(5), scalar2=None, op0=ALU.is_equal), reads=[rs_], writes=[rs_])
            K.op("dve", lambda e: e.tensor_tensor(out=sc(6), in0=sc(5), in1=sc(4), op=ALU.subtract), reads=[rs_], writes=[rs_])
            K.op("act", lambda e: e.activation(out=sc(7), in_=sc(6), func=AF.Exp), reads=[rs_], writes=[rs_])
            K.op("dve", lambda e: e.tensor_scalar(out=sc(8), in0=sc(7), scalar1=1.0, scalar2=None, op0=ALU.add), reads=[rs_], writes=[rs_])
            K.op("dve", lambda e: e.reciprocal(out=sc(9), in_=sc(8)), reads=[rs_], writes=[rs_])
            K.op("dve", lambda e: e.tensor_tensor(out=sc(10), in0=sc(9), in1=sc(3), op=ALU.mult), reads=[rs_], writes=[rs_])
            K.op("dve", lambda e: e.tensor_tensor(out=sc(11), in0=sc(3), in1=sc(10), op=ALU.subtract), reads=[rs_], writes=[rs_])
            K.op("dve", lambda e: e.tensor_scalar(out=mk1, in0=mk1, scalar1=sc(10), scalar2=None, op0=ALU.mult), reads=[rs_], writes=[rs_])
            K.op("dve", lambda e: e.scalar_tensor_tensor(out=mk1, in0=mk2, scalar=sc(11), in1=mk1, op0=ALU.mult, op1=ALU.add), reads=[rs_], writes=[rs_])
            K.op("dve", lambda e, t=t: e.tensor_tensor(out=comb[:, t * 32:(t + 1) * 32].rearrange("p (g e) -> p g e", g=4), in0=V(rs_.t, 36, (1, 4), (0, 8)),
                                                       in1=V(rs_.t, 80, (0, 4), (1, 8)), op=ALU.mult), reads=[rs_], writes=[comb])
        for g in range(3):
            ps = PS()
            nt_ = 4 if g < 2 else 1
            for j in range(nt_):
                t = g * 4 + j
                K.op("pe", lambda e, ps=ps, j=j, t=t: e.transpose(ps[0:32, j * 128:(j + 1) * 128], comb[:, t * 32:(t + 1) * 32], ident), reads=[comb, cst], writes=[ps])
            K.op("act", lambda e, ps=ps, g=g, nt_=nt_: e.activation(out=cTf[0:32, g * 512:g * 512 + nt_ * 128], in_=ps[0:32, 0:nt_ * 128], func=AF.Copy), reads=[ps], writes=[cTf])
        K.op("dve", lambda e: e.tensor_copy(out=cThi[0:32, :], in_=cTf[0:32, :]), reads=[cTf], writes=[cThi])
        K.op("dve", lambda e: e.tensor_tensor(out=cTlo[0:32, :], in0=cTf[0:32, :], in1=cThi[0:32, :], op=ALU.subtract), reads=[cTf, cThi], writes=[cTlo])
        TB3 = ((0, 512), (512, 512), (1024, 128))
        for xe in range(32):
            se = sel[xe % 2]
            K.op("pool", lambda e, se=se, xe=xe: e.tensor_scalar(out=se[0:32, :], in0=cst[0:32, C_ONES:C_ONES + 128], scalar1=cst[0:32, C_ID + xe:C_ID + xe + 1], scalar2=None, op0=ALU.mult),
                 reads=[cst], writes=[se])
            for (t0, n) in TB3:
                ps = PS()
                mm_group(ps[:, 0:n], ps, [(se[0:32, :], cThi[0:32, t0:t0 + n]), (se[0:32, :], cTlo[0:32, t0:t0 + n])], [se, cThi, cTlo])
                K.op("act", lambda e, ps=ps, t0=t0, n=n: e.activation(out=cbc[:, t0:t0 + n], in_=ps[:, 0:n], func=AF.Copy), reads=[ps], writes=[cbc])
            for fp in range(2):
                slg, wg = wload([I["e_wg"][xe][:, fp * 256:(fp + 1) * 256]])
                slu, wu = wload([I["e_wu"][xe][:, fp * 256:(fp + 1) * 256]])
                for c2 in range(2):
                    fb = fp * 2 + c2
                    cs_ = slice(c2 * 128, (c2 + 1) * 128)
                    for (t0, n) in TB3:
                        pg = PS()
                        mm_group(pg[:, 0:n], pg, [(wg[:, kc, cs_], h2Tv[:, kc, t0:t0 + n]) for kc in range(16)], [slg, xT])
                        K.op("act", lambda e, pg=pg, n=n: e.activation(out=sgt[:, 0:n], in_=pg[:, 0:n], func=AF.Silu), reads=[pg], writes=[sgt])
                        pu = PS()
                        mm_group(pu[:, 0:n], pu, [(wu[:, kc, cs_], h2Tv[:, kc, t0:t0 + n]) for kc in range(16)], [slu, xT])
                        K.op("dve", lambda e, pu=pu, n=n, t0=t0: e.tensor_tensor(out=ut[:, 0:n], in0=pu[:, 0:n], in1=cbc[:, t0:t0 + n], op=ALU.mult), reads=[pu, cbc], writes=[ut])
                        K.op("dve", lambda e, n=n, t0=t0, fb=fb: e.tensor_tensor(out=hidv[:, fb, t0:t0 + n], in0=sgt[:, 0:n], in1=ut[:, 0:n], op=ALU.mult), reads=[sgt, ut], writes=[hid])
            for dh in range(2):
                sld, wd = wload([I["e_wd"][xe][:, dh * 1024:(dh + 1) * 1024]])
                for t in range(NT):
                    for db in range(2):
                        ps = PS()
                        mm_group(ps[:, :], ps, [(hidv[:, fc, t * 128:(t + 1) * 128], wd[:, fc, db * 512:(db + 1) * 512]) for fc in range(4)], [hid, sld])
                        c0 = dh * 1024 + db * 512
                        K.op("dve", lambda e, ps=ps, t=t, c0=c0: e.tensor_tensor(out=acc_t[t][:, c0:c0 + 512], in0=acc_t[t][:, c0:c0 + 512], in1=ps[:, :], op=ALU.add),
                             reads=[ps], writes=[acc_t[t]])
        K.barrier()
        A.release(m_after_acc)
        lng = A.alloc("lngc", D, F32)
        lnb = A.alloc("lnbc", D, F32)
        lnst = A.alloc("lnstc", 32, F32)
        layer_norm_tiles(I["ln3_g"], I["ln3_b"], None, None, acc_t, lng, lnb, lnst, out_dram=None,
                         out_fn=lambda t: (O["y_p"][t * 128:(t + 1) * 128, :] if t < 8 else O["y_s"]), inplace=True)

    def emit_prompt_state_outputs():
        for h in range(4):
            K.dma("sp", O["Cp"][h].rearrange("(dc p) v -> p dc v", p=128), V(Cst[h].t, 0, (257, 2), (1, 256)), reads=[Cst[h]])
            K.dma("sp", O["np_"][h].rearrange("(dc p) -> p dc", p=128), V(Cst[h].t, 256, (257, 2)), reads=[Cst[h]])
        K.dma("sp", O["mp"], mstate[0:1, :], reads=[mstate])
        for hh in range(8):
            K.dma("sp", O["Sp"][hh], Sst[hh][:, :], reads=[Sst[hh]])

    if stage == 1:
        emit_prompt_state_outputs()

    nc._K = K
    nc._A = A
    nc._I = I
    nc._O = O
    with nc.allow_non_contiguous_dma(reason="small strided parameter / state layouts"):
        K.finalize()
    return nc


def prep_core_inputs(inp, c, shared):
    j, hh = c // 2, c % 2
    sl = slice(16 * c, 16 * c + 16)
    m = dict(shared)
    m["xp"] = np.ascontiguousarray(inp["x_prompt"][j, hh * TP:(hh + 1) * TP])
    m["xpre"] = np.ascontiguousarray(inp["x_prompt"][j, 0:TP])
    m["xs"] = np.ascontiguousarray(inp["x_sample"][sl].reshape(128, D))
    m["mem"] = np.ascontiguousarray(inp["mem_prompt"][j])
    m["ck"] = np.ascontiguousarray(inp["cache_mem_k"][0, sl].reshape(16, 256, D))
    m["cv"] = np.ascontiguousarray(inp["cache_mem_v"][0, sl].reshape(16, 256, D))
    m["C0"] = np.ascontiguousarray(inp["state_mlstm_C"][0, sl])
    m["n0"] = np.ascontiguousarray(inp["state_mlstm_n"][0, sl])
    m["m0"] = np.ascontiguousarray(inp["state_mlstm_m"][0, sl])
    m["conv0"] = np.ascontiguousarray(inp["state_mlstm_conv"][0, sl].reshape(48, D))
    m["S0"] = np.ascontiguousarray(inp["state_hgrn_S"][0, sl])
    m["flag"] = np.full((128, 1), float(hh), np.float32)
    return m


def prep_shared(inp):
    s = {"consts": host_consts()}
    for k in ["w_in", "conv_w", "lb_logits", "w_bm", "w_bh", "w_out", "xa_wq", "xa_wk", "xa_wv", "xa_wo", "e_wg", "e_wu", "e_wd"]:
        a = inp[k]
        s[k] = np.ascontiguousarray(a[0] if k != "lb_logits" else a)
    for k in ["b_in", "mlstm_gn", "hgrn_gn", "ln1_g", "ln1_b", "ln2_g", "ln2_b", "ln3_g", "ln3_b"]:
        s[k] = np.ascontiguousarray(inp[k].reshape(1, -1))
    s["rw"] = np.ascontiguousarray(np.concatenate([inp["r1_w"][0], inp["r2_w"][0].transpose(1, 0, 2).reshape(D, 32)], axis=1))
    s["rb"] = np.ascontiguousarray(np.concatenate([inp["r1_b"][0].reshape(-1), inp["r2_b"][0].reshape(-1)]).reshape(1, 36))
    return s


def assemble(res):
    f = np.float32
    y_p = np.zeros((4, 2048, D), f)
    y_s = np.zeros((128, 8, D), f)
    mk = np.zeros((1, 4, 256, 4, 512), f)
    mv = np.zeros((1, 4, 256, 4, 512), f)
    C_p = np.zeros((1, 4, 4, 256, 256), f)
    n_p = np.zeros((1, 4, 4, 256), f)
    m_p = np.zeros((1, 4, 4), f)
    conv_p = np.zeros((1, 4, 3, D), f)
    S_p = np.zeros((1, 4, 8, 128, 128), f)
    C_s = np.zeros((1, 128, 4, 256, 256), f)
    n_s = np.zeros((1, 128, 4, 256), f)
    m_s = np.zeros((1, 128, 4), f)
    conv_s = np.zeros((1, 128, 3, D), f)
    S_s = np.zeros((1, 128, 8, 128, 128), f)
    for c, r in res.items():
        j, hh = c // 2, c % 2
        sl = slice(16 * c, 16 * c + 16)
        y_p[j, hh * TP:(hh + 1) * TP] = r["y_p"]
        y_s[sl] = r["y_s"].reshape(16, 8, D)
        if hh == 0:
            mk[0, j] = r["mk"].reshape(256, 4, 512)
            mv[0, j] = r["mv"].reshape(256, 4, 512)
        else:
            C_p[0, j] = r["Cp"]
            n_p[0, j] = r["np_"]
            m_p[0, j] = r["mp"].reshape(4)
            conv_p[0, j] = r["convp"]
            S_p[0, j] = r["Sp"]
        C_s[0, sl] = r["Cs"]
        n_s[0, sl] = r["ns"]
        m_s[0, sl] = r["ms"]
        conv_s[0, sl] = r["convs"]
        S_s[0, sl] = r["Ss"]
    return (y_p, y_s, mk, mv, C_p, n_p, m_p, conv_p, S_p, C_s, n_s, m_s, conv_s, S_s)


def kernel(**inputs):
    inp = {k: np.asarray(v) for k, v in inputs.items()}
    shared = prep_shared(inp)
    in_maps = [prep_core_inputs(inp, c, shared) for c in range(NCORES)]
    nc = build_program()
    res = run_bass_kernel_spmd(nc, in_maps, core_ids=list(range(NCORES)))
    return assemble({c: res.results[c] for c in range(NCORES)})
```
